# Optimizing a Trainium2 kernel written in Bass

```python
import jax, jax.numpy as jnp
from jax import lax
import numpy as np

D_MODEL = 1024
BATCH = 8
SEQ = 4096
DEPTH = 4

GRID_W = 64
CTX_LEN = 256
N_MIXERS = 3
QUERY_BLOCK = 128
NORM_EPS = 1e-6
ROPE_BASE = 10000.0

A_HEADS = 16
A_KV_HEADS = 4
A_GROUP = A_HEADS // A_KV_HEADS
A_HEAD_DIM = D_MODEL // A_HEADS
F_GROUPS = 4
M_HEADS = 16
M_Q_LORA = 3 * D_MODEL // 8
M_KV_LORA = D_MODEL // 4
M_NOPE = D_MODEL // 16
M_ROPE = D_MODEL // 32
M_V = D_MODEL // 16
FF_DENSE = 11 * D_MODEL // 4
N_EXPERTS = 8
TOP_K = 2
FF_EXPERT = 7 * D_MODEL // 2

N_A = (DEPTH + 2) // 3
N_B = (DEPTH + 1) // 3
N_C = DEPTH // 3
N_DENSE = (DEPTH + 1) // 2
N_MOE = DEPTH // 2

kernel_name = "hybrid_gqa_fnet_mla_moe_dit"


def rmsnorm(x, g):
    x32 = x.astype(jnp.float32)
    y = x32 * lax.rsqrt(jnp.mean(x32 * x32, axis=-1, keepdims=True) + NORM_EPS)
    return (y * g.astype(jnp.float32)).astype(x.dtype)


def modulate(h, shift, scale):
    return h * (1 + scale) + shift


def axial_rope_tables(rows, cols, rot_dim):
    nf = rot_dim // 4
    inv = ROPE_BASE ** (-jnp.arange(nf, dtype=jnp.float32) / nf)
    ang_r = rows[:, None] * inv[None, :]
    ang_c = cols[:, None] * inv[None, :]
    return (jnp.cos(ang_r), jnp.sin(ang_r), jnp.cos(ang_c), jnp.sin(ang_c))


def _rotate(x, cos, sin):
    x1, x2 = jnp.split(x, 2, axis=-1)
    cos = cos[None, :, None, :]
    sin = sin[None, :, None, :]
    return jnp.concatenate([x1 * cos - x2 * sin, x2 * cos + x1 * sin], axis=-1)


def apply_axial_rope(x, tabs):
    cr, sr, cc, sc = tabs
    xr, xc = jnp.split(x, 2, axis=-1)
    out = jnp.concatenate([_rotate(xr, cr, sr), _rotate(xc, cc, sc)], axis=-1)
    return out.astype(x.dtype)


def sweep_attention(q, k, v, scale):
    B, Sq, KV, G, dk = q.shape
    dv = v.shape[-1]
    nb = Sq // QUERY_BLOCK
    qb = q.reshape(B, nb, QUERY_BLOCK, KV, G, dk).transpose(1, 0, 2, 3, 4, 5)

    def one_block(qblk):
        s = jnp.einsum('bqkgd,bskd->bkgqs', qblk, k, preferred_element_type=jnp.float32) * scale
        p = jax.nn.softmax(s, axis=-1).astype(v.dtype)
        return jnp.einsum('bkgqs,bskd->bqkgd', p, v)

    o = lax.map(one_block, qb)
    return o.transpose(1, 0, 2, 3, 4, 5).reshape(B, Sq, KV * G * dv)


def _gqa_project(h, w_qkv, g_q, g_k, with_q):
    B, S, _ = h.shape
    nq = A_HEADS * A_HEAD_DIM
    nkv = A_KV_HEADS * A_HEAD_DIM
    out = h @ (w_qkv if with_q else w_qkv[:, nq:])
    q = None
    if with_q:
        q = rmsnorm(out[..., :nq].reshape(B, S, A_HEADS, A_HEAD_DIM), g_q)
        out = out[..., nq:]
    k = rmsnorm(out[..., :nkv].reshape(B, S, A_KV_HEADS, A_HEAD_DIM), g_k)
    v = out[..., nkv:].reshape(B, S, A_KV_HEADS, A_HEAD_DIM)
    return q, k, v


def gqa_mixer(hc, hl, w_qkv, g_q, g_k, w_o, rope, ctx_queries):
    scale = A_HEAD_DIM ** -0.5
    ql, kl, vl = _gqa_project(hl, w_qkv, g_q, g_k, True)
    ql = apply_axial_rope(ql, rope)
    kl = apply_axial_rope(kl, rope)
    qc, kc, vc = _gqa_project(hc, w_qkv, g_q, g_k, ctx_queries)
    k_all = jnp.concatenate([kc, kl], axis=1)
    v_all = jnp.concatenate([vc, vl], axis=1)
    B, S = hl.shape[:2]
    yl = sweep_attention(ql.reshape(B, S, A_KV_HEADS, A_GROUP, A_HEAD_DIM), k_all, v_all, scale) @ w_o
    yc = None
    if ctx_queries:
        L = hc.shape[1]
        yc = sweep_attention(qc.reshape(B, L, A_KV_HEADS, A_GROUP, A_HEAD_DIM), kc, vc, scale) @ w_o
    return yc, yl


def fourier_mixer(h, w_f):
    B, S, D = h.shape
    hg = h.astype(jnp.float32).reshape(B, S, F_GROUPS, D // F_GROUPS)
    f = jnp.fft.fftn(hg, axes=(1, 3), norm='ortho').real
    return f.reshape(B, S, D).astype(h.dtype) @ w_f


def _mla_project(h, w_down, g_cq, g_ckv, w_uq, w_ukv, rope, with_q):
    B, S, _ = h.shape
    d = h @ (w_down if with_q else w_down[:, M_Q_LORA:])
    cq = None
    if with_q:
        cq = d[..., :M_Q_LORA]
        d = d[..., M_Q_LORA:]
    ckv = d[..., :M_KV_LORA]
    kr = d[..., M_KV_LORA:].reshape(B, S, 1, M_ROPE)
    kv = (rmsnorm(ckv, g_ckv) @ w_ukv).reshape(B, S, M_HEADS, M_NOPE + M_V)
    k_nope, v = kv[..., :M_NOPE], kv[..., M_NOPE:]
    if rope is not None:
        kr = apply_axial_rope(kr, rope)
    k = jnp.concatenate([k_nope, jnp.broadcast_to(kr, (B, S, M_HEADS, M_ROPE))], axis=-1)
    q = None
    if with_q:
        q = (rmsnorm(cq, g_cq) @ w_uq).reshape(B, S, M_HEADS, M_NOPE + M_ROPE)
        if rope is not None:
            q = jnp.concatenate([q[..., :M_NOPE], apply_axial_rope(q[..., M_NOPE:], rope)], axis=-1)
    return q, k, v


def mla_mixer(hc, hl, w_down, g_cq, g_ckv, w_uq, w_ukv, w_o, rope, ctx_queries):
    scale = (M_NOPE + M_ROPE) ** -0.5
    ql, kl, vl = _mla_project(hl, w_down, g_cq, g_ckv, w_uq, w_ukv, rope, True)
    qc, kc, vc = _mla_project(hc, w_down, g_cq, g_ckv, w_uq, w_ukv, None, ctx_queries)
    k_all = jnp.concatenate([kc, kl], axis=1)
    v_all = jnp.concatenate([vc, vl], axis=1)
    yl = sweep_attention(ql[:, :, :, None, :], k_all, v_all, scale) @ w_o
    yc = None
    if ctx_queries:
        yc = sweep_attention(qc[:, :, :, None, :], kc, vc, scale) @ w_o
    return yc, yl


def swiglu(h, w_gu, w_down):
    g, u = jnp.split(h @ w_gu, 2, axis=-1)
    return (jax.nn.silu(g) * u) @ w_down


def moe_swiglu(h, w_router, w_gu, w_down):
    B, S, D = h.shape
    t = h.reshape(B * S, D)
    logits = (t @ w_router).astype(jnp.float32)
    top_v, top_i = lax.top_k(logits, TOP_K)
    wts = jax.nn.softmax(top_v, axis=-1)
    gates = jnp.sum(jax.nn.one_hot(top_i, N_EXPERTS, dtype=jnp.float32) * wts[..., None], axis=1).astype(h.dtype)
    out = jnp.zeros_like(t)
    for e in range(N_EXPERTS):
        out = out + gates[:, e:e + 1] * swiglu(t, w_gu[e], w_down[e])
    return out.reshape(B, S, D)


def setup_inputs(seed: int = 0) -> dict:
    key = jax.random.key(seed)
    ks = jax.random.split(key, 24)
    D = D_MODEL

    def nrm(k, shape, std):
        return jax.random.normal(k, shape, dtype=jnp.float32) * std

    def gain(k, shape):
        return 1.0 + 0.02 * jax.random.normal(k, shape, dtype=jnp.float32)

    return {
        'x': nrm(ks[0], (BATCH, SEQ, D), 1.0),
        'c': nrm(ks[1], (BATCH, D), 1.0),
        'ctx': nrm(ks[2], (BATCH, CTX_LEN, D), 1.0),
        'c_ctx': nrm(ks[3], (D,), 1.0),
        'w_mod': nrm(ks[4], (DEPTH, D, 6 * D), 0.5 * D ** -0.5),
        'b_mod': nrm(ks[5], (DEPTH, 6 * D), 0.02),
        'norm_g': gain(ks[6], (DEPTH, 4, D)),
        'a_w_qkv': nrm(ks[7], (N_A, D, (A_HEADS + 2 * A_KV_HEADS) * A_HEAD_DIM), D ** -0.5),
        'a_g_q': gain(ks[8], (N_A, A_HEAD_DIM)),
        'a_g_k': gain(ks[9], (N_A, A_HEAD_DIM)),
        'a_w_o': nrm(ks[10], (N_A, A_HEADS * A_HEAD_DIM, D), (A_HEADS * A_HEAD_DIM) ** -0.5),
        'f_w': nrm(ks[11], (N_B, D, D), D ** -0.5),
        'm_w_down': nrm(ks[12], (N_C, D, M_Q_LORA + M_KV_LORA + M_ROPE), D ** -0.5),
        'm_g_cq': gain(ks[13], (N_C, M_Q_LORA)),
        'm_g_ckv': gain(ks[14], (N_C, M_KV_LORA)),
        'm_w_uq': nrm(ks[15], (N_C, M_Q_LORA, M_HEADS * (M_NOPE + M_ROPE)), M_Q_LORA ** -0.5),
        'm_w_ukv': nrm(ks[16], (N_C, M_KV_LORA, M_HEADS * (M_NOPE + M_V)), M_KV_LORA ** -0.5),
        'm_w_o': nrm(ks[17], (N_C, M_HEADS * M_V, D), (M_HEADS * M_V) ** -0.5),
        'd_w_gu': nrm(ks[18], (N_DENSE, D, 2 * FF_DENSE), D ** -0.5),
        'd_w_down': nrm(ks[19], (N_DENSE, FF_DENSE, D), FF_DENSE ** -0.5),
        'e_w_router': nrm(ks[20], (N_MOE, D, N_EXPERTS), D ** -0.5),
        'e_w_gu': nrm(ks[21], (N_MOE, N_EXPERTS, D, 2 * FF_EXPERT), D ** -0.5),
        'e_w_down': nrm(ks[22], (N_MOE, N_EXPERTS, FF_EXPERT, D), FF_EXPERT ** -0.5),
    }


def reference(x, c, ctx, c_ctx, w_mod, b_mod, norm_g, a_w_qkv, a_g_q, a_g_k, a_w_o, f_w,
              m_w_down, m_g_cq, m_g_ckv, m_w_uq, m_w_ukv, m_w_o, d_w_gu, d_w_down,
              e_w_router, e_w_gu, e_w_down):
    B, S, D = x.shape
    L = ctx.shape[1]
    ROWS = S // GRID_W
    rows = jnp.repeat(jnp.arange(ROWS, dtype=jnp.float32), GRID_W)
    cols = jnp.tile(jnp.arange(GRID_W, dtype=jnp.float32), ROWS)
    rope_a = axial_rope_tables(rows, cols, A_HEAD_DIM)
    rope_m = axial_rope_tables(rows, cols, M_ROPE)
    sc_lat = jax.nn.silu(c)
    sc_ctx = jax.nn.silu(c_ctx)
    xl, xc = x, ctx

    for i in range(DEPTH):
        last = i == DEPTH - 1
        kind = i % N_MIXERS
        j = i // N_MIXERS
        ctx_in = (not last) or kind != 1

        mod_l = sc_lat @ w_mod[i] + b_mod[i]
        sh_m, s_m, g_m, sh_f, s_f, g_f = [t[:, None, :] for t in jnp.split(mod_l, 6, axis=-1)]
        hl = modulate(rmsnorm(xl, norm_g[i, 0]), sh_m, s_m)
        hc = None
        mc = None
        if ctx_in:
            n_c = 6 * D if not last else 2 * D
            mod_c = sc_ctx @ w_mod[i, :, :n_c] + b_mod[i, :n_c]
            mc = jnp.split(mod_c, n_c // D)
            hc = modulate(rmsnorm(xc, norm_g[i, 0]), mc[0], mc[1])

        if kind == 0:
            yc, yl = gqa_mixer(hc, hl, a_w_qkv[j], a_g_q[j], a_g_k[j], a_w_o[j], rope_a, not last)
        elif kind == 1:
            yl = fourier_mixer(hl, f_w[j])
            yc = fourier_mixer(hc, f_w[j]) if not last else None
        else:
            yc, yl = mla_mixer(hc, hl, m_w_down[j], m_g_cq[j], m_g_ckv[j], m_w_uq[j], m_w_ukv[j],
                               m_w_o[j], rope_m, not last)
        xl = xl + g_m * rmsnorm(yl, norm_g[i, 1])
        if not last:
            xc = xc + mc[2] * rmsnorm(yc, norm_g[i, 1])

        hl2 = modulate(rmsnorm(xl, norm_g[i, 2]), sh_f, s_f)
        if not last:
            hc2 = modulate(rmsnorm(xc, norm_g[i, 2]), mc[3], mc[4])
            h2 = jnp.concatenate([hc2, hl2], axis=1)
        else:
            h2 = hl2
        if i % 2 == 0:
            y2 = swiglu(h2, d_w_gu[i // 2], d_w_down[i // 2])
        else:
            y2 = moe_swiglu(h2, e_w_router[i // 2], e_w_gu[i // 2], e_w_down[i // 2])
        if not last:
            xc = xc + mc[5] * rmsnorm(y2[:, :L], norm_g[i, 3])
            yl2 = y2[:, L:]
        else:
            yl2 = y2
        xl = xl + g_f * rmsnorm(yl2, norm_g[i, 3])

    return xl
```

```python
import contextlib
import numpy as np
import ml_dtypes
import concourse.bass as bass
import concourse.mybir as mybir
from concourse.bass_utils import run_bass_kernel_spmd

F32 = mybir.dt.float32
BF16 = mybir.dt.bfloat16
AF = mybir.ActivationFunctionType
ALU = mybir.AluOpType
AX = mybir.AxisListType

D = 1024
T = 4352
NT = 34
NCTX_T = 2
DEPTH = 4
EPS = 1e-6
FF_D = 2816
FF_E = 3584
NE = 8


class Tile:
    __slots__ = ("name", "writers", "readers")

    def __init__(self, name=""):
        self.name = name
        self.writers = {}
        self.readers = {}


class Op:
    __slots__ = ("stream", "tl", "deps", "fn", "marked", "epoch", "value", "seq")


class Timeline:
    def __init__(self, name, inc, limit):
        self.name = name
        self.inc = inc
        self.limit = limit
        self.ops = []
        self.epoch = None
        self.cnt = 0
        self.last = None


class Sched:
    STREAMS = ("sp", "act", "dve", "pool", "pe")
    ENG = {"sp": "sync", "act": "scalar", "dve": "vector", "pool": "gpsimd", "pe": "tensor"}

    def __init__(self, nc, semalloc, ndma=8):
        self.nc = nc
        self.semalloc = semalloc
        self.ops = {s: [] for s in self.STREAMS}
        self.tls = {s: Timeline(s, 1, 30000) for s in ("act", "dve", "pool", "pe")}
        self.dma_tls = {q: [Timeline(f"dma_{q}{i}", 16, 1800) for i in range(ndma)] for q in ("sp", "pool")}
        self.dma_rr = {q: 0 for q in self.dma_tls}
        self.all_tiles = []
        self.nseq = 0
        self.waited = {s: {} for s in self.STREAMS}
        self.nops = 0

    def all_tls(self):
        return list(self.tls.values()) + [t for q in self.dma_tls.values() for t in q]

    def tile(self, name=""):
        t = Tile(name)
        self.all_tiles.append(t)
        return t

    def _mk(self, stream, tl, fn, reads, writes, extra=()):
        op = Op()
        op.stream = stream
        op.tl = tl
        op.fn = fn
        op.marked = False
        op.epoch = None
        op.value = None
        op.seq = self.nseq
        self.nseq += 1
        deps = {}

        def add(d):
            k = d.tl
            if k not in deps or deps[k].seq < d.seq:
                deps[k] = d

        for t in reads:
            for d in t.writers.values():
                add(d)
        for t in writes:
            for d in t.writers.values():
                add(d)
            for d in t.readers.values():
                add(d)
        for d in extra:
            add(d)
        if stream == "pe" and self.tls["pe"] in deps:
            del deps[self.tls["pe"]]
        op.deps = list(deps.values())
        for d in op.deps:
            d.marked = True
        for t in reads:
            t.readers[tl] = op
        for t in writes:
            t.writers = {tl: op}
            t.readers = {}
        tl.ops.append(op)
        tl.last = op
        self.ops[stream].append(op)
        self.nops += 1
        return op

    def op(self, stream, fn, reads=(), writes=()):
        return self._mk(stream, self.tls[stream], fn, reads, writes)

    def dma(self, queue, fn, reads=(), writes=()):
        tls = self.dma_tls[queue]
        i = self.dma_rr[queue]
        self.dma_rr[queue] = (i + 1) % len(tls)
        tl = tls[i]
        prev = tl.ops[-1] if tl.ops else None
        op = self._mk(queue, tl, fn, reads, writes, extra=(prev,) if prev is not None else ())
        op.marked = True
        return op

    def barrier_and_emit(self):
        lasts = [tl.ops[-1] for tl in self.all_tls() if tl.ops]
        for s in self.STREAMS:
            op = Op()
            op.stream = s
            op.tl = None
            op.fn = None
            op.marked = False
            op.epoch = None
            op.value = None
            op.seq = self.nseq
            self.nseq += 1
            op.deps = [d for d in lasts if not (s == "pe" and d.tl is self.tls["pe"])]
            for d in op.deps:
                d.marked = True
            self.ops[s].append(op)
        for t in self.all_tiles:
            t.writers = {}
            t.readers = {}
        self._emit()

    def _emit(self):
        nc = self.nc
        for tl in self.all_tls():
            for op in tl.ops:
                if not op.marked:
                    continue
                if tl.epoch is None or tl.cnt >= tl.limit:
                    tl.epoch = self.semalloc(tl.name)
                    tl.cnt = 0
                tl.cnt += 1
                op.epoch = tl.epoch
                op.value = tl.cnt * tl.inc
        with nc.Block() as block:
            for s in self.STREAMS:
                ops = self.ops[s]
                if not ops:
                    continue

                def body(e, ops=ops, waited=self.waited[s]):
                    for op in ops:
                        for d in op.deps:
                            key = id(d.epoch)
                            if waited.get(key, 0) >= d.value:
                                continue
                            e.wait_ge(d.epoch, d.value)
                            waited[key] = d.value
                        if op.fn is None:
                            continue
                        ins = op.fn(e)
                        if op.marked:
                            ins.then_inc(op.epoch, op.tl.inc)

                getattr(block, self.ENG[s])(body)
        for s in self.STREAMS:
            self.ops[s] = []
        for tl in self.all_tls():
            tl.ops = []


class Rot:
    def __init__(self, S, alloc, name, shape, dt, n):
        self.bufs = [alloc(f"{name}{i}", shape, dt) for i in range(n)]
        self.tiles = [S.tile(f"{name}{i}") for i in range(n)]
        self.i = 0

    def next(self):
        b, t = self.bufs[self.i], self.tiles[self.i]
        self.i = (self.i + 1) % len(self.bufs)
        return b, t


class Builder:
    def __init__(self, nlayers=DEPTH, debug=False):
        self.nlayers = nlayers
        self.debug = debug
        nc = self.nc = bass.Bass("TRN2", target_bir_lowering=False)
        self.es = contextlib.ExitStack()
        self.uid = 0

        def semalloc(name):
            self.uid += 1
            return self.es.enter_context(nc.semaphore(f"{name}_{self.uid}"))

        self.S = Sched(nc, semalloc)

    def din(self, name, shape, dt=F32):
        return self.nc.dram_tensor(name, list(shape), dt, kind="ExternalInput").ap()

    def dscr(self, name, shape, dt):
        return self.nc.dram_tensor(name, list(shape), dt, kind="Internal").ap()

    def declare(self):
        d = self.din
        self.xin = d("xin", [T, D])
        self.cvT = d("cvT", [128, 16])
        self.ident_d = d("ident", [128, 128])
        self.w_mod = d("w_mod", [DEPTH, D, 6 * D])
        self.b_mod = d("b_mod", [DEPTH, 6 * D])
        self.norm_g = d("norm_g", [DEPTH, 4, D])
        self.a_w_qkv = d("a_w_qkv", [2, D, 1536])
        self.a_g_q = d("a_g_q", [2, 64])
        self.a_g_k = d("a_g_k", [2, 64])
        self.a_w_o = d("a_w_o", [2, D, D])
        self.f_w = d("f_w", [1, D, D])
        self.m_w_down = d("m_w_down", [1, D, 672])
        self.m_g_cq = d("m_g_cq", [1, 384])
        self.m_g_ckv = d("m_g_ckv", [1, 256])
        self.m_wuqn = d("m_wuqn", [384, 1024])
        self.m_wuqr = d("m_wuqr", [384, 512])
        self.m_wuqrs = d("m_wuqrs", [384, 512])
        self.m_wuk = d("m_wuk", [256, 1024])
        self.m_wuv = d("m_wuv", [256, 1024])
        self.m_w_o = d("m_w_o", [1, D, D])
        self.d_w_gu = d("d_w_gu", [2, D, 2 * FF_D])
        self.d_w_down = d("d_w_down", [2, FF_D, D])
        self.e_w_router = d("e_w_router", [2, D, NE])
        self.e_w_gu = d("e_w_gu", [2, NE, D, 2 * FF_E])
        self.e_w_down = d("e_w_down", [2, NE, FF_E, D])
        self.ropeA_cos = d("ropeA_cos", [T, 64])
        self.ropeA_sin = d("ropeA_sin", [T, 64])
        self.ropeM_cos = d("ropeM_cos", [T, 32])
        self.ropeM_sin = d("ropeM_sin", [T, 32])
        self.ropeMT_cos = d("ropeMT_cos", [64, T])
        self.ropeMT_sin = d("ropeMT_sin", [64, T])
        self.dft256 = d("dft256", [256, 512])
        self.cos4096 = d("cos4096", [4096, 4096], BF16)
        self.nsin4096 = d("nsin4096", [4096, 4096], BF16)
        self.cos256 = d("cos256", [256, 256], BF16)
        self.nsin256 = d("nsin256", [256, 256], BF16)
        self.sel_d = d("sel", [NE, NE * 128])
        self.out = self.nc.dram_tensor("out", [4096, D], F32, kind="ExternalOutput").ap()
        s = self.dscr
        if self.debug:
            self.xres = self.nc.dram_tensor("xres", [T, D], F32, kind="ExternalOutput").ap()
        else:
            self.xres = s("xres", [T, D], F32)
        self.modrows = s("modrows", [DEPTH, 2, 6 * D], F32)
        self.QT_d = s("QT_d", [8, 128, T], BF16)
        self.QrT_d = s("QrT_d", [8, 64, T], BF16)
        self.KnT_d = s("KnT_d", [8, 128, T], BF16)
        self.V_d = s("V_d", [NT, 128, 16 * 65], BF16)
        self.XCS_d = s("XCS_d", [4, T, 512], BF16)
        self.h2T_d = s("h2T_d", [128, 8, T], BF16)
        self.h2f_d = s("h2f_d", [T, D], F32)

    def scope(self):
        b = self

        class _Scope:
            def __enter__(s):
                s.es = contextlib.ExitStack()
                s.es.__enter__()
                return s

            def sb(s, name, shape, dt):
                b.uid += 1
                return s.es.enter_context(b.nc.sbuf_tensor(f"{name}_{b.uid}", list(shape), dt))

            def ps(s, name, shape, dt):
                b.uid += 1
                return s.es.enter_context(b.nc.psum_tensor(f"{name}_{b.uid}", list(shape), dt))

            def rot(s, name, shape, dt, n, psum=False):
                return Rot(b.S, s.ps if psum else s.sb, name, shape, dt, n)

            def __exit__(s, *a):
                if a[0] is None:
                    b.S.barrier_and_emit()
                return s.es.__exit__(*a)

        return _Scope()

    def dma(self, q, out, in_, reads=(), writes=()):
        return self.S.dma(q, lambda e: e.dma_start(out=out, in_=in_), reads, writes)

    def act(self, out, in_, func, reads, writes, **kw):
        return self.S.op("act", lambda e: e.activation(out=out, in_=in_, func=func, **kw), reads, writes)

    def tt(self, eng, out, in0, in1, op, reads, writes):
        return self.S.op(eng, lambda e: e.tensor_tensor(out=out, in0=in0, in1=in1, op=op), reads, writes)

    def stt(self, out, in0, scalar, in1, op0, op1, reads, writes):
        return self.S.op("dve", lambda e: e.scalar_tensor_tensor(out=out, in0=in0, scalar=scalar, in1=in1,
                                                                  op0=op0, op1=op1), reads, writes)

    def ts(self, eng, out, in0, s1, op0, reads, writes, s2=None, op1=None):
        if op1 is None:
            return self.S.op(eng, lambda e: e.tensor_scalar(out=out, in0=in0, scalar1=s1, scalar2=None, op0=op0),
                             reads, writes)
        return self.S.op(eng, lambda e: e.tensor_scalar(out=out, in0=in0, scalar1=s1, scalar2=s2, op0=op0, op1=op1),
                         reads, writes)

    def copy(self, eng, out, in_, reads, writes):
        if eng == "act":
            return self.act(out, in_, AF.Copy, reads, writes)
        return self.S.op(eng, lambda e: e.tensor_copy(out=out, in_=in_), reads, writes)

    def mm(self, out, lhsT, rhs, start, stop, reads, writes):
        return self.S.op("pe", lambda e: e.matmul(out, lhsT=lhsT, rhs=rhs, start=start, stop=stop), reads, writes)

    def tr(self, out, in_, ident, reads, writes):
        return self.S.op("pe", lambda e: e.transpose(out=out, in_=in_, identity=ident), reads, writes)

    def recip(self, out, in_, reads, writes):
        return self.S.op("dve", lambda e: e.reciprocal(out=out, in_=in_), reads, writes)

    def memset(self, eng, ap, val, writes):
        return self.S.op(eng, lambda e: e.memset(ap, val), (), writes)

    def common(self, sc, njunk=1536):
        c = type("C", (), {})()
        c.ss = sc.rot("ss", [128, 1], F32, 4)
        c.rs = sc.rot("rs", [128, 1], F32, 4)
        c.junk = sc.rot("junk", [128, njunk], BF16, 2)
        c.identb = sc.sb("identb", [128, 128], BF16)
        c.identf = sc.sb("identf", [128, 128], F32)
        c.t_id = self.S.tile("ident")
        self.dma("sp", c.identf[:], self.ident_d[:, :], writes=[c.t_id])
        self.copy("dve", c.identb[:], c.identf[:], [c.t_id], [c.t_id])
        return c

    def rstd_of(self, c, src, n, reads, eps=EPS):
        ss, t_ss = c.ss.next()
        jk, t_jk = c.junk.next()
        self.act(jk[:, 0:n], src, AF.Square, reads, [t_jk, t_ss], accum_out=ss[:])
        rs, t_rs = c.rs.next()
        self.act(rs[:], ss[:], AF.Sqrt, [t_ss], [t_rs], scale=1.0 / n, bias=eps)
        self.recip(rs[:], rs[:], [t_rs], [t_rs])
        return rs, t_rs

    def load_mod(self, sc, layer, which, ctx_rows=True):
        res = {}
        gt = {}
        for (j, kind, nrow) in which:
            if kind != "shift" and nrow not in gt:
                g = sc.sb(f"gbc{nrow}", [128, D], F32)
                tg = self.S.tile()
                self.dma("sp", g[:], self.norm_g[layer, nrow:nrow + 1, :].partition_broadcast(128), writes=[tg])
                gt[nrow] = (g, tg)
            for r in range(2 if ctx_rows else 1):
                m = sc.sb(f"mod{j}_{r}", [128, D], F32)
                tm = self.S.tile()
                self.dma("sp", m[:], self.modrows[layer, r:r + 1, j * D:(j + 1) * D].partition_broadcast(128),
                         writes=[tm])
                if kind == "scale":
                    g, tg = gt[nrow]
                    self.stt(m[:], m[:], 1.0, g[:], ALU.add, ALU.mult, [tm, tg], [tm])
                elif kind == "gate":
                    g, tg = gt[nrow]
                    self.tt("dve", m[:], m[:], g[:], ALU.mult, [tm, tg], [tm])
                res[(j, r)] = (m, tm)
        return res

    def phase_mod(self):
        with self.scope() as sc:
            cv = sc.sb("cv", [128, 16], F32)
            scT = sc.sb("scT", [128, 16], F32)
            t_cv = self.S.tile()
            self.dma("sp", cv[:], self.cvT[:, :], writes=[t_cv])
            self.act(scT[:], cv[:], AF.Silu, [t_cv], [t_cv])
            wm = sc.rot("wm", [128, 8, 512], F32, 3)
            bm = sc.rot("bm", [2, 512], F32, 3)
            mr = sc.rot("mr", [2, 512], F32, 3)
            ps = sc.rot("mps", [2, 512], F32, 2, psum=True)
            for i in range(self.nlayers):
                for nb in range(12):
                    w, t_w = wm.next()
                    self.dma("sp", w[:], self.w_mod[i, :, nb * 512:(nb + 1) * 512].rearrange("(c p) n -> p c n", p=128),
                             writes=[t_w])
                    b_, t_b = bm.next()
                    self.dma("sp", b_[:], self.b_mod[i:i + 1, nb * 512:(nb + 1) * 512].partition_broadcast(2),
                             writes=[t_b])
                    p, t_p = ps.next()
                    for k in range(8):
                        self.mm(p[:], scT[:, 2 * k:2 * k + 2], w[:, k, :], k == 0, k == 7, [t_cv, t_w], [t_p])
                    m, t_m = mr.next()
                    self.tt("dve", m[:], p[:], b_[:], ALU.add, [t_p, t_b], [t_m])
                    self.dma("sp", self.modrows[i, :, nb * 512:(nb + 1) * 512], m[:], reads=[t_m])

    def mk_pre(self, sc, c):
        p = type("P", (), {})()
        p.x = sc.rot("px", [128, D], F32, 3)
        p.tmp = sc.rot("ptmp", [128, D], F32, 2)
        p.hb = sc.rot("phb", [128, D], BF16, 2)
        p.hT = sc.rot("phT", [128, 8, 128], BF16, 3)
        return p

    def prenorm(self, c, p, pT, xsrc, tt_, A, B):
        A_ap, t_A = A
        B_ap, t_B = B
        xt, t_x = p.x.next()
        self.dma("sp", xt[:], xsrc[tt_ * 128:(tt_ + 1) * 128, :], writes=[t_x])
        r, t_r = self.rstd_of(c, xt[:], D, [t_x])
        tmp, t_tmp = p.tmp.next()
        self.stt(tmp[:], xt[:], r[:], A_ap[:], ALU.mult, ALU.mult, [t_x, t_r, t_A], [t_tmp])
        hb, t_hb = p.hb.next()
        self.tt("pool", hb[:], tmp[:], B_ap[:], ALU.add, [t_tmp, t_B], [t_hb])
        return self.transpose8(c, p, pT, hb, t_hb)

    def transpose8(self, c, p, pT, hb, t_hb):
        ps, t_ps = pT.next()
        for k in range(8):
            self.tr(ps[:, k, :], hb[:, k * 128:(k + 1) * 128], c.identb[:], [t_hb, c.t_id], [t_ps])
        hT, t_hT = p.hT.next()
        self.copy("act", hT[:], ps[:], [t_ps], [t_hT])
        return hT, t_hT

    def mk_epi(self, sc, c, moe):
        p = type("E", (), {})()
        p.x = sc.rot("ex", [128, D], F32, 2)
        p.tmp = sc.rot("etmp", [128, D], F32, 2)
        p.xn = sc.rot("exn", [128, D], F32, 2)
        p.hb = sc.rot("ehb", [128, D], BF16, 2)
        p.hT = sc.rot("ehT", [128, 8, 128], BF16, 2)
        p.hf = sc.rot("ehf", [128, D], F32, 2) if moe else None
        return p

    def epilogue(self, c, p, pT, xsrc, tt_, y_ap, y_tiles, G, A2, B2, do_h2, moe):
        G_ap, t_G = G
        rows = slice(tt_ * 128, (tt_ + 1) * 128)
        xt, t_x = p.x.next()
        self.dma("sp", xt[:], xsrc[rows, :], writes=[t_x])
        r, t_r = self.rstd_of(c, y_ap, D, y_tiles)
        tmp, t_tmp = p.tmp.next()
        self.stt(tmp[:], y_ap, r[:], G_ap[:], ALU.mult, ALU.mult, list(y_tiles) + [t_r, t_G], [t_tmp])
        xn, t_xn = p.xn.next()
        self.tt("pool", xn[:], xt[:], tmp[:], ALU.add, [t_x, t_tmp], [t_xn])
        self.dma("sp", self.xres[rows, :], xn[:], reads=[t_xn])
        if not do_h2:
            return
        A_ap, t_A = A2
        B_ap, t_B = B2
        r2, t_r2 = self.rstd_of(c, xn[:], D, [t_xn])
        t2, t_t2 = p.tmp.next()
        self.stt(t2[:], xn[:], r2[:], A_ap[:], ALU.mult, ALU.mult, [t_xn, t_r2, t_A], [t_t2])
        hb, t_hb = p.hb.next()
        if moe:
            hf, t_hf = p.hf.next()
            self.tt("pool", hf[:], t2[:], B_ap[:], ALU.add, [t_t2, t_B], [t_hf])
            self.dma("sp", self.h2f_d[rows, :], hf[:], reads=[t_hf])
            self.copy("pool", hb[:], hf[:], [t_hf], [t_hb])
        else:
            self.tt("pool", hb[:], t2[:], B_ap[:], ALU.add, [t_t2, t_B], [t_hb])
        hT, t_hT = self.transpose8(c, p, pT, hb, t_hb)
        self.dma("sp", self.h2T_d[:, :, rows], hT[:], reads=[t_hT])

    def phase_gqa_proj(self, layer, j, xsrc, lay):
        KT, V_aug = lay.KT, lay.V
        with self.scope() as sc:
            c = self.common(sc)
            p = self.mk_pre(sc, c)
            mods = self.load_mod(sc, layer, [(0, "shift", None), (1, "scale", 0)])
            wq = sc.sb("wqkv", [128, 8, 1536], BF16)
            t_w = self.S.tile()
            for nb in range(3):
                self.dma("pool", wq[:, :, nb * 512:(nb + 1) * 512],
                         self.a_w_qkv[j, :, nb * 512:(nb + 1) * 512].rearrange("(c p) n -> p c n", p=128), writes=[t_w])
            gain = sc.sb("gain", [128, 20, 64], F32)
            t_g = self.S.tile()
            for hh in range(20):
                src = self.a_g_q if hh < 16 else self.a_g_k
                self.dma("sp", gain[:, hh, :], src[j:j + 1, :].partition_broadcast(128), writes=[t_g])
            cosT = sc.sb("cosT", [128, NT, 64], F32)
            sinT = sc.sb("sinT", [128, NT, 64], F32)
            t_tab = self.S.tile()
            self.dma("sp", cosT[:], self.ropeA_cos.rearrange("(t p) d -> p t d", p=128), writes=[t_tab])
            self.dma("sp", sinT[:], self.ropeA_sin.rearrange("(t p) d -> p t d", p=128), writes=[t_tab])
            self.memset("pool", V_aug[:, :, :, 64:65], 1.0, [lay.t_V])
            pT = sc.rot("pT", [128, 8, 128], BF16, 2, psum=True)
            qps = sc.rot("qps", [128, 1536], F32, 2, psum=True)
            sq = sc.rot("sq", [128, 1280], F32, 1)
            ssq = sc.rot("ssq", [128, 20], F32, 2)
            qn = sc.rot("qn", [128, 20, 64], F32, 2)
            qa = sc.rot("qa", [128, 20, 64], F32, 1)
            qb = sc.rot("qb", [128, 20, 64], F32, 1)
            qr = sc.rot("qr", [128, 20, 64], BF16, 2)
            kd = sc.rot("kd", [128, 4, 2, 64], BF16, 2)
            qst = sc.rot("qst", [128, 8, 128], BF16, 2)
            for tt_ in range(NT):
                r_ = 1 if tt_ < NCTX_T else 0
                hT, t_hT = self.prenorm(c, p, pT, xsrc, tt_, mods[(1, r_)], mods[(0, r_)])
                ps, t_ps = qps.next()
                for nb in range(3):
                    for k in range(8):
                        self.mm(ps[:, nb * 512:(nb + 1) * 512], hT[:, k, :], wq[:, k, nb * 512:(nb + 1) * 512],
                                k == 0, k == 7, [t_hT, t_w], [t_ps])
                s_, t_s = sq.next()
                self.act(s_[:], ps[:, 0:1280], AF.Square, [t_ps], [t_s])
                ss_, t_ss = ssq.next()
                self.S.op("dve", lambda e, o=ss_, i=s_: e.tensor_reduce(
                    out=o[:], in_=i[:].rearrange("p (h d) -> p h d", d=64), axis=AX.X, op=ALU.add), [t_s], [t_ss])
                self.act(ss_[:], ss_[:], AF.Sqrt, [t_ss], [t_ss], scale=1.0 / 64, bias=EPS)
                self.recip(ss_[:], ss_[:], [t_ss], [t_ss])
                q_, t_q = qn.next()
                self.tt("dve", q_[:], ps[:, 0:1280].rearrange("p (h d) -> p h d", d=64),
                        ss_[:].unsqueeze(2).to_broadcast([128, 20, 64]), ALU.mult, [t_ps, t_ss], [t_q])
                self.tt("pool", q_[:], q_[:], gain[:], ALU.mult, [t_q, t_g], [t_q])
                a_, t_a = qa.next()
                self.tt("dve", a_[:], q_[:], cosT[:, tt_, :].unsqueeze(1).to_broadcast([128, 20, 64]), ALU.mult,
                        [t_q, t_tab], [t_a])
                b_, t_b = qb.next()
                q5 = q_[:].rearrange("p h (b s f) -> p h b s f", b=2, s=2)
                b5 = b_[:].rearrange("p h (b s f) -> p h b s f", b=2, s=2)
                s5 = sinT[:, tt_, :].rearrange("p (b s f) -> p b s f", b=2, s=2)
                for s in range(2):
                    self.tt("pool", b5[:, :, :, s, :], q5[:, :, :, 1 - s, :],
                            s5[:, :, s, :].unsqueeze(1).to_broadcast([128, 20, 2, 16]), ALU.mult, [t_q, t_tab], [t_b])
                o_, t_o = qr.next()
                self.tt("dve", o_[:], a_[:], b_[:], ALU.add, [t_a, t_b], [t_o])
                self.copy("act", V_aug[:, tt_, :, 0:64], ps[:, 1280:1536].rearrange("p (h d) -> p h d", d=64),
                          [t_ps], [lay.t_V])
                kd_, t_kd = kd.next()
                for dd in range(2):
                    self.copy("pool", kd_[:, :, dd, :], o_[:, 16:20, :], [t_o], [t_kd])
                pk, t_pk = pT.next()
                for kv in range(4):
                    self.tr(pk[:, kv, :], kd_[:, kv, :, :].rearrange("p a d -> p (a d)"), c.identb[:], [t_kd, c.t_id],
                            [t_pk])
                self.copy("act", KT[:, :, tt_ * 128:(tt_ + 1) * 128], pk[:, 0:4, :], [t_pk], [lay.t_KT])
                pq, t_pq = pT.next()
                of = o_[:].rearrange("p h d -> p (h d)")
                for k in range(8):
                    self.tr(pq[:, k, :], of[:, k * 128:(k + 1) * 128], c.identb[:], [t_o, c.t_id], [t_pq])
                st, t_st = qst.next()
                self.copy("dve", st[:], pq[:], [t_pq], [t_st])
                self.dma("sp", self.QT_d[:, :, tt_ * 128:(tt_ + 1) * 128].rearrange("c p n -> p c n"), st[:],
                         reads=[t_st])

    def phase_attn(self, kind, layer, j, xsrc, lay, last, moe):
        scale = 0.125 if kind == "gqa" else (96.0 ** -0.5)
        w_o_d = self.a_w_o[j] if kind == "gqa" else self.m_w_o[0]
        with self.scope() as sc:
            c = self.common(sc, njunk=1024)
            ep = self.mk_epi(sc, c, moe)
            wl = [(2, "gate", 1), (3, "shift", None), (4, "scale", 2)]
            mods = self.load_mod(sc, layer, wl, ctx_rows=not last)
            wo = sc.sb("wo", [128, 8, D], BF16)
            t_wo = self.S.tile()
            self.dma("pool", wo[:], w_o_d.rearrange("(c p) n -> p c n", p=128), writes=[t_wo])
            Sps = sc.ps("Sps", [128, 2, 512], F32)
            t_S = [self.S.tile(), self.S.tile()]
            Ops = [sc.ps(f"Ops{i}", [128, 512], F32) for i in range(4)]
            t_O = [self.S.tile() for _ in range(4)]
            pT = sc.rot("pT", [128, 8, 128], BF16, 2, psum=True)
            PT = sc.rot("PT", [128, 512], BF16, 4)
            qp = sc.rot("qp", [128, 512], BF16, 3)
            attn = sc.rot("attn", [128, 4, D], BF16, 2)
            aT = sc.rot("aT", [128, 8, 128], BF16, 2)
            rec = sc.rot("rec", [128, 1], F32, 8)
            if kind == "mla":
                qrp = sc.rot("qrp", [64, 512], BF16, 3)
                knp = sc.rot("knp", [128, T], BF16, 2)
                vpp = sc.rot("vpp", [128, NT, 130], BF16, 2)
            groups = []
            if not last:
                groups.append((0, 2, [0, 1]))
            for g in range(8):
                groups.append((256 + g * 512, 4, list(range(NT))))
            sidx = 0
            for (tok0, nsub, kts) in groups:
                N = nsub * 128
                nk = len(kts)
                at, t_at = attn.next()
                for pj in range(8):
                    q_, t_q = qp.next()
                    self.dma("sp", q_[:, 0:N], self.QT_d[pj, :, tok0:tok0 + N], writes=[t_q])
                    if kind == "mla":
                        qr_, t_qr = qrp.next()
                        self.dma("sp", qr_[:, 0:N], self.QrT_d[pj, :, tok0:tok0 + N], writes=[t_qr])
                        kn_, t_kn = knp.next()
                        self.dma("sp", kn_[:, 0:nk * 128], self.KnT_d[pj, :, 0:nk * 128], writes=[t_kn])
                        vp_, t_vp = vpp.next()
                        self.dma("sp", vp_[:, 0:nk, :],
                                 self.V_d[0:nk, :, pj * 130:(pj + 1) * 130].rearrange("t p c -> p t c"), writes=[t_vp])
                    for r in range(2):
                        h = 2 * pj + r
                        kvh = h // 4
                        for ki, kt in enumerate(kts):
                            sb_ = sidx % 2
                            sidx += 1
                            s_ap = Sps[:, sb_, 0:N]
                            ksl = slice(kt * 128, (kt + 1) * 128)
                            if kind == "gqa":
                                self.mm(s_ap, lay.KT[64 * r:64 * r + 64, kvh, ksl], q_[64 * r:64 * r + 64, 0:N],
                                        True, True, [t_q], [t_S[sb_]])
                            else:
                                self.mm(s_ap, kn_[64 * r:64 * r + 64, ksl], q_[64 * r:64 * r + 64, 0:N],
                                        True, False, [t_q, t_kn], [t_S[sb_]])
                                self.mm(s_ap, lay.KrT[32 * r:32 * r + 32, ksl], qr_[32 * r:32 * r + 32, 0:N],
                                        False, True, [t_qr], [t_S[sb_]])
                            pt, t_pt = PT.next()
                            self.act(pt[:, 0:N], s_ap, AF.Exp, [t_S[sb_]], [t_pt], scale=scale)
                            for sub in range(nsub):
                                if kind == "gqa":
                                    v_ap = lay.V[:, kt, kvh, :]
                                    rd = [t_pt]
                                else:
                                    v_ap = vp_[:, kt, r * 65:(r + 1) * 65]
                                    rd = [t_pt, t_vp]
                                self.mm(Ops[sub][:, 0:65], pt[:, sub * 128:(sub + 1) * 128], v_ap,
                                        ki == 0, ki == nk - 1, rd, [t_O[sub]])
                        for sub in range(nsub):
                            rc, t_rc = rec.next()
                            self.recip(rc[:], Ops[sub][:, 64:65], [t_O[sub]], [t_rc])
                            self.ts("dve", at[:, sub, h * 64:(h + 1) * 64], Ops[sub][:, 0:64], rc[:], ALU.mult,
                                    [t_O[sub], t_rc], [t_at])
                for sub in range(nsub):
                    tt_ = tok0 // 128 + sub
                    r_ = 1 if tt_ < NCTX_T else 0
                    ps, t_ps = pT.next()
                    for k in range(8):
                        self.tr(ps[:, k, :], at[:, sub, k * 128:(k + 1) * 128], c.identb[:], [t_at, c.t_id], [t_ps])
                    a_, t_a = aT.next()
                    self.copy("act", a_[:], ps[:], [t_ps], [t_a])
                    y_ap = Sps[:].rearrange("p a n -> p (a n)")
                    for nb in range(2):
                        for k in range(8):
                            self.mm(Sps[:, nb, :], a_[:, k, :], wo[:, k, nb * 512:(nb + 1) * 512], k == 0, k == 7,
                                    [t_a, t_wo], [t_S[nb]])
                    self.epilogue(c, ep, pT, xsrc, tt_, y_ap, t_S, mods[(2, r_)], mods[(4, r_)], mods[(3, r_)],
                                  True, moe)

    def phase_fourier_1(self, layer, xsrc):
        with self.scope() as sc:
            c = self.common(sc)
            p = self.mk_pre(sc, c)
            mods = self.load_mod(sc, layer, [(0, "shift", None), (1, "scale", 0)])
            cs = sc.sb("cs", [128, 2, 512], BF16)
            t_cs = self.S.tile()
            self.dma("pool", cs[:], self.dft256.rearrange("(c p) n -> p c n", p=128), writes=[t_cs])
            pT = sc.rot("pT", [128, 8, 128], BF16, 2, psum=True)
            xps = sc.rot("xps", [128, 4, 512], F32, 1, psum=True)
            xsb = sc.rot("xsb", [128, 4, 512], BF16, 2)
            for tt_ in range(NT):
                r_ = 1 if tt_ < NCTX_T else 0
                hT, t_hT = self.prenorm(c, p, pT, xsrc, tt_, mods[(1, r_)], mods[(0, r_)])
                ps, t_ps = xps.next()
                for g in range(4):
                    for k in range(2):
                        self.mm(ps[:, g, :], hT[:, 2 * g + k, :], cs[:, k, :], k == 0, k == 1, [t_hT, t_cs], [t_ps])
                xs, t_xs = xsb.next()
                self.copy("act", xs[:], ps[:], [t_ps], [t_xs])
                self.dma("sp", self.XCS_d[:, tt_ * 128:(tt_ + 1) * 128, :].rearrange("g p n -> p g n"), xs[:],
                         reads=[t_xs])

    def phase_fourier_2(self, layer, xsrc, last, moe):
        with self.scope() as sc:
            c = self.common(sc, njunk=1024)
            ep = self.mk_epi(sc, c, moe)
            mods = self.load_mod(sc, layer, [(2, "gate", 1), (3, "shift", None), (4, "scale", 2)], ctx_rows=not last)
            fw = sc.sb("fw", [128, 8, D], BF16)
            t_fw = self.S.tile()
            self.dma("pool", fw[:], self.f_w[0].rearrange("(c p) n -> p c n", p=128), writes=[t_fw])
            Tc = sc.sb("Tc", [128, 32, 512], BF16)
            Ts = sc.sb("Ts", [128, 32, 512], BF16)
            t_T = self.S.tile()
            X = sc.rot("X", [128, 32, 512], BF16, 1)
            fT = sc.rot("fT", [128, 8, 512], BF16, 1)
            pT = sc.rot("pT", [128, 8, 128], BF16, 2, psum=True)
            fps = sc.rot("fps", [128, 512], F32, 2, psum=True)
            yps = sc.ps("yps", [128, 2, 512], F32)
            t_y = [self.S.tile(), self.S.tile()]
            groups = []
            if not last:
                groups.append(("ctx", 0, 2, 2))
            for g in range(8):
                groups.append(("lat", 256 + g * 512, 4, 32))
            for (kind, tok0, nsub, na) in groups:
                N = nsub * 128
                if kind == "ctx":
                    self.dma("sp", Tc[:, 0:2, 0:256], self.cos256.rearrange("(a p) n -> p a n", p=128), writes=[t_T])
                    self.dma("sp", Ts[:, 0:2, 0:256], self.nsin256.rearrange("(a p) n -> p a n", p=128), writes=[t_T])
                    fscale = 1.0 / 256
                else:
                    k0 = tok0 - 256
                    self.dma("sp", Tc[:], self.cos4096[:, k0:k0 + 512].rearrange("(a p) n -> p a n", p=128),
                             writes=[t_T])
                    self.dma("sp", Ts[:], self.nsin4096[:, k0:k0 + 512].rearrange("(a p) n -> p a n", p=128),
                             writes=[t_T])
                    fscale = 1.0 / 1024
                f_, t_f = fT.next()
                for g in range(4):
                    x_, t_x = X.next()
                    if kind == "ctx":
                        self.dma("sp", x_[:, 0:2, :], self.XCS_d[g, 0:256, :].rearrange("(a p) n -> p a n", p=128),
                                 writes=[t_x])
                    else:
                        self.dma("sp", x_[:], self.XCS_d[g, 256:T, :].rearrange("(a p) n -> p a n", p=128),
                                 writes=[t_x])
                    for lc in range(2):
                        ps, t_ps = fps.next()
                        n_mm = 2 * na
                        i_mm = 0
                        for (tab, off) in ((Tc, 0), (Ts, 256)):
                            for a in range(na):
                                self.mm(ps[:, 0:N], x_[:, a, off + lc * 128:off + (lc + 1) * 128], tab[:, a, 0:N],
                                        i_mm == 0, i_mm == n_mm - 1, [t_x, t_T], [t_ps])
                                i_mm += 1
                        self.act(f_[:, 2 * g + lc, 0:N], ps[:, 0:N], AF.Copy, [t_ps], [t_f], scale=fscale)
                for sub in range(nsub):
                    tt_ = tok0 // 128 + sub
                    r_ = 1 if tt_ < NCTX_T else 0
                    for nb in range(2):
                        for k in range(8):
                            self.mm(yps[:, nb, :], f_[:, k, sub * 128:(sub + 1) * 128], fw[:, k, nb * 512:(nb + 1) * 512],
                                    k == 0, k == 7, [t_f, t_fw], [t_y[nb]])
                    self.epilogue(c, ep, pT, xsrc, tt_, yps[:].rearrange("p a n -> p (a n)"), t_y, mods[(2, r_)],
                                  mods[(4, r_)], mods[(3, r_)], True, moe)

    def phase_mla_proj(self, layer, xsrc, lay):
        with self.scope() as sc:
            c = self.common(sc)
            p = self.mk_pre(sc, c)
            mods = self.load_mod(sc, layer, [(0, "shift", None), (1, "scale", 0)])
            t_w = self.S.tile()
            wdn = sc.sb("wdn", [128, 8, 672], BF16)
            self.dma("pool", wdn[:], self.m_w_down[0].rearrange("(c p) n -> p c n", p=128), writes=[t_w])
            wqn = sc.sb("wqn", [128, 3, 1024], BF16)
            self.dma("pool", wqn[:], self.m_wuqn.rearrange("(c p) n -> p c n", p=128), writes=[t_w])
            wqr = sc.sb("wqr", [128, 3, 512], BF16)
            self.dma("pool", wqr[:], self.m_wuqr.rearrange("(c p) n -> p c n", p=128), writes=[t_w])
            wqs = sc.sb("wqs", [128, 3, 512], BF16)
            self.dma("pool", wqs[:], self.m_wuqrs.rearrange("(c p) n -> p c n", p=128), writes=[t_w])
            wuk = sc.sb("wuk", [128, 2, 1024], BF16)
            self.dma("pool", wuk[:], self.m_wuk.rearrange("(c p) n -> p c n", p=128), writes=[t_w])
            wuv = sc.sb("wuv", [128, 2, 1024], BF16)
            self.dma("pool", wuv[:], self.m_wuv.rearrange("(c p) n -> p c n", p=128), writes=[t_w])
            gcq = sc.sb("gcq", [128, 384], F32)
            gckv = sc.sb("gckv", [128, 256], F32)
            t_g = self.S.tile()
            self.dma("sp", gcq[:], self.m_g_cq[0:1, :].partition_broadcast(128), writes=[t_g])
            self.dma("sp", gckv[:], self.m_g_ckv[0:1, :].partition_broadcast(128), writes=[t_g])
            cosT = sc.sb("cosT", [128, NT, 32], F32)
            sinT = sc.sb("sinT", [128, NT, 32], F32)
            t_tab = self.S.tile()
            self.dma("sp", cosT[:], self.ropeM_cos.rearrange("(t p) d -> p t d", p=128), writes=[t_tab])
            self.dma("sp", sinT[:], self.ropeM_sin.rearrange("(t p) d -> p t d", p=128), writes=[t_tab])
            pT = sc.rot("pT", [128, 8, 128], BF16, 2, psum=True)
            dps = sc.rot("dps", [128, 1024], F32, 1, psum=True)
            pps = sc.rot("pps", [128, 512], F32, 2, psum=True)
            vps = sc.rot("vps", [128, 1024], F32, 1, psum=True)
            cqn = sc.rot("cqn", [128, 384], BF16, 2)
            ckvn = sc.rot("ckvn", [128, 256], BF16, 2)
            cqT = sc.rot("cqT", [128, 3, 512], BF16, 2)
            ckT = sc.rot("ckT", [128, 2, 512], BF16, 2)
            ka = sc.rot("ka", [128, 32], F32, 2)
            kb = sc.rot("kb", [128, 32], F32, 2)
            kd = sc.rot("kd", [128, 2, 32], BF16, 2)
            st = sc.rot("st", [128, 512], BF16, 3)
            ctab = sc.rot("ctab", [64, 512], F32, 2)
            stab = sc.rot("stab", [64, 512], F32, 2)
            ra = sc.rot("ra", [64, 512], F32, 2)
            rb = sc.rot("rb", [64, 512], F32, 2)
            rst = sc.rot("rst", [64, 512], BF16, 2)
            vst = sc.rot("vst", [128, 16, 65], BF16, 2)
            for i in range(2):
                self.memset("pool", vst.bufs[i][:, :, 64:65], 1.0, [vst.tiles[i]])
            groups = [(0, 2)] + [(2 + 4 * g, 4) for g in range(8)]
            for (t0, nsub) in groups:
                N = nsub * 128
                tok0 = t0 * 128
                cq_T, t_cqT = cqT.next()
                ck_T, t_ckT = ckT.next()
                for sub in range(nsub):
                    tt_ = t0 + sub
                    r_ = 1 if tt_ < NCTX_T else 0
                    hT, t_hT = self.prenorm(c, p, pT, xsrc, tt_, mods[(1, r_)], mods[(0, r_)])
                    d_, t_d = dps.next()
                    for (o0, o1) in ((0, 512), (512, 672)):
                        for k in range(8):
                            self.mm(d_[:, o0:o1], hT[:, k, :], wdn[:, k, o0:o1], k == 0, k == 7, [t_hT, t_w], [t_d])
                    r1, t_r1 = self.rstd_of(c, d_[:, 0:384], 384, [t_d])
                    cq_, t_cq = cqn.next()
                    self.stt(cq_[:], d_[:, 0:384], r1[:], gcq[:], ALU.mult, ALU.mult, [t_d, t_r1, t_g], [t_cq])
                    r2, t_r2 = self.rstd_of(c, d_[:, 384:640], 256, [t_d])
                    ck_, t_ck = ckvn.next()
                    self.stt(ck_[:], d_[:, 384:640], r2[:], gckv[:], ALU.mult, ALU.mult, [t_d, t_r2, t_g], [t_ck])
                    a_, t_a = ka.next()
                    self.tt("dve", a_[:], d_[:, 640:672], cosT[:, tt_, :], ALU.mult, [t_d, t_tab], [t_a])
                    b_, t_b = kb.next()
                    d4 = d_[:, 640:672].rearrange("p (b s f) -> p b s f", b=2, s=2)
                    b4 = b_[:].rearrange("p (b s f) -> p b s f", b=2, s=2)
                    s4 = sinT[:, tt_, :].rearrange("p (b s f) -> p b s f", b=2, s=2)
                    for s in range(2):
                        self.tt("dve", b4[:, :, s, :], d4[:, :, 1 - s, :], s4[:, :, s, :], ALU.mult, [t_d, t_tab], [t_b])
                    kd_, t_kd = kd.next()
                    for dd in range(2):
                        self.tt("pool", kd_[:, dd, :], a_[:], b_[:], ALU.add, [t_a, t_b], [t_kd])
                    ps, t_ps = pT.next()
                    for k in range(3):
                        self.tr(ps[:, k, :], cq_[:, k * 128:(k + 1) * 128], c.identb[:], [t_cq, c.t_id], [t_ps])
                    for k in range(2):
                        self.tr(ps[:, 3 + k, :], ck_[:, k * 128:(k + 1) * 128], c.identb[:], [t_ck, c.t_id], [t_ps])
                    self.tr(ps[0:64, 5, :], kd_[:].rearrange("p a d -> p (a d)"), c.identb[:], [t_kd, c.t_id], [t_ps])
                    self.copy("act", cq_T[:, :, sub * 128:(sub + 1) * 128], ps[:, 0:3, :], [t_ps], [t_cqT])
                    self.copy("act", ck_T[:, :, sub * 128:(sub + 1) * 128], ps[:, 3:5, :], [t_ps], [t_ckT])
                    self.copy("dve", lay.KrT[:, tt_ * 128:(tt_ + 1) * 128], ps[0:64, 5, :], [t_ps], [lay.t_KrT])
                for sub in range(nsub):
                    tt_ = t0 + sub
                    v_, t_v = vps.next()
                    for nb in range(2):
                        for k in range(2):
                            self.mm(v_[:, nb * 512:(nb + 1) * 512], ck_T[:, k, sub * 128:(sub + 1) * 128],
                                    wuv[:, k, nb * 512:(nb + 1) * 512], k == 0, k == 1, [t_ckT, t_w], [t_v])
                    vs, t_vs = vst.next()
                    self.copy("act", vs[:, :, 0:64], v_[:].rearrange("p (h d) -> p h d", d=64), [t_v], [t_vs])
                    self.dma("sp", self.V_d[tt_, :, :], vs[:].rearrange("p h d -> p (h d)"), reads=[t_vs])
                ct_, t_ct = ctab.next()
                st_, t_stb = stab.next()
                self.dma("sp", ct_[:, 0:N], self.ropeMT_cos[:, tok0:tok0 + N], writes=[t_ct])
                self.dma("sp", st_[:, 0:N], self.ropeMT_sin[:, tok0:tok0 + N], writes=[t_stb])
                for pj in range(8):
                    ps, t_ps = pps.next()
                    for k in range(3):
                        self.mm(ps[:, 0:N], wqn[:, k, pj * 128:(pj + 1) * 128], cq_T[:, k, 0:N], k == 0, k == 2,
                                [t_cqT, t_w], [t_ps])
                    s_, t_s = st.next()
                    self.copy("act", s_[:, 0:N], ps[:, 0:N], [t_ps], [t_s])
                    self.dma("sp", self.QT_d[pj, :, tok0:tok0 + N], s_[:, 0:N], reads=[t_s])
                    ps, t_ps = pps.next()
                    for k in range(2):
                        self.mm(ps[:, 0:N], wuk[:, k, pj * 128:(pj + 1) * 128], ck_T[:, k, 0:N], k == 0, k == 1,
                                [t_ckT, t_w], [t_ps])
                    s_, t_s = st.next()
                    self.copy("act", s_[:, 0:N], ps[:, 0:N], [t_ps], [t_s])
                    self.dma("sp", self.KnT_d[pj, :, tok0:tok0 + N], s_[:, 0:N], reads=[t_s])
                    ps, t_ps = pps.next()
                    for k in range(3):
                        self.mm(ps[0:64, 0:N], wqr[:, k, pj * 64:(pj + 1) * 64], cq_T[:, k, 0:N], k == 0, k == 2,
                                [t_cqT, t_w], [t_ps])
                    a_, t_a = ra.next()
                    self.tt("dve", a_[:, 0:N], ps[0:64, 0:N], ct_[:, 0:N], ALU.mult, [t_ps, t_ct], [t_a])
                    ps, t_ps = pps.next()
                    for k in range(3):
                        self.mm(ps[0:64, 0:N], wqs[:, k, pj * 64:(pj + 1) * 64], cq_T[:, k, 0:N], k == 0, k == 2,
                                [t_cqT, t_w], [t_ps])
                    b_, t_b = rb.next()
                    self.tt("dve", b_[:, 0:N], ps[0:64, 0:N], st_[:, 0:N], ALU.mult, [t_ps, t_stb], [t_b])
                    o_, t_o = rst.next()
                    self.tt("pool", o_[:, 0:N], a_[:, 0:N], b_[:, 0:N], ALU.add, [t_a, t_b], [t_o])
                    self.dma("sp", self.QrT_d[pj, :, tok0:tok0 + N], o_[:, 0:N], reads=[t_o])

    def phase_ffn(self, layer, moe, last, xsrc_unused):
        idx = layer // 2
        tiles = list(range(NCTX_T, NT)) if last else list(range(NT))
        nh = len(tiles) // 2
        halves = [tiles[:nh], tiles[nh:]]
        E = NE if moe else 1
        F = FF_E if moe else FF_D
        nfb = F // 256
        for hi, half in enumerate(halves):
            nt = len(half)
            tok0 = half[0] * 128
            NTOK = nt * 128
            with contextlib.ExitStack() as hes:
                yacc = hes.enter_context(self.nc.sbuf_tensor(f"yacc{layer}_{hi}", [128, nt, D], F32))
                with self.scope() as sc:
                    c = self.common(sc, njunk=1024)
                    h2T = sc.sb("h2T", [128, 8, NTOK], BF16)
                    t_h = self.S.tile()
                    self.dma("sp", h2T[:], self.h2T_d[:, :, tok0:tok0 + NTOK], writes=[t_h])
                    t_ya = [self.S.tile() for _ in range(nt)]
                    gps = sc.rot("gps", [128, 512], F32, 2, psum=True)
                    ups = sc.rot("ups", [128, 512], F32, 2, psum=True)
                    yps = sc.rot("yps", [128, 2, 512], F32, 1, psum=True)
                    misc = sc.ps("misc", [128, 512], F32)
                    t_misc = self.S.tile()
                    wg = sc.rot("wg", [128, 8, 256], BF16, 3)
                    wu = sc.rot("wu", [128, 8, 256], BF16, 3)
                    wd = sc.rot("wd", [128, 2, D], BF16, 3)
                    sg = sc.rot("sg", [128, 512], F32, 2)
                    aT = sc.rot("aT", [128, 2, 512], BF16, 2)
                    if moe:
                        t1 = sc.rot("t1", [128, 512], F32, 2)
                        gbs = sc.rot("gbs", [128, NTOK], F32, 2)
                        gatesT = sc.sb("gatesT", [NE, NTOK], F32)
                        t_gT = self.S.tile()
                        sel = sc.sb("sel", [NE, NE * 128], F32)
                        t_sel = self.S.tile()
                        self.dma("sp", sel[:], self.sel_d[:, :], writes=[t_sel])
                        self.router(sc, c, idx, half, tok0, gatesT, t_gT, yps, misc, t_misc)
                    groups = []
                    g0 = 0
                    while g0 < NTOK:
                        n = min(512, NTOK - g0)
                        groups.append((g0, n))
                        g0 += n
                    for e in range(E):
                        if moe:
                            wgu_d = self.e_w_gu[idx, e]
                            wdn_d = self.e_w_down[idx, e]
                            gb_, t_gb = gbs.next()
                            for (g0, n) in groups:
                                self.mm(misc[:, 0:n], sel[:, e * 128:(e + 1) * 128], gatesT[:, g0:g0 + n], True, True,
                                        [t_sel, t_gT], [t_misc])
                                self.copy("act", gb_[:, g0:g0 + n], misc[:, 0:n], [t_misc], [t_gb])
                        else:
                            wgu_d = self.d_w_gu[idx]
                            wdn_d = self.d_w_down[idx]
                        for fb in range(nfb):
                            wg_, t_wg = wg.next()
                            wu_, t_wu = wu.next()
                            wd_, t_wd = wd.next()
                            f0 = fb * 256
                            self.dma("pool", wg_[:], wgu_d[:, f0:f0 + 256].rearrange("(c p) n -> p c n", p=128),
                                     writes=[t_wg])
                            self.dma("pool", wu_[:], wgu_d[:, F + f0:F + f0 + 256].rearrange("(c p) n -> p c n", p=128),
                                     writes=[t_wu])
                            self.dma("pool", wd_[:], wdn_d[f0:f0 + 256, :].rearrange("(c p) n -> p c n", p=128),
                                     writes=[t_wd])
                            first = (e == 0 and fb == 0)
                            for (g0, n) in groups:
                                a_, t_a = aT.next()
                                for fc in range(2):
                                    g_, t_g = gps.next()
                                    u_, t_u = ups.next()
                                    for k in range(8):
                                        self.mm(g_[:, 0:n], wg_[:, k, fc * 128:(fc + 1) * 128], h2T[:, k, g0:g0 + n],
                                                k == 0, k == 7, [t_wg, t_h], [t_g])
                                    for k in range(8):
                                        self.mm(u_[:, 0:n], wu_[:, k, fc * 128:(fc + 1) * 128], h2T[:, k, g0:g0 + n],
                                                k == 0, k == 7, [t_wu, t_h], [t_u])
                                    s_, t_s = sg.next()
                                    self.act(s_[:, 0:n], g_[:, 0:n], AF.Silu, [t_g], [t_s])
                                    if moe:
                                        t1_, t_t1 = t1.next()
                                        self.tt("dve", t1_[:, 0:n], s_[:, 0:n], u_[:, 0:n], ALU.mult, [t_s, t_u], [t_t1])
                                        self.tt("dve", a_[:, fc, 0:n], t1_[:, 0:n], gb_[:, g0:g0 + n], ALU.mult,
                                                [t_t1, t_gb], [t_a])
                                    else:
                                        self.tt("dve", a_[:, fc, 0:n], s_[:, 0:n], u_[:, 0:n], ALU.mult, [t_s, t_u],
                                                [t_a])
                                for sub in range(n // 128):
                                    ti = g0 // 128 + sub
                                    y_, t_y = yps.next()
                                    for nb in range(2):
                                        for fc in range(2):
                                            self.mm(y_[:, nb, :], a_[:, fc, sub * 128:(sub + 1) * 128],
                                                    wd_[:, fc, nb * 512:(nb + 1) * 512], fc == 0, fc == 1, [t_a, t_wd],
                                                    [t_y])
                                    yf = y_[:].rearrange("p a n -> p (a n)")
                                    if first:
                                        self.copy("act", yacc[:, ti, :], yf, [t_y], [t_ya[ti]])
                                    else:
                                        self.tt("dve", yacc[:, ti, :], yacc[:, ti, :], yf, ALU.add, [t_y, t_ya[ti]],
                                                [t_ya[ti]])
                with self.scope() as sc:
                    c = self.common(sc, njunk=1024)
                    mods = self.load_mod(sc, layer, [(5, "gate", 3)], ctx_rows=not last)
                    xr = sc.rot("xr", [128, D], F32, 3)
                    tm = sc.rot("tm", [128, D], F32, 2)
                    xo = sc.rot("xo", [128, D], F32, 3)
                    for ti, tt_ in enumerate(half):
                        r_ = 1 if tt_ < NCTX_T else 0
                        G_ap, t_G = mods[(5, r_)]
                        rows = slice(tt_ * 128, (tt_ + 1) * 128)
                        x_, t_x = xr.next()
                        self.dma("sp", x_[:], self.xres[rows, :], writes=[t_x])
                        r, t_r = self.rstd_of(c, yacc[:, ti, :], D, [])
                        t_, t_t = tm.next()
                        self.stt(t_[:], yacc[:, ti, :], r[:], G_ap[:], ALU.mult, ALU.mult, [t_r, t_G], [t_t])
                        o_, t_o = xo.next()
                        self.tt("pool", o_[:], x_[:], t_[:], ALU.add, [t_x, t_t], [t_o])
                        if layer == self.nlayers - 1:
                            if tt_ >= NCTX_T:
                                self.dma("sp", self.out[(tt_ - NCTX_T) * 128:(tt_ - NCTX_T + 1) * 128, :], o_[:],
                                         reads=[t_o])
                            if self.debug:
                                self.dma("sp", self.xres[rows, :], o_[:], reads=[t_o])
                        else:
                            self.dma("sp", self.xres[rows, :], o_[:], reads=[t_o])

    def router(self, sc, c, idx, half, tok0, gatesT, t_gT, yps, misc, t_misc):
        wr = sc.sb("wr", [128, 8, NE], F32)
        t_wr = self.S.tile()
        self.dma("sp", wr[:], self.e_w_router[idx].rearrange("(c p) n -> p c n", p=128), writes=[t_wr])
        hf = sc.rot("rhf", [128, D], F32, 2)
        hfT = sc.rot("rhfT", [128, 8, 128], F32, 2)
        lg = sc.rot("lg", [128, NE], F32, 2)
        m8 = sc.rot("m8", [128, 8], F32, 2)
        dd = sc.rot("dd", [128, NE], F32, 2)
        ee = sc.rot("ee", [128, NE], F32, 2)
        mk = sc.rot("mk", [128, NE], F32, 2)
        sm = sc.rot("sm", [128, 1], F32, 2)
        gt = sc.rot("gt", [128, NE], F32, 2)
        for ti, tt_ in enumerate(half):
            rows = slice(tt_ * 128, (tt_ + 1) * 128)
            h_, t_h = hf.next()
            self.dma("sp", h_[:], self.h2f_d[rows, :], writes=[t_h])
            y_, t_y = yps.next()
            yT = y_[:].rearrange("p a (b n) -> p (a b) n", n=128)
            for k in range(8):
                self.tr(yT[:, k, :], h_[:, k * 128:(k + 1) * 128], c.identf[:], [t_h, c.t_id], [t_y])
            hT_, t_hT = hfT.next()
            self.copy("act", hT_[:], yT, [t_y], [t_hT])
            for k in range(8):
                self.mm(misc[:, 0:NE], hT_[:, k, :], wr[:, k, :], k == 0, k == 7, [t_hT, t_wr], [t_misc])
            l_, t_l = lg.next()
            self.copy("dve", l_[:], misc[:, 0:NE], [t_misc], [t_l])
            m_, t_m = m8.next()
            self.S.op("dve", lambda e, o=m_, i=l_: e.max(out=o[:], in_=i[:]), [t_l], [t_m])
            d_, t_d = dd.next()
            self.ts("dve", d_[:], l_[:], m_[:, 0:1], ALU.subtract, [t_l, t_m], [t_d])
            e_, t_e = ee.next()
            self.act(e_[:], d_[:], AF.Exp, [t_d], [t_e])
            k_, t_k = mk.next()
            self.ts("dve", k_[:], l_[:], m_[:, 1:2], ALU.is_ge, [t_l, t_m], [t_k])
            self.tt("dve", e_[:], e_[:], k_[:], ALU.mult, [t_e, t_k], [t_e])
            s_, t_s = sm.next()
            self.S.op("dve", lambda e, o=s_, i=e_: e.tensor_reduce(out=o[:], in_=i[:], axis=AX.X, op=ALU.add),
                      [t_e], [t_s])
            self.recip(s_[:], s_[:], [t_s], [t_s])
            g_, t_g = gt.next()
            self.ts("dve", g_[:], e_[:], s_[:], ALU.mult, [t_e, t_s], [t_g])
            self.tr(misc[0:NE, 128:256], g_[:], c.identf[:], [t_g, c.t_id], [t_misc])
            self.copy("dve", gatesT[:, ti * 128:(ti + 1) * 128], misc[0:NE, 128:256], [t_misc], [t_gT])

    def build(self):
        with self.es:
            self.declare()
            self.phase_mod()
            for layer in range(self.nlayers):
                last = layer == DEPTH - 1
                kind = layer % 3
                j = layer // 3
                moe = layer % 2 == 1
                xsrc = self.xin if layer == 0 else self.xres
                with contextlib.ExitStack() as les:
                    lay = type("L", (), {})()
                    if kind == 0:
                        lay.KT = les.enter_context(self.nc.sbuf_tensor(f"KT{layer}", [128, 4, T], BF16))
                        lay.V = les.enter_context(self.nc.sbuf_tensor(f"Vaug{layer}", [128, NT, 4, 65], BF16))
                        lay.t_KT = self.S.tile()
                        lay.t_V = self.S.tile()
                        self.phase_gqa_proj(layer, j, xsrc, lay)
                        self.phase_attn("gqa", layer, j, xsrc, lay, last, moe)
                    elif kind == 1:
                        self.phase_fourier_1(layer, xsrc)
                        self.phase_fourier_2(layer, xsrc, last, moe)
                    else:
                        lay.KrT = les.enter_context(self.nc.sbuf_tensor(f"KrT{layer}", [64, T], BF16))
                        lay.t_KrT = self.S.tile()
                        self.phase_mla_proj(layer, xsrc, lay)
                        self.phase_attn("mla", layer, j, xsrc, lay, last, moe)
                self.phase_ffn(layer, moe, last, xsrc)
        return self.nc


def _rope_tables(rot_dim):
    nf = rot_dim // 4
    inv = (10000.0 ** (-np.arange(nf, dtype=np.float32) / nf)).astype(np.float32)
    rows = np.repeat(np.arange(64, dtype=np.float32), 64)
    cols = np.tile(np.arange(64, dtype=np.float32), 64)
    ar = rows[:, None] * inv[None, :]
    ac = cols[:, None] * inv[None, :]
    cr, sr, cc, sc_ = np.cos(ar), np.sin(ar), np.cos(ac), np.sin(ac)
    cos = np.concatenate([cr, cr, cc, cc], axis=1).astype(np.float32)
    sin = np.concatenate([-sr, sr, -sc_, sc_], axis=1).astype(np.float32)
    cos = np.concatenate([np.ones((256, rot_dim), np.float32), cos], axis=0)
    sin = np.concatenate([np.zeros((256, rot_dim), np.float32), sin], axis=0)
    return cos, sin


_CONST = {}


def _constants():
    if _CONST:
        return _CONST
    c = {}
    c["ident"] = np.eye(128, dtype=np.float32)
    c["ropeA_cos"], c["ropeA_sin"] = _rope_tables(64)
    mc, ms = _rope_tables(32)
    c["ropeM_cos"], c["ropeM_sin"] = mc, ms
    c["ropeMT_cos"] = np.ascontiguousarray(np.concatenate([mc, mc], axis=1).T)
    c["ropeMT_sin"] = np.ascontiguousarray(np.concatenate([ms, ms], axis=1).T)
    k = np.arange(256)
    ang = 2 * np.pi * ((k[:, None] * k[None, :]) % 256) / 256.0
    c["dft256"] = np.concatenate([np.cos(ang), np.sin(ang)], axis=1).astype(np.float32)
    c["cos256"] = np.cos(ang).astype(ml_dtypes.bfloat16)
    c["nsin256"] = (-np.sin(ang)).astype(ml_dtypes.bfloat16)
    k = np.arange(4096, dtype=np.int64)
    ang = 2 * np.pi * ((k[:, None] * k[None, :]) % 4096) / 4096.0
    c["cos4096"] = np.cos(ang).astype(ml_dtypes.bfloat16)
    c["nsin4096"] = (-np.sin(ang)).astype(ml_dtypes.bfloat16)
    sel = np.zeros((NE, NE, 128), np.float32)
    for e in range(NE):
        sel[e, e, :] = 1.0
    c["sel"] = sel.reshape(NE, NE * 128)
    _CONST.update(c)
    return _CONST


def _prep_inputs(inputs):
    f = lambda a: np.ascontiguousarray(np.asarray(a, dtype=np.float32))
    shared = dict(_constants())
    for k_ in ["w_mod", "b_mod", "norm_g", "a_w_qkv", "a_g_q", "a_g_k", "a_w_o", "f_w", "m_w_down", "m_g_cq", "m_g_ckv",
               "m_w_o", "d_w_gu", "d_w_down", "e_w_router", "e_w_gu", "e_w_down"]:
        shared[k_] = f(inputs[k_])
    wuq = f(inputs["m_w_uq"])[0].reshape(384, 16, 96)
    shared["m_wuqn"] = np.ascontiguousarray(wuq[:, :, 0:64].reshape(384, 1024))
    rope = wuq[:, :, 64:96]
    shared["m_wuqr"] = np.ascontiguousarray(rope.reshape(384, 512))
    perm = np.array([(r + 8) if (r % 16) < 8 else (r - 8) for r in range(32)])
    shared["m_wuqrs"] = np.ascontiguousarray(rope[:, :, perm].reshape(384, 512))
    wukv = f(inputs["m_w_ukv"])[0].reshape(256, 16, 128)
    shared["m_wuk"] = np.ascontiguousarray(wukv[:, :, 0:64].reshape(256, 1024))
    shared["m_wuv"] = np.ascontiguousarray(wukv[:, :, 64:128].reshape(256, 1024))
    x = f(inputs["x"])
    ctx = f(inputs["ctx"])
    cc = f(inputs["c"])
    c_ctx = f(inputs["c_ctx"])
    maps = []
    for b in range(8):
        m = dict(shared)
        m["xin"] = np.ascontiguousarray(np.concatenate([ctx[b], x[b]], axis=0))
        cv = np.stack([cc[b], c_ctx], axis=1)
        m["cvT"] = np.ascontiguousarray(cv.reshape(8, 128, 2).transpose(1, 0, 2).reshape(128, 16))
        maps.append(m)
    return maps


_NC_CACHE = {}


def kernel(**inputs):
    maps = _prep_inputs(inputs)
    if "nc" not in _NC_CACHE:
        _NC_CACHE["nc"] = Builder().build()
    nc = _NC_CACHE["nc"]
    res = run_bass_kernel_spmd(nc, maps, core_ids=list(range(8)))
    out = np.stack([np.asarray(r["out"], dtype=np.float32) for r in res.results], axis=0)
    return out
```

```python
import contextlib
import numpy as np
import ml_dtypes
import concourse.bass as bass
import concourse.mybir as mybir
from concourse.bass_utils import run_bass_kernel_spmd

F32 = mybir.dt.float32
BF16 = mybir.dt.bfloat16
AF = mybir.ActivationFunctionType
ALU = mybir.AluOpType
AX = mybir.AxisListType

D = 1024
T = 4352
NT = 34
NCTX_T = 2
DEPTH = 4
EPS = 1e-6
FF_D = 2816
FF_E = 3584
NE = 8


class Tile:
    __slots__ = ("name", "writers", "readers")

    def __init__(self, name=""):
        self.name = name
        self.writers = {}
        self.readers = {}


class Op:
    __slots__ = ("stream", "tl", "deps", "fn", "marked", "epoch", "value", "seq")


class Timeline:
    def __init__(self, name, inc, limit):
        self.name = name
        self.inc = inc
        self.limit = limit
        self.ops = []
        self.epoch = None
        self.cnt = 0
        self.last = None


class Sched:
    STREAMS = ("sp", "act", "dve", "pool", "pe")
    ENG = {"sp": "sync", "act": "scalar", "dve": "vector", "pool": "gpsimd", "pe": "tensor"}

    def __init__(self, nc, semalloc, ndma=8):
        self.nc = nc
        self.semalloc = semalloc
        self.ops = {s: [] for s in self.STREAMS}
        self.tls = {s: Timeline(s, 1, 30000) for s in ("act", "dve", "pool", "pe")}
        self.dma_tls = {q: [Timeline(f"dma_{q}{i}", 16, 1800) for i in range(ndma)] for q in ("sp", "pool")}
        self.dma_rr = {q: 0 for q in self.dma_tls}
        self.all_tiles = []
        self.nseq = 0
        self.waited = {s: {} for s in self.STREAMS}
        self.nops = 0

    def all_tls(self):
        return list(self.tls.values()) + [t for q in self.dma_tls.values() for t in q]

    def tile(self, name=""):
        t = Tile(name)
        self.all_tiles.append(t)
        return t

    def _mk(self, stream, tl, fn, reads, writes, extra=()):
        op = Op()
        op.stream = stream
        op.tl = tl
        op.fn = fn
        op.marked = False
        op.epoch = None
        op.value = None
        op.seq = self.nseq
        self.nseq += 1
        deps = {}

        def add(d):
            k = d.tl
            if k not in deps or deps[k].seq < d.seq:
                deps[k] = d

        for t in reads:
            for d in t.writers.values():
                add(d)
        for t in writes:
            for d in t.writers.values():
                add(d)
            for d in t.readers.values():
                add(d)
        for d in extra:
            add(d)
        if stream == "pe" and self.tls["pe"] in deps:
            del deps[self.tls["pe"]]
        op.deps = list(deps.values())
        for d in op.deps:
            d.marked = True
        for t in reads:
            t.readers[tl] = op
        for t in writes:
            t.writers = {tl: op}
            t.readers = {}
        tl.ops.append(op)
        tl.last = op
        self.ops[stream].append(op)
        self.nops += 1
        return op

    def op(self, stream, fn, reads=(), writes=()):
        return self._mk(stream, self.tls[stream], fn, reads, writes)

    def dma(self, queue, fn, reads=(), writes=()):
        tls = self.dma_tls[queue]
        i = self.dma_rr[queue]
        self.dma_rr[queue] = (i + 1) % len(tls)
        tl = tls[i]
        prev = tl.ops[-1] if tl.ops else None
        op = self._mk(queue, tl, fn, reads, writes, extra=(prev,) if prev is not None else ())
        op.marked = True
        return op

    def barrier_and_emit(self):
        lasts = [tl.ops[-1] for tl in self.all_tls() if tl.ops]
        for s in self.STREAMS:
            op = Op()
            op.stream = s
            op.tl = None
            op.fn = None
            op.marked = False
            op.epoch = None
            op.value = None
            op.seq = self.nseq
            self.nseq += 1
            op.deps = [d for d in lasts if not (s == "pe" and d.tl is self.tls["pe"])]
            for d in op.deps:
                d.marked = True
            self.ops[s].append(op)
        for t in self.all_tiles:
            t.writers = {}
            t.readers = {}
        self._emit()

    def _emit(self):
        nc = self.nc
        for tl in self.all_tls():
            for op in tl.ops:
                if not op.marked:
                    continue
                if tl.epoch is None or tl.cnt >= tl.limit:
                    tl.epoch = self.semalloc(tl.name)
                    tl.cnt = 0
                tl.cnt += 1
                op.epoch = tl.epoch
                op.value = tl.cnt * tl.inc
        with nc.Block() as block:
            for s in self.STREAMS:
                ops = self.ops[s]
                if not ops:
                    continue

                def body(e, ops=ops, waited=self.waited[s]):
                    for op in ops:
                        for d in op.deps:
                            key = id(d.epoch)
                            if waited.get(key, 0) >= d.value:
                                continue
                            e.wait_ge(d.epoch, d.value)
                            waited[key] = d.value
                        if op.fn is None:
                            continue
                        ins = op.fn(e)
                        if op.marked:
                            ins.then_inc(op.epoch, op.tl.inc)

                getattr(block, self.ENG[s])(body)
        for s in self.STREAMS:
            self.ops[s] = []
        for tl in self.all_tls():
            tl.ops = []


class Rot:
    def __init__(self, S, alloc, name, shape, dt, n):
        self.bufs = [alloc(f"{name}{i}", shape, dt) for i in range(n)]
        self.tiles = [S.tile(f"{name}{i}") for i in range(n)]
        self.i = 0

    def next(self):
        b, t = self.bufs[self.i], self.tiles[self.i]
        self.i = (self.i + 1) % len(self.bufs)
        return b, t


class _Stop(Exception):
    pass


class Builder:
    def __init__(self, nlayers=DEPTH, debug=False, nphases=None):
        self.nphases = nphases
        self.phase_i = 0
        self.nlayers = nlayers
        self.debug = debug
        nc = self.nc = bass.Bass("TRN2", target_bir_lowering=False)
        self.es = contextlib.ExitStack()
        self.uid = 0

        def semalloc(name):
            self.uid += 1
            return self.es.enter_context(nc.semaphore(f"{name}_{self.uid}"))

        self.S = Sched(nc, semalloc)

    def din(self, name, shape, dt=F32):
        return self.nc.dram_tensor(name, list(shape), dt, kind="ExternalInput").ap()

    def dscr(self, name, shape, dt):
        return self.nc.dram_tensor(name, list(shape), dt, kind="Internal").ap()

    def declare(self):
        d = self.din
        self.xin = d("xin", [T, D])
        self.cvT = d("cvT", [128, 16])
        self.ident_d = d("ident", [128, 128])
        self.w_mod = d("w_mod", [DEPTH, D, 6 * D])
        self.b_mod = d("b_mod", [DEPTH, 6 * D])
        self.norm_g = d("norm_g", [DEPTH, 4, D])
        self.a_w_qkv = d("a_w_qkv", [2, D, 1536])
        self.a_g_q = d("a_g_q", [2, 64])
        self.a_g_k = d("a_g_k", [2, 64])
        self.a_w_o = d("a_w_o", [2, D, D])
        self.f_w = d("f_w", [1, D, D])
        self.m_w_down = d("m_w_down", [1, D, 672])
        self.m_g_cq = d("m_g_cq", [1, 384])
        self.m_g_ckv = d("m_g_ckv", [1, 256])
        self.m_wuqn = d("m_wuqn", [384, 1024])
        self.m_wuqr = d("m_wuqr", [384, 512])
        self.m_wuqrs = d("m_wuqrs", [384, 512])
        self.m_wuk = d("m_wuk", [256, 1024])
        self.m_wuv = d("m_wuv", [256, 1024])
        self.m_w_o = d("m_w_o", [1, D, D])
        self.d_w_gu = d("d_w_gu", [2, D, 2 * FF_D])
        self.d_w_down = d("d_w_down", [2, FF_D, D])
        self.e_w_router = d("e_w_router", [2, D, NE])
        self.e_w_gu = d("e_w_gu", [2, NE, D, 2 * FF_E])
        self.e_w_down = d("e_w_down", [2, NE, FF_E, D])
        self.ropeA_cos = d("ropeA_cos", [T, 64])
        self.ropeA_sin = d("ropeA_sin", [T, 64])
        self.ropeM_cos = d("ropeM_cos", [T, 32])
        self.ropeM_sin = d("ropeM_sin", [T, 32])
        self.ropeMT_cos = d("ropeMT_cos", [64, T])
        self.ropeMT_sin = d("ropeMT_sin", [64, T])
        self.dft256 = d("dft256", [256, 512])
        self.cos4096 = d("cos4096", [4096, 4096], BF16)
        self.nsin4096 = d("nsin4096", [4096, 4096], BF16)
        self.cos256 = d("cos256", [256, 256], BF16)
        self.nsin256 = d("nsin256", [256, 256], BF16)
        self.sel_d = d("sel", [NE, NE * 128])
        self.out = self.nc.dram_tensor("out", [4096, D], F32, kind="ExternalOutput").ap()
        s = self.dscr
        if self.debug:
            self.xres = self.nc.dram_tensor("xres", [T, D], F32, kind="ExternalOutput").ap()
        else:
            self.xres = s("xres", [T, D], F32)
        self.modrows = s("modrows", [DEPTH, 2, 6 * D], F32)
        self.QT_d = s("QT_d", [8, 128, T], BF16)
        self.QrT_d = s("QrT_d", [8, 64, T], BF16)
        self.KnT_d = s("KnT_d", [8, 128, T], BF16)
        self.V_d = s("V_d", [NT, 128, 16 * 65], BF16)
        self.XCS_d = s("XCS_d", [4, T, 512], BF16)
        self.h2T_d = s("h2T_d", [128, 8, T], BF16)
        self.h2f_d = s("h2f_d", [T, D], F32)

    def scope(self):
        b = self

        class _Scope:
            def __enter__(s):
                if b.nphases is not None and b.phase_i >= b.nphases:
                    raise _Stop()
                b.phase_i += 1
                s.es = contextlib.ExitStack()
                s.es.__enter__()
                return s

            def sb(s, name, shape, dt):
                b.uid += 1
                return s.es.enter_context(b.nc.sbuf_tensor(f"{name}_{b.uid}", list(shape), dt))

            def ps(s, name, shape, dt):
                b.uid += 1
                return s.es.enter_context(b.nc.psum_tensor(f"{name}_{b.uid}", list(shape), dt))

            def rot(s, name, shape, dt, n, psum=False):
                return Rot(b.S, s.ps if psum else s.sb, name, shape, dt, n)

            def __exit__(s, *a):
                if a[0] is None:
                    b.S.barrier_and_emit()
                return s.es.__exit__(*a)

        return _Scope()

    def dma(self, q, out, in_, reads=(), writes=()):
        return self.S.dma(q, lambda e: e.dma_start(out=out, in_=in_), reads, writes)

    def act(self, out, in_, func, reads, writes, **kw):
        return self.S.op("act", lambda e: e.activation(out=out, in_=in_, func=func, **kw), reads, writes)

    def tt(self, eng, out, in0, in1, op, reads, writes):
        return self.S.op(eng, lambda e: e.tensor_tensor(out=out, in0=in0, in1=in1, op=op), reads, writes)

    def stt(self, out, in0, scalar, in1, op0, op1, reads, writes):
        return self.S.op("dve", lambda e: e.scalar_tensor_tensor(out=out, in0=in0, scalar=scalar, in1=in1,
                                                                  op0=op0, op1=op1), reads, writes)

    def ts(self, eng, out, in0, s1, op0, reads, writes, s2=None, op1=None):
        if op1 is None:
            return self.S.op(eng, lambda e: e.tensor_scalar(out=out, in0=in0, scalar1=s1, scalar2=None, op0=op0),
                             reads, writes)
        return self.S.op(eng, lambda e: e.tensor_scalar(out=out, in0=in0, scalar1=s1, scalar2=s2, op0=op0, op1=op1),
                         reads, writes)

    def copy(self, eng, out, in_, reads, writes):
        if eng == "act":
            return self.act(out, in_, AF.Copy, reads, writes)
        return self.S.op(eng, lambda e: e.tensor_copy(out=out, in_=in_), reads, writes)

    def mm(self, out, lhsT, rhs, start, stop, reads, writes):
        return self.S.op("pe", lambda e: e.matmul(out, lhsT=lhsT, rhs=rhs, start=start, stop=stop), reads, writes)

    def tr(self, out, in_, ident, reads, writes):
        return self.S.op("pe", lambda e: e.transpose(out=out, in_=in_, identity=ident), reads, writes)

    def recip(self, out, in_, reads, writes):
        return self.S.op("dve", lambda e: e.reciprocal(out=out, in_=in_), reads, writes)

    def memset(self, eng, ap, val, writes):
        return self.S.op(eng, lambda e: e.memset(ap, val), (), writes)

    def common(self, sc, njunk=1536):
        c = type("C", (), {})()
        c.ss = sc.rot("ss", [128, 1], F32, 4)
        c.rs = sc.rot("rs", [128, 1], F32, 4)
        c.junk = sc.rot("junk", [128, njunk], BF16, 2)
        c.identb = sc.sb("identb", [128, 128], BF16)
        c.identf = sc.sb("identf", [128, 128], F32)
        c.t_id = self.S.tile("ident")
        self.dma("sp", c.identf[:], self.ident_d[:, :], writes=[c.t_id])
        self.copy("dve", c.identb[:], c.identf[:], [c.t_id], [c.t_id])
        return c

    def rstd_of(self, c, src, n, reads, eps=EPS):
        ss, t_ss = c.ss.next()
        jk, t_jk = c.junk.next()
        self.act(jk[:, 0:n], src, AF.Square, reads, [t_jk, t_ss], accum_out=ss[:])
        rs, t_rs = c.rs.next()
        self.act(rs[:], ss[:], AF.Sqrt, [t_ss], [t_rs], scale=1.0 / n, bias=eps)
        self.recip(rs[:], rs[:], [t_rs], [t_rs])
        return rs, t_rs

    def load_mod(self, sc, layer, which, ctx_rows=True):
        res = {}
        gt = {}
        for (j, kind, nrow) in which:
            if kind != "shift" and nrow not in gt:
                g = sc.sb(f"gbc{nrow}", [128, D], F32)
                tg = self.S.tile()
                self.dma("sp", g[:], self.norm_g[layer, nrow:nrow + 1, :].partition_broadcast(128), writes=[tg])
                gt[nrow] = (g, tg)
            for r in range(2 if ctx_rows else 1):
                m = sc.sb(f"mod{j}_{r}", [128, D], F32)
                tm = self.S.tile()
                self.dma("sp", m[:], self.modrows[layer, r:r + 1, j * D:(j + 1) * D].partition_broadcast(128),
                         writes=[tm])
                if kind == "scale":
                    g, tg = gt[nrow]
                    self.stt(m[:], m[:], 1.0, g[:], ALU.add, ALU.mult, [tm, tg], [tm])
                elif kind == "gate":
                    g, tg = gt[nrow]
                    self.tt("dve", m[:], m[:], g[:], ALU.mult, [tm, tg], [tm])
                res[(j, r)] = (m, tm)
        return res

    def phase_mod(self):
        with self.scope() as sc:
            cv = sc.sb("cv", [128, 16], F32)
            scT = sc.sb("scT", [128, 16], F32)
            t_cv = self.S.tile()
            self.dma("sp", cv[:], self.cvT[:, :], writes=[t_cv])
            self.act(scT[:], cv[:], AF.Silu, [t_cv], [t_cv])
            wm = sc.rot("wm", [128, 8, 512], F32, 3)
            bm = sc.rot("bm", [2, 512], F32, 3)
            mr = sc.rot("mr", [2, 512], F32, 3)
            ps = sc.rot("mps", [2, 512], F32, 2, psum=True)
            for i in range(self.nlayers):
                for nb in range(12):
                    w, t_w = wm.next()
                    self.dma("sp", w[:], self.w_mod[i, :, nb * 512:(nb + 1) * 512].rearrange("(c p) n -> p c n", p=128),
                             writes=[t_w])
                    b_, t_b = bm.next()
                    self.dma("sp", b_[:], self.b_mod[i:i + 1, nb * 512:(nb + 1) * 512].partition_broadcast(2),
                             writes=[t_b])
                    p, t_p = ps.next()
                    for k in range(8):
                        self.mm(p[:], scT[:, 2 * k:2 * k + 2], w[:, k, :], k == 0, k == 7, [t_cv, t_w], [t_p])
                    m, t_m = mr.next()
                    self.tt("dve", m[:], p[:], b_[:], ALU.add, [t_p, t_b], [t_m])
                    self.dma("sp", self.modrows[i, :, nb * 512:(nb + 1) * 512], m[:], reads=[t_m])

    def mk_pre(self, sc, c):
        p = type("P", (), {})()
        p.x = sc.rot("px", [128, D], F32, 3)
        p.tmp = sc.rot("ptmp", [128, D], F32, 2)
        p.hb = sc.rot("phb", [128, D], BF16, 2)
        p.hT = sc.rot("phT", [128, 8, 128], BF16, 3)
        return p

    def prenorm(self, c, p, pT, xsrc, tt_, A, B):
        A_ap, t_A = A
        B_ap, t_B = B
        xt, t_x = p.x.next()
        self.dma("sp", xt[:], xsrc[tt_ * 128:(tt_ + 1) * 128, :], writes=[t_x])
        r, t_r = self.rstd_of(c, xt[:], D, [t_x])
        tmp, t_tmp = p.tmp.next()
        self.stt(tmp[:], xt[:], r[:], A_ap[:], ALU.mult, ALU.mult, [t_x, t_r, t_A], [t_tmp])
        hb, t_hb = p.hb.next()
        self.tt("pool", hb[:], tmp[:], B_ap[:], ALU.add, [t_tmp, t_B], [t_hb])
        return self.transpose8(c, p, pT, hb, t_hb)

    def transpose8(self, c, p, pT, hb, t_hb):
        ps, t_ps = pT.next()
        for k in range(8):
            self.tr(ps[:, k, :], hb[:, k * 128:(k + 1) * 128], c.identb[:], [t_hb, c.t_id], [t_ps])
        hT, t_hT = p.hT.next()
        self.copy("act", hT[:], ps[:], [t_ps], [t_hT])
        return hT, t_hT

    def mk_epi(self, sc, c, moe):
        p = type("E", (), {})()
        p.x = sc.rot("ex", [128, D], F32, 2)
        p.tmp = sc.rot("etmp", [128, D], F32, 2)
        p.xn = sc.rot("exn", [128, D], F32, 2)
        p.hb = sc.rot("ehb", [128, D], BF16, 2)
        p.hT = sc.rot("ehT", [128, 8, 128], BF16, 2)
        p.hf = sc.rot("ehf", [128, D], F32, 2) if moe else None
        return p

    def epilogue(self, c, p, pT, xsrc, tt_, y_ap, y_tiles, G, A2, B2, do_h2, moe):
        G_ap, t_G = G
        rows = slice(tt_ * 128, (tt_ + 1) * 128)
        xt, t_x = p.x.next()
        self.dma("sp", xt[:], xsrc[rows, :], writes=[t_x])
        r, t_r = self.rstd_of(c, y_ap, D, y_tiles)
        tmp, t_tmp = p.tmp.next()
        self.stt(tmp[:], y_ap, r[:], G_ap[:], ALU.mult, ALU.mult, list(y_tiles) + [t_r, t_G], [t_tmp])
        xn, t_xn = p.xn.next()
        self.tt("pool", xn[:], xt[:], tmp[:], ALU.add, [t_x, t_tmp], [t_xn])
        self.dma("sp", self.xres[rows, :], xn[:], reads=[t_xn])
        if not do_h2:
            return
        A_ap, t_A = A2
        B_ap, t_B = B2
        r2, t_r2 = self.rstd_of(c, xn[:], D, [t_xn])
        t2, t_t2 = p.tmp.next()
        self.stt(t2[:], xn[:], r2[:], A_ap[:], ALU.mult, ALU.mult, [t_xn, t_r2, t_A], [t_t2])
        hb, t_hb = p.hb.next()
        if moe:
            hf, t_hf = p.hf.next()
            self.tt("pool", hf[:], t2[:], B_ap[:], ALU.add, [t_t2, t_B], [t_hf])
            self.dma("sp", self.h2f_d[rows, :], hf[:], reads=[t_hf])
            self.copy("pool", hb[:], hf[:], [t_hf], [t_hb])
        else:
            self.tt("pool", hb[:], t2[:], B_ap[:], ALU.add, [t_t2, t_B], [t_hb])
        hT, t_hT = self.transpose8(c, p, pT, hb, t_hb)
        self.dma("sp", self.h2T_d[:, :, rows], hT[:], reads=[t_hT])

    def phase_gqa_proj(self, layer, j, xsrc, lay):
        KT, V_aug = lay.KT, lay.V
        with self.scope() as sc:
            c = self.common(sc)
            p = self.mk_pre(sc, c)
            mods = self.load_mod(sc, layer, [(0, "shift", None), (1, "scale", 0)])
            wq = sc.sb("wqkv", [128, 8, 1536], BF16)
            t_w = self.S.tile()
            for nb in range(3):
                self.dma("pool", wq[:, :, nb * 512:(nb + 1) * 512],
                         self.a_w_qkv[j, :, nb * 512:(nb + 1) * 512].rearrange("(c p) n -> p c n", p=128), writes=[t_w])
            gain = sc.sb("gain", [128, 20, 64], F32)
            t_g = self.S.tile()
            for hh in range(20):
                src = self.a_g_q if hh < 16 else self.a_g_k
                self.dma("sp", gain[:, hh, :], src[j:j + 1, :].partition_broadcast(128), writes=[t_g])
            cosT = sc.sb("cosT", [128, NT, 64], F32)
            sinT = sc.sb("sinT", [128, NT, 64], F32)
            t_tab = self.S.tile()
            self.dma("sp", cosT[:], self.ropeA_cos.rearrange("(t p) d -> p t d", p=128), writes=[t_tab])
            self.dma("sp", sinT[:], self.ropeA_sin.rearrange("(t p) d -> p t d", p=128), writes=[t_tab])
            self.memset("pool", V_aug[:, :, :, 64:65], 1.0, [lay.t_V])
            pT = sc.rot("pT", [128, 8, 128], BF16, 2, psum=True)
            qps = sc.rot("qps", [128, 1536], F32, 2, psum=True)
            sq = sc.rot("sq", [128, 1280], F32, 1)
            ssq = sc.rot("ssq", [128, 20], F32, 2)
            qn = sc.rot("qn", [128, 20, 64], F32, 2)
            qa = sc.rot("qa", [128, 20, 64], F32, 1)
            qb = sc.rot("qb", [128, 20, 64], F32, 1)
            qr = sc.rot("qr", [128, 20, 64], BF16, 2)
            kd = sc.rot("kd", [128, 4, 2, 64], BF16, 2)
            qst = sc.rot("qst", [128, 8, 128], BF16, 2)
            for tt_ in range(NT):
                r_ = 1 if tt_ < NCTX_T else 0
                hT, t_hT = self.prenorm(c, p, pT, xsrc, tt_, mods[(1, r_)], mods[(0, r_)])
                ps, t_ps = qps.next()
                for nb in range(3):
                    for k in range(8):
                        self.mm(ps[:, nb * 512:(nb + 1) * 512], hT[:, k, :], wq[:, k, nb * 512:(nb + 1) * 512],
                                k == 0, k == 7, [t_hT, t_w], [t_ps])
                s_, t_s = sq.next()
                self.act(s_[:], ps[:, 0:1280], AF.Square, [t_ps], [t_s])
                ss_, t_ss = ssq.next()
                self.S.op("dve", lambda e, o=ss_, i=s_: e.tensor_reduce(
                    out=o[:], in_=i[:].rearrange("p (h d) -> p h d", d=64), axis=AX.X, op=ALU.add), [t_s], [t_ss])
                self.act(ss_[:], ss_[:], AF.Sqrt, [t_ss], [t_ss], scale=1.0 / 64, bias=EPS)
                self.recip(ss_[:], ss_[:], [t_ss], [t_ss])
                q_, t_q = qn.next()
                self.tt("dve", q_[:], ps[:, 0:1280].rearrange("p (h d) -> p h d", d=64),
                        ss_[:].unsqueeze(2).to_broadcast([128, 20, 64]), ALU.mult, [t_ps, t_ss], [t_q])
                self.tt("pool", q_[:], q_[:], gain[:], ALU.mult, [t_q, t_g], [t_q])
                a_, t_a = qa.next()
                self.tt("dve", a_[:], q_[:], cosT[:, tt_, :].unsqueeze(1).to_broadcast([128, 20, 64]), ALU.mult,
                        [t_q, t_tab], [t_a])
                b_, t_b = qb.next()
                q5 = q_[:].rearrange("p h (b s f) -> p h b s f", b=2, s=2)
                b5 = b_[:].rearrange("p h (b s f) -> p h b s f", b=2, s=2)
                s5 = sinT[:, tt_, :].rearrange("p (b s f) -> p b s f", b=2, s=2)
                for s in range(2):
                    self.tt("pool", b5[:, :, :, s, :], q5[:, :, :, 1 - s, :],
                            s5[:, :, s, :].unsqueeze(1).to_broadcast([128, 20, 2, 16]), ALU.mult, [t_q, t_tab], [t_b])
                o_, t_o = qr.next()
                self.tt("dve", o_[:], a_[:], b_[:], ALU.add, [t_a, t_b], [t_o])
                self.copy("act", V_aug[:, tt_, :, 0:64], ps[:, 1280:1536].rearrange("p (h d) -> p h d", d=64),
                          [t_ps], [lay.t_V])
                kd_, t_kd = kd.next()
                for dd in range(2):
                    self.copy("pool", kd_[:, :, dd, :], o_[:, 16:20, :], [t_o], [t_kd])
                pk, t_pk = pT.next()
                for kv in range(4):
                    self.tr(pk[:, kv, :], kd_[:, kv, :, :].rearrange("p a d -> p (a d)"), c.identb[:], [t_kd, c.t_id],
                            [t_pk])
                self.copy("act", KT[:, :, tt_ * 128:(tt_ + 1) * 128], pk[:, 0:4, :], [t_pk], [lay.t_KT])
                pq, t_pq = pT.next()
                of = o_[:].rearrange("p h d -> p (h d)")
                for k in range(8):
                    self.tr(pq[:, k, :], of[:, k * 128:(k + 1) * 128], c.identb[:], [t_o, c.t_id], [t_pq])
                st, t_st = qst.next()
                self.copy("dve", st[:], pq[:], [t_pq], [t_st])
                self.dma("sp", self.QT_d[:, :, tt_ * 128:(tt_ + 1) * 128].rearrange("c p n -> p c n"), st[:],
                         reads=[t_st])

    def phase_attn(self, kind, layer, j, xsrc, lay, last, moe):
        scale = 0.125 if kind == "gqa" else (96.0 ** -0.5)
        w_o_d = self.a_w_o[j] if kind == "gqa" else self.m_w_o[0]
        with self.scope() as sc:
            c = self.common(sc, njunk=1024)
            ep = self.mk_epi(sc, c, moe)
            wl = [(2, "gate", 1), (3, "shift", None), (4, "scale", 2)]
            mods = self.load_mod(sc, layer, wl, ctx_rows=not last)
            wo = sc.sb("wo", [128, 8, D], BF16)
            t_wo = self.S.tile()
            self.dma("pool", wo[:], w_o_d.rearrange("(c p) n -> p c n", p=128), writes=[t_wo])
            Sps = sc.ps("Sps", [128, 2, 512], F32)
            t_S = [self.S.tile(), self.S.tile()]
            Ops = [sc.ps(f"Ops{i}", [128, 512], F32) for i in range(4)]
            t_O = [self.S.tile() for _ in range(4)]
            pT = sc.rot("pT", [128, 8, 128], BF16, 2, psum=True)
            PT = sc.rot("PT", [128, 512], BF16, 4)
            qp = sc.rot("qp", [128, 512], BF16, 3)
            attn = sc.rot("attn", [128, 4, D], BF16, 2)
            aT = sc.rot("aT", [128, 8, 128], BF16, 2)
            rec = sc.rot("rec", [128, 1], F32, 8)
            if kind == "mla":
                qrp = sc.rot("qrp", [64, 512], BF16, 3)
                knp = sc.rot("knp", [128, T], BF16, 2)
                vpp = sc.rot("vpp", [128, NT, 130], BF16, 2)
            groups = []
            if not last:
                groups.append((0, 2, [0, 1]))
            for g in range(8):
                groups.append((256 + g * 512, 4, list(range(NT))))
            sidx = 0
            for (tok0, nsub, kts) in groups:
                N = nsub * 128
                nk = len(kts)
                at, t_at = attn.next()
                its = []
                for pj in range(8):
                    for r in range(2):
                        for ki, kt in enumerate(kts):
                            its.append(dict(pj=pj, r=r, ki=ki, kt=kt))
                cur = {}

                def emit_S(it):
                    nonlocal sidx
                    pj, r, ki, kt = it["pj"], it["r"], it["ki"], it["kt"]
                    if r == 0 and ki == 0:
                        q_, t_q = qp.next()
                        self.dma("sp", q_[:, 0:N], self.QT_d[pj, :, tok0:tok0 + N], writes=[t_q])
                        cur["q"] = (q_, t_q)
                        if kind == "mla":
                            qr_, t_qr = qrp.next()
                            self.dma("sp", qr_[:, 0:N], self.QrT_d[pj, :, tok0:tok0 + N], writes=[t_qr])
                            kn_, t_kn = knp.next()
                            self.dma("sp", kn_[:, 0:nk * 128], self.KnT_d[pj, :, 0:nk * 128], writes=[t_kn])
                            vp_, t_vp = vpp.next()
                            self.dma("sp", vp_[:, 0:nk, :],
                                     self.V_d[0:nk, :, pj * 130:(pj + 1) * 130].rearrange("t p c -> p t c"),
                                     writes=[t_vp])
                            cur["qr"] = (qr_, t_qr)
                            cur["kn"] = (kn_, t_kn)
                            cur["vp"] = (vp_, t_vp)
                    q_, t_q = cur["q"]
                    h = 2 * pj + r
                    kvh = h // 4
                    sb_ = sidx % 2
                    sidx += 1
                    s_ap = Sps[:, sb_, 0:N]
                    ksl = slice(kt * 128, (kt + 1) * 128)
                    if kind == "gqa":
                        self.mm(s_ap, lay.KT[64 * r:64 * r + 64, kvh, ksl], q_[64 * r:64 * r + 64, 0:N],
                                True, True, [t_q], [t_S[sb_]])
                    else:
                        qr_, t_qr = cur["qr"]
                        kn_, t_kn = cur["kn"]
                        it["vp"] = cur["vp"]
                        self.mm(s_ap, kn_[64 * r:64 * r + 64, ksl], q_[64 * r:64 * r + 64, 0:N],
                                True, False, [t_q, t_kn], [t_S[sb_]])
                        self.mm(s_ap, lay.KrT[32 * r:32 * r + 32, ksl], qr_[32 * r:32 * r + 32, 0:N],
                                False, True, [t_qr], [t_S[sb_]])
                    pt, t_pt = PT.next()
                    self.act(pt[:, 0:N], s_ap, AF.Exp, [t_S[sb_]], [t_pt], scale=scale)
                    it["pt"] = (pt, t_pt)

                def emit_PV(it):
                    pj, r, ki, kt = it["pj"], it["r"], it["ki"], it["kt"]
                    h = 2 * pj + r
                    kvh = h // 4
                    pt, t_pt = it["pt"]
                    for sub in range(nsub):
                        if kind == "gqa":
                            v_ap = lay.V[:, kt, kvh, :]
                            rd = [t_pt]
                        else:
                            vp_, t_vp = it["vp"]
                            v_ap = vp_[:, kt, r * 65:(r + 1) * 65]
                            rd = [t_pt, t_vp]
                        self.mm(Ops[sub][:, 0:65], pt[:, sub * 128:(sub + 1) * 128], v_ap,
                                ki == 0, ki == nk - 1, rd, [t_O[sub]])
                    if ki == nk - 1:
                        for sub in range(nsub):
                            rc, t_rc = rec.next()
                            self.recip(rc[:], Ops[sub][:, 64:65], [t_O[sub]], [t_rc])
                            self.ts("dve", at[:, sub, h * 64:(h + 1) * 64], Ops[sub][:, 0:64], rc[:], ALU.mult,
                                    [t_O[sub], t_rc], [t_at])

                for i in range(len(its) + 1):
                    if i < len(its):
                        emit_S(its[i])
                    if i >= 1:
                        emit_PV(its[i - 1])
                for sub in range(nsub):
                    tt_ = tok0 // 128 + sub
                    r_ = 1 if tt_ < NCTX_T else 0
                    ps, t_ps = pT.next()
                    for k in range(8):
                        self.tr(ps[:, k, :], at[:, sub, k * 128:(k + 1) * 128], c.identb[:], [t_at, c.t_id], [t_ps])
                    a_, t_a = aT.next()
                    self.copy("act", a_[:], ps[:], [t_ps], [t_a])
                    y_ap = Sps[:].rearrange("p a n -> p (a n)")
                    for nb in range(2):
                        for k in range(8):
                            self.mm(Sps[:, nb, :], a_[:, k, :], wo[:, k, nb * 512:(nb + 1) * 512], k == 0, k == 7,
                                    [t_a, t_wo], [t_S[nb]])
                    self.epilogue(c, ep, pT, xsrc, tt_, y_ap, t_S, mods[(2, r_)], mods[(4, r_)], mods[(3, r_)],
                                  True, moe)

    def phase_fourier_1(self, layer, xsrc):
        with self.scope() as sc:
            c = self.common(sc)
            p = self.mk_pre(sc, c)
            mods = self.load_mod(sc, layer, [(0, "shift", None), (1, "scale", 0)])
            cs = sc.sb("cs", [128, 2, 512], BF16)
            t_cs = self.S.tile()
            self.dma("pool", cs[:], self.dft256.rearrange("(c p) n -> p c n", p=128), writes=[t_cs])
            pT = sc.rot("pT", [128, 8, 128], BF16, 2, psum=True)
            xps = sc.rot("xps", [128, 4, 512], F32, 1, psum=True)
            xsb = sc.rot("xsb", [128, 4, 512], BF16, 2)
            for tt_ in range(NT):
                r_ = 1 if tt_ < NCTX_T else 0
                hT, t_hT = self.prenorm(c, p, pT, xsrc, tt_, mods[(1, r_)], mods[(0, r_)])
                ps, t_ps = xps.next()
                for g in range(4):
                    for k in range(2):
                        self.mm(ps[:, g, :], hT[:, 2 * g + k, :], cs[:, k, :], k == 0, k == 1, [t_hT, t_cs], [t_ps])
                xs, t_xs = xsb.next()
                self.copy("act", xs[:], ps[:], [t_ps], [t_xs])
                self.dma("sp", self.XCS_d[:, tt_ * 128:(tt_ + 1) * 128, :].rearrange("g p n -> p g n"), xs[:],
                         reads=[t_xs])

    def phase_fourier_2(self, layer, xsrc, last, moe):
        with self.scope() as sc:
            c = self.common(sc, njunk=1024)
            ep = self.mk_epi(sc, c, moe)
            mods = self.load_mod(sc, layer, [(2, "gate", 1), (3, "shift", None), (4, "scale", 2)], ctx_rows=not last)
            fw = sc.sb("fw", [128, 8, D], BF16)
            t_fw = self.S.tile()
            self.dma("pool", fw[:], self.f_w[0].rearrange("(c p) n -> p c n", p=128), writes=[t_fw])
            Tc = sc.sb("Tc", [128, 32, 512], BF16)
            Ts = sc.sb("Ts", [128, 32, 512], BF16)
            t_T = self.S.tile()
            X = sc.rot("X", [128, 32, 512], BF16, 1)
            fT = sc.rot("fT", [128, 8, 512], BF16, 1)
            pT = sc.rot("pT", [128, 8, 128], BF16, 2, psum=True)
            fps = sc.rot("fps", [128, 512], F32, 2, psum=True)
            yps = sc.ps("yps", [128, 2, 512], F32)
            t_y = [self.S.tile(), self.S.tile()]
            groups = []
            if not last:
                groups.append(("ctx", 0, 2, 2))
            for g in range(8):
                groups.append(("lat", 256 + g * 512, 4, 32))
            for (kind, tok0, nsub, na) in groups:
                N = nsub * 128
                if kind == "ctx":
                    self.dma("sp", Tc[:, 0:2, 0:256], self.cos256.rearrange("(a p) n -> p a n", p=128), writes=[t_T])
                    self.dma("sp", Ts[:, 0:2, 0:256], self.nsin256.rearrange("(a p) n -> p a n", p=128), writes=[t_T])
                    fscale = 1.0 / 256
                else:
                    k0 = tok0 - 256
                    self.dma("sp", Tc[:], self.cos4096[:, k0:k0 + 512].rearrange("(a p) n -> p a n", p=128),
                             writes=[t_T])
                    self.dma("sp", Ts[:], self.nsin4096[:, k0:k0 + 512].rearrange("(a p) n -> p a n", p=128),
                             writes=[t_T])
                    fscale = 1.0 / 1024
                f_, t_f = fT.next()
                for g in range(4):
                    x_, t_x = X.next()
                    if kind == "ctx":
                        self.dma("sp", x_[:, 0:2, :], self.XCS_d[g, 0:256, :].rearrange("(a p) n -> p a n", p=128),
                                 writes=[t_x])
                    else:
                        self.dma("sp", x_[:], self.XCS_d[g, 256:T, :].rearrange("(a p) n -> p a n", p=128),
                                 writes=[t_x])
                    for lc in range(2):
                        ps, t_ps = fps.next()
                        n_mm = 2 * na
                        i_mm = 0
                        for (tab, off) in ((Tc, 0), (Ts, 256)):
                            for a in range(na):
                                self.mm(ps[:, 0:N], x_[:, a, off + lc * 128:off + (lc + 1) * 128], tab[:, a, 0:N],
                                        i_mm == 0, i_mm == n_mm - 1, [t_x, t_T], [t_ps])
                                i_mm += 1
                        self.act(f_[:, 2 * g + lc, 0:N], ps[:, 0:N], AF.Copy, [t_ps], [t_f], scale=fscale)
                for sub in range(nsub):
                    tt_ = tok0 // 128 + sub
                    r_ = 1 if tt_ < NCTX_T else 0
                    for nb in range(2):
                        for k in range(8):
                            self.mm(yps[:, nb, :], f_[:, k, sub * 128:(sub + 1) * 128], fw[:, k, nb * 512:(nb + 1) * 512],
                                    k == 0, k == 7, [t_f, t_fw], [t_y[nb]])
                    self.epilogue(c, ep, pT, xsrc, tt_, yps[:].rearrange("p a n -> p (a n)"), t_y, mods[(2, r_)],
                                  mods[(4, r_)], mods[(3, r_)], True, moe)

    def phase_mla_proj(self, layer, xsrc, lay):
        with self.scope() as sc:
            c = self.common(sc)
            p = self.mk_pre(sc, c)
            mods = self.load_mod(sc, layer, [(0, "shift", None), (1, "scale", 0)])
            t_w = self.S.tile()
            wdn = sc.sb("wdn", [128, 8, 672], BF16)
            self.dma("pool", wdn[:], self.m_w_down[0].rearrange("(c p) n -> p c n", p=128), writes=[t_w])
            wqn = sc.sb("wqn", [128, 3, 1024], BF16)
            self.dma("pool", wqn[:], self.m_wuqn.rearrange("(c p) n -> p c n", p=128), writes=[t_w])
            wqr = sc.sb("wqr", [128, 3, 512], BF16)
            self.dma("pool", wqr[:], self.m_wuqr.rearrange("(c p) n -> p c n", p=128), writes=[t_w])
            wqs = sc.sb("wqs", [128, 3, 512], BF16)
            self.dma("pool", wqs[:], self.m_wuqrs.rearrange("(c p) n -> p c n", p=128), writes=[t_w])
            wuk = sc.sb("wuk", [128, 2, 1024], BF16)
            self.dma("pool", wuk[:], self.m_wuk.rearrange("(c p) n -> p c n", p=128), writes=[t_w])
            wuv = sc.sb("wuv", [128, 2, 1024], BF16)
            self.dma("pool", wuv[:], self.m_wuv.rearrange("(c p) n -> p c n", p=128), writes=[t_w])
            gcq = sc.sb("gcq", [128, 384], F32)
            gckv = sc.sb("gckv", [128, 256], F32)
            t_g = self.S.tile()
            self.dma("sp", gcq[:], self.m_g_cq[0:1, :].partition_broadcast(128), writes=[t_g])
            self.dma("sp", gckv[:], self.m_g_ckv[0:1, :].partition_broadcast(128), writes=[t_g])
            cosT = sc.sb("cosT", [128, NT, 32], F32)
            sinT = sc.sb("sinT", [128, NT, 32], F32)
            t_tab = self.S.tile()
            self.dma("sp", cosT[:], self.ropeM_cos.rearrange("(t p) d -> p t d", p=128), writes=[t_tab])
            self.dma("sp", sinT[:], self.ropeM_sin.rearrange("(t p) d -> p t d", p=128), writes=[t_tab])
            pT = sc.rot("pT", [128, 8, 128], BF16, 2, psum=True)
            dps = sc.rot("dps", [128, 1024], F32, 1, psum=True)
            pps = sc.rot("pps", [128, 512], F32, 2, psum=True)
            vps = sc.rot("vps", [128, 1024], F32, 1, psum=True)
            cqn = sc.rot("cqn", [128, 384], BF16, 2)
            ckvn = sc.rot("ckvn", [128, 256], BF16, 2)
            cqT = sc.rot("cqT", [128, 3, 512], BF16, 2)
            ckT = sc.rot("ckT", [128, 2, 512], BF16, 2)
            ka = sc.rot("ka", [128, 32], F32, 2)
            kb = sc.rot("kb", [128, 32], F32, 2)
            kd = sc.rot("kd", [128, 2, 32], BF16, 2)
            st = sc.rot("st", [128, 512], BF16, 3)
            ctab = sc.rot("ctab", [64, 512], F32, 2)
            stab = sc.rot("stab", [64, 512], F32, 2)
            ra = sc.rot("ra", [64, 512], F32, 2)
            rb = sc.rot("rb", [64, 512], F32, 2)
            rst = sc.rot("rst", [64, 512], BF16, 2)
            vst = sc.rot("vst", [128, 16, 65], BF16, 2)
            for i in range(2):
                self.memset("pool", vst.bufs[i][:, :, 64:65], 1.0, [vst.tiles[i]])
            groups = [(0, 2)] + [(2 + 4 * g, 4) for g in range(8)]
            for (t0, nsub) in groups:
                N = nsub * 128
                tok0 = t0 * 128
                cq_T, t_cqT = cqT.next()
                ck_T, t_ckT = ckT.next()
                for sub in range(nsub):
                    tt_ = t0 + sub
                    r_ = 1 if tt_ < NCTX_T else 0
                    hT, t_hT = self.prenorm(c, p, pT, xsrc, tt_, mods[(1, r_)], mods[(0, r_)])
                    d_, t_d = dps.next()
                    for (o0, o1) in ((0, 512), (512, 672)):
                        for k in range(8):
                            self.mm(d_[:, o0:o1], hT[:, k, :], wdn[:, k, o0:o1], k == 0, k == 7, [t_hT, t_w], [t_d])
                    r1, t_r1 = self.rstd_of(c, d_[:, 0:384], 384, [t_d])
                    cq_, t_cq = cqn.next()
                    self.stt(cq_[:], d_[:, 0:384], r1[:], gcq[:], ALU.mult, ALU.mult, [t_d, t_r1, t_g], [t_cq])
                    r2, t_r2 = self.rstd_of(c, d_[:, 384:640], 256, [t_d])
                    ck_, t_ck = ckvn.next()
                    self.stt(ck_[:], d_[:, 384:640], r2[:], gckv[:], ALU.mult, ALU.mult, [t_d, t_r2, t_g], [t_ck])
                    a_, t_a = ka.next()
                    self.tt("dve", a_[:], d_[:, 640:672], cosT[:, tt_, :], ALU.mult, [t_d, t_tab], [t_a])
                    b_, t_b = kb.next()
                    d4 = d_[:, 640:672].rearrange("p (b s f) -> p b s f", b=2, s=2)
                    b4 = b_[:].rearrange("p (b s f) -> p b s f", b=2, s=2)
                    s4 = sinT[:, tt_, :].rearrange("p (b s f) -> p b s f", b=2, s=2)
                    for s in range(2):
                        self.tt("dve", b4[:, :, s, :], d4[:, :, 1 - s, :], s4[:, :, s, :], ALU.mult, [t_d, t_tab], [t_b])
                    kd_, t_kd = kd.next()
                    for dd in range(2):
                        self.tt("pool", kd_[:, dd, :], a_[:], b_[:], ALU.add, [t_a, t_b], [t_kd])
                    ps, t_ps = pT.next()
                    for k in range(3):
                        self.tr(ps[:, k, :], cq_[:, k * 128:(k + 1) * 128], c.identb[:], [t_cq, c.t_id], [t_ps])
                    for k in range(2):
                        self.tr(ps[:, 3 + k, :], ck_[:, k * 128:(k + 1) * 128], c.identb[:], [t_ck, c.t_id], [t_ps])
                    self.tr(ps[0:64, 5, :], kd_[:].rearrange("p a d -> p (a d)"), c.identb[:], [t_kd, c.t_id], [t_ps])
                    self.copy("act", cq_T[:, :, sub * 128:(sub + 1) * 128], ps[:, 0:3, :], [t_ps], [t_cqT])
                    self.copy("act", ck_T[:, :, sub * 128:(sub + 1) * 128], ps[:, 3:5, :], [t_ps], [t_ckT])
                    self.copy("dve", lay.KrT[:, tt_ * 128:(tt_ + 1) * 128], ps[0:64, 5, :], [t_ps], [lay.t_KrT])
                for sub in range(nsub):
                    tt_ = t0 + sub
                    v_, t_v = vps.next()
                    for nb in range(2):
                        for k in range(2):
                            self.mm(v_[:, nb * 512:(nb + 1) * 512], ck_T[:, k, sub * 128:(sub + 1) * 128],
                                    wuv[:, k, nb * 512:(nb + 1) * 512], k == 0, k == 1, [t_ckT, t_w], [t_v])
                    vs, t_vs = vst.next()
                    self.copy("act", vs[:, :, 0:64], v_[:].rearrange("p (h d) -> p h d", d=64), [t_v], [t_vs])
                    self.dma("sp", self.V_d[tt_, :, :], vs[:].rearrange("p h d -> p (h d)"), reads=[t_vs])
                ct_, t_ct = ctab.next()
                st_, t_stb = stab.next()
                self.dma("sp", ct_[:, 0:N], self.ropeMT_cos[:, tok0:tok0 + N], writes=[t_ct])
                self.dma("sp", st_[:, 0:N], self.ropeMT_sin[:, tok0:tok0 + N], writes=[t_stb])
                for pj in range(8):
                    ps, t_ps = pps.next()
                    for k in range(3):
                        self.mm(ps[:, 0:N], wqn[:, k, pj * 128:(pj + 1) * 128], cq_T[:, k, 0:N], k == 0, k == 2,
                                [t_cqT, t_w], [t_ps])
                    s_, t_s = st.next()
                    self.copy("act", s_[:, 0:N], ps[:, 0:N], [t_ps], [t_s])
                    self.dma("sp", self.QT_d[pj, :, tok0:tok0 + N], s_[:, 0:N], reads=[t_s])
                    ps, t_ps = pps.next()
                    for k in range(2):
                        self.mm(ps[:, 0:N], wuk[:, k, pj * 128:(pj + 1) * 128], ck_T[:, k, 0:N], k == 0, k == 1,
                                [t_ckT, t_w], [t_ps])
                    s_, t_s = st.next()
                    self.copy("act", s_[:, 0:N], ps[:, 0:N], [t_ps], [t_s])
                    self.dma("sp", self.KnT_d[pj, :, tok0:tok0 + N], s_[:, 0:N], reads=[t_s])
                    ps, t_ps = pps.next()
                    for k in range(3):
                        self.mm(ps[0:64, 0:N], wqr[:, k, pj * 64:(pj + 1) * 64], cq_T[:, k, 0:N], k == 0, k == 2,
                                [t_cqT, t_w], [t_ps])
                    a_, t_a = ra.next()
                    self.tt("dve", a_[:, 0:N], ps[0:64, 0:N], ct_[:, 0:N], ALU.mult, [t_ps, t_ct], [t_a])
                    ps, t_ps = pps.next()
                    for k in range(3):
                        self.mm(ps[0:64, 0:N], wqs[:, k, pj * 64:(pj + 1) * 64], cq_T[:, k, 0:N], k == 0, k == 2,
                                [t_cqT, t_w], [t_ps])
                    b_, t_b = rb.next()
                    self.tt("dve", b_[:, 0:N], ps[0:64, 0:N], st_[:, 0:N], ALU.mult, [t_ps, t_stb], [t_b])
                    o_, t_o = rst.next()
                    self.tt("pool", o_[:, 0:N], a_[:, 0:N], b_[:, 0:N], ALU.add, [t_a, t_b], [t_o])
                    self.dma("sp", self.QrT_d[pj, :, tok0:tok0 + N], o_[:, 0:N], reads=[t_o])

    def phase_ffn(self, layer, moe, last, xsrc_unused):
        idx = layer // 2
        tiles = list(range(NCTX_T, NT)) if last else list(range(NT))
        nh = len(tiles) // 2
        halves = [tiles[:nh], tiles[nh:]]
        E = NE if moe else 1
        F = FF_E if moe else FF_D
        nfb = F // 256
        for hi, half in enumerate(halves):
            nt = len(half)
            tok0 = half[0] * 128
            NTOK = nt * 128
            with contextlib.ExitStack() as hes:
                yacc = hes.enter_context(self.nc.sbuf_tensor(f"yacc{layer}_{hi}", [128, nt, D], F32))
                with self.scope() as sc:
                    c = self.common(sc, njunk=1024)
                    h2T = sc.sb("h2T", [128, 8, NTOK], BF16)
                    t_h = self.S.tile()
                    self.dma("sp", h2T[:], self.h2T_d[:, :, tok0:tok0 + NTOK], writes=[t_h])
                    t_ya = [self.S.tile() for _ in range(nt)]
                    gps = sc.rot("gps", [128, 512], F32, 2, psum=True)
                    ups = sc.rot("ups", [128, 512], F32, 2, psum=True)
                    yps = sc.rot("yps", [128, 2, 512], F32, 2, psum=True)
                    wg = sc.rot("wg", [128, 8, 256], BF16, 3)
                    wu = sc.rot("wu", [128, 8, 256], BF16, 3)
                    wd = sc.rot("wd", [128, 2, D], BF16, 3)
                    sg = sc.rot("sg", [128, 512], F32, 2)
                    aT = sc.rot("aT", [128, 2, 512], BF16, 2)
                    if moe:
                        t1 = sc.rot("t1", [128, 512], F32, 2)
                        gbs = sc.rot("gbs", [128, NTOK], F32, 2)
                        gatesT = sc.sb("gatesT", [NE, NTOK], F32)
                        t_gT = self.S.tile()
                        sel = sc.sb("sel", [NE, NE * 128], F32)
                        t_sel = self.S.tile()
                        self.dma("sp", sel[:], self.sel_d[:, :], writes=[t_sel])
                        self.router(sc, c, idx, half, tok0, gatesT, t_gT, yps, ups)
                    groups = []
                    g0 = 0
                    while g0 < NTOK:
                        n = min(512, NTOK - g0)
                        groups.append((g0, n))
                        g0 += n
                    its = []
                    for e in range(E):
                        for fb in range(nfb):
                            for gi, (g0, n) in enumerate(groups):
                                its.append(dict(e=e, fb=fb, gi=gi, g0=g0, n=n))
                    cur = {}

                    def stage_a(it):
                        e, fb, gi, g0, n = it["e"], it["fb"], it["gi"], it["g0"], it["n"]
                        if moe:
                            wgu_d = self.e_w_gu[idx, e]
                            wdn_d = self.e_w_down[idx, e]
                        else:
                            wgu_d = self.d_w_gu[idx]
                            wdn_d = self.d_w_down[idx]
                        if gi == 0:
                            if moe and fb == 0:
                                gb_, t_gb = gbs.next()
                                for (gg0, gn) in groups:
                                    m_, t_m = gps.next()
                                    self.mm(m_[:, 0:gn], sel[:, e * 128:(e + 1) * 128], gatesT[:, gg0:gg0 + gn],
                                            True, True, [t_sel, t_gT], [t_m])
                                    self.copy("act", gb_[:, gg0:gg0 + gn], m_[:, 0:gn], [t_m], [t_gb])
                                cur["gb"] = (gb_, t_gb)
                            wg_, t_wg = wg.next()
                            wu_, t_wu = wu.next()
                            wd_, t_wd = wd.next()
                            f0 = fb * 256
                            self.dma("pool", wg_[:], wgu_d[:, f0:f0 + 256].rearrange("(c p) n -> p c n", p=128),
                                     writes=[t_wg])
                            self.dma("pool", wu_[:], wgu_d[:, F + f0:F + f0 + 256].rearrange("(c p) n -> p c n", p=128),
                                     writes=[t_wu])
                            self.dma("pool", wd_[:], wdn_d[f0:f0 + 256, :].rearrange("(c p) n -> p c n", p=128),
                                     writes=[t_wd])
                            cur["w"] = (wg_, t_wg, wu_, t_wu, wd_, t_wd)
                        wg_, t_wg, wu_, t_wu, wd_, t_wd = cur["w"]
                        it["wd"] = (wd_, t_wd)
                        a_, t_a = aT.next()
                        it["a"] = (a_, t_a)
                        for fc in range(2):
                            g_, t_g = gps.next()
                            u_, t_u = ups.next()
                            for k in range(8):
                                self.mm(g_[:, 0:n], wg_[:, k, fc * 128:(fc + 1) * 128], h2T[:, k, g0:g0 + n],
                                        k == 0, k == 7, [t_wg, t_h], [t_g])
                            for k in range(8):
                                self.mm(u_[:, 0:n], wu_[:, k, fc * 128:(fc + 1) * 128], h2T[:, k, g0:g0 + n],
                                        k == 0, k == 7, [t_wu, t_h], [t_u])
                            s_, t_s = sg.next()
                            self.act(s_[:, 0:n], g_[:, 0:n], AF.Silu, [t_g], [t_s])
                            if moe:
                                gb_, t_gb = cur["gb"]
                                t1_, t_t1 = t1.next()
                                self.tt("dve", t1_[:, 0:n], s_[:, 0:n], u_[:, 0:n], ALU.mult, [t_s, t_u], [t_t1])
                                self.tt("dve", a_[:, fc, 0:n], t1_[:, 0:n], gb_[:, g0:g0 + n], ALU.mult,
                                        [t_t1, t_gb], [t_a])
                            else:
                                self.tt("dve", a_[:, fc, 0:n], s_[:, 0:n], u_[:, 0:n], ALU.mult, [t_s, t_u], [t_a])

                    def stage_b(it):
                        e, fb, g0, n = it["e"], it["fb"], it["g0"], it["n"]
                        first = (e == 0 and fb == 0)
                        a_, t_a = it["a"]
                        wd_, t_wd = it["wd"]
                        for sub in range(n // 128):
                            ti = g0 // 128 + sub
                            y_, t_y = yps.next()
                            for nb in range(2):
                                for fc in range(2):
                                    self.mm(y_[:, nb, :], a_[:, fc, sub * 128:(sub + 1) * 128],
                                            wd_[:, fc, nb * 512:(nb + 1) * 512], fc == 0, fc == 1, [t_a, t_wd], [t_y])
                            yf = y_[:].rearrange("p a n -> p (a n)")
                            if first:
                                self.copy("act", yacc[:, ti, :], yf, [t_y], [t_ya[ti]])
                            else:
                                self.tt("dve", yacc[:, ti, :], yacc[:, ti, :], yf, ALU.add, [t_y, t_ya[ti]],
                                        [t_ya[ti]])

                    for i in range(len(its) + 1):
                        if i < len(its):
                            stage_a(its[i])
                        if i >= 1:
                            stage_b(its[i - 1])
                with self.scope() as sc:
                    c = self.common(sc, njunk=1024)
                    mods = self.load_mod(sc, layer, [(5, "gate", 3)], ctx_rows=not last)
                    xr = sc.rot("xr", [128, D], F32, 3)
                    tm = sc.rot("tm", [128, D], F32, 2)
                    xo = sc.rot("xo", [128, D], F32, 3)
                    for ti, tt_ in enumerate(half):
                        r_ = 1 if tt_ < NCTX_T else 0
                        G_ap, t_G = mods[(5, r_)]
                        rows = slice(tt_ * 128, (tt_ + 1) * 128)
                        x_, t_x = xr.next()
                        self.dma("sp", x_[:], self.xres[rows, :], writes=[t_x])
                        r, t_r = self.rstd_of(c, yacc[:, ti, :], D, [])
                        t_, t_t = tm.next()
                        self.stt(t_[:], yacc[:, ti, :], r[:], G_ap[:], ALU.mult, ALU.mult, [t_r, t_G], [t_t])
                        o_, t_o = xo.next()
                        self.tt("pool", o_[:], x_[:], t_[:], ALU.add, [t_x, t_t], [t_o])
                        if layer == self.nlayers - 1:
                            if tt_ >= NCTX_T:
                                self.dma("sp", self.out[(tt_ - NCTX_T) * 128:(tt_ - NCTX_T + 1) * 128, :], o_[:],
                                         reads=[t_o])
                            if self.debug:
                                self.dma("sp", self.xres[rows, :], o_[:], reads=[t_o])
                        else:
                            self.dma("sp", self.xres[rows, :], o_[:], reads=[t_o])

    def router(self, sc, c, idx, half, tok0, gatesT, t_gT, yps, ups):
        wr = sc.sb("wr", [128, 8, NE], F32)
        t_wr = self.S.tile()
        self.dma("sp", wr[:], self.e_w_router[idx].rearrange("(c p) n -> p c n", p=128), writes=[t_wr])
        hf = sc.rot("rhf", [128, D], F32, 2)
        hfT = sc.rot("rhfT", [128, 8, 128], F32, 2)
        lg = sc.rot("lg", [128, NE], F32, 2)
        m8 = sc.rot("m8", [128, 8], F32, 2)
        dd = sc.rot("dd", [128, NE], F32, 2)
        ee = sc.rot("ee", [128, NE], F32, 2)
        mk = sc.rot("mk", [128, NE], F32, 2)
        sm = sc.rot("sm", [128, 1], F32, 2)
        gt = sc.rot("gt", [128, NE], F32, 2)
        for ti, tt_ in enumerate(half):
            rows = slice(tt_ * 128, (tt_ + 1) * 128)
            h_, t_h = hf.next()
            self.dma("sp", h_[:], self.h2f_d[rows, :], writes=[t_h])
            y_, t_y = yps.next()
            yT = y_[:].rearrange("p a (b n) -> p (a b) n", n=128)
            for k in range(8):
                self.tr(yT[:, k, :], h_[:, k * 128:(k + 1) * 128], c.identf[:], [t_h, c.t_id], [t_y])
            hT_, t_hT = hfT.next()
            self.copy("act", hT_[:], yT, [t_y], [t_hT])
            misc, t_misc = ups.next()
            for k in range(8):
                self.mm(misc[:, 0:NE], hT_[:, k, :], wr[:, k, :], k == 0, k == 7, [t_hT, t_wr], [t_misc])
            l_, t_l = lg.next()
            self.copy("dve", l_[:], misc[:, 0:NE], [t_misc], [t_l])
            m_, t_m = m8.next()
            self.S.op("dve", lambda e, o=m_, i=l_: e.max(out=o[:], in_=i[:]), [t_l], [t_m])
            d_, t_d = dd.next()
            self.ts("dve", d_[:], l_[:], m_[:, 0:1], ALU.subtract, [t_l, t_m], [t_d])
            e_, t_e = ee.next()
            self.act(e_[:], d_[:], AF.Exp, [t_d], [t_e])
            k_, t_k = mk.next()
            self.ts("dve", k_[:], l_[:], m_[:, 1:2], ALU.is_ge, [t_l, t_m], [t_k])
            self.tt("dve", e_[:], e_[:], k_[:], ALU.mult, [t_e, t_k], [t_e])
            s_, t_s = sm.next()
            self.S.op("dve", lambda e, o=s_, i=e_: e.tensor_reduce(out=o[:], in_=i[:], axis=AX.X, op=ALU.add),
                      [t_e], [t_s])
            self.recip(s_[:], s_[:], [t_s], [t_s])
            g_, t_g = gt.next()
            self.ts("dve", g_[:], e_[:], s_[:], ALU.mult, [t_e, t_s], [t_g])
            self.tr(misc[0:NE, 128:256], g_[:], c.identf[:], [t_g, c.t_id], [t_misc])
            self.copy("dve", gatesT[:, ti * 128:(ti + 1) * 128], misc[0:NE, 128:256], [t_misc], [t_gT])

    def build(self):
        try:
            self._build()
        except _Stop:
            pass
        return self.nc

    def _build(self):
        with self.es:
            self.declare()
            self.phase_mod()
            for layer in range(self.nlayers):
                last = layer == DEPTH - 1
                kind = layer % 3
                j = layer // 3
                moe = layer % 2 == 1
                xsrc = self.xin if layer == 0 else self.xres
                with contextlib.ExitStack() as les:
                    lay = type("L", (), {})()
                    if kind == 0:
                        lay.KT = les.enter_context(self.nc.sbuf_tensor(f"KT{layer}", [128, 4, T], BF16))
                        lay.V = les.enter_context(self.nc.sbuf_tensor(f"Vaug{layer}", [128, NT, 4, 65], BF16))
                        lay.t_KT = self.S.tile()
                        lay.t_V = self.S.tile()
                        self.phase_gqa_proj(layer, j, xsrc, lay)
                        self.phase_attn("gqa", layer, j, xsrc, lay, last, moe)
                    elif kind == 1:
                        self.phase_fourier_1(layer, xsrc)
                        self.phase_fourier_2(layer, xsrc, last, moe)
                    else:
                        lay.KrT = les.enter_context(self.nc.sbuf_tensor(f"KrT{layer}", [64, T], BF16))
                        lay.t_KrT = self.S.tile()
                        self.phase_mla_proj(layer, xsrc, lay)
                        self.phase_attn("mla", layer, j, xsrc, lay, last, moe)
                self.phase_ffn(layer, moe, last, xsrc)
        return self.nc


def _rope_tables(rot_dim):
    nf = rot_dim // 4
    inv = (10000.0 ** (-np.arange(nf, dtype=np.float32) / nf)).astype(np.float32)
    rows = np.repeat(np.arange(64, dtype=np.float32), 64)
    cols = np.tile(np.arange(64, dtype=np.float32), 64)
    ar = rows[:, None] * inv[None, :]
    ac = cols[:, None] * inv[None, :]
    cr, sr, cc, sc_ = np.cos(ar), np.sin(ar), np.cos(ac), np.sin(ac)
    cos = np.concatenate([cr, cr, cc, cc], axis=1).astype(np.float32)
    sin = np.concatenate([-sr, sr, -sc_, sc_], axis=1).astype(np.float32)
    cos = np.concatenate([np.ones((256, rot_dim), np.float32), cos], axis=0)
    sin = np.concatenate([np.zeros((256, rot_dim), np.float32), sin], axis=0)
    return cos, sin


_CONST = {}


def _constants():
    if _CONST:
        return _CONST
    c = {}
    c["ident"] = np.eye(128, dtype=np.float32)
    c["ropeA_cos"], c["ropeA_sin"] = _rope_tables(64)
    mc, ms = _rope_tables(32)
    c["ropeM_cos"], c["ropeM_sin"] = mc, ms
    c["ropeMT_cos"] = np.ascontiguousarray(np.concatenate([mc, mc], axis=1).T)
    c["ropeMT_sin"] = np.ascontiguousarray(np.concatenate([ms, ms], axis=1).T)
    k = np.arange(256)
    ang = 2 * np.pi * ((k[:, None] * k[None, :]) % 256) / 256.0
    c["dft256"] = np.concatenate([np.cos(ang), np.sin(ang)], axis=1).astype(np.float32)
    c["cos256"] = np.cos(ang).astype(ml_dtypes.bfloat16)
    c["nsin256"] = (-np.sin(ang)).astype(ml_dtypes.bfloat16)
    k = np.arange(4096, dtype=np.int64)
    ang = 2 * np.pi * ((k[:, None] * k[None, :]) % 4096) / 4096.0
    c["cos4096"] = np.cos(ang).astype(ml_dtypes.bfloat16)
    c["nsin4096"] = (-np.sin(ang)).astype(ml_dtypes.bfloat16)
    sel = np.zeros((NE, NE, 128), np.float32)
    for e in range(NE):
        sel[e, e, :] = 1.0
    c["sel"] = sel.reshape(NE, NE * 128)
    _CONST.update(c)
    return _CONST


def _prep_inputs(inputs):
    f = lambda a: np.ascontiguousarray(np.asarray(a, dtype=np.float32))
    shared = dict(_constants())
    for k_ in ["w_mod", "b_mod", "norm_g", "a_w_qkv", "a_g_q", "a_g_k", "a_w_o", "f_w", "m_w_down", "m_g_cq", "m_g_ckv",
               "m_w_o", "d_w_gu", "d_w_down", "e_w_router", "e_w_gu", "e_w_down"]:
        shared[k_] = f(inputs[k_])
    wuq = f(inputs["m_w_uq"])[0].reshape(384, 16, 96)
    shared["m_wuqn"] = np.ascontiguousarray(wuq[:, :, 0:64].reshape(384, 1024))
    rope = wuq[:, :, 64:96]
    shared["m_wuqr"] = np.ascontiguousarray(rope.reshape(384, 512))
    perm = np.array([(r + 8) if (r % 16) < 8 else (r - 8) for r in range(32)])
    shared["m_wuqrs"] = np.ascontiguousarray(rope[:, :, perm].reshape(384, 512))
    wukv = f(inputs["m_w_ukv"])[0].reshape(256, 16, 128)
    shared["m_wuk"] = np.ascontiguousarray(wukv[:, :, 0:64].reshape(256, 1024))
    shared["m_wuv"] = np.ascontiguousarray(wukv[:, :, 64:128].reshape(256, 1024))
    x = f(inputs["x"])
    ctx = f(inputs["ctx"])
    cc = f(inputs["c"])
    c_ctx = f(inputs["c_ctx"])
    maps = []
    for b in range(8):
        m = dict(shared)
        m["xin"] = np.ascontiguousarray(np.concatenate([ctx[b], x[b]], axis=0))
        cv = np.stack([cc[b], c_ctx], axis=1)
        m["cvT"] = np.ascontiguousarray(cv.reshape(8, 128, 2).transpose(1, 0, 2).reshape(128, 16))
        maps.append(m)
    return maps


_NC_CACHE = {}


def kernel(**inputs):
    maps = _prep_inputs(inputs)
    if "nc" not in _NC_CACHE:
        _NC_CACHE["nc"] = Builder().build()
    nc = _NC_CACHE["nc"]
    res = run_bass_kernel_spmd(nc, maps, core_ids=list(range(8)))
    out = np.stack([np.asarray(r["out"], dtype=np.float32) for r in res.results], axis=0)
    return out
```

```python
import contextlib
import numpy as np
import ml_dtypes
import concourse.bass as bass
import concourse.mybir as mybir
from concourse.bass_utils import run_bass_kernel_spmd

F32 = mybir.dt.float32
BF16 = mybir.dt.bfloat16
AF = mybir.ActivationFunctionType
ALU = mybir.AluOpType
AX = mybir.AxisListType

D = 1024
T = 4352
NT = 34
NCTX_T = 2
DEPTH = 4
EPS = 1e-6
FF_D = 2816
FF_E = 3584
NE = 8
NSLOT = 2176
I32 = mybir.dt.int32


class Tile:
    __slots__ = ("name", "writers", "readers")

    def __init__(self, name=""):
        self.name = name
        self.writers = {}
        self.readers = {}


class Op:
    __slots__ = ("stream", "tl", "deps", "fn", "marked", "epoch", "value", "seq", "cond")


class Timeline:
    def __init__(self, name, inc, limit):
        self.name = name
        self.inc = inc
        self.limit = limit
        self.ops = []
        self.epoch = None
        self.cnt = 0
        self.last = None


class Sched:
    STREAMS = ("sp", "act", "dve", "pool", "pe")
    ENG = {"sp": "sync", "act": "scalar", "dve": "vector", "pool": "gpsimd", "pe": "tensor"}

    def __init__(self, nc, semalloc, ndma=8):
        self.nc = nc
        self.semalloc = semalloc
        self.ops = {s: [] for s in self.STREAMS}
        self.tls = {s: Timeline(s, 1, 30000) for s in ("act", "dve", "pool", "pe")}
        self.dma_tls = {q: [Timeline(f"dma_{q}{i}", 16, 1800) for i in range(ndma)] for q in ("sp", "pool")}
        self.dma_rr = {q: 0 for q in self.dma_tls}
        self.all_tiles = []
        self.nseq = 0
        self.waited = {s: {} for s in self.STREAMS}
        self.nops = 0
        self.cur_cond = None
        self.valcache = {}

    def begin_cond(self, cnt_ap, thr, maxv):
        self.cur_cond = (cnt_ap, thr, maxv)

    def end_cond(self):
        self.cur_cond = None

    def all_tls(self):
        return list(self.tls.values()) + [t for q in self.dma_tls.values() for t in q]

    def tile(self, name=""):
        t = Tile(name)
        self.all_tiles.append(t)
        return t

    def _mk(self, stream, tl, fn, reads, writes, extra=()):
        op = Op()
        op.stream = stream
        op.tl = tl
        op.fn = fn
        op.marked = False
        op.epoch = None
        op.value = None
        op.seq = self.nseq
        op.cond = self.cur_cond
        assert not (op.cond is not None and tl.inc == 16), "no DMA inside conditional regions"
        self.nseq += 1
        deps = {}

        def add(d):
            k = d.tl
            if k not in deps or deps[k].seq < d.seq:
                deps[k] = d

        for t in reads:
            for d in t.writers.values():
                add(d)
        for t in writes:
            for d in t.writers.values():
                add(d)
            for d in t.readers.values():
                add(d)
        for d in extra:
            add(d)
        if stream == "pe" and self.tls["pe"] in deps:
            del deps[self.tls["pe"]]
        op.deps = list(deps.values())
        for d in op.deps:
            d.marked = True
        for t in reads:
            t.readers[tl] = op
        for t in writes:
            t.writers = {tl: op}
            t.readers = {}
        tl.ops.append(op)
        tl.last = op
        self.ops[stream].append(op)
        self.nops += 1
        return op

    def op(self, stream, fn, reads=(), writes=()):
        return self._mk(stream, self.tls[stream], fn, reads, writes)

    def dma(self, queue, fn, reads=(), writes=()):
        tls = self.dma_tls[queue]
        i = self.dma_rr[queue]
        self.dma_rr[queue] = (i + 1) % len(tls)
        tl = tls[i]
        prev = tl.ops[-1] if tl.ops else None
        op = self._mk(queue, tl, fn, reads, writes, extra=(prev,) if prev is not None else ())
        op.marked = True
        return op

    def barrier_and_emit(self):
        lasts = [tl.ops[-1] for tl in self.all_tls() if tl.ops]
        for s in self.STREAMS:
            op = Op()
            op.stream = s
            op.tl = None
            op.fn = None
            op.marked = False
            op.epoch = None
            op.value = None
            op.seq = self.nseq
            op.cond = None
            self.nseq += 1
            op.deps = [d for d in lasts if not (s == "pe" and d.tl is self.tls["pe"])]
            for d in op.deps:
                d.marked = True
            self.ops[s].append(op)
        for t in self.all_tiles:
            t.writers = {}
            t.readers = {}
        self._emit()

    def _emit(self):
        nc = self.nc
        for tl in self.all_tls():
            for op in tl.ops:
                if not op.marked:
                    continue
                if tl.epoch is None or tl.cnt >= tl.limit:
                    tl.epoch = self.semalloc(tl.name)
                    tl.cnt = 0
                tl.cnt += 1
                op.epoch = tl.epoch
                op.value = tl.cnt * tl.inc
        with nc.Block() as block:
            for s in self.STREAMS:
                ops = self.ops[s]
                if not ops:
                    continue

                def body(e, ops=ops, waited=self.waited[s], sname=s):
                    def emit_op(op):
                        for d in op.deps:
                            key = id(d.epoch)
                            if waited.get(key, 0) >= d.value:
                                continue
                            e.wait_ge(d.epoch, d.value)
                            waited[key] = d.value
                        if op.fn is None:
                            return
                        ins = op.fn(e)
                        if op.marked:
                            ins.then_inc(op.epoch, op.tl.inc)

                    i = 0
                    while i < len(ops):
                        op = ops[i]
                        if op.cond is None:
                            emit_op(op)
                            i += 1
                            continue
                        j = i
                        while j < len(ops) and ops[j].cond is op.cond:
                            j += 1
                        region = ops[i:j]
                        cnt_ap, thr, maxv = op.cond
                        ck = id(cnt_ap)
                        st_ = self.valcache.setdefault(sname, {"reg": None, "key": None, "val": None})
                        if st_["reg"] is None:
                            st_["reg"] = self.cond_regs[sname]
                        if st_["key"] != ck:
                            e.reg_load(st_["reg"], cnt_ap)
                            st_["val"] = e.snap(st_["reg"])
                            st_["key"] = ck
                        val = st_["val"]
                        saved = dict(waited)
                        with e.If(val > thr):
                            for rop in region:
                                emit_op(rop)
                        with e.Else():
                            incs = {}
                            for rop in region:
                                if rop.marked:
                                    k_ = id(rop.epoch)
                                    if k_ not in incs:
                                        incs[k_] = [rop.epoch, 0]
                                    incs[k_][1] += rop.tl.inc
                            if incs:
                                e.drain()
                            for ep_, n_ in incs.values():
                                e.sem_inc(ep_, n_)
                        waited.clear()
                        waited.update(saved)
                        i = j

                getattr(block, self.ENG[s])(body)
        for s in self.STREAMS:
            self.ops[s] = []
        for tl in self.all_tls():
            tl.ops = []
        for st_ in self.valcache.values():
            st_["key"] = None
            st_["val"] = None


class Rot:
    def __init__(self, S, alloc, name, shape, dt, n):
        self.bufs = [alloc(f"{name}{i}", shape, dt) for i in range(n)]
        self.tiles = [S.tile(f"{name}{i}") for i in range(n)]
        self.i = 0

    def next(self):
        b, t = self.bufs[self.i], self.tiles[self.i]
        self.i = (self.i + 1) % len(self.bufs)
        return b, t


class _Stop(Exception):
    pass


class Builder:
    def __init__(self, nlayers=DEPTH, debug=False, nphases=None):
        self.nphases = nphases
        self.phase_i = 0
        self.nlayers = nlayers
        self.debug = debug
        nc = self.nc = bass.Bass("TRN2", target_bir_lowering=False)
        self.es = contextlib.ExitStack()
        self.uid = 0

        def semalloc(name):
            self.uid += 1
            return self.es.enter_context(nc.semaphore(f"{name}_{self.uid}"))

        self.S = Sched(nc, semalloc)
        self.S.cond_regs = {"pe": nc.tensor.alloc_register("cr_pe"), "act": nc.scalar.alloc_register("cr_act"),
                            "dve": nc.vector.alloc_register("cr_dve"), "pool": nc.gpsimd.alloc_register("cr_pool")}

    def din(self, name, shape, dt=F32):
        return self.nc.dram_tensor(name, list(shape), dt, kind="ExternalInput").ap()

    def dscr(self, name, shape, dt):
        return self.nc.dram_tensor(name, list(shape), dt, kind="Internal").ap()

    def declare(self):
        d = self.din
        self.xin = d("xin", [T, D])
        self.cvT = d("cvT", [128, 16])
        self.ident_d = d("ident", [128, 128])
        self.w_mod = d("w_mod", [DEPTH, D, 6 * D])
        self.b_mod = d("b_mod", [DEPTH, 6 * D])
        self.norm_g = d("norm_g", [DEPTH, 4, D])
        self.a_w_qkv = d("a_w_qkv", [2, D, 1536])
        self.a_g_q = d("a_g_q", [2, 64])
        self.a_g_k = d("a_g_k", [2, 64])
        self.a_w_o = d("a_w_o", [2, D, D])
        self.f_w = d("f_w", [1, D, D])
        self.m_w_down = d("m_w_down", [1, D, 672])
        self.m_g_cq = d("m_g_cq", [1, 384])
        self.m_g_ckv = d("m_g_ckv", [1, 256])
        self.m_wuqn = d("m_wuqn", [384, 1024])
        self.m_wuqr = d("m_wuqr", [384, 512])
        self.m_wuqrs = d("m_wuqrs", [384, 512])
        self.m_wuk = d("m_wuk", [256, 1024])
        self.m_wuv = d("m_wuv", [256, 1024])
        self.m_w_o = d("m_w_o", [1, D, D])
        self.d_w_gu = d("d_w_gu", [2, D, 2 * FF_D])
        self.d_w_down = d("d_w_down", [2, FF_D, D])
        self.e_w_router = d("e_w_router", [2, D, NE])
        self.e_w_gu = d("e_w_gu", [2, NE, D, 2 * FF_E])
        self.e_w_down = d("e_w_down", [2, NE, FF_E, D])
        self.ropeA_cos = d("ropeA_cos", [T, 64])
        self.ropeA_sin = d("ropeA_sin", [T, 64])
        self.ropeM_cos = d("ropeM_cos", [T, 32])
        self.ropeM_sin = d("ropeM_sin", [T, 32])
        self.ropeMT_cos = d("ropeMT_cos", [64, T])
        self.ropeMT_sin = d("ropeMT_sin", [64, T])
        self.dft256 = d("dft256", [256, 512])
        self.cos4096 = d("cos4096", [4096, 4096], BF16)
        self.nsin4096 = d("nsin4096", [4096, 4096], BF16)
        self.cos256 = d("cos256", [256, 256], BF16)
        self.nsin256 = d("nsin256", [256, 256], BF16)
        self.sel_d = d("sel", [NE, NE * 128])
        self.tri_d = d("tri", [128, 128])
        self.iota_d = d("iota512", [128, 512])
        self.eoff_d = d("eoff", [128, NE])
        self.out = self.nc.dram_tensor("out", [4096, D], F32, kind="ExternalOutput").ap()
        s = self.dscr
        if self.debug:
            self.xres = self.nc.dram_tensor("xres", [T, D], F32, kind="ExternalOutput").ap()
        else:
            self.xres = s("xres", [T, D], F32)
        self.modrows = s("modrows", [DEPTH, 2, 6 * D], F32)
        self.ye_d = s("arena", [NE * NSLOT, D], F32)
        ab = self.ye_d.bitcast(BF16)
        r0 = [0]

        def carve(nrows):
            v = ab[r0[0]:r0[0] + nrows, :].rearrange("r c -> (r c)")
            r0[0] += nrows
            return v

        self.QT_d = carve(2176).rearrange("(c p n) -> c p n", c=8, p=128)
        self.QrT_d = carve(1088).rearrange("(c p n) -> c p n", c=8, p=64)
        self.KnT_d = carve(2176).rearrange("(c p n) -> c p n", c=8, p=128)
        self.V_d = carve(2210).rearrange("(t p n) -> t p n", t=NT, p=128)
        self.XCS_d = carve(4352).rearrange("(g t n) -> g t n", g=4, t=T)
        self.h2T_d = carve(2176).rearrange("(p c n) -> p c n", p=128, c=8)
        self.h2f_d = s("h2f_d", [T, D], F32)

    def scope(self):
        b = self

        class _Scope:
            def __enter__(s):
                if b.nphases is not None and b.phase_i >= b.nphases:
                    raise _Stop()
                b.phase_i += 1
                s.es = contextlib.ExitStack()
                s.es.__enter__()
                return s

            def sb(s, name, shape, dt):
                b.uid += 1
                return s.es.enter_context(b.nc.sbuf_tensor(f"{name}_{b.uid}", list(shape), dt))

            def ps(s, name, shape, dt):
                b.uid += 1
                return s.es.enter_context(b.nc.psum_tensor(f"{name}_{b.uid}", list(shape), dt))

            def rot(s, name, shape, dt, n, psum=False):
                return Rot(b.S, s.ps if psum else s.sb, name, shape, dt, n)

            def __exit__(s, *a):
                if a[0] is None:
                    b.S.barrier_and_emit()
                return s.es.__exit__(*a)

        return _Scope()

    def dma(self, q, out, in_, reads=(), writes=()):
        return self.S.dma(q, lambda e: e.dma_start(out=out, in_=in_), reads, writes)

    def act(self, out, in_, func, reads, writes, **kw):
        return self.S.op("act", lambda e: e.activation(out=out, in_=in_, func=func, **kw), reads, writes)

    def tt(self, eng, out, in0, in1, op, reads, writes):
        return self.S.op(eng, lambda e: e.tensor_tensor(out=out, in0=in0, in1=in1, op=op), reads, writes)

    def stt(self, out, in0, scalar, in1, op0, op1, reads, writes):
        return self.S.op("dve", lambda e: e.scalar_tensor_tensor(out=out, in0=in0, scalar=scalar, in1=in1,
                                                                  op0=op0, op1=op1), reads, writes)

    def ts(self, eng, out, in0, s1, op0, reads, writes, s2=None, op1=None):
        if op1 is None:
            return self.S.op(eng, lambda e: e.tensor_scalar(out=out, in0=in0, scalar1=s1, scalar2=None, op0=op0),
                             reads, writes)
        return self.S.op(eng, lambda e: e.tensor_scalar(out=out, in0=in0, scalar1=s1, scalar2=s2, op0=op0, op1=op1),
                         reads, writes)

    def copy(self, eng, out, in_, reads, writes):
        if eng == "act":
            return self.act(out, in_, AF.Copy, reads, writes)
        return self.S.op(eng, lambda e: e.tensor_copy(out=out, in_=in_), reads, writes)

    def mm(self, out, lhsT, rhs, start, stop, reads, writes):
        return self.S.op("pe", lambda e: e.matmul(out, lhsT=lhsT, rhs=rhs, start=start, stop=stop), reads, writes)

    def tr(self, out, in_, ident, reads, writes):
        return self.S.op("pe", lambda e: e.transpose(out=out, in_=in_, identity=ident), reads, writes)

    def recip(self, out, in_, reads, writes):
        return self.S.op("dve", lambda e: e.reciprocal(out=out, in_=in_), reads, writes)

    def memset(self, eng, ap, val, writes):
        return self.S.op(eng, lambda e: e.memset(ap, val), (), writes)

    def common(self, sc, njunk=1536):
        c = type("C", (), {})()
        c.ss = sc.rot("ss", [128, 1], F32, 4)
        c.rs = sc.rot("rs", [128, 1], F32, 4)
        c.junk = sc.rot("junk", [128, njunk], BF16, 2)
        c.identb = sc.sb("identb", [128, 128], BF16)
        c.identf = sc.sb("identf", [128, 128], F32)
        c.t_id = self.S.tile("ident")
        self.dma("sp", c.identf[:], self.ident_d[:, :], writes=[c.t_id])
        self.copy("dve", c.identb[:], c.identf[:], [c.t_id], [c.t_id])
        return c

    def rstd_of(self, c, src, n, reads, eps=EPS):
        ss, t_ss = c.ss.next()
        jk, t_jk = c.junk.next()
        self.act(jk[:, 0:n], src, AF.Square, reads, [t_jk, t_ss], accum_out=ss[:])
        rs, t_rs = c.rs.next()
        self.act(rs[:], ss[:], AF.Sqrt, [t_ss], [t_rs], scale=1.0 / n, bias=eps)
        self.recip(rs[:], rs[:], [t_rs], [t_rs])
        return rs, t_rs

    def load_mod(self, sc, layer, which, ctx_rows=True):
        res = {}
        gt = {}
        for (j, kind, nrow) in which:
            if kind != "shift" and nrow not in gt:
                g = sc.sb(f"gbc{nrow}", [128, D], F32)
                tg = self.S.tile()
                self.dma("sp", g[:], self.norm_g[layer, nrow:nrow + 1, :].partition_broadcast(128), writes=[tg])
                gt[nrow] = (g, tg)
            for r in range(2 if ctx_rows else 1):
                m = sc.sb(f"mod{j}_{r}", [128, D], F32)
                tm = self.S.tile()
                self.dma("sp", m[:], self.modrows[layer, r:r + 1, j * D:(j + 1) * D].partition_broadcast(128),
                         writes=[tm])
                if kind == "scale":
                    g, tg = gt[nrow]
                    self.stt(m[:], m[:], 1.0, g[:], ALU.add, ALU.mult, [tm, tg], [tm])
                elif kind == "gate":
                    g, tg = gt[nrow]
                    self.tt("dve", m[:], m[:], g[:], ALU.mult, [tm, tg], [tm])
                res[(j, r)] = (m, tm)
        return res

    def phase_mod(self):
        with self.scope() as sc:
            cv = sc.sb("cv", [128, 16], F32)
            scT = sc.sb("scT", [128, 16], F32)
            t_cv = self.S.tile()
            self.dma("sp", cv[:], self.cvT[:, :], writes=[t_cv])
            self.act(scT[:], cv[:], AF.Silu, [t_cv], [t_cv])
            wm = sc.rot("wm", [128, 8, 512], F32, 3)
            bm = sc.rot("bm", [2, 512], F32, 3)
            mr = sc.rot("mr", [2, 512], F32, 3)
            ps = sc.rot("mps", [2, 512], F32, 2, psum=True)
            for i in range(self.nlayers):
                for nb in range(12):
                    w, t_w = wm.next()
                    self.dma("sp", w[:], self.w_mod[i, :, nb * 512:(nb + 1) * 512].rearrange("(c p) n -> p c n", p=128),
                             writes=[t_w])
                    b_, t_b = bm.next()
                    self.dma("sp", b_[:], self.b_mod[i:i + 1, nb * 512:(nb + 1) * 512].partition_broadcast(2),
                             writes=[t_b])
                    p, t_p = ps.next()
                    for k in range(8):
                        self.mm(p[:], scT[:, 2 * k:2 * k + 2], w[:, k, :], k == 0, k == 7, [t_cv, t_w], [t_p])
                    m, t_m = mr.next()
                    self.tt("dve", m[:], p[:], b_[:], ALU.add, [t_p, t_b], [t_m])
                    self.dma("sp", self.modrows[i, :, nb * 512:(nb + 1) * 512], m[:], reads=[t_m])

    def mk_pre(self, sc, c):
        p = type("P", (), {})()
        p.x = sc.rot("px", [128, D], F32, 3)
        p.tmp = sc.rot("ptmp", [128, D], F32, 2)
        p.hb = sc.rot("phb", [128, D], BF16, 2)
        p.hT = sc.rot("phT", [128, 8, 128], BF16, 3)
        return p

    def prenorm(self, c, p, pT, xsrc, tt_, A, B):
        A_ap, t_A = A
        B_ap, t_B = B
        xt, t_x = p.x.next()
        self.dma("sp", xt[:], xsrc[tt_ * 128:(tt_ + 1) * 128, :], writes=[t_x])
        r, t_r = self.rstd_of(c, xt[:], D, [t_x])
        tmp, t_tmp = p.tmp.next()
        self.stt(tmp[:], xt[:], r[:], A_ap[:], ALU.mult, ALU.mult, [t_x, t_r, t_A], [t_tmp])
        hb, t_hb = p.hb.next()
        self.tt("pool", hb[:], tmp[:], B_ap[:], ALU.add, [t_tmp, t_B], [t_hb])
        return self.transpose8(c, p, pT, hb, t_hb)

    def transpose8(self, c, p, pT, hb, t_hb):
        ps, t_ps = pT.next()
        for k in range(8):
            self.tr(ps[:, k, :], hb[:, k * 128:(k + 1) * 128], c.identb[:], [t_hb, c.t_id], [t_ps])
        hT, t_hT = p.hT.next()
        self.copy("act", hT[:], ps[:], [t_ps], [t_hT])
        return hT, t_hT

    def mk_epi(self, sc, c, moe):
        p = type("E", (), {})()
        p.x = sc.rot("ex", [128, D], F32, 2)
        p.tmp = sc.rot("etmp", [128, D], F32, 2)
        p.xn = sc.rot("exn", [128, D], F32, 2)
        p.hb = sc.rot("ehb", [128, D], BF16, 2)
        p.hT = sc.rot("ehT", [128, 8, 128], BF16, 2)
        p.hf = sc.rot("ehf", [128, D], F32, 2) if moe else None
        return p

    def epilogue(self, c, p, pT, xsrc, tt_, y_ap, y_tiles, G, A2, B2, do_h2, moe):
        G_ap, t_G = G
        rows = slice(tt_ * 128, (tt_ + 1) * 128)
        xt, t_x = p.x.next()
        self.dma("sp", xt[:], xsrc[rows, :], writes=[t_x])
        r, t_r = self.rstd_of(c, y_ap, D, y_tiles)
        tmp, t_tmp = p.tmp.next()
        self.stt(tmp[:], y_ap, r[:], G_ap[:], ALU.mult, ALU.mult, list(y_tiles) + [t_r, t_G], [t_tmp])
        xn, t_xn = p.xn.next()
        self.tt("pool", xn[:], xt[:], tmp[:], ALU.add, [t_x, t_tmp], [t_xn])
        self.dma("sp", self.xres[rows, :], xn[:], reads=[t_xn])
        if not do_h2:
            return
        A_ap, t_A = A2
        B_ap, t_B = B2
        r2, t_r2 = self.rstd_of(c, xn[:], D, [t_xn])
        t2, t_t2 = p.tmp.next()
        self.stt(t2[:], xn[:], r2[:], A_ap[:], ALU.mult, ALU.mult, [t_xn, t_r2, t_A], [t_t2])
        hb, t_hb = p.hb.next()
        if moe:
            hf, t_hf = p.hf.next()
            self.tt("pool", hf[:], t2[:], B_ap[:], ALU.add, [t_t2, t_B], [t_hf])
            self.dma("sp", self.h2f_d[rows, :], hf[:], reads=[t_hf])
            if self.cur_sparse:
                return
            self.copy("pool", hb[:], hf[:], [t_hf], [t_hb])
        else:
            self.tt("pool", hb[:], t2[:], B_ap[:], ALU.add, [t_t2, t_B], [t_hb])
        hT, t_hT = self.transpose8(c, p, pT, hb, t_hb)
        self.dma("sp", self.h2T_d[:, :, rows], hT[:], reads=[t_hT])

    def phase_gqa_proj(self, layer, j, xsrc, lay):
        KT, V_aug = lay.KT, lay.V
        with self.scope() as sc:
            c = self.common(sc)
            p = self.mk_pre(sc, c)
            mods = self.load_mod(sc, layer, [(0, "shift", None), (1, "scale", 0)])
            wq = sc.sb("wqkv", [128, 8, 1536], BF16)
            t_w = self.S.tile()
            for nb in range(3):
                self.dma("pool", wq[:, :, nb * 512:(nb + 1) * 512],
                         self.a_w_qkv[j, :, nb * 512:(nb + 1) * 512].rearrange("(c p) n -> p c n", p=128), writes=[t_w])
            gain = sc.sb("gain", [128, 20, 64], F32)
            t_g = self.S.tile()
            for hh in range(20):
                src = self.a_g_q if hh < 16 else self.a_g_k
                self.dma("sp", gain[:, hh, :], src[j:j + 1, :].partition_broadcast(128), writes=[t_g])
            cosT = sc.sb("cosT", [128, NT, 64], F32)
            sinT = sc.sb("sinT", [128, NT, 64], F32)
            t_tab = self.S.tile()
            self.dma("sp", cosT[:], self.ropeA_cos.rearrange("(t p) d -> p t d", p=128), writes=[t_tab])
            self.dma("sp", sinT[:], self.ropeA_sin.rearrange("(t p) d -> p t d", p=128), writes=[t_tab])
            self.memset("pool", V_aug[:, :, :, 64:65], 1.0, [lay.t_V])
            pT = sc.rot("pT", [128, 8, 128], BF16, 2, psum=True)
            qps = sc.rot("qps", [128, 1536], F32, 2, psum=True)
            sq = sc.rot("sq", [128, 1280], F32, 1)
            ssq = sc.rot("ssq", [128, 20], F32, 2)
            qn = sc.rot("qn", [128, 20, 64], F32, 2)
            qa = sc.rot("qa", [128, 20, 64], F32, 1)
            qb = sc.rot("qb", [128, 20, 64], F32, 1)
            qr = sc.rot("qr", [128, 20, 64], BF16, 2)
            kd = sc.rot("kd", [128, 4, 2, 64], BF16, 2)
            qst = sc.rot("qst", [128, 8, 128], BF16, 2)
            for tt_ in range(NT):
                r_ = 1 if tt_ < NCTX_T else 0
                hT, t_hT = self.prenorm(c, p, pT, xsrc, tt_, mods[(1, r_)], mods[(0, r_)])
                ps, t_ps = qps.next()
                for nb in range(3):
                    for k in range(8):
                        self.mm(ps[:, nb * 512:(nb + 1) * 512], hT[:, k, :], wq[:, k, nb * 512:(nb + 1) * 512],
                                k == 0, k == 7, [t_hT, t_w], [t_ps])
                s_, t_s = sq.next()
                self.act(s_[:], ps[:, 0:1280], AF.Square, [t_ps], [t_s])
                ss_, t_ss = ssq.next()
                self.S.op("dve", lambda e, o=ss_, i=s_: e.tensor_reduce(
                    out=o[:], in_=i[:].rearrange("p (h d) -> p h d", d=64), axis=AX.X, op=ALU.add), [t_s], [t_ss])
                self.act(ss_[:], ss_[:], AF.Sqrt, [t_ss], [t_ss], scale=1.0 / 64, bias=EPS)
                self.recip(ss_[:], ss_[:], [t_ss], [t_ss])
                q_, t_q = qn.next()
                self.tt("dve", q_[:], ps[:, 0:1280].rearrange("p (h d) -> p h d", d=64),
                        ss_[:].unsqueeze(2).to_broadcast([128, 20, 64]), ALU.mult, [t_ps, t_ss], [t_q])
                self.tt("pool", q_[:], q_[:], gain[:], ALU.mult, [t_q, t_g], [t_q])
                a_, t_a = qa.next()
                self.tt("dve", a_[:], q_[:], cosT[:, tt_, :].unsqueeze(1).to_broadcast([128, 20, 64]), ALU.mult,
                        [t_q, t_tab], [t_a])
                b_, t_b = qb.next()
                q5 = q_[:].rearrange("p h (b s f) -> p h b s f", b=2, s=2)
                b5 = b_[:].rearrange("p h (b s f) -> p h b s f", b=2, s=2)
                s5 = sinT[:, tt_, :].rearrange("p (b s f) -> p b s f", b=2, s=2)
                for s in range(2):
                    self.tt("pool", b5[:, :, :, s, :], q5[:, :, :, 1 - s, :],
                            s5[:, :, s, :].unsqueeze(1).to_broadcast([128, 20, 2, 16]), ALU.mult, [t_q, t_tab], [t_b])
                o_, t_o = qr.next()
                self.tt("dve", o_[:], a_[:], b_[:], ALU.add, [t_a, t_b], [t_o])
                self.copy("act", V_aug[:, tt_, :, 0:64], ps[:, 1280:1536].rearrange("p (h d) -> p h d", d=64),
                          [t_ps], [lay.t_V])
                kd_, t_kd = kd.next()
                for dd in range(2):
                    self.copy("pool", kd_[:, :, dd, :], o_[:, 16:20, :], [t_o], [t_kd])
                pk, t_pk = pT.next()
                for kv in range(4):
                    self.tr(pk[:, kv, :], kd_[:, kv, :, :].rearrange("p a d -> p (a d)"), c.identb[:], [t_kd, c.t_id],
                            [t_pk])
                self.copy("act", KT[:, :, tt_ * 128:(tt_ + 1) * 128], pk[:, 0:4, :], [t_pk], [lay.t_KT])
                pq, t_pq = pT.next()
                of = o_[:].rearrange("p h d -> p (h d)")
                for k in range(8):
                    self.tr(pq[:, k, :], of[:, k * 128:(k + 1) * 128], c.identb[:], [t_o, c.t_id], [t_pq])
                st, t_st = qst.next()
                self.copy("dve", st[:], pq[:], [t_pq], [t_st])
                self.dma("sp", self.QT_d[:, :, tt_ * 128:(tt_ + 1) * 128].rearrange("c p n -> p c n"), st[:],
                         reads=[t_st])

    def phase_attn(self, kind, layer, j, xsrc, lay, last, moe):
        scale = 0.125 if kind == "gqa" else (96.0 ** -0.5)
        w_o_d = self.a_w_o[j] if kind == "gqa" else self.m_w_o[0]
        with self.scope() as sc:
            c = self.common(sc, njunk=1024)
            ep = self.mk_epi(sc, c, moe)
            wl = [(2, "gate", 1), (3, "shift", None), (4, "scale", 2)]
            mods = self.load_mod(sc, layer, wl, ctx_rows=not last)
            wo = sc.sb("wo", [128, 8, D], BF16)
            t_wo = self.S.tile()
            self.dma("pool", wo[:], w_o_d.rearrange("(c p) n -> p c n", p=128), writes=[t_wo])
            Sps = sc.ps("Sps", [128, 2, 512], F32)
            t_S = [self.S.tile(), self.S.tile()]
            Ops = [sc.ps(f"Ops{i}", [128, 512], F32) for i in range(4)]
            t_O = [self.S.tile() for _ in range(4)]
            pT = sc.rot("pT", [128, 8, 128], BF16, 2, psum=True)
            PT = sc.rot("PT", [128, 512], BF16, 4)
            qp = sc.rot("qp", [128, 512], BF16, 3)
            attn = sc.rot("attn", [128, 4, D], BF16, 2)
            aT = sc.rot("aT", [128, 8, 128], BF16, 2)
            rec = sc.rot("rec", [128, 1], F32, 8)
            if kind == "mla":
                qrp = sc.rot("qrp", [64, 512], BF16, 3)
                knp = sc.rot("knp", [128, T], BF16, 2)
                vpp = sc.rot("vpp", [128, NT, 130], BF16, 2)
            groups = []
            if not last:
                groups.append((0, 2, [0, 1]))
            for g in range(8):
                groups.append((256 + g * 512, 4, list(range(NT))))
            sidx = 0
            for (tok0, nsub, kts) in groups:
                N = nsub * 128
                nk = len(kts)
                at, t_at = attn.next()
                its = []
                for pj in range(8):
                    for r in range(2):
                        for ki, kt in enumerate(kts):
                            its.append(dict(pj=pj, r=r, ki=ki, kt=kt))
                cur = {}

                def emit_S(it):
                    nonlocal sidx
                    pj, r, ki, kt = it["pj"], it["r"], it["ki"], it["kt"]
                    if r == 0 and ki == 0:
                        q_, t_q = qp.next()
                        self.dma("sp", q_[:, 0:N], self.QT_d[pj, :, tok0:tok0 + N], writes=[t_q])
                        cur["q"] = (q_, t_q)
                        if kind == "mla":
                            qr_, t_qr = qrp.next()
                            self.dma("sp", qr_[:, 0:N], self.QrT_d[pj, :, tok0:tok0 + N], writes=[t_qr])
                            kn_, t_kn = knp.next()
                            self.dma("sp", kn_[:, 0:nk * 128], self.KnT_d[pj, :, 0:nk * 128], writes=[t_kn])
                            vp_, t_vp = vpp.next()
                            self.dma("sp", vp_[:, 0:nk, :],
                                     self.V_d[0:nk, :, pj * 130:(pj + 1) * 130].rearrange("t p c -> p t c"),
                                     writes=[t_vp])
                            cur["qr"] = (qr_, t_qr)
                            cur["kn"] = (kn_, t_kn)
                            cur["vp"] = (vp_, t_vp)
                    q_, t_q = cur["q"]
                    h = 2 * pj + r
                    kvh = h // 4
                    sb_ = sidx % 2
                    sidx += 1
                    s_ap = Sps[:, sb_, 0:N]
                    ksl = slice(kt * 128, (kt + 1) * 128)
                    if kind == "gqa":
                        self.mm(s_ap, lay.KT[64 * r:64 * r + 64, kvh, ksl], q_[64 * r:64 * r + 64, 0:N],
                                True, True, [t_q], [t_S[sb_]])
                    else:
                        qr_, t_qr = cur["qr"]
                        kn_, t_kn = cur["kn"]
                        it["vp"] = cur["vp"]
                        self.mm(s_ap, kn_[64 * r:64 * r + 64, ksl], q_[64 * r:64 * r + 64, 0:N],
                                True, False, [t_q, t_kn], [t_S[sb_]])
                        self.mm(s_ap, lay.KrT[32 * r:32 * r + 32, ksl], qr_[32 * r:32 * r + 32, 0:N],
                                False, True, [t_qr], [t_S[sb_]])
                    pt, t_pt = PT.next()
                    self.act(pt[:, 0:N], s_ap, AF.Exp, [t_S[sb_]], [t_pt], scale=scale)
                    it["pt"] = (pt, t_pt)

                def emit_PV(it):
                    pj, r, ki, kt = it["pj"], it["r"], it["ki"], it["kt"]
                    h = 2 * pj + r
                    kvh = h // 4
                    pt, t_pt = it["pt"]
                    for sub in range(nsub):
                        if kind == "gqa":
                            v_ap = lay.V[:, kt, kvh, :]
                            rd = [t_pt]
                        else:
                            vp_, t_vp = it["vp"]
                            v_ap = vp_[:, kt, r * 65:(r + 1) * 65]
                            rd = [t_pt, t_vp]
                        self.mm(Ops[sub][:, 0:65], pt[:, sub * 128:(sub + 1) * 128], v_ap,
                                ki == 0, ki == nk - 1, rd, [t_O[sub]])
                    if ki == nk - 1:
                        for sub in range(nsub):
                            rc, t_rc = rec.next()
                            self.recip(rc[:], Ops[sub][:, 64:65], [t_O[sub]], [t_rc])
                            self.ts("dve", at[:, sub, h * 64:(h + 1) * 64], Ops[sub][:, 0:64], rc[:], ALU.mult,
                                    [t_O[sub], t_rc], [t_at])

                for i in range(len(its) + 1):
                    if i < len(its):
                        emit_S(its[i])
                    if i >= 1:
                        emit_PV(its[i - 1])
                for sub in range(nsub):
                    tt_ = tok0 // 128 + sub
                    r_ = 1 if tt_ < NCTX_T else 0
                    ps, t_ps = pT.next()
                    for k in range(8):
                        self.tr(ps[:, k, :], at[:, sub, k * 128:(k + 1) * 128], c.identb[:], [t_at, c.t_id], [t_ps])
                    a_, t_a = aT.next()
                    self.copy("act", a_[:], ps[:], [t_ps], [t_a])
                    y_ap = Sps[:].rearrange("p a n -> p (a n)")
                    for nb in range(2):
                        for k in range(8):
                            self.mm(Sps[:, nb, :], a_[:, k, :], wo[:, k, nb * 512:(nb + 1) * 512], k == 0, k == 7,
                                    [t_a, t_wo], [t_S[nb]])
                    self.epilogue(c, ep, pT, xsrc, tt_, y_ap, t_S, mods[(2, r_)], mods[(4, r_)], mods[(3, r_)],
                                  True, moe)

    def phase_fourier_1(self, layer, xsrc):
        with self.scope() as sc:
            c = self.common(sc)
            p = self.mk_pre(sc, c)
            mods = self.load_mod(sc, layer, [(0, "shift", None), (1, "scale", 0)])
            cs = sc.sb("cs", [128, 2, 512], BF16)
            t_cs = self.S.tile()
            self.dma("pool", cs[:], self.dft256.rearrange("(c p) n -> p c n", p=128), writes=[t_cs])
            pT = sc.rot("pT", [128, 8, 128], BF16, 2, psum=True)
            xps = sc.rot("xps", [128, 4, 512], F32, 1, psum=True)
            xsb = sc.rot("xsb", [128, 4, 512], BF16, 2)
            for tt_ in range(NT):
                r_ = 1 if tt_ < NCTX_T else 0
                hT, t_hT = self.prenorm(c, p, pT, xsrc, tt_, mods[(1, r_)], mods[(0, r_)])
                ps, t_ps = xps.next()
                for g in range(4):
                    for k in range(2):
                        self.mm(ps[:, g, :], hT[:, 2 * g + k, :], cs[:, k, :], k == 0, k == 1, [t_hT, t_cs], [t_ps])
                xs, t_xs = xsb.next()
                self.copy("act", xs[:], ps[:], [t_ps], [t_xs])
                self.dma("sp", self.XCS_d[:, tt_ * 128:(tt_ + 1) * 128, :].rearrange("g p n -> p g n"), xs[:],
                         reads=[t_xs])

    def phase_fourier_2(self, layer, xsrc, last, moe):
        with self.scope() as sc:
            c = self.common(sc, njunk=1024)
            ep = self.mk_epi(sc, c, moe)
            mods = self.load_mod(sc, layer, [(2, "gate", 1), (3, "shift", None), (4, "scale", 2)], ctx_rows=not last)
            fw = sc.sb("fw", [128, 8, D], BF16)
            t_fw = self.S.tile()
            self.dma("pool", fw[:], self.f_w[0].rearrange("(c p) n -> p c n", p=128), writes=[t_fw])
            Tc = sc.sb("Tc", [128, 32, 512], BF16)
            Ts = sc.sb("Ts", [128, 32, 512], BF16)
            t_T = self.S.tile()
            X = sc.rot("X", [128, 32, 512], BF16, 1)
            fT = sc.rot("fT", [128, 8, 512], BF16, 1)
            pT = sc.rot("pT", [128, 8, 128], BF16, 2, psum=True)
            fps = sc.rot("fps", [128, 512], F32, 2, psum=True)
            yps = sc.ps("yps", [128, 2, 512], F32)
            t_y = [self.S.tile(), self.S.tile()]
            groups = []
            if not last:
                groups.append(("ctx", 0, 2, 2))
            for g in range(8):
                groups.append(("lat", 256 + g * 512, 4, 32))
            for (kind, tok0, nsub, na) in groups:
                N = nsub * 128
                if kind == "ctx":
                    self.dma("sp", Tc[:, 0:2, 0:256], self.cos256.rearrange("(a p) n -> p a n", p=128), writes=[t_T])
                    self.dma("sp", Ts[:, 0:2, 0:256], self.nsin256.rearrange("(a p) n -> p a n", p=128), writes=[t_T])
                    fscale = 1.0 / 256
                else:
                    k0 = tok0 - 256
                    self.dma("sp", Tc[:], self.cos4096[:, k0:k0 + 512].rearrange("(a p) n -> p a n", p=128),
                             writes=[t_T])
                    self.dma("sp", Ts[:], self.nsin4096[:, k0:k0 + 512].rearrange("(a p) n -> p a n", p=128),
                             writes=[t_T])
                    fscale = 1.0 / 1024
                f_, t_f = fT.next()
                for g in range(4):
                    x_, t_x = X.next()
                    if kind == "ctx":
                        self.dma("sp", x_[:, 0:2, :], self.XCS_d[g, 0:256, :].rearrange("(a p) n -> p a n", p=128),
                                 writes=[t_x])
                    else:
                        self.dma("sp", x_[:], self.XCS_d[g, 256:T, :].rearrange("(a p) n -> p a n", p=128),
                                 writes=[t_x])
                    for lc in range(2):
                        ps, t_ps = fps.next()
                        n_mm = 2 * na
                        i_mm = 0
                        for (tab, off) in ((Tc, 0), (Ts, 256)):
                            for a in range(na):
                                self.mm(ps[:, 0:N], x_[:, a, off + lc * 128:off + (lc + 1) * 128], tab[:, a, 0:N],
                                        i_mm == 0, i_mm == n_mm - 1, [t_x, t_T], [t_ps])
                                i_mm += 1
                        self.act(f_[:, 2 * g + lc, 0:N], ps[:, 0:N], AF.Copy, [t_ps], [t_f], scale=fscale)
                for sub in range(nsub):
                    tt_ = tok0 // 128 + sub
                    r_ = 1 if tt_ < NCTX_T else 0
                    for nb in range(2):
                        for k in range(8):
                            self.mm(yps[:, nb, :], f_[:, k, sub * 128:(sub + 1) * 128], fw[:, k, nb * 512:(nb + 1) * 512],
                                    k == 0, k == 7, [t_f, t_fw], [t_y[nb]])
                    self.epilogue(c, ep, pT, xsrc, tt_, yps[:].rearrange("p a n -> p (a n)"), t_y, mods[(2, r_)],
                                  mods[(4, r_)], mods[(3, r_)], True, moe)

    def phase_mla_proj(self, layer, xsrc, lay):
        with self.scope() as sc:
            c = self.common(sc)
            p = self.mk_pre(sc, c)
            mods = self.load_mod(sc, layer, [(0, "shift", None), (1, "scale", 0)])
            t_w = self.S.tile()
            wdn = sc.sb("wdn", [128, 8, 672], BF16)
            self.dma("pool", wdn[:], self.m_w_down[0].rearrange("(c p) n -> p c n", p=128), writes=[t_w])
            wqn = sc.sb("wqn", [128, 3, 1024], BF16)
            self.dma("pool", wqn[:], self.m_wuqn.rearrange("(c p) n -> p c n", p=128), writes=[t_w])
            wqr = sc.sb("wqr", [128, 3, 512], BF16)
            self.dma("pool", wqr[:], self.m_wuqr.rearrange("(c p) n -> p c n", p=128), writes=[t_w])
            wqs = sc.sb("wqs", [128, 3, 512], BF16)
            self.dma("pool", wqs[:], self.m_wuqrs.rearrange("(c p) n -> p c n", p=128), writes=[t_w])
            wuk = sc.sb("wuk", [128, 2, 1024], BF16)
            self.dma("pool", wuk[:], self.m_wuk.rearrange("(c p) n -> p c n", p=128), writes=[t_w])
            wuv = sc.sb("wuv", [128, 2, 1024], BF16)
            self.dma("pool", wuv[:], self.m_wuv.rearrange("(c p) n -> p c n", p=128), writes=[t_w])
            gcq = sc.sb("gcq", [128, 384], F32)
            gckv = sc.sb("gckv", [128, 256], F32)
            t_g = self.S.tile()
            self.dma("sp", gcq[:], self.m_g_cq[0:1, :].partition_broadcast(128), writes=[t_g])
            self.dma("sp", gckv[:], self.m_g_ckv[0:1, :].partition_broadcast(128), writes=[t_g])
            cosT = sc.sb("cosT", [128, NT, 32], F32)
            sinT = sc.sb("sinT", [128, NT, 32], F32)
            t_tab = self.S.tile()
            self.dma("sp", cosT[:], self.ropeM_cos.rearrange("(t p) d -> p t d", p=128), writes=[t_tab])
            self.dma("sp", sinT[:], self.ropeM_sin.rearrange("(t p) d -> p t d", p=128), writes=[t_tab])
            pT = sc.rot("pT", [128, 8, 128], BF16, 2, psum=True)
            dps = sc.rot("dps", [128, 1024], F32, 1, psum=True)
            pps = sc.rot("pps", [128, 512], F32, 2, psum=True)
            vps = sc.rot("vps", [128, 1024], F32, 1, psum=True)
            cqn = sc.rot("cqn", [128, 384], BF16, 2)
            ckvn = sc.rot("ckvn", [128, 256], BF16, 2)
            cqT = sc.rot("cqT", [128, 3, 512], BF16, 2)
            ckT = sc.rot("ckT", [128, 2, 512], BF16, 2)
            ka = sc.rot("ka", [128, 32], F32, 2)
            kb = sc.rot("kb", [128, 32], F32, 2)
            kd = sc.rot("kd", [128, 2, 32], BF16, 2)
            st = sc.rot("st", [128, 512], BF16, 3)
            ctab = sc.rot("ctab", [64, 512], F32, 2)
            stab = sc.rot("stab", [64, 512], F32, 2)
            ra = sc.rot("ra", [64, 512], F32, 2)
            rb = sc.rot("rb", [64, 512], F32, 2)
            rst = sc.rot("rst", [64, 512], BF16, 2)
            vst = sc.rot("vst", [128, 16, 65], BF16, 2)
            for i in range(2):
                self.memset("pool", vst.bufs[i][:, :, 64:65], 1.0, [vst.tiles[i]])
            groups = [(0, 2)] + [(2 + 4 * g, 4) for g in range(8)]
            for (t0, nsub) in groups:
                N = nsub * 128
                tok0 = t0 * 128
                cq_T, t_cqT = cqT.next()
                ck_T, t_ckT = ckT.next()
                for sub in range(nsub):
                    tt_ = t0 + sub
                    r_ = 1 if tt_ < NCTX_T else 0
                    hT, t_hT = self.prenorm(c, p, pT, xsrc, tt_, mods[(1, r_)], mods[(0, r_)])
                    d_, t_d = dps.next()
                    for (o0, o1) in ((0, 512), (512, 672)):
                        for k in range(8):
                            self.mm(d_[:, o0:o1], hT[:, k, :], wdn[:, k, o0:o1], k == 0, k == 7, [t_hT, t_w], [t_d])
                    r1, t_r1 = self.rstd_of(c, d_[:, 0:384], 384, [t_d])
                    cq_, t_cq = cqn.next()
                    self.stt(cq_[:], d_[:, 0:384], r1[:], gcq[:], ALU.mult, ALU.mult, [t_d, t_r1, t_g], [t_cq])
                    r2, t_r2 = self.rstd_of(c, d_[:, 384:640], 256, [t_d])
                    ck_, t_ck = ckvn.next()
                    self.stt(ck_[:], d_[:, 384:640], r2[:], gckv[:], ALU.mult, ALU.mult, [t_d, t_r2, t_g], [t_ck])
                    a_, t_a = ka.next()
                    self.tt("dve", a_[:], d_[:, 640:672], cosT[:, tt_, :], ALU.mult, [t_d, t_tab], [t_a])
                    b_, t_b = kb.next()
                    d4 = d_[:, 640:672].rearrange("p (b s f) -> p b s f", b=2, s=2)
                    b4 = b_[:].rearrange("p (b s f) -> p b s f", b=2, s=2)
                    s4 = sinT[:, tt_, :].rearrange("p (b s f) -> p b s f", b=2, s=2)
                    for s in range(2):
                        self.tt("dve", b4[:, :, s, :], d4[:, :, 1 - s, :], s4[:, :, s, :], ALU.mult, [t_d, t_tab], [t_b])
                    kd_, t_kd = kd.next()
                    for dd in range(2):
                        self.tt("pool", kd_[:, dd, :], a_[:], b_[:], ALU.add, [t_a, t_b], [t_kd])
                    ps, t_ps = pT.next()
                    for k in range(3):
                        self.tr(ps[:, k, :], cq_[:, k * 128:(k + 1) * 128], c.identb[:], [t_cq, c.t_id], [t_ps])
                    for k in range(2):
                        self.tr(ps[:, 3 + k, :], ck_[:, k * 128:(k + 1) * 128], c.identb[:], [t_ck, c.t_id], [t_ps])
                    if True:
                        self.tr(ps[0:64, 5, :], kd_[:].rearrange("p a d -> p (a d)"), c.identb[:], [t_kd, c.t_id], [t_ps])
                    self.copy("act", cq_T[:, :, sub * 128:(sub + 1) * 128], ps[:, 0:3, :], [t_ps], [t_cqT])
                    self.copy("act", ck_T[:, :, sub * 128:(sub + 1) * 128], ps[:, 3:5, :], [t_ps], [t_ckT])
                    self.copy("dve", lay.KrT[:, tt_ * 128:(tt_ + 1) * 128], ps[0:64, 5, :], [t_ps], [lay.t_KrT])
                for sub in range(nsub):
                    tt_ = t0 + sub
                    v_, t_v = vps.next()
                    for nb in range(2):
                        for k in range(2):
                            self.mm(v_[:, nb * 512:(nb + 1) * 512], ck_T[:, k, sub * 128:(sub + 1) * 128],
                                    wuv[:, k, nb * 512:(nb + 1) * 512], k == 0, k == 1, [t_ckT, t_w], [t_v])
                    vs, t_vs = vst.next()
                    self.copy("act", vs[:, :, 0:64], v_[:].rearrange("p (h d) -> p h d", d=64), [t_v], [t_vs])
                    self.dma("sp", self.V_d[tt_, :, :], vs[:].rearrange("p h d -> p (h d)"), reads=[t_vs])
                ct_, t_ct = ctab.next()
                st_, t_stb = stab.next()
                self.dma("sp", ct_[:, 0:N], self.ropeMT_cos[:, tok0:tok0 + N], writes=[t_ct])
                self.dma("sp", st_[:, 0:N], self.ropeMT_sin[:, tok0:tok0 + N], writes=[t_stb])
                for pj in range(8):
                    ps, t_ps = pps.next()
                    for k in range(3):
                        self.mm(ps[:, 0:N], wqn[:, k, pj * 128:(pj + 1) * 128], cq_T[:, k, 0:N], k == 0, k == 2,
                                [t_cqT, t_w], [t_ps])
                    s_, t_s = st.next()
                    self.copy("act", s_[:, 0:N], ps[:, 0:N], [t_ps], [t_s])
                    self.dma("sp", self.QT_d[pj, :, tok0:tok0 + N], s_[:, 0:N], reads=[t_s])
                    ps, t_ps = pps.next()
                    for k in range(2):
                        self.mm(ps[:, 0:N], wuk[:, k, pj * 128:(pj + 1) * 128], ck_T[:, k, 0:N], k == 0, k == 1,
                                [t_ckT, t_w], [t_ps])
                    s_, t_s = st.next()
                    self.copy("act", s_[:, 0:N], ps[:, 0:N], [t_ps], [t_s])
                    self.dma("sp", self.KnT_d[pj, :, tok0:tok0 + N], s_[:, 0:N], reads=[t_s])
                    ps, t_ps = pps.next()
                    for k in range(3):
                        self.mm(ps[0:64, 0:N], wqr[:, k, pj * 64:(pj + 1) * 64], cq_T[:, k, 0:N], k == 0, k == 2,
                                [t_cqT, t_w], [t_ps])
                    a_, t_a = ra.next()
                    self.tt("dve", a_[:, 0:N], ps[0:64, 0:N], ct_[:, 0:N], ALU.mult, [t_ps, t_ct], [t_a])
                    ps, t_ps = pps.next()
                    for k in range(3):
                        self.mm(ps[0:64, 0:N], wqs[:, k, pj * 64:(pj + 1) * 64], cq_T[:, k, 0:N], k == 0, k == 2,
                                [t_cqT, t_w], [t_ps])
                    b_, t_b = rb.next()
                    self.tt("dve", b_[:, 0:N], ps[0:64, 0:N], st_[:, 0:N], ALU.mult, [t_ps, t_stb], [t_b])
                    o_, t_o = rst.next()
                    self.tt("pool", o_[:, 0:N], a_[:, 0:N], b_[:, 0:N], ALU.add, [t_a, t_b], [t_o])
                    self.dma("sp", self.QrT_d[pj, :, tok0:tok0 + N], o_[:, 0:N], reads=[t_o])

    def phase_ffn(self, layer, moe, last, xsrc_unused):
        idx = layer // 2
        tiles = list(range(NCTX_T, NT)) if last else list(range(NT))
        nh = len(tiles) // 2
        halves = [tiles[:nh], tiles[nh:]]
        E = NE if moe else 1
        F = FF_E if moe else FF_D
        nfb = F // 256
        for hi, half in enumerate(halves):
            nt = len(half)
            tok0 = half[0] * 128
            NTOK = nt * 128
            with contextlib.ExitStack() as hes:
                yacc = hes.enter_context(self.nc.sbuf_tensor(f"yacc{layer}_{hi}", [128, nt, D], F32))
                with self.scope() as sc:
                    c = self.common(sc, njunk=1024)
                    h2T = sc.sb("h2T", [128, 8, NTOK], BF16)
                    t_h = self.S.tile()
                    self.dma("sp", h2T[:], self.h2T_d[:, :, tok0:tok0 + NTOK], writes=[t_h])
                    t_ya = [self.S.tile() for _ in range(nt)]
                    gps = sc.rot("gps", [128, 512], F32, 2, psum=True)
                    ups = sc.rot("ups", [128, 512], F32, 2, psum=True)
                    yps = sc.rot("yps", [128, 2, 512], F32, 2, psum=True)
                    wg = sc.rot("wg", [128, 8, 256], BF16, 3)
                    wu = sc.rot("wu", [128, 8, 256], BF16, 3)
                    wd = sc.rot("wd", [128, 2, D], BF16, 3)
                    sg = sc.rot("sg", [128, 512], F32, 2)
                    aT = sc.rot("aT", [128, 2, 512], BF16, 2)
                    if moe:
                        t1 = sc.rot("t1", [128, 512], F32, 2)
                        gbs = sc.rot("gbs", [128, NTOK], F32, 2)
                        gatesT = sc.sb("gatesT", [NE, NTOK], F32)
                        t_gT = self.S.tile()
                        sel = sc.sb("sel", [NE, NE * 128], F32)
                        t_sel = self.S.tile()
                        self.dma("sp", sel[:], self.sel_d[:, :], writes=[t_sel])
                        self.router(sc, c, idx, half, tok0, gatesT, t_gT, yps, ups)
                    groups = []
                    g0 = 0
                    while g0 < NTOK:
                        n = min(512, NTOK - g0)
                        groups.append((g0, n))
                        g0 += n
                    its = []
                    for e in range(E):
                        for fb in range(nfb):
                            for gi, (g0, n) in enumerate(groups):
                                its.append(dict(e=e, fb=fb, gi=gi, g0=g0, n=n))
                    cur = {}

                    def stage_a(it):
                        e, fb, gi, g0, n = it["e"], it["fb"], it["gi"], it["g0"], it["n"]
                        if moe:
                            wgu_d = self.e_w_gu[idx, e]
                            wdn_d = self.e_w_down[idx, e]
                        else:
                            wgu_d = self.d_w_gu[idx]
                            wdn_d = self.d_w_down[idx]
                        if gi == 0:
                            if moe and fb == 0:
                                gb_, t_gb = gbs.next()
                                for (gg0, gn) in groups:
                                    m_, t_m = gps.next()
                                    self.mm(m_[:, 0:gn], sel[:, e * 128:(e + 1) * 128], gatesT[:, gg0:gg0 + gn],
                                            True, True, [t_sel, t_gT], [t_m])
                                    self.copy("act", gb_[:, gg0:gg0 + gn], m_[:, 0:gn], [t_m], [t_gb])
                                cur["gb"] = (gb_, t_gb)
                            wg_, t_wg = wg.next()
                            wu_, t_wu = wu.next()
                            wd_, t_wd = wd.next()
                            f0 = fb * 256
                            self.dma("pool", wg_[:], wgu_d[:, f0:f0 + 256].rearrange("(c p) n -> p c n", p=128),
                                     writes=[t_wg])
                            self.dma("pool", wu_[:], wgu_d[:, F + f0:F + f0 + 256].rearrange("(c p) n -> p c n", p=128),
                                     writes=[t_wu])
                            self.dma("pool", wd_[:], wdn_d[f0:f0 + 256, :].rearrange("(c p) n -> p c n", p=128),
                                     writes=[t_wd])
                            cur["w"] = (wg_, t_wg, wu_, t_wu, wd_, t_wd)
                        wg_, t_wg, wu_, t_wu, wd_, t_wd = cur["w"]
                        it["wd"] = (wd_, t_wd)
                        a_, t_a = aT.next()
                        it["a"] = (a_, t_a)
                        for fc in range(2):
                            g_, t_g = gps.next()
                            u_, t_u = ups.next()
                            for k in range(8):
                                self.mm(g_[:, 0:n], wg_[:, k, fc * 128:(fc + 1) * 128], h2T[:, k, g0:g0 + n],
                                        k == 0, k == 7, [t_wg, t_h], [t_g])
                            for k in range(8):
                                self.mm(u_[:, 0:n], wu_[:, k, fc * 128:(fc + 1) * 128], h2T[:, k, g0:g0 + n],
                                        k == 0, k == 7, [t_wu, t_h], [t_u])
                            s_, t_s = sg.next()
                            self.act(s_[:, 0:n], g_[:, 0:n], AF.Silu, [t_g], [t_s])
                            if moe:
                                gb_, t_gb = cur["gb"]
                                t1_, t_t1 = t1.next()
                                self.tt("dve", t1_[:, 0:n], s_[:, 0:n], u_[:, 0:n], ALU.mult, [t_s, t_u], [t_t1])
                                self.tt("dve", a_[:, fc, 0:n], t1_[:, 0:n], gb_[:, g0:g0 + n], ALU.mult,
                                        [t_t1, t_gb], [t_a])
                            else:
                                self.tt("dve", a_[:, fc, 0:n], s_[:, 0:n], u_[:, 0:n], ALU.mult, [t_s, t_u], [t_a])

                    def stage_b(it):
                        e, fb, g0, n = it["e"], it["fb"], it["g0"], it["n"]
                        first = (e == 0 and fb == 0)
                        a_, t_a = it["a"]
                        wd_, t_wd = it["wd"]
                        for sub in range(n // 128):
                            ti = g0 // 128 + sub
                            y_, t_y = yps.next()
                            for nb in range(2):
                                for fc in range(2):
                                    self.mm(y_[:, nb, :], a_[:, fc, sub * 128:(sub + 1) * 128],
                                            wd_[:, fc, nb * 512:(nb + 1) * 512], fc == 0, fc == 1, [t_a, t_wd], [t_y])
                            yf = y_[:].rearrange("p a n -> p (a n)")
                            if first:
                                self.copy("act", yacc[:, ti, :], yf, [t_y], [t_ya[ti]])
                            else:
                                self.tt("dve", yacc[:, ti, :], yacc[:, ti, :], yf, ALU.add, [t_y, t_ya[ti]],
                                        [t_ya[ti]])

                    for i in range(len(its) + 1):
                        if i < len(its):
                            stage_a(its[i])
                        if i >= 1:
                            stage_b(its[i - 1])
                with self.scope() as sc:
                    c = self.common(sc, njunk=1024)
                    mods = self.load_mod(sc, layer, [(5, "gate", 3)], ctx_rows=not last)
                    xr = sc.rot("xr", [128, D], F32, 3)
                    tm = sc.rot("tm", [128, D], F32, 2)
                    xo = sc.rot("xo", [128, D], F32, 3)
                    for ti, tt_ in enumerate(half):
                        r_ = 1 if tt_ < NCTX_T else 0
                        G_ap, t_G = mods[(5, r_)]
                        rows = slice(tt_ * 128, (tt_ + 1) * 128)
                        x_, t_x = xr.next()
                        self.dma("sp", x_[:], self.xres[rows, :], writes=[t_x])
                        r, t_r = self.rstd_of(c, yacc[:, ti, :], D, [])
                        t_, t_t = tm.next()
                        self.stt(t_[:], yacc[:, ti, :], r[:], G_ap[:], ALU.mult, ALU.mult, [t_r, t_G], [t_t])
                        o_, t_o = xo.next()
                        self.tt("pool", o_[:], x_[:], t_[:], ALU.add, [t_x, t_t], [t_o])
                        if layer == self.nlayers - 1:
                            if tt_ >= NCTX_T:
                                self.dma("sp", self.out[(tt_ - NCTX_T) * 128:(tt_ - NCTX_T + 1) * 128, :], o_[:],
                                         reads=[t_o])
                            if self.debug:
                                self.dma("sp", self.xres[rows, :], o_[:], reads=[t_o])
                        else:
                            self.dma("sp", self.xres[rows, :], o_[:], reads=[t_o])

    def phase_moe(self, layer, last):
        idx = layer // 2
        tiles = list(range(NCTX_T, NT)) if last else list(range(NT))
        nh = len(tiles) // 2
        halves = [tiles[:nh], tiles[nh:]]
        F = FF_E
        nfb = F // 256
        for hi, half in enumerate(halves):
            nt = len(half)
            tok0 = half[0] * 128
            NTOK = nt * 128
            groups = []
            g0 = 0
            while g0 < NTOK:
                n = min(512, NTOK - g0)
                groups.append((g0, n))
                g0 += n
            with contextlib.ExitStack() as hes:
                hsb = lambda name, shape, dt: hes.enter_context(
                    self.nc.sbuf_tensor(f"{name}{layer}_{hi}", list(shape), dt))
                gates = hsb("gates", [128, nt, NE], F32)
                posm = hsb("posm", [128, nt, NE], F32)
                cnt_i = hsb("cnti", [1, NE], I32)
                with self.scope() as sc:
                    c = self.common(sc, njunk=1024)
                    wr = sc.sb("wr", [128, 8, NE], F32)
                    t_wr = self.S.tile()
                    self.dma("sp", wr[:], self.e_w_router[idx].rearrange("(c p) n -> p c n", p=128), writes=[t_wr])
                    trif = sc.sb("trif", [128, 128], F32)
                    trib = sc.sb("trib", [128, 128], BF16)
                    oneb = sc.sb("oneb", [128, 128], BF16)
                    t_tri = self.S.tile()
                    self.dma("sp", trif[:], self.tri_d[:, :], writes=[t_tri])
                    self.copy("dve", trib[:], trif[:], [t_tri], [t_tri])
                    self.memset("pool", oneb[:], 1.0, [t_tri])
                    maskb = sc.sb("maskb", [128, nt, NE], BF16)
                    maskf = sc.sb("maskf", [128, nt, NE], F32)
                    t_mask = self.S.tile()
                    t_gates = self.S.tile()
                    t_posm = self.S.tile()
                    hf = sc.rot("rhf", [128, D], F32, 2)
                    hfT = sc.rot("rhfT", [128, 8, 128], F32, 2)
                    lg = sc.rot("lg", [128, NE], F32, 2)
                    m8 = sc.rot("m8", [128, 8], F32, 2)
                    dd = sc.rot("dd", [128, NE], F32, 2)
                    ee = sc.rot("ee", [128, NE], F32, 2)
                    sm = sc.rot("sm", [128, 1], F32, 2)
                    tps = sc.rot("tps", [128, 2, 512], F32, 2, psum=True)
                    lps = sc.rot("lps", [128, 512], F32, 2, psum=True)
                    for ti, tt_ in enumerate(half):
                        rows = slice(tt_ * 128, (tt_ + 1) * 128)
                        h_, t_h = hf.next()
                        self.dma("sp", h_[:], self.h2f_d[rows, :], writes=[t_h])
                        y_, t_y = tps.next()
                        yT = y_[:].rearrange("p a (b n) -> p (a b) n", n=128)
                        for k in range(8):
                            self.tr(yT[:, k, :], h_[:, k * 128:(k + 1) * 128], c.identf[:], [t_h, c.t_id], [t_y])
                        hT_, t_hT = hfT.next()
                        self.copy("act", hT_[:], yT, [t_y], [t_hT])
                        lp, t_lp = lps.next()
                        for k in range(8):
                            self.mm(lp[:, 0:NE], hT_[:, k, :], wr[:, k, :], k == 0, k == 7, [t_hT, t_wr], [t_lp])
                        l_, t_l = lg.next()
                        self.copy("dve", l_[:], lp[:, 0:NE], [t_lp], [t_l])
                        m_, t_m = m8.next()
                        self.S.op("dve", lambda e, o=m_, i=l_: e.max(out=o[:], in_=i[:]), [t_l], [t_m])
                        d_, t_d = dd.next()
                        self.ts("dve", d_[:], l_[:], m_[:, 0:1], ALU.subtract, [t_l, t_m], [t_d])
                        e_, t_e = ee.next()
                        self.act(e_[:], d_[:], AF.Exp, [t_d], [t_e])
                        self.ts("dve", maskf[:, ti, :], l_[:], m_[:, 1:2], ALU.is_ge, [t_l, t_m], [t_mask])
                        self.copy("dve", maskb[:, ti, :], maskf[:, ti, :], [t_mask], [t_mask])
                        self.tt("dve", e_[:], e_[:], maskf[:, ti, :], ALU.mult, [t_e, t_mask], [t_e])
                        s_, t_s = sm.next()
                        self.S.op("dve", lambda e, o=s_, i=e_: e.tensor_reduce(out=o[:], in_=i[:], axis=AX.X, op=ALU.add),
                                  [t_e], [t_s])
                        self.recip(s_[:], s_[:], [t_s], [t_s])
                        self.ts("dve", gates[:, ti, :], e_[:], s_[:], ALU.mult, [t_e, t_s], [t_gates])
                    for ti in range(nt):
                        lp, t_lp = lps.next()
                        self.mm(lp[:, 0:NE], trib[:], maskb[:, ti, :], True, ti == 0, [t_tri, t_mask], [t_lp])
                        for tp in range(ti):
                            self.mm(lp[:, 0:NE], oneb[:], maskb[:, tp, :], False, tp == ti - 1, [t_tri, t_mask], [t_lp])
                        self.tt("dve", posm[:, ti, :], lp[:, 0:NE], maskf[:, ti, :], ALU.mult, [t_lp, t_mask], [t_posm])
                        self.ts("dve", posm[:, ti, :], posm[:, ti, :], -1.0, ALU.add, [t_posm], [t_posm])
                    lp, t_lp = lps.next()
                    for ti in range(nt):
                        self.mm(lp[0:1, 0:NE], oneb[:, 0:1], maskb[:, ti, :], ti == 0, ti == nt - 1, [t_tri, t_mask],
                                [t_lp])
                    cf = sc.sb("cntf", [1, NE], F32)
                    t_cf = self.S.tile()
                    self.copy("dve", cf[:], lp[0:1, 0:NE], [t_lp], [t_cf])
                    self.copy("dve", cnt_i[:], cf[:], [t_cf], [t_cf])
                with self.scope() as sc:
                    c = self.common(sc, njunk=1024)
                    yacc = sc.sb("yacc", [128, nt, D], F32)
                    t_ya = [self.S.tile() for _ in range(nt)]
                    h2tok = sc.sb("h2tok", [128, nt, D], BF16)
                    t_htok = self.S.tile()
                    self.dma("pool", h2tok[:], self.h2f_d[tok0:tok0 + NTOK, :].rearrange("(t p) d -> p t d", p=128),
                             writes=[t_htok])
                    iot = sc.sb("iot", [128, 512], F32)
                    t_io = self.S.tile()
                    self.dma("sp", iot[:], self.iota_d[:, :], writes=[t_io])
                    hTe = sc.sb("hTe", [128, 8, NTOK], BF16)
                    t_hTe = [self.S.tile() for _ in groups]
                    pb = sc.rot("pb", [128, nt, 512], BF16, 1)
                    gps = sc.rot("gps", [128, 512], F32, 2, psum=True)
                    ups = sc.rot("ups", [128, 512], F32, 2, psum=True)
                    yps = sc.rot("yps", [128, 2, 512], F32, 2, psum=True)
                    wg = sc.rot("wg", [128, 8, 256], BF16, 2)
                    wu = sc.rot("wu", [128, 8, 256], BF16, 2)
                    wd = sc.rot("wd", [128, 2, D], BF16, 2)
                    sg = sc.rot("sg", [128, 512], F32, 2)
                    aT = sc.rot("aT", [128, 2, 512], BF16, 2)
                    for e in range(NE):
                        cap = cnt_i[0:1, e:e + 1]
                        wgu_d = self.e_w_gu[idx, e]
                        wdn_d = self.e_w_down[idx, e]
                        for gi, (g0, n) in enumerate(groups):
                            self.S.begin_cond(cap, g0, NTOK)
                            pb_, t_pb = pb.next()
                            for ti in range(nt):
                                eng = "dve" if ti % 2 == 0 else "pool"
                                self.ts(eng, pb_[:, ti, 0:n], iot[:, 0:n], float(g0), ALU.add, [t_io], [t_pb],
                                        s2=posm[:, ti, e:e + 1], op1=ALU.is_equal)
                            for k in range(8):
                                ps, t_ps = (gps if k % 2 == 0 else ups).next()
                                for ti in range(nt):
                                    self.mm(ps[:, 0:n], h2tok[:, ti, k * 128:(k + 1) * 128], pb_[:, ti, 0:n],
                                            ti == 0, ti == nt - 1, [t_htok, t_pb], [t_ps])
                                self.copy("act" if k % 2 == 0 else "dve", hTe[:, k, g0:g0 + n], ps[:, 0:n], [t_ps],
                                          [t_hTe[gi]])
                            self.S.end_cond()
                        its = []
                        for fb in range(nfb):
                            for gi, (g0, n) in enumerate(groups):
                                its.append(dict(fb=fb, gi=gi, g0=g0, n=n))
                        cur = {}

                        def stage_a(it):
                            fb, gi, g0, n = it["fb"], it["gi"], it["g0"], it["n"]
                            if gi == 0:
                                wg_, t_wg = wg.next()
                                wu_, t_wu = wu.next()
                                wd_, t_wd = wd.next()
                                f0 = fb * 256
                                self.dma("pool", wg_[:], wgu_d[:, f0:f0 + 256].rearrange("(c p) n -> p c n", p=128),
                                         writes=[t_wg])
                                self.dma("pool", wu_[:],
                                         wgu_d[:, F + f0:F + f0 + 256].rearrange("(c p) n -> p c n", p=128),
                                         writes=[t_wu])
                                self.dma("pool", wd_[:], wdn_d[f0:f0 + 256, :].rearrange("(c p) n -> p c n", p=128),
                                         writes=[t_wd])
                                cur["w"] = (wg_, t_wg, wu_, t_wu, wd_, t_wd)
                            wg_, t_wg, wu_, t_wu, wd_, t_wd = cur["w"]
                            it["wd"] = (wd_, t_wd)
                            self.S.begin_cond(cap, g0, NTOK)
                            a_, t_a = aT.next()
                            it["a"] = (a_, t_a)
                            for fc in range(2):
                                g_, t_g = gps.next()
                                u_, t_u = ups.next()
                                for k in range(8):
                                    self.mm(g_[:, 0:n], wg_[:, k, fc * 128:(fc + 1) * 128], hTe[:, k, g0:g0 + n],
                                            k == 0, k == 7, [t_wg, t_hTe[gi]], [t_g])
                                for k in range(8):
                                    self.mm(u_[:, 0:n], wu_[:, k, fc * 128:(fc + 1) * 128], hTe[:, k, g0:g0 + n],
                                            k == 0, k == 7, [t_wu, t_hTe[gi]], [t_u])
                                s_, t_s = sg.next()
                                self.act(s_[:, 0:n], g_[:, 0:n], AF.Silu, [t_g], [t_s])
                                self.tt("dve", a_[:, fc, 0:n], s_[:, 0:n], u_[:, 0:n], ALU.mult, [t_s, t_u], [t_a])
                            self.S.end_cond()

                        def stage_b(it):
                            fb, g0, n = it["fb"], it["g0"], it["n"]
                            a_, t_a = it["a"]
                            wd_, t_wd = it["wd"]
                            self.S.begin_cond(cap, g0, NTOK)
                            for sub in range(n // 128):
                                ti = g0 // 128 + sub
                                y_, t_y = yps.next()
                                for nb in range(2):
                                    for fc in range(2):
                                        self.mm(y_[:, nb, :], a_[:, fc, sub * 128:(sub + 1) * 128],
                                                wd_[:, fc, nb * 512:(nb + 1) * 512], fc == 0, fc == 1, [t_a, t_wd],
                                                [t_y])
                                yf = y_[:].rearrange("p a n -> p (a n)")
                                if fb == 0:
                                    self.copy("act", yacc[:, ti, :], yf, [t_y], [t_ya[ti]])
                                else:
                                    self.tt("dve", yacc[:, ti, :], yacc[:, ti, :], yf, ALU.add, [t_y, t_ya[ti]],
                                            [t_ya[ti]])
                            self.S.end_cond()
                            if fb == nfb - 1:
                                for sub in range(n // 128):
                                    ti = g0 // 128 + sub
                                    r0 = e * NSLOT + ti * 128
                                    self.dma("sp", self.ye_d[r0:r0 + 128, :], yacc[:, ti, :], reads=[t_ya[ti]])

                        for i in range(len(its) + 1):
                            if i < len(its):
                                stage_a(its[i])
                            if i >= 1:
                                stage_b(its[i - 1])
                with self.scope() as sc:
                    c = self.common(sc, njunk=1024)
                    mods = self.load_mod(sc, layer, [(5, "gate", 3)], ctx_rows=not last)
                    eoff = sc.sb("eoff", [128, NE], F32)
                    t_eo = self.S.tile()
                    self.dma("sp", eoff[:], self.eoff_d[:, :], writes=[t_eo])
                    xr = sc.rot("xr", [128, D], F32, 2)
                    tm = sc.rot("tm", [128, D], F32, 2)
                    xo = sc.rot("xo", [128, D], F32, 2)
                    yh = sc.rot("yh", [128, D], F32, 2)
                    yl = sc.rot("yl", [128, D], F32, 2)
                    mk = sc.rot("mk", [128, NE], F32, 2)
                    fl = sc.rot("fl", [128, NE], F32, 2)
                    a8 = sc.rot("a8", [128, 8], F32, 2)
                    eq = sc.rot("eq", [128, NE], F32, 4)
                    gg = sc.rot("gg", [128, 2], F32, 2)
                    ii = sc.rot("ii", [128, 2], I32, 2)
                    fi = sc.rot("fi", [128, 2], F32, 2)
                    for ti, tt_ in enumerate(half):
                        r_ = 1 if tt_ < NCTX_T else 0
                        G_ap, t_G = mods[(5, r_)]
                        rows = slice(tt_ * 128, (tt_ + 1) * 128)
                        x_, t_x = xr.next()
                        self.dma("sp", x_[:], self.xres[rows, :], writes=[t_x])
                        mk_, t_mk = mk.next()
                        self.ts("dve", mk_[:], posm[:, ti, :], 0.0, ALU.is_ge, [], [t_mk])
                        self.tt("dve", mk_[:], mk_[:], eoff[:], ALU.mult, [t_mk, t_eo], [t_mk])
                        fl_, t_fl = fl.next()
                        self.stt(fl_[:], posm[:, ti, :], 1.0, mk_[:], ALU.add, ALU.add, [t_mk], [t_fl])
                        a_, t_a = a8.next()
                        self.S.op("dve", lambda e, o=a_, i=fl_: e.max(out=o[:], in_=i[:]), [t_fl], [t_a])
                        g_, t_g = gg.next()
                        for w in range(2):
                            q_, t_q = eq.next()
                            self.ts("dve", q_[:], fl_[:], a_[:, w:w + 1], ALU.is_equal, [t_fl, t_a], [t_q])
                            self.tt("dve", q_[:], q_[:], gates[:, ti, :], ALU.mult, [t_q], [t_q])
                            self.S.op("dve", lambda e, o=g_, i=q_, w=w: e.tensor_reduce(
                                out=o[:, w:w + 1], in_=i[:], axis=AX.X, op=ALU.add), [t_q], [t_g])
                        f_, t_f = fi.next()
                        self.ts("dve", f_[:], a_[:, 0:2], -1.0, ALU.add, [t_a], [t_f])
                        i_, t_i = ii.next()
                        self.copy("dve", i_[:], f_[:], [t_f], [t_i])
                        yh_, t_yh = yh.next()
                        yl_, t_yl = yl.next()
                        self.S.dma("pool", lambda e, o=yh_, i=i_: e.indirect_dma_start(
                            out=o[:], out_offset=None, in_=self.ye_d[:, :],
                            in_offset=bass.IndirectOffsetOnAxis(ap=i[:, 0:1], axis=0)), [t_i], [t_yh])
                        self.S.dma("pool", lambda e, o=yl_, i=i_: e.indirect_dma_start(
                            out=o[:], out_offset=None, in_=self.ye_d[:, :],
                            in_offset=bass.IndirectOffsetOnAxis(ap=i[:, 1:2], axis=0)), [t_i], [t_yl])
                        self.ts("dve", yh_[:], yh_[:], g_[:, 0:1], ALU.mult, [t_yh, t_g], [t_yh])
                        self.stt(yh_[:], yl_[:], g_[:, 1:2], yh_[:], ALU.mult, ALU.add, [t_yl, t_g, t_yh], [t_yh])
                        r, t_r = self.rstd_of(c, yh_[:], D, [t_yh])
                        t_, t_t = tm.next()
                        self.stt(t_[:], yh_[:], r[:], G_ap[:], ALU.mult, ALU.mult, [t_yh, t_r, t_G], [t_t])
                        o_, t_o = xo.next()
                        self.tt("pool", o_[:], x_[:], t_[:], ALU.add, [t_x, t_t], [t_o])
                        if layer == self.nlayers - 1:
                            if tt_ >= NCTX_T:
                                self.dma("sp", self.out[(tt_ - NCTX_T) * 128:(tt_ - NCTX_T + 1) * 128, :], o_[:],
                                         reads=[t_o])
                            if self.debug:
                                self.dma("sp", self.xres[rows, :], o_[:], reads=[t_o])
                        else:
                            self.dma("sp", self.xres[rows, :], o_[:], reads=[t_o])

    def router(self, sc, c, idx, half, tok0, gatesT, t_gT, yps, ups):
        wr = sc.sb("wr", [128, 8, NE], F32)
        t_wr = self.S.tile()
        self.dma("sp", wr[:], self.e_w_router[idx].rearrange("(c p) n -> p c n", p=128), writes=[t_wr])
        hf = sc.rot("rhf", [128, D], F32, 2)
        hfT = sc.rot("rhfT", [128, 8, 128], F32, 2)
        lg = sc.rot("lg", [128, NE], F32, 2)
        m8 = sc.rot("m8", [128, 8], F32, 2)
        dd = sc.rot("dd", [128, NE], F32, 2)
        ee = sc.rot("ee", [128, NE], F32, 2)
        mk = sc.rot("mk", [128, NE], F32, 2)
        sm = sc.rot("sm", [128, 1], F32, 2)
        gt = sc.rot("gt", [128, NE], F32, 2)
        for ti, tt_ in enumerate(half):
            rows = slice(tt_ * 128, (tt_ + 1) * 128)
            h_, t_h = hf.next()
            self.dma("sp", h_[:], self.h2f_d[rows, :], writes=[t_h])
            y_, t_y = yps.next()
            yT = y_[:].rearrange("p a (b n) -> p (a b) n", n=128)
            for k in range(8):
                self.tr(yT[:, k, :], h_[:, k * 128:(k + 1) * 128], c.identf[:], [t_h, c.t_id], [t_y])
            hT_, t_hT = hfT.next()
            self.copy("act", hT_[:], yT, [t_y], [t_hT])
            misc, t_misc = ups.next()
            for k in range(8):
                self.mm(misc[:, 0:NE], hT_[:, k, :], wr[:, k, :], k == 0, k == 7, [t_hT, t_wr], [t_misc])
            l_, t_l = lg.next()
            self.copy("dve", l_[:], misc[:, 0:NE], [t_misc], [t_l])
            m_, t_m = m8.next()
            self.S.op("dve", lambda e, o=m_, i=l_: e.max(out=o[:], in_=i[:]), [t_l], [t_m])
            d_, t_d = dd.next()
            self.ts("dve", d_[:], l_[:], m_[:, 0:1], ALU.subtract, [t_l, t_m], [t_d])
            e_, t_e = ee.next()
            self.act(e_[:], d_[:], AF.Exp, [t_d], [t_e])
            k_, t_k = mk.next()
            self.ts("dve", k_[:], l_[:], m_[:, 1:2], ALU.is_ge, [t_l, t_m], [t_k])
            self.tt("dve", e_[:], e_[:], k_[:], ALU.mult, [t_e, t_k], [t_e])
            s_, t_s = sm.next()
            self.S.op("dve", lambda e, o=s_, i=e_: e.tensor_reduce(out=o[:], in_=i[:], axis=AX.X, op=ALU.add),
                      [t_e], [t_s])
            self.recip(s_[:], s_[:], [t_s], [t_s])
            g_, t_g = gt.next()
            self.ts("dve", g_[:], e_[:], s_[:], ALU.mult, [t_e, t_s], [t_g])
            self.tr(misc[0:NE, 128:256], g_[:], c.identf[:], [t_g, c.t_id], [t_misc])
            self.copy("dve", gatesT[:, ti * 128:(ti + 1) * 128], misc[0:NE, 128:256], [t_misc], [t_gT])

    def build(self):
        try:
            self._build()
        except _Stop:
            pass
        return self.nc

    def _build(self):
        with self.es:
            self.declare()
            self.phase_mod()
            for layer in range(self.nlayers):
                last = layer == DEPTH - 1
                kind = layer % 3
                j = layer // 3
                moe = layer % 2 == 1
                xsrc = self.xin if layer == 0 else self.xres
                self.cur_sparse = moe and last
                with contextlib.ExitStack() as les:
                    lay = type("L", (), {})()
                    if kind == 0:
                        lay.KT = les.enter_context(self.nc.sbuf_tensor(f"KT{layer}", [128, 4, T], BF16))
                        lay.V = les.enter_context(self.nc.sbuf_tensor(f"Vaug{layer}", [128, NT, 4, 65], BF16))
                        lay.t_KT = self.S.tile()
                        lay.t_V = self.S.tile()
                        self.phase_gqa_proj(layer, j, xsrc, lay)
                        self.phase_attn("gqa", layer, j, xsrc, lay, last, moe)
                    elif kind == 1:
                        self.phase_fourier_1(layer, xsrc)
                        self.phase_fourier_2(layer, xsrc, last, moe)
                    else:
                        lay.KrT = les.enter_context(self.nc.sbuf_tensor(f"KrT{layer}", [64, T], BF16))
                        lay.t_KrT = self.S.tile()
                        self.phase_mla_proj(layer, xsrc, lay)
                        self.phase_attn("mla", layer, j, xsrc, lay, last, moe)
                if self.cur_sparse:
                    self.phase_moe(layer, last)
                else:
                    self.phase_ffn(layer, moe, last, xsrc)
        return self.nc


def _rope_tables(rot_dim):
    nf = rot_dim // 4
    inv = (10000.0 ** (-np.arange(nf, dtype=np.float32) / nf)).astype(np.float32)
    rows = np.repeat(np.arange(64, dtype=np.float32), 64)
    cols = np.tile(np.arange(64, dtype=np.float32), 64)
    ar = rows[:, None] * inv[None, :]
    ac = cols[:, None] * inv[None, :]
    cr, sr, cc, sc_ = np.cos(ar), np.sin(ar), np.cos(ac), np.sin(ac)
    cos = np.concatenate([cr, cr, cc, cc], axis=1).astype(np.float32)
    sin = np.concatenate([-sr, sr, -sc_, sc_], axis=1).astype(np.float32)
    cos = np.concatenate([np.ones((256, rot_dim), np.float32), cos], axis=0)
    sin = np.concatenate([np.zeros((256, rot_dim), np.float32), sin], axis=0)
    return cos, sin


_CONST = {}


def _constants():
    if _CONST:
        return _CONST
    c = {}
    c["ident"] = np.eye(128, dtype=np.float32)
    c["ropeA_cos"], c["ropeA_sin"] = _rope_tables(64)
    mc, ms = _rope_tables(32)
    c["ropeM_cos"], c["ropeM_sin"] = mc, ms
    c["ropeMT_cos"] = np.ascontiguousarray(np.concatenate([mc, mc], axis=1).T)
    c["ropeMT_sin"] = np.ascontiguousarray(np.concatenate([ms, ms], axis=1).T)
    k = np.arange(256)
    ang = 2 * np.pi * ((k[:, None] * k[None, :]) % 256) / 256.0
    c["dft256"] = np.concatenate([np.cos(ang), np.sin(ang)], axis=1).astype(np.float32)
    c["cos256"] = np.cos(ang).astype(ml_dtypes.bfloat16)
    c["nsin256"] = (-np.sin(ang)).astype(ml_dtypes.bfloat16)
    k = np.arange(4096, dtype=np.int64)
    ang = 2 * np.pi * ((k[:, None] * k[None, :]) % 4096) / 4096.0
    c["cos4096"] = np.cos(ang).astype(ml_dtypes.bfloat16)
    c["nsin4096"] = (-np.sin(ang)).astype(ml_dtypes.bfloat16)
    sel = np.zeros((NE, NE, 128), np.float32)
    for e in range(NE):
        sel[e, e, :] = 1.0
    c["sel"] = sel.reshape(NE, NE * 128)
    c["tri"] = np.triu(np.ones((128, 128), np.float32))
    c["iota512"] = np.tile(np.arange(512, dtype=np.float32)[None, :], (128, 1))
    c["eoff"] = np.tile((np.arange(NE, dtype=np.float32) * NSLOT)[None, :], (128, 1))
    _CONST.update(c)
    return _CONST


def _prep_inputs(inputs):
    f = lambda a: np.ascontiguousarray(np.asarray(a, dtype=np.float32))
    shared = dict(_constants())
    for k_ in ["w_mod", "b_mod", "norm_g", "a_w_qkv", "a_g_q", "a_g_k", "a_w_o", "f_w", "m_w_down", "m_g_cq", "m_g_ckv",
               "m_w_o", "d_w_gu", "d_w_down", "e_w_router", "e_w_gu", "e_w_down"]:
        shared[k_] = f(inputs[k_])
    wuq = f(inputs["m_w_uq"])[0].reshape(384, 16, 96)
    shared["m_wuqn"] = np.ascontiguousarray(wuq[:, :, 0:64].reshape(384, 1024))
    rope = wuq[:, :, 64:96]
    shared["m_wuqr"] = np.ascontiguousarray(rope.reshape(384, 512))
    perm = np.array([(r + 8) if (r % 16) < 8 else (r - 8) for r in range(32)])
    shared["m_wuqrs"] = np.ascontiguousarray(rope[:, :, perm].reshape(384, 512))
    wukv = f(inputs["m_w_ukv"])[0].reshape(256, 16, 128)
    shared["m_wuk"] = np.ascontiguousarray(wukv[:, :, 0:64].reshape(256, 1024))
    shared["m_wuv"] = np.ascontiguousarray(wukv[:, :, 64:128].reshape(256, 1024))
    x = f(inputs["x"])
    ctx = f(inputs["ctx"])
    cc = f(inputs["c"])
    c_ctx = f(inputs["c_ctx"])
    maps = []
    for b in range(8):
        m = dict(shared)
        m["xin"] = np.ascontiguousarray(np.concatenate([ctx[b], x[b]], axis=0))
        cv = np.stack([cc[b], c_ctx], axis=1)
        m["cvT"] = np.ascontiguousarray(cv.reshape(8, 128, 2).transpose(1, 0, 2).reshape(128, 16))
        maps.append(m)
    return maps


_NC_CACHE = {}


def kernel(**inputs):
    maps = _prep_inputs(inputs)
    if "nc" not in _NC_CACHE:
        _NC_CACHE["nc"] = Builder().build()
    nc = _NC_CACHE["nc"]
    res = run_bass_kernel_spmd(nc, maps, core_ids=list(range(8)))
    out = np.stack([np.asarray(r["out"], dtype=np.float32) for r in res.results], axis=0)
    return out
```

```python
import contextlib
import numpy as np
import ml_dtypes
import concourse.bass as bass
import concourse.mybir as mybir
from concourse.bass_utils import run_bass_kernel_spmd

F32 = mybir.dt.float32
BF16 = mybir.dt.bfloat16
AF = mybir.ActivationFunctionType
ALU = mybir.AluOpType
AX = mybir.AxisListType

D = 1024
T = 4352
NT = 34
NCTX_T = 2
DEPTH = 4
EPS = 1e-6
FF_D = 2816
FF_E = 3584
NE = 8
NSLOT = 2176
I32 = mybir.dt.int32


class Tile:
    __slots__ = ("name", "writers", "readers")

    def __init__(self, name=""):
        self.name = name
        self.writers = {}
        self.readers = {}


class Op:
    __slots__ = ("stream", "tl", "deps", "fn", "marked", "epoch", "value", "seq", "cond")


class Timeline:
    def __init__(self, name, inc, limit):
        self.name = name
        self.inc = inc
        self.limit = limit
        self.ops = []
        self.epoch = None
        self.cnt = 0
        self.last = None


class Sched:
    STREAMS = ("sp", "act", "dve", "pool", "pe")
    ENG = {"sp": "sync", "act": "scalar", "dve": "vector", "pool": "gpsimd", "pe": "tensor"}

    def __init__(self, nc, semalloc, ndma=8):
        self.nc = nc
        self.semalloc = semalloc
        self.ops = {s: [] for s in self.STREAMS}
        self.tls = {s: Timeline(s, 1, 30000) for s in ("act", "dve", "pool", "pe")}
        self.dma_tls = {q: [Timeline(f"dma_{q}{i}", 16, 1800) for i in range(ndma)] for q in ("sp", "pool")}
        self.dma_rr = {q: 0 for q in self.dma_tls}
        self.all_tiles = []
        self.nseq = 0
        self.waited = {s: {} for s in self.STREAMS}
        self.nops = 0
        self.cur_cond = None
        self.valcache = {}

    def begin_cond(self, cnt_ap, thr, maxv):
        self.cur_cond = (cnt_ap, thr, maxv)

    def end_cond(self):
        self.cur_cond = None

    def all_tls(self):
        return list(self.tls.values()) + [t for q in self.dma_tls.values() for t in q]

    def tile(self, name=""):
        t = Tile(name)
        self.all_tiles.append(t)
        return t

    def _mk(self, stream, tl, fn, reads, writes, extra=()):
        op = Op()
        op.stream = stream
        op.tl = tl
        op.fn = fn
        op.marked = False
        op.epoch = None
        op.value = None
        op.seq = self.nseq
        op.cond = self.cur_cond
        assert not (op.cond is not None and tl.inc == 16), "no DMA inside conditional regions"
        self.nseq += 1
        deps = {}

        def add(d):
            k = d.tl
            if k not in deps or deps[k].seq < d.seq:
                deps[k] = d

        for t in reads:
            for d in t.writers.values():
                add(d)
        for t in writes:
            for d in t.writers.values():
                add(d)
            for d in t.readers.values():
                add(d)
        for d in extra:
            add(d)
        if stream == "pe" and self.tls["pe"] in deps:
            del deps[self.tls["pe"]]
        op.deps = list(deps.values())
        for d in op.deps:
            d.marked = True
        for t in reads:
            t.readers[tl] = op
        for t in writes:
            t.writers = {tl: op}
            t.readers = {}
        tl.ops.append(op)
        tl.last = op
        self.ops[stream].append(op)
        self.nops += 1
        return op

    def op(self, stream, fn, reads=(), writes=()):
        return self._mk(stream, self.tls[stream], fn, reads, writes)

    def dma(self, queue, fn, reads=(), writes=()):
        tls = self.dma_tls[queue]
        i = self.dma_rr[queue]
        self.dma_rr[queue] = (i + 1) % len(tls)
        tl = tls[i]
        prev = tl.ops[-1] if tl.ops else None
        op = self._mk(queue, tl, fn, reads, writes, extra=(prev,) if prev is not None else ())
        op.marked = True
        return op

    def barrier_and_emit(self):
        lasts = [tl.ops[-1] for tl in self.all_tls() if tl.ops]
        for s in self.STREAMS:
            op = Op()
            op.stream = s
            op.tl = None
            op.fn = None
            op.marked = False
            op.epoch = None
            op.value = None
            op.seq = self.nseq
            op.cond = None
            self.nseq += 1
            op.deps = [d for d in lasts if not (s == "pe" and d.tl is self.tls["pe"])]
            for d in op.deps:
                d.marked = True
            self.ops[s].append(op)
        for t in self.all_tiles:
            t.writers = {}
            t.readers = {}
        self._emit()

    def _emit(self):
        nc = self.nc
        for tl in self.all_tls():
            for op in tl.ops:
                if not op.marked:
                    continue
                if tl.epoch is None or tl.cnt >= tl.limit:
                    tl.epoch = self.semalloc(tl.name)
                    tl.cnt = 0
                tl.cnt += 1
                op.epoch = tl.epoch
                op.value = tl.cnt * tl.inc
        with nc.Block() as block:
            for s in self.STREAMS:
                ops = self.ops[s]
                if not ops:
                    continue

                def body(e, ops=ops, waited=self.waited[s], sname=s):
                    def emit_op(op):
                        for d in op.deps:
                            key = id(d.epoch)
                            if waited.get(key, 0) >= d.value:
                                continue
                            e.wait_ge(d.epoch, d.value)
                            waited[key] = d.value
                        if op.fn is None:
                            return
                        ins = op.fn(e)
                        if op.marked:
                            ins.then_inc(op.epoch, op.tl.inc)

                    i = 0
                    while i < len(ops):
                        op = ops[i]
                        if op.cond is None:
                            emit_op(op)
                            i += 1
                            continue
                        j = i
                        while j < len(ops) and ops[j].cond is op.cond:
                            j += 1
                        region = ops[i:j]
                        cnt_ap, thr, maxv = op.cond
                        ck = id(cnt_ap)
                        st_ = self.valcache.setdefault(sname, {"reg": None, "key": None, "val": None})
                        if st_["reg"] is None:
                            st_["reg"] = self.cond_regs[sname]
                        if st_["key"] != ck:
                            e.reg_load(st_["reg"], cnt_ap)
                            st_["val"] = e.snap(st_["reg"])
                            st_["key"] = ck
                        val = st_["val"]
                        saved = dict(waited)
                        with e.If(val > thr):
                            for rop in region:
                                emit_op(rop)
                        with e.Else():
                            incs = {}
                            for rop in region:
                                if rop.marked:
                                    k_ = id(rop.epoch)
                                    if k_ not in incs:
                                        incs[k_] = [rop.epoch, 0]
                                    incs[k_][1] += rop.tl.inc
                            if incs:
                                e.drain()
                            for ep_, n_ in incs.values():
                                e.sem_inc(ep_, n_)
                        waited.clear()
                        waited.update(saved)
                        i = j

                getattr(block, self.ENG[s])(body)
        for s in self.STREAMS:
            self.ops[s] = []
        for tl in self.all_tls():
            tl.ops = []
        for st_ in self.valcache.values():
            st_["key"] = None
            st_["val"] = None


class Rot:
    def __init__(self, S, alloc, name, shape, dt, n):
        self.bufs = [alloc(f"{name}{i}", shape, dt) for i in range(n)]
        self.tiles = [S.tile(f"{name}{i}") for i in range(n)]
        self.i = 0

    def next(self):
        b, t = self.bufs[self.i], self.tiles[self.i]
        self.i = (self.i + 1) % len(self.bufs)
        return b, t


class _Stop(Exception):
    pass


class Builder:
    def __init__(self, nlayers=DEPTH, debug=False, nphases=None):
        self.nphases = nphases
        self.phase_i = 0
        self.nlayers = nlayers
        self.debug = debug
        nc = self.nc = bass.Bass("TRN2", target_bir_lowering=False)
        self.es = contextlib.ExitStack()
        self.uid = 0

        def semalloc(name):
            self.uid += 1
            return self.es.enter_context(nc.semaphore(f"{name}_{self.uid}"))

        self.S = Sched(nc, semalloc)
        self.S.cond_regs = {"pe": nc.tensor.alloc_register("cr_pe"), "act": nc.scalar.alloc_register("cr_act"),
                            "dve": nc.vector.alloc_register("cr_dve"), "pool": nc.gpsimd.alloc_register("cr_pool")}

    def din(self, name, shape, dt=F32):
        return self.nc.dram_tensor(name, list(shape), dt, kind="ExternalInput").ap()

    def dscr(self, name, shape, dt):
        return self.nc.dram_tensor(name, list(shape), dt, kind="Internal").ap()

    def declare(self):
        d = self.din
        self.xin = d("xin", [T, D])
        self.cvT = d("cvT", [128, 16])
        self.ident_d = d("ident", [128, 128])
        self.w_mod = d("w_mod", [DEPTH, D, 6 * D])
        self.b_mod = d("b_mod", [DEPTH, 6 * D])
        self.norm_g = d("norm_g", [DEPTH, 4, D])
        self.a_w_qkv = d("a_w_qkv", [2, D, 1536])
        self.a_g_q = d("a_g_q", [2, 64])
        self.a_g_k = d("a_g_k", [2, 64])
        self.a_w_o = d("a_w_o", [2, D, D])
        self.f_w = d("f_w", [1, D, D])
        self.m_w_down = d("m_w_down", [1, D, 672])
        self.m_g_cq = d("m_g_cq", [1, 384])
        self.m_g_ckv = d("m_g_ckv", [1, 256])
        self.m_wuqn = d("m_wuqn", [384, 1024])
        self.m_wuqr = d("m_wuqr", [384, 512])
        self.m_wuqrs = d("m_wuqrs", [384, 512])
        self.m_wuk = d("m_wuk", [256, 1024])
        self.m_wuv = d("m_wuv", [256, 1024])
        self.m_w_o = d("m_w_o", [1, D, D])
        self.d_w_gu = d("d_w_gu", [2, D, 2 * FF_D])
        self.d_w_down = d("d_w_down", [2, FF_D, D])
        self.e_w_router = d("e_w_router", [2, D, NE])
        self.e_w_gu = d("e_w_gu", [2, NE, D, 2 * FF_E])
        self.e_w_down = d("e_w_down", [2, NE, FF_E, D])
        self.ropeA_cos = d("ropeA_cos", [T, 64])
        self.ropeA_sin = d("ropeA_sin", [T, 64])
        self.ropeM_cos = d("ropeM_cos", [T, 32])
        self.ropeM_sin = d("ropeM_sin", [T, 32])
        self.ropeMT_cos = d("ropeMT_cos", [64, T])
        self.ropeMT_sin = d("ropeMT_sin", [64, T])
        self.dft256 = d("dft256", [256, 512])
        self.cos4096 = d("cos4096", [4096, 4096], BF16)
        self.nsin4096 = d("nsin4096", [4096, 4096], BF16)
        self.cos256 = d("cos256", [256, 256], BF16)
        self.nsin256 = d("nsin256", [256, 256], BF16)
        self.sel_d = d("sel", [NE, NE * 128])
        self.tri_d = d("tri", [128, 128])
        self.iota_d = d("iota512", [128, 512])
        self.eoff_d = d("eoff", [128, NE])
        self.out = self.nc.dram_tensor("out", [4096, D], F32, kind="ExternalOutput").ap()
        s = self.dscr
        if self.debug:
            self.xres = self.nc.dram_tensor("xres", [T, D], F32, kind="ExternalOutput").ap()
        else:
            self.xres = s("xres", [T, D], F32)
        self.modrows = s("modrows", [DEPTH, 2, 6 * D], F32)
        self.ye_d = s("arena", [NE * NSLOT, D], F32)
        ab = self.ye_d.bitcast(BF16)
        r0 = [0]

        def carve(nrows):
            v = ab[r0[0]:r0[0] + nrows, :].rearrange("r c -> (r c)")
            r0[0] += nrows
            return v

        self.QT_d = carve(2176).rearrange("(c p n) -> c p n", c=8, p=128)
        self.QrT_d = carve(1088).rearrange("(c p n) -> c p n", c=8, p=64)
        self.KnT_d = carve(2176).rearrange("(c p n) -> c p n", c=8, p=128)
        self.V_d = carve(2210).rearrange("(t p n) -> t p n", t=NT, p=128)
        self.XCS_d = carve(4352).rearrange("(g t n) -> g t n", g=4, t=T)
        self.h2T_d = carve(2176).rearrange("(p c n) -> p c n", p=128, c=8)
        self.h2f_d = s("h2f_d", [T, D], F32)

    def scope(self):
        b = self

        class _Scope:
            def __enter__(s):
                if b.nphases is not None and b.phase_i >= b.nphases:
                    raise _Stop()
                b.phase_i += 1
                s.es = contextlib.ExitStack()
                s.es.__enter__()
                return s

            def sb(s, name, shape, dt):
                b.uid += 1
                return s.es.enter_context(b.nc.sbuf_tensor(f"{name}_{b.uid}", list(shape), dt))

            def ps(s, name, shape, dt):
                b.uid += 1
                return s.es.enter_context(b.nc.psum_tensor(f"{name}_{b.uid}", list(shape), dt))

            def rot(s, name, shape, dt, n, psum=False):
                return Rot(b.S, s.ps if psum else s.sb, name, shape, dt, n)

            def __exit__(s, *a):
                if a[0] is None:
                    b.S.barrier_and_emit()
                return s.es.__exit__(*a)

        return _Scope()

    def dma(self, q, out, in_, reads=(), writes=()):
        return self.S.dma(q, lambda e: e.dma_start(out=out, in_=in_), reads, writes)

    def act(self, out, in_, func, reads, writes, **kw):
        return self.S.op("act", lambda e: e.activation(out=out, in_=in_, func=func, **kw), reads, writes)

    def tt(self, eng, out, in0, in1, op, reads, writes):
        return self.S.op(eng, lambda e: e.tensor_tensor(out=out, in0=in0, in1=in1, op=op), reads, writes)

    def stt(self, out, in0, scalar, in1, op0, op1, reads, writes):
        return self.S.op("dve", lambda e: e.scalar_tensor_tensor(out=out, in0=in0, scalar=scalar, in1=in1,
                                                                  op0=op0, op1=op1), reads, writes)

    def ts(self, eng, out, in0, s1, op0, reads, writes, s2=None, op1=None):
        if op1 is None:
            return self.S.op(eng, lambda e: e.tensor_scalar(out=out, in0=in0, scalar1=s1, scalar2=None, op0=op0),
                             reads, writes)
        return self.S.op(eng, lambda e: e.tensor_scalar(out=out, in0=in0, scalar1=s1, scalar2=s2, op0=op0, op1=op1),
                         reads, writes)

    def copy(self, eng, out, in_, reads, writes):
        if eng == "act":
            return self.act(out, in_, AF.Copy, reads, writes)
        return self.S.op(eng, lambda e: e.tensor_copy(out=out, in_=in_), reads, writes)

    def mm(self, out, lhsT, rhs, start, stop, reads, writes):
        return self.S.op("pe", lambda e: e.matmul(out, lhsT=lhsT, rhs=rhs, start=start, stop=stop), reads, writes)

    def tr(self, out, in_, ident, reads, writes):
        return self.S.op("pe", lambda e: e.transpose(out=out, in_=in_, identity=ident), reads, writes)

    def recip(self, out, in_, reads, writes):
        return self.S.op("dve", lambda e: e.reciprocal(out=out, in_=in_), reads, writes)

    def memset(self, eng, ap, val, writes):
        return self.S.op(eng, lambda e: e.memset(ap, val), (), writes)

    def common(self, sc, njunk=1536):
        c = type("C", (), {})()
        c.ss = sc.rot("ss", [128, 1], F32, 4)
        c.rs = sc.rot("rs", [128, 1], F32, 4)
        c.junk = sc.rot("junk", [128, njunk], BF16, 2)
        c.identb = sc.sb("identb", [128, 128], BF16)
        c.identf = sc.sb("identf", [128, 128], F32)
        c.t_id = self.S.tile("ident")
        self.dma("sp", c.identf[:], self.ident_d[:, :], writes=[c.t_id])
        self.copy("dve", c.identb[:], c.identf[:], [c.t_id], [c.t_id])
        return c

    def rstd_of(self, c, src, n, reads, eps=EPS):
        ss, t_ss = c.ss.next()
        jk, t_jk = c.junk.next()
        self.act(jk[:, 0:n], src, AF.Square, reads, [t_jk, t_ss], accum_out=ss[:])
        rs, t_rs = c.rs.next()
        self.act(rs[:], ss[:], AF.Sqrt, [t_ss], [t_rs], scale=1.0 / n, bias=eps)
        self.recip(rs[:], rs[:], [t_rs], [t_rs])
        return rs, t_rs

    def load_mod(self, sc, layer, which, ctx_rows=True):
        res = {}
        gt = {}
        for (j, kind, nrow) in which:
            if kind != "shift" and nrow not in gt:
                g = sc.sb(f"gbc{nrow}", [128, D], F32)
                tg = self.S.tile()
                self.dma("sp", g[:], self.norm_g[layer, nrow:nrow + 1, :].partition_broadcast(128), writes=[tg])
                gt[nrow] = (g, tg)
            for r in range(2 if ctx_rows else 1):
                m = sc.sb(f"mod{j}_{r}", [128, D], F32)
                tm = self.S.tile()
                self.dma("sp", m[:], self.modrows[layer, r:r + 1, j * D:(j + 1) * D].partition_broadcast(128),
                         writes=[tm])
                if kind == "scale":
                    g, tg = gt[nrow]
                    self.stt(m[:], m[:], 1.0, g[:], ALU.add, ALU.mult, [tm, tg], [tm])
                elif kind == "gate":
                    g, tg = gt[nrow]
                    self.tt("dve", m[:], m[:], g[:], ALU.mult, [tm, tg], [tm])
                res[(j, r)] = (m, tm)
        return res

    def phase_mod(self):
        with self.scope() as sc:
            cv = sc.sb("cv", [128, 16], F32)
            scT = sc.sb("scT", [128, 16], F32)
            t_cv = self.S.tile()
            self.dma("sp", cv[:], self.cvT[:, :], writes=[t_cv])
            self.act(scT[:], cv[:], AF.Silu, [t_cv], [t_cv])
            wm = sc.rot("wm", [128, 8, 512], F32, 3)
            bm = sc.rot("bm", [2, 512], F32, 3)
            mr = sc.rot("mr", [2, 512], F32, 3)
            ps = sc.rot("mps", [2, 512], F32, 2, psum=True)
            for i in range(self.nlayers):
                for nb in range(12):
                    w, t_w = wm.next()
                    self.dma("sp", w[:], self.w_mod[i, :, nb * 512:(nb + 1) * 512].rearrange("(c p) n -> p c n", p=128),
                             writes=[t_w])
                    b_, t_b = bm.next()
                    self.dma("sp", b_[:], self.b_mod[i:i + 1, nb * 512:(nb + 1) * 512].partition_broadcast(2),
                             writes=[t_b])
                    p, t_p = ps.next()
                    for k in range(8):
                        self.mm(p[:], scT[:, 2 * k:2 * k + 2], w[:, k, :], k == 0, k == 7, [t_cv, t_w], [t_p])
                    m, t_m = mr.next()
                    self.tt("dve", m[:], p[:], b_[:], ALU.add, [t_p, t_b], [t_m])
                    self.dma("sp", self.modrows[i, :, nb * 512:(nb + 1) * 512], m[:], reads=[t_m])

    def mk_pre(self, sc, c):
        p = type("P", (), {})()
        p.x = sc.rot("px", [128, D], F32, 3)
        p.tmp = sc.rot("ptmp", [128, D], F32, 2)
        p.hb = sc.rot("phb", [128, D], BF16, 2)
        p.hT = sc.rot("phT", [128, 8, 128], BF16, 3)
        return p

    def prenorm(self, c, p, pT, xsrc, tt_, A, B):
        A_ap, t_A = A
        B_ap, t_B = B
        xt, t_x = p.x.next()
        self.dma("sp", xt[:], xsrc[tt_ * 128:(tt_ + 1) * 128, :], writes=[t_x])
        r, t_r = self.rstd_of(c, xt[:], D, [t_x])
        tmp, t_tmp = p.tmp.next()
        self.stt(tmp[:], xt[:], r[:], A_ap[:], ALU.mult, ALU.mult, [t_x, t_r, t_A], [t_tmp])
        hb, t_hb = p.hb.next()
        self.tt("pool", hb[:], tmp[:], B_ap[:], ALU.add, [t_tmp, t_B], [t_hb])
        return self.transpose8(c, p, pT, hb, t_hb)

    def transpose8(self, c, p, pT, hb, t_hb):
        ps, t_ps = pT.next()
        for k in range(8):
            self.tr(ps[:, k, :], hb[:, k * 128:(k + 1) * 128], c.identb[:], [t_hb, c.t_id], [t_ps])
        hT, t_hT = p.hT.next()
        self.copy("act", hT[:], ps[:], [t_ps], [t_hT])
        return hT, t_hT

    def mk_epi(self, sc, c, moe):
        p = type("E", (), {})()
        p.x = sc.rot("ex", [128, D], F32, 2)
        p.tmp = sc.rot("etmp", [128, D], F32, 2)
        p.xn = sc.rot("exn", [128, D], F32, 2)
        p.hb = sc.rot("ehb", [128, D], BF16, 2)
        p.hT = sc.rot("ehT", [128, 8, 128], BF16, 2)
        p.hf = sc.rot("ehf", [128, D], F32, 2) if moe else None
        return p

    def epilogue(self, c, p, pT, xsrc, tt_, y_ap, y_tiles, G, A2, B2, do_h2, moe):
        G_ap, t_G = G
        rows = slice(tt_ * 128, (tt_ + 1) * 128)
        xt, t_x = p.x.next()
        self.dma("sp", xt[:], xsrc[rows, :], writes=[t_x])
        r, t_r = self.rstd_of(c, y_ap, D, y_tiles)
        tmp, t_tmp = p.tmp.next()
        self.stt(tmp[:], y_ap, r[:], G_ap[:], ALU.mult, ALU.mult, list(y_tiles) + [t_r, t_G], [t_tmp])
        xn, t_xn = p.xn.next()
        self.tt("pool", xn[:], xt[:], tmp[:], ALU.add, [t_x, t_tmp], [t_xn])
        self.dma("sp", self.xres[rows, :], xn[:], reads=[t_xn])
        if not do_h2:
            return
        A_ap, t_A = A2
        B_ap, t_B = B2
        r2, t_r2 = self.rstd_of(c, xn[:], D, [t_xn])
        t2, t_t2 = p.tmp.next()
        self.stt(t2[:], xn[:], r2[:], A_ap[:], ALU.mult, ALU.mult, [t_xn, t_r2, t_A], [t_t2])
        hb, t_hb = p.hb.next()
        if moe:
            hf, t_hf = p.hf.next()
            self.tt("pool", hf[:], t2[:], B_ap[:], ALU.add, [t_t2, t_B], [t_hf])
            self.dma("sp", self.h2f_d[rows, :], hf[:], reads=[t_hf])
            if self.cur_sparse:
                return
            self.copy("pool", hb[:], hf[:], [t_hf], [t_hb])
        else:
            self.tt("pool", hb[:], t2[:], B_ap[:], ALU.add, [t_t2, t_B], [t_hb])
        hT, t_hT = self.transpose8(c, p, pT, hb, t_hb)
        self.dma("sp", self.h2T_d[:, :, rows], hT[:], reads=[t_hT])

    def phase_gqa_proj(self, layer, j, xsrc, lay):
        KT, V_aug = lay.KT, lay.V
        with self.scope() as sc:
            c = self.common(sc)
            p = self.mk_pre(sc, c)
            mods = self.load_mod(sc, layer, [(0, "shift", None), (1, "scale", 0)])
            wq = sc.sb("wqkv", [128, 8, 1536], BF16)
            t_w = self.S.tile()
            for nb in range(3):
                self.dma("pool", wq[:, :, nb * 512:(nb + 1) * 512],
                         self.a_w_qkv[j, :, nb * 512:(nb + 1) * 512].rearrange("(c p) n -> p c n", p=128), writes=[t_w])
            gain = sc.sb("gain", [128, 20, 64], F32)
            t_g = self.S.tile()
            for hh in range(20):
                src = self.a_g_q if hh < 16 else self.a_g_k
                self.dma("sp", gain[:, hh, :], src[j:j + 1, :].partition_broadcast(128), writes=[t_g])
            cosT = sc.sb("cosT", [128, NT, 64], F32)
            sinT = sc.sb("sinT", [128, NT, 64], F32)
            t_tab = self.S.tile()
            self.dma("sp", cosT[:], self.ropeA_cos.rearrange("(t p) d -> p t d", p=128), writes=[t_tab])
            self.dma("sp", sinT[:], self.ropeA_sin.rearrange("(t p) d -> p t d", p=128), writes=[t_tab])
            self.memset("pool", V_aug[:, :, :, 64:65], 1.0, [lay.t_V])
            pT = sc.rot("pT", [128, 8, 128], BF16, 2, psum=True)
            qps = sc.rot("qps", [128, 1536], F32, 2, psum=True)
            sq = sc.rot("sq", [128, 1280], F32, 1)
            ssq = sc.rot("ssq", [128, 20], F32, 2)
            qn = sc.rot("qn", [128, 20, 64], F32, 2)
            qa = sc.rot("qa", [128, 20, 64], F32, 1)
            qb = sc.rot("qb", [128, 20, 64], F32, 1)
            qr = sc.rot("qr", [128, 20, 64], BF16, 2)
            kd = sc.rot("kd", [128, 4, 2, 64], BF16, 2)
            qst = sc.rot("qst", [128, 8, 128], BF16, 2)
            for tt_ in range(NT):
                r_ = 1 if tt_ < NCTX_T else 0
                hT, t_hT = self.prenorm(c, p, pT, xsrc, tt_, mods[(1, r_)], mods[(0, r_)])
                ps, t_ps = qps.next()
                for nb in range(3):
                    for k in range(8):
                        self.mm(ps[:, nb * 512:(nb + 1) * 512], hT[:, k, :], wq[:, k, nb * 512:(nb + 1) * 512],
                                k == 0, k == 7, [t_hT, t_w], [t_ps])
                s_, t_s = sq.next()
                self.act(s_[:], ps[:, 0:1280], AF.Square, [t_ps], [t_s])
                ss_, t_ss = ssq.next()
                self.S.op("dve", lambda e, o=ss_, i=s_: e.tensor_reduce(
                    out=o[:], in_=i[:].rearrange("p (h d) -> p h d", d=64), axis=AX.X, op=ALU.add), [t_s], [t_ss])
                self.act(ss_[:], ss_[:], AF.Sqrt, [t_ss], [t_ss], scale=1.0 / 64, bias=EPS)
                self.recip(ss_[:], ss_[:], [t_ss], [t_ss])
                q_, t_q = qn.next()
                self.tt("dve", q_[:], ps[:, 0:1280].rearrange("p (h d) -> p h d", d=64),
                        ss_[:].unsqueeze(2).to_broadcast([128, 20, 64]), ALU.mult, [t_ps, t_ss], [t_q])
                self.tt("pool", q_[:], q_[:], gain[:], ALU.mult, [t_q, t_g], [t_q])
                a_, t_a = qa.next()
                self.tt("dve", a_[:], q_[:], cosT[:, tt_, :].unsqueeze(1).to_broadcast([128, 20, 64]), ALU.mult,
                        [t_q, t_tab], [t_a])
                b_, t_b = qb.next()
                q5 = q_[:].rearrange("p h (b s f) -> p h b s f", b=2, s=2)
                b5 = b_[:].rearrange("p h (b s f) -> p h b s f", b=2, s=2)
                s5 = sinT[:, tt_, :].rearrange("p (b s f) -> p b s f", b=2, s=2)
                for s in range(2):
                    self.tt("pool", b5[:, :, :, s, :], q5[:, :, :, 1 - s, :],
                            s5[:, :, s, :].unsqueeze(1).to_broadcast([128, 20, 2, 16]), ALU.mult, [t_q, t_tab], [t_b])
                o_, t_o = qr.next()
                self.tt("dve", o_[:], a_[:], b_[:], ALU.add, [t_a, t_b], [t_o])
                self.copy("act", V_aug[:, tt_, :, 0:64], ps[:, 1280:1536].rearrange("p (h d) -> p h d", d=64),
                          [t_ps], [lay.t_V])
                kd_, t_kd = kd.next()
                for dd in range(2):
                    self.copy("pool", kd_[:, :, dd, :], o_[:, 16:20, :], [t_o], [t_kd])
                pk, t_pk = pT.next()
                for kv in range(4):
                    self.tr(pk[:, kv, :], kd_[:, kv, :, :].rearrange("p a d -> p (a d)"), c.identb[:], [t_kd, c.t_id],
                            [t_pk])
                self.copy("act", KT[:, :, tt_ * 128:(tt_ + 1) * 128], pk[:, 0:4, :], [t_pk], [lay.t_KT])
                pq, t_pq = pT.next()
                of = o_[:].rearrange("p h d -> p (h d)")
                for k in range(8):
                    self.tr(pq[:, k, :], of[:, k * 128:(k + 1) * 128], c.identb[:], [t_o, c.t_id], [t_pq])
                st, t_st = qst.next()
                self.copy("dve", st[:], pq[:], [t_pq], [t_st])
                self.dma("sp", self.QT_d[:, :, tt_ * 128:(tt_ + 1) * 128].rearrange("c p n -> p c n"), st[:],
                         reads=[t_st])

    def phase_attn(self, kind, layer, j, xsrc, lay, last, moe):
        scale = 0.125 if kind == "gqa" else (96.0 ** -0.5)
        w_o_d = self.a_w_o[j] if kind == "gqa" else self.m_w_o[0]
        with self.scope() as sc:
            c = self.common(sc, njunk=1024)
            ep = self.mk_epi(sc, c, moe)
            wl = [(2, "gate", 1), (3, "shift", None), (4, "scale", 2)]
            mods = self.load_mod(sc, layer, wl, ctx_rows=not last)
            wo = sc.sb("wo", [128, 8, D], BF16)
            t_wo = self.S.tile()
            self.dma("pool", wo[:], w_o_d.rearrange("(c p) n -> p c n", p=128), writes=[t_wo])
            Sps = sc.ps("Sps", [128, 2, 512], F32)
            t_S = [self.S.tile(), self.S.tile()]
            Ops = [sc.ps(f"Ops{i}", [128, 512], F32) for i in range(4)]
            t_O = [self.S.tile() for _ in range(4)]
            pT = sc.rot("pT", [128, 8, 128], BF16, 2, psum=True)
            PT = sc.rot("PT", [128, 512], BF16, 4)
            qp = sc.rot("qp", [128, 512], BF16, 3)
            attn = sc.rot("attn", [128, 4, D], BF16, 2)
            aT = sc.rot("aT", [128, 8, 128], BF16, 2)
            rec = sc.rot("rec", [128, 1], F32, 8)
            if kind == "mla":
                qrp = sc.rot("qrp", [64, 512], BF16, 3)
                knp = sc.rot("knp", [128, T], BF16, 2)
                vpp = sc.rot("vpp", [128, NT, 130], BF16, 2)
            groups = []
            if not last:
                groups.append((0, 2, [0, 1]))
            for g in range(8):
                groups.append((256 + g * 512, 4, list(range(NT))))
            sidx = 0
            for (tok0, nsub, kts) in groups:
                N = nsub * 128
                nk = len(kts)
                at, t_at = attn.next()
                its = []
                for pj in range(8):
                    for r in range(2):
                        for ki, kt in enumerate(kts):
                            its.append(dict(pj=pj, r=r, ki=ki, kt=kt))
                cur = {}

                def emit_S(it):
                    nonlocal sidx
                    pj, r, ki, kt = it["pj"], it["r"], it["ki"], it["kt"]
                    if r == 0 and ki == 0:
                        q_, t_q = qp.next()
                        self.dma("sp", q_[:, 0:N], self.QT_d[pj, :, tok0:tok0 + N], writes=[t_q])
                        cur["q"] = (q_, t_q)
                        if kind == "mla":
                            qr_, t_qr = qrp.next()
                            self.dma("sp", qr_[:, 0:N], self.QrT_d[pj, :, tok0:tok0 + N], writes=[t_qr])
                            kn_, t_kn = knp.next()
                            self.dma("sp", kn_[:, 0:nk * 128], self.KnT_d[pj, :, 0:nk * 128], writes=[t_kn])
                            vp_, t_vp = vpp.next()
                            self.dma("sp", vp_[:, 0:nk, :],
                                     self.V_d[0:nk, :, pj * 130:(pj + 1) * 130].rearrange("t p c -> p t c"),
                                     writes=[t_vp])
                            cur["qr"] = (qr_, t_qr)
                            cur["kn"] = (kn_, t_kn)
                            cur["vp"] = (vp_, t_vp)
                    q_, t_q = cur["q"]
                    h = 2 * pj + r
                    kvh = h // 4
                    sb_ = sidx % 2
                    sidx += 1
                    s_ap = Sps[:, sb_, 0:N]
                    ksl = slice(kt * 128, (kt + 1) * 128)
                    if kind == "gqa":
                        self.mm(s_ap, lay.KT[64 * r:64 * r + 64, kvh, ksl], q_[64 * r:64 * r + 64, 0:N],
                                True, True, [t_q], [t_S[sb_]])
                    else:
                        qr_, t_qr = cur["qr"]
                        kn_, t_kn = cur["kn"]
                        it["vp"] = cur["vp"]
                        self.mm(s_ap, kn_[64 * r:64 * r + 64, ksl], q_[64 * r:64 * r + 64, 0:N],
                                True, False, [t_q, t_kn], [t_S[sb_]])
                        self.mm(s_ap, lay.KrT[32 * r:32 * r + 32, ksl], qr_[32 * r:32 * r + 32, 0:N],
                                False, True, [t_qr], [t_S[sb_]])
                    pt, t_pt = PT.next()
                    self.act(pt[:, 0:N], s_ap, AF.Exp, [t_S[sb_]], [t_pt], scale=scale)
                    it["pt"] = (pt, t_pt)

                def emit_PV(it):
                    pj, r, ki, kt = it["pj"], it["r"], it["ki"], it["kt"]
                    h = 2 * pj + r
                    kvh = h // 4
                    pt, t_pt = it["pt"]
                    for sub in range(nsub):
                        if kind == "gqa":
                            v_ap = lay.V[:, kt, kvh, :]
                            rd = [t_pt]
                        else:
                            vp_, t_vp = it["vp"]
                            v_ap = vp_[:, kt, r * 65:(r + 1) * 65]
                            rd = [t_pt, t_vp]
                        self.mm(Ops[sub][:, 0:65], pt[:, sub * 128:(sub + 1) * 128], v_ap,
                                ki == 0, ki == nk - 1, rd, [t_O[sub]])
                    if ki == nk - 1:
                        for sub in range(nsub):
                            rc, t_rc = rec.next()
                            self.recip(rc[:], Ops[sub][:, 64:65], [t_O[sub]], [t_rc])
                            self.ts("dve", at[:, sub, h * 64:(h + 1) * 64], Ops[sub][:, 0:64], rc[:], ALU.mult,
                                    [t_O[sub], t_rc], [t_at])

                for i in range(len(its) + 1):
                    if i < len(its):
                        emit_S(its[i])
                    if i >= 1:
                        emit_PV(its[i - 1])
                for sub in range(nsub):
                    tt_ = tok0 // 128 + sub
                    r_ = 1 if tt_ < NCTX_T else 0
                    ps, t_ps = pT.next()
                    for k in range(8):
                        self.tr(ps[:, k, :], at[:, sub, k * 128:(k + 1) * 128], c.identb[:], [t_at, c.t_id], [t_ps])
                    a_, t_a = aT.next()
                    self.copy("act", a_[:], ps[:], [t_ps], [t_a])
                    y_ap = Sps[:].rearrange("p a n -> p (a n)")
                    for nb in range(2):
                        for k in range(8):
                            self.mm(Sps[:, nb, :], a_[:, k, :], wo[:, k, nb * 512:(nb + 1) * 512], k == 0, k == 7,
                                    [t_a, t_wo], [t_S[nb]])
                    self.epilogue(c, ep, pT, xsrc, tt_, y_ap, t_S, mods[(2, r_)], mods[(4, r_)], mods[(3, r_)],
                                  True, moe)

    def phase_fourier_1(self, layer, xsrc):
        with self.scope() as sc:
            c = self.common(sc)
            p = self.mk_pre(sc, c)
            mods = self.load_mod(sc, layer, [(0, "shift", None), (1, "scale", 0)])
            cs = sc.sb("cs", [128, 2, 512], BF16)
            t_cs = self.S.tile()
            self.dma("pool", cs[:], self.dft256.rearrange("(c p) n -> p c n", p=128), writes=[t_cs])
            pT = sc.rot("pT", [128, 8, 128], BF16, 2, psum=True)
            xps = sc.rot("xps", [128, 4, 512], F32, 1, psum=True)
            xsb = sc.rot("xsb", [128, 4, 512], BF16, 2)
            for tt_ in range(NT):
                r_ = 1 if tt_ < NCTX_T else 0
                hT, t_hT = self.prenorm(c, p, pT, xsrc, tt_, mods[(1, r_)], mods[(0, r_)])
                ps, t_ps = xps.next()
                for g in range(4):
                    for k in range(2):
                        self.mm(ps[:, g, :], hT[:, 2 * g + k, :], cs[:, k, :], k == 0, k == 1, [t_hT, t_cs], [t_ps])
                xs, t_xs = xsb.next()
                self.copy("act", xs[:], ps[:], [t_ps], [t_xs])
                self.dma("sp", self.XCS_d[:, tt_ * 128:(tt_ + 1) * 128, :].rearrange("g p n -> p g n"), xs[:],
                         reads=[t_xs])

    def phase_fourier_2(self, layer, xsrc, last, moe):
        with self.scope() as sc:
            c = self.common(sc, njunk=1024)
            ep = self.mk_epi(sc, c, moe)
            mods = self.load_mod(sc, layer, [(2, "gate", 1), (3, "shift", None), (4, "scale", 2)], ctx_rows=not last)
            fw = sc.sb("fw", [128, 8, D], BF16)
            t_fw = self.S.tile()
            self.dma("pool", fw[:], self.f_w[0].rearrange("(c p) n -> p c n", p=128), writes=[t_fw])
            Tc = sc.sb("Tc", [128, 32, 512], BF16)
            Ts = sc.sb("Ts", [128, 32, 512], BF16)
            t_T = self.S.tile()
            X = sc.rot("X", [128, 32, 512], BF16, 1)
            fT = sc.rot("fT", [128, 8, 512], BF16, 1)
            pT = sc.rot("pT", [128, 8, 128], BF16, 2, psum=True)
            fps = sc.rot("fps", [128, 512], F32, 2, psum=True)
            yps = sc.ps("yps", [128, 2, 512], F32)
            t_y = [self.S.tile(), self.S.tile()]
            groups = []
            if not last:
                groups.append(("ctx", 0, 2, 2))
            for g in range(8):
                groups.append(("lat", 256 + g * 512, 4, 32))
            for (kind, tok0, nsub, na) in groups:
                N = nsub * 128
                if kind == "ctx":
                    self.dma("sp", Tc[:, 0:2, 0:256], self.cos256.rearrange("(a p) n -> p a n", p=128), writes=[t_T])
                    self.dma("sp", Ts[:, 0:2, 0:256], self.nsin256.rearrange("(a p) n -> p a n", p=128), writes=[t_T])
                    fscale = 1.0 / 256
                else:
                    k0 = tok0 - 256
                    self.dma("sp", Tc[:], self.cos4096[:, k0:k0 + 512].rearrange("(a p) n -> p a n", p=128),
                             writes=[t_T])
                    self.dma("sp", Ts[:], self.nsin4096[:, k0:k0 + 512].rearrange("(a p) n -> p a n", p=128),
                             writes=[t_T])
                    fscale = 1.0 / 1024
                f_, t_f = fT.next()
                for g in range(4):
                    x_, t_x = X.next()
                    if kind == "ctx":
                        self.dma("sp", x_[:, 0:2, :], self.XCS_d[g, 0:256, :].rearrange("(a p) n -> p a n", p=128),
                                 writes=[t_x])
                    else:
                        self.dma("sp", x_[:], self.XCS_d[g, 256:T, :].rearrange("(a p) n -> p a n", p=128),
                                 writes=[t_x])
                    for lc in range(2):
                        ps, t_ps = fps.next()
                        n_mm = 2 * na
                        i_mm = 0
                        for (tab, off) in ((Tc, 0), (Ts, 256)):
                            for a in range(na):
                                self.mm(ps[:, 0:N], x_[:, a, off + lc * 128:off + (lc + 1) * 128], tab[:, a, 0:N],
                                        i_mm == 0, i_mm == n_mm - 1, [t_x, t_T], [t_ps])
                                i_mm += 1
                        self.act(f_[:, 2 * g + lc, 0:N], ps[:, 0:N], AF.Copy, [t_ps], [t_f], scale=fscale)
                for sub in range(nsub):
                    tt_ = tok0 // 128 + sub
                    r_ = 1 if tt_ < NCTX_T else 0
                    for nb in range(2):
                        for k in range(8):
                            self.mm(yps[:, nb, :], f_[:, k, sub * 128:(sub + 1) * 128], fw[:, k, nb * 512:(nb + 1) * 512],
                                    k == 0, k == 7, [t_f, t_fw], [t_y[nb]])
                    self.epilogue(c, ep, pT, xsrc, tt_, yps[:].rearrange("p a n -> p (a n)"), t_y, mods[(2, r_)],
                                  mods[(4, r_)], mods[(3, r_)], True, moe)

    def phase_mla_proj(self, layer, xsrc, lay):
        with self.scope() as sc:
            c = self.common(sc)
            p = self.mk_pre(sc, c)
            mods = self.load_mod(sc, layer, [(0, "shift", None), (1, "scale", 0)])
            t_w = self.S.tile()
            wdn = sc.sb("wdn", [128, 8, 672], BF16)
            self.dma("pool", wdn[:], self.m_w_down[0].rearrange("(c p) n -> p c n", p=128), writes=[t_w])
            wqn = sc.sb("wqn", [128, 3, 1024], BF16)
            self.dma("pool", wqn[:], self.m_wuqn.rearrange("(c p) n -> p c n", p=128), writes=[t_w])
            wqr = sc.sb("wqr", [128, 3, 512], BF16)
            self.dma("pool", wqr[:], self.m_wuqr.rearrange("(c p) n -> p c n", p=128), writes=[t_w])
            wqs = sc.sb("wqs", [128, 3, 512], BF16)
            self.dma("pool", wqs[:], self.m_wuqrs.rearrange("(c p) n -> p c n", p=128), writes=[t_w])
            wuk = sc.sb("wuk", [128, 2, 1024], BF16)
            self.dma("pool", wuk[:], self.m_wuk.rearrange("(c p) n -> p c n", p=128), writes=[t_w])
            wuv = sc.sb("wuv", [128, 2, 1024], BF16)
            self.dma("pool", wuv[:], self.m_wuv.rearrange("(c p) n -> p c n", p=128), writes=[t_w])
            gcq = sc.sb("gcq", [128, 384], F32)
            gckv = sc.sb("gckv", [128, 256], F32)
            t_g = self.S.tile()
            self.dma("sp", gcq[:], self.m_g_cq[0:1, :].partition_broadcast(128), writes=[t_g])
            self.dma("sp", gckv[:], self.m_g_ckv[0:1, :].partition_broadcast(128), writes=[t_g])
            cosT = sc.sb("cosT", [128, NT, 32], F32)
            sinT = sc.sb("sinT", [128, NT, 32], F32)
            t_tab = self.S.tile()
            self.dma("sp", cosT[:], self.ropeM_cos.rearrange("(t p) d -> p t d", p=128), writes=[t_tab])
            self.dma("sp", sinT[:], self.ropeM_sin.rearrange("(t p) d -> p t d", p=128), writes=[t_tab])
            pT = sc.rot("pT", [128, 8, 128], BF16, 2, psum=True)
            dps = sc.rot("dps", [128, 1024], F32, 1, psum=True)
            pps = sc.rot("pps", [128, 512], F32, 2, psum=True)
            vps = sc.rot("vps", [128, 1024], F32, 1, psum=True)
            cqn = sc.rot("cqn", [128, 384], BF16, 2)
            ckvn = sc.rot("ckvn", [128, 256], BF16, 2)
            cqT = sc.rot("cqT", [128, 3, 512], BF16, 2)
            ckT = sc.rot("ckT", [128, 2, 512], BF16, 2)
            ka = sc.rot("ka", [128, 32], F32, 2)
            kb = sc.rot("kb", [128, 32], F32, 2)
            kd = sc.rot("kd", [128, 2, 32], BF16, 2)
            st = sc.rot("st", [128, 512], BF16, 3)
            ctab = sc.rot("ctab", [64, 512], F32, 2)
            stab = sc.rot("stab", [64, 512], F32, 2)
            ra = sc.rot("ra", [64, 512], F32, 2)
            rb = sc.rot("rb", [64, 512], F32, 2)
            rst = sc.rot("rst", [64, 512], BF16, 2)
            vst = sc.rot("vst", [128, 16, 65], BF16, 2)
            for i in range(2):
                self.memset("pool", vst.bufs[i][:, :, 64:65], 1.0, [vst.tiles[i]])
            groups = [(0, 2)] + [(2 + 4 * g, 4) for g in range(8)]
            for (t0, nsub) in groups:
                N = nsub * 128
                tok0 = t0 * 128
                cq_T, t_cqT = cqT.next()
                ck_T, t_ckT = ckT.next()
                for sub in range(nsub):
                    tt_ = t0 + sub
                    r_ = 1 if tt_ < NCTX_T else 0
                    hT, t_hT = self.prenorm(c, p, pT, xsrc, tt_, mods[(1, r_)], mods[(0, r_)])
                    d_, t_d = dps.next()
                    for (o0, o1) in ((0, 512), (512, 672)):
                        for k in range(8):
                            self.mm(d_[:, o0:o1], hT[:, k, :], wdn[:, k, o0:o1], k == 0, k == 7, [t_hT, t_w], [t_d])
                    r1, t_r1 = self.rstd_of(c, d_[:, 0:384], 384, [t_d])
                    cq_, t_cq = cqn.next()
                    self.stt(cq_[:], d_[:, 0:384], r1[:], gcq[:], ALU.mult, ALU.mult, [t_d, t_r1, t_g], [t_cq])
                    r2, t_r2 = self.rstd_of(c, d_[:, 384:640], 256, [t_d])
                    ck_, t_ck = ckvn.next()
                    self.stt(ck_[:], d_[:, 384:640], r2[:], gckv[:], ALU.mult, ALU.mult, [t_d, t_r2, t_g], [t_ck])
                    a_, t_a = ka.next()
                    self.tt("dve", a_[:], d_[:, 640:672], cosT[:, tt_, :], ALU.mult, [t_d, t_tab], [t_a])
                    b_, t_b = kb.next()
                    d4 = d_[:, 640:672].rearrange("p (b s f) -> p b s f", b=2, s=2)
                    b4 = b_[:].rearrange("p (b s f) -> p b s f", b=2, s=2)
                    s4 = sinT[:, tt_, :].rearrange("p (b s f) -> p b s f", b=2, s=2)
                    for s in range(2):
                        self.tt("dve", b4[:, :, s, :], d4[:, :, 1 - s, :], s4[:, :, s, :], ALU.mult, [t_d, t_tab], [t_b])
                    kd_, t_kd = kd.next()
                    for dd in range(2):
                        self.tt("pool", kd_[:, dd, :], a_[:], b_[:], ALU.add, [t_a, t_b], [t_kd])
                    ps, t_ps = pT.next()
                    for k in range(3):
                        self.tr(ps[:, k, :], cq_[:, k * 128:(k + 1) * 128], c.identb[:], [t_cq, c.t_id], [t_ps])
                    for k in range(2):
                        self.tr(ps[:, 3 + k, :], ck_[:, k * 128:(k + 1) * 128], c.identb[:], [t_ck, c.t_id], [t_ps])
                    if True:
                        self.tr(ps[0:64, 5, :], kd_[:].rearrange("p a d -> p (a d)"), c.identb[:], [t_kd, c.t_id], [t_ps])
                    self.copy("act", cq_T[:, :, sub * 128:(sub + 1) * 128], ps[:, 0:3, :], [t_ps], [t_cqT])
                    self.copy("act", ck_T[:, :, sub * 128:(sub + 1) * 128], ps[:, 3:5, :], [t_ps], [t_ckT])
                    self.copy("dve", lay.KrT[:, tt_ * 128:(tt_ + 1) * 128], ps[0:64, 5, :], [t_ps], [lay.t_KrT])
                for sub in range(nsub):
                    tt_ = t0 + sub
                    v_, t_v = vps.next()
                    for nb in range(2):
                        for k in range(2):
                            self.mm(v_[:, nb * 512:(nb + 1) * 512], ck_T[:, k, sub * 128:(sub + 1) * 128],
                                    wuv[:, k, nb * 512:(nb + 1) * 512], k == 0, k == 1, [t_ckT, t_w], [t_v])
                    vs, t_vs = vst.next()
                    self.copy("act", vs[:, :, 0:64], v_[:].rearrange("p (h d) -> p h d", d=64), [t_v], [t_vs])
                    self.dma("sp", self.V_d[tt_, :, :], vs[:].rearrange("p h d -> p (h d)"), reads=[t_vs])
                ct_, t_ct = ctab.next()
                st_, t_stb = stab.next()
                self.dma("sp", ct_[:, 0:N], self.ropeMT_cos[:, tok0:tok0 + N], writes=[t_ct])
                self.dma("sp", st_[:, 0:N], self.ropeMT_sin[:, tok0:tok0 + N], writes=[t_stb])
                for pj in range(8):
                    ps, t_ps = pps.next()
                    for k in range(3):
                        self.mm(ps[:, 0:N], wqn[:, k, pj * 128:(pj + 1) * 128], cq_T[:, k, 0:N], k == 0, k == 2,
                                [t_cqT, t_w], [t_ps])
                    s_, t_s = st.next()
                    self.copy("act", s_[:, 0:N], ps[:, 0:N], [t_ps], [t_s])
                    self.dma("sp", self.QT_d[pj, :, tok0:tok0 + N], s_[:, 0:N], reads=[t_s])
                    ps, t_ps = pps.next()
                    for k in range(2):
                        self.mm(ps[:, 0:N], wuk[:, k, pj * 128:(pj + 1) * 128], ck_T[:, k, 0:N], k == 0, k == 1,
                                [t_ckT, t_w], [t_ps])
                    s_, t_s = st.next()
                    self.copy("act", s_[:, 0:N], ps[:, 0:N], [t_ps], [t_s])
                    self.dma("sp", self.KnT_d[pj, :, tok0:tok0 + N], s_[:, 0:N], reads=[t_s])
                    ps, t_ps = pps.next()
                    for k in range(3):
                        self.mm(ps[0:64, 0:N], wqr[:, k, pj * 64:(pj + 1) * 64], cq_T[:, k, 0:N], k == 0, k == 2,
                                [t_cqT, t_w], [t_ps])
                    a_, t_a = ra.next()
                    self.tt("dve", a_[:, 0:N], ps[0:64, 0:N], ct_[:, 0:N], ALU.mult, [t_ps, t_ct], [t_a])
                    ps, t_ps = pps.next()
                    for k in range(3):
                        self.mm(ps[0:64, 0:N], wqs[:, k, pj * 64:(pj + 1) * 64], cq_T[:, k, 0:N], k == 0, k == 2,
                                [t_cqT, t_w], [t_ps])
                    b_, t_b = rb.next()
                    self.tt("dve", b_[:, 0:N], ps[0:64, 0:N], st_[:, 0:N], ALU.mult, [t_ps, t_stb], [t_b])
                    o_, t_o = rst.next()
                    self.tt("pool", o_[:, 0:N], a_[:, 0:N], b_[:, 0:N], ALU.add, [t_a, t_b], [t_o])
                    self.dma("sp", self.QrT_d[pj, :, tok0:tok0 + N], o_[:, 0:N], reads=[t_o])

    def phase_ffn(self, layer, moe, last, xsrc_unused):
        idx = layer // 2
        tiles = list(range(NCTX_T, NT)) if last else list(range(NT))
        nh = len(tiles) // 2
        halves = [tiles[:nh], tiles[nh:]]
        E = NE if moe else 1
        F = FF_E if moe else FF_D
        nfb = F // 256
        for hi, half in enumerate(halves):
            nt = len(half)
            tok0 = half[0] * 128
            NTOK = nt * 128
            with contextlib.ExitStack() as hes:
                yacc = hes.enter_context(self.nc.sbuf_tensor(f"yacc{layer}_{hi}", [128, nt, D], F32))
                with self.scope() as sc:
                    c = self.common(sc, njunk=1024)
                    h2T = sc.sb("h2T", [128, 8, NTOK], BF16)
                    t_h = self.S.tile()
                    self.dma("sp", h2T[:], self.h2T_d[:, :, tok0:tok0 + NTOK], writes=[t_h])
                    t_ya = [self.S.tile() for _ in range(nt)]
                    gps = sc.rot("gps", [128, 512], F32, 2, psum=True)
                    ups = sc.rot("ups", [128, 512], F32, 2, psum=True)
                    yps = sc.rot("yps", [128, 2, 512], F32, 2, psum=True)
                    wg = sc.rot("wg", [128, 8, 256], BF16, 3)
                    wu = sc.rot("wu", [128, 8, 256], BF16, 3)
                    wd = sc.rot("wd", [128, 2, D], BF16, 3)
                    sg = sc.rot("sg", [128, 512], F32, 2)
                    aT = sc.rot("aT", [128, 2, 512], BF16, 2)
                    if moe:
                        t1 = sc.rot("t1", [128, 512], F32, 2)
                        gbs = sc.rot("gbs", [128, NTOK], F32, 2)
                        gatesT = sc.sb("gatesT", [NE, NTOK], F32)
                        t_gT = self.S.tile()
                        sel = sc.sb("sel", [NE, NE * 128], F32)
                        t_sel = self.S.tile()
                        self.dma("sp", sel[:], self.sel_d[:, :], writes=[t_sel])
                        self.router(sc, c, idx, half, tok0, gatesT, t_gT, yps, ups)
                    groups = []
                    g0 = 0
                    while g0 < NTOK:
                        n = min(512, NTOK - g0)
                        groups.append((g0, n))
                        g0 += n
                    its = []
                    for e in range(E):
                        for fb in range(nfb):
                            for gi, (g0, n) in enumerate(groups):
                                its.append(dict(e=e, fb=fb, gi=gi, g0=g0, n=n))
                    cur = {}

                    def stage_a(it):
                        e, fb, gi, g0, n = it["e"], it["fb"], it["gi"], it["g0"], it["n"]
                        if moe:
                            wgu_d = self.e_w_gu[idx, e]
                            wdn_d = self.e_w_down[idx, e]
                        else:
                            wgu_d = self.d_w_gu[idx]
                            wdn_d = self.d_w_down[idx]
                        if gi == 0:
                            if moe and fb == 0:
                                gb_, t_gb = gbs.next()
                                for (gg0, gn) in groups:
                                    m_, t_m = gps.next()
                                    self.mm(m_[:, 0:gn], sel[:, e * 128:(e + 1) * 128], gatesT[:, gg0:gg0 + gn],
                                            True, True, [t_sel, t_gT], [t_m])
                                    self.copy("act", gb_[:, gg0:gg0 + gn], m_[:, 0:gn], [t_m], [t_gb])
                                cur["gb"] = (gb_, t_gb)
                            wg_, t_wg = wg.next()
                            wu_, t_wu = wu.next()
                            wd_, t_wd = wd.next()
                            f0 = fb * 256
                            self.dma("pool", wg_[:], wgu_d[:, f0:f0 + 256].rearrange("(c p) n -> p c n", p=128),
                                     writes=[t_wg])
                            self.dma("pool", wu_[:], wgu_d[:, F + f0:F + f0 + 256].rearrange("(c p) n -> p c n", p=128),
                                     writes=[t_wu])
                            self.dma("pool", wd_[:], wdn_d[f0:f0 + 256, :].rearrange("(c p) n -> p c n", p=128),
                                     writes=[t_wd])
                            cur["w"] = (wg_, t_wg, wu_, t_wu, wd_, t_wd)
                        wg_, t_wg, wu_, t_wu, wd_, t_wd = cur["w"]
                        it["wd"] = (wd_, t_wd)
                        a_, t_a = aT.next()
                        it["a"] = (a_, t_a)
                        for fc in range(2):
                            g_, t_g = gps.next()
                            u_, t_u = ups.next()
                            for k in range(8):
                                self.mm(g_[:, 0:n], wg_[:, k, fc * 128:(fc + 1) * 128], h2T[:, k, g0:g0 + n],
                                        k == 0, k == 7, [t_wg, t_h], [t_g])
                            for k in range(8):
                                self.mm(u_[:, 0:n], wu_[:, k, fc * 128:(fc + 1) * 128], h2T[:, k, g0:g0 + n],
                                        k == 0, k == 7, [t_wu, t_h], [t_u])
                            s_, t_s = sg.next()
                            self.act(s_[:, 0:n], g_[:, 0:n], AF.Silu, [t_g], [t_s])
                            if moe:
                                gb_, t_gb = cur["gb"]
                                t1_, t_t1 = t1.next()
                                self.tt("dve", t1_[:, 0:n], s_[:, 0:n], u_[:, 0:n], ALU.mult, [t_s, t_u], [t_t1])
                                self.tt("dve", a_[:, fc, 0:n], t1_[:, 0:n], gb_[:, g0:g0 + n], ALU.mult,
                                        [t_t1, t_gb], [t_a])
                            else:
                                self.tt("dve", a_[:, fc, 0:n], s_[:, 0:n], u_[:, 0:n], ALU.mult, [t_s, t_u], [t_a])

                    def stage_b(it):
                        e, fb, g0, n = it["e"], it["fb"], it["g0"], it["n"]
                        first = (e == 0 and fb == 0)
                        a_, t_a = it["a"]
                        wd_, t_wd = it["wd"]
                        for sub in range(n // 128):
                            ti = g0 // 128 + sub
                            y_, t_y = yps.next()
                            for nb in range(2):
                                for fc in range(2):
                                    self.mm(y_[:, nb, :], a_[:, fc, sub * 128:(sub + 1) * 128],
                                            wd_[:, fc, nb * 512:(nb + 1) * 512], fc == 0, fc == 1, [t_a, t_wd], [t_y])
                            yf = y_[:].rearrange("p a n -> p (a n)")
                            if first:
                                self.copy("act", yacc[:, ti, :], yf, [t_y], [t_ya[ti]])
                            else:
                                self.tt("dve", yacc[:, ti, :], yacc[:, ti, :], yf, ALU.add, [t_y, t_ya[ti]],
                                        [t_ya[ti]])

                    for i in range(len(its) + 1):
                        if i < len(its):
                            stage_a(its[i])
                        if i >= 1:
                            stage_b(its[i - 1])
                with self.scope() as sc:
                    c = self.common(sc, njunk=1024)
                    mods = self.load_mod(sc, layer, [(5, "gate", 3)], ctx_rows=not last)
                    xr = sc.rot("xr", [128, D], F32, 3)
                    tm = sc.rot("tm", [128, D], F32, 2)
                    xo = sc.rot("xo", [128, D], F32, 3)
                    for ti, tt_ in enumerate(half):
                        r_ = 1 if tt_ < NCTX_T else 0
                        G_ap, t_G = mods[(5, r_)]
                        rows = slice(tt_ * 128, (tt_ + 1) * 128)
                        x_, t_x = xr.next()
                        self.dma("sp", x_[:], self.xres[rows, :], writes=[t_x])
                        r, t_r = self.rstd_of(c, yacc[:, ti, :], D, [])
                        t_, t_t = tm.next()
                        self.stt(t_[:], yacc[:, ti, :], r[:], G_ap[:], ALU.mult, ALU.mult, [t_r, t_G], [t_t])
                        o_, t_o = xo.next()
                        self.tt("pool", o_[:], x_[:], t_[:], ALU.add, [t_x, t_t], [t_o])
                        if layer == self.nlayers - 1:
                            if tt_ >= NCTX_T:
                                self.dma("sp", self.out[(tt_ - NCTX_T) * 128:(tt_ - NCTX_T + 1) * 128, :], o_[:],
                                         reads=[t_o])
                            if self.debug:
                                self.dma("sp", self.xres[rows, :], o_[:], reads=[t_o])
                        else:
                            self.dma("sp", self.xres[rows, :], o_[:], reads=[t_o])

    def phase_moe(self, layer, last):
        idx = layer // 2
        tiles = list(range(NCTX_T, NT)) if last else list(range(NT))
        nh = len(tiles) // 2
        halves = [tiles[:nh], tiles[nh:]]
        F = FF_E
        nfb = F // 256
        for hi, half in enumerate(halves):
            nt = len(half)
            tok0 = half[0] * 128
            NTOK = nt * 128
            groups = []
            g0 = 0
            while g0 < NTOK:
                n = min(512, NTOK - g0)
                groups.append((g0, n))
                g0 += n
            with contextlib.ExitStack() as hes:
                hsb = lambda name, shape, dt: hes.enter_context(
                    self.nc.sbuf_tensor(f"{name}{layer}_{hi}", list(shape), dt))
                gates = hsb("gates", [128, nt, NE], F32)
                posm = hsb("posm", [128, nt, NE], F32)
                cnt_i = hsb("cnti", [1, NE], I32)
                with self.scope() as sc:
                    c = self.common(sc, njunk=1024)
                    wr = sc.sb("wr", [128, 8, NE], F32)
                    t_wr = self.S.tile()
                    self.dma("sp", wr[:], self.e_w_router[idx].rearrange("(c p) n -> p c n", p=128), writes=[t_wr])
                    trif = sc.sb("trif", [128, 128], F32)
                    trib = sc.sb("trib", [128, 128], BF16)
                    oneb = sc.sb("oneb", [128, 128], BF16)
                    t_tri = self.S.tile()
                    self.dma("sp", trif[:], self.tri_d[:, :], writes=[t_tri])
                    self.copy("dve", trib[:], trif[:], [t_tri], [t_tri])
                    self.memset("pool", oneb[:], 1.0, [t_tri])
                    maskb = sc.sb("maskb", [128, nt, NE], BF16)
                    maskf = sc.sb("maskf", [128, nt, NE], F32)
                    t_mask = self.S.tile()
                    t_gates = self.S.tile()
                    t_posm = self.S.tile()
                    hf = sc.rot("rhf", [128, D], F32, 2)
                    hfT = sc.rot("rhfT", [128, 8, 128], F32, 2)
                    lg = sc.rot("lg", [128, NE], F32, 2)
                    m8 = sc.rot("m8", [128, 8], F32, 2)
                    dd = sc.rot("dd", [128, NE], F32, 2)
                    ee = sc.rot("ee", [128, NE], F32, 2)
                    sm = sc.rot("sm", [128, 1], F32, 2)
                    tps = sc.rot("tps", [128, 2, 512], F32, 2, psum=True)
                    lps = sc.rot("lps", [128, 512], F32, 2, psum=True)
                    for ti, tt_ in enumerate(half):
                        rows = slice(tt_ * 128, (tt_ + 1) * 128)
                        h_, t_h = hf.next()
                        self.dma("sp", h_[:], self.h2f_d[rows, :], writes=[t_h])
                        y_, t_y = tps.next()
                        yT = y_[:].rearrange("p a (b n) -> p (a b) n", n=128)
                        for k in range(8):
                            self.tr(yT[:, k, :], h_[:, k * 128:(k + 1) * 128], c.identf[:], [t_h, c.t_id], [t_y])
                        hT_, t_hT = hfT.next()
                        self.copy("act", hT_[:], yT, [t_y], [t_hT])
                        lp, t_lp = lps.next()
                        for k in range(8):
                            self.mm(lp[:, 0:NE], hT_[:, k, :], wr[:, k, :], k == 0, k == 7, [t_hT, t_wr], [t_lp])
                        l_, t_l = lg.next()
                        self.copy("dve", l_[:], lp[:, 0:NE], [t_lp], [t_l])
                        m_, t_m = m8.next()
                        self.S.op("dve", lambda e, o=m_, i=l_: e.max(out=o[:], in_=i[:]), [t_l], [t_m])
                        d_, t_d = dd.next()
                        self.ts("dve", d_[:], l_[:], m_[:, 0:1], ALU.subtract, [t_l, t_m], [t_d])
                        e_, t_e = ee.next()
                        self.act(e_[:], d_[:], AF.Exp, [t_d], [t_e])
                        self.ts("dve", maskf[:, ti, :], l_[:], m_[:, 1:2], ALU.is_ge, [t_l, t_m], [t_mask])
                        self.copy("dve", maskb[:, ti, :], maskf[:, ti, :], [t_mask], [t_mask])
                        self.tt("dve", e_[:], e_[:], maskf[:, ti, :], ALU.mult, [t_e, t_mask], [t_e])
                        s_, t_s = sm.next()
                        self.S.op("dve", lambda e, o=s_, i=e_: e.tensor_reduce(out=o[:], in_=i[:], axis=AX.X, op=ALU.add),
                                  [t_e], [t_s])
                        self.recip(s_[:], s_[:], [t_s], [t_s])
                        self.ts("dve", gates[:, ti, :], e_[:], s_[:], ALU.mult, [t_e, t_s], [t_gates])
                    for ti in range(nt):
                        lp, t_lp = lps.next()
                        self.mm(lp[:, 0:NE], trib[:], maskb[:, ti, :], True, ti == 0, [t_tri, t_mask], [t_lp])
                        for tp in range(ti):
                            self.mm(lp[:, 0:NE], oneb[:], maskb[:, tp, :], False, tp == ti - 1, [t_tri, t_mask], [t_lp])
                        self.tt("dve", posm[:, ti, :], lp[:, 0:NE], maskf[:, ti, :], ALU.mult, [t_lp, t_mask], [t_posm])
                        self.ts("dve", posm[:, ti, :], posm[:, ti, :], -1.0, ALU.add, [t_posm], [t_posm])
                    lp, t_lp = lps.next()
                    for ti in range(nt):
                        self.mm(lp[0:1, 0:NE], oneb[:, 0:1], maskb[:, ti, :], ti == 0, ti == nt - 1, [t_tri, t_mask],
                                [t_lp])
                    cf = sc.sb("cntf", [1, NE], F32)
                    t_cf = self.S.tile()
                    self.copy("dve", cf[:], lp[0:1, 0:NE], [t_lp], [t_cf])
                    self.copy("dve", cnt_i[:], cf[:], [t_cf], [t_cf])
                with self.scope() as sc:
                    c = self.common(sc, njunk=1024)
                    yacc = sc.sb("yacc", [128, nt, D], F32)
                    t_ya = [self.S.tile() for _ in range(nt)]
                    h2tok = sc.sb("h2tok", [128, nt, D], BF16)
                    t_htok = self.S.tile()
                    self.dma("pool", h2tok[:], self.h2f_d[tok0:tok0 + NTOK, :].rearrange("(t p) d -> p t d", p=128),
                             writes=[t_htok])
                    iot = sc.sb("iot", [128, 512], F32)
                    t_io = self.S.tile()
                    self.dma("sp", iot[:], self.iota_d[:, :], writes=[t_io])
                    hTe = sc.sb("hTe", [128, 8, NTOK], BF16)
                    t_hTe = [self.S.tile() for _ in groups]
                    pb = sc.rot("pb", [128, nt, 512], BF16, 1)
                    gps = sc.rot("gps", [128, 512], F32, 2, psum=True)
                    ups = sc.rot("ups", [128, 512], F32, 2, psum=True)
                    yps = sc.rot("yps", [128, 2, 512], F32, 2, psum=True)
                    wg = sc.rot("wg", [128, 8, 256], BF16, 2)
                    wu = sc.rot("wu", [128, 8, 256], BF16, 2)
                    wd = sc.rot("wd", [128, 2, D], BF16, 2)
                    wdst = sc.rot("wdst", [128, 2, D], F32, 1)
                    sg = sc.rot("sg", [128, 512], F32, 2)
                    aT = sc.rot("aT", [128, 2, 512], BF16, 2)
                    for e in range(NE):
                        cap = cnt_i[0:1, e:e + 1]
                        wgu_d = self.e_w_gu[idx, e]
                        wdn_d = self.e_w_down[idx, e]
                        for gi, (g0, n) in enumerate(groups):
                            self.S.begin_cond(cap, g0, NTOK)
                            pb_, t_pb = pb.next()
                            for ti in range(nt):
                                eng = "dve" if ti % 2 == 0 else "pool"
                                self.ts(eng, pb_[:, ti, 0:n], iot[:, 0:n], float(g0), ALU.add, [t_io], [t_pb],
                                        s2=posm[:, ti, e:e + 1], op1=ALU.is_equal)
                            for k in range(8):
                                ps, t_ps = (gps if k % 2 == 0 else ups).next()
                                for ti in range(nt):
                                    self.mm(ps[:, 0:n], h2tok[:, ti, k * 128:(k + 1) * 128], pb_[:, ti, 0:n],
                                            ti == 0, ti == nt - 1, [t_htok, t_pb], [t_ps])
                                self.copy("act" if k % 2 == 0 else "dve", hTe[:, k, g0:g0 + n], ps[:, 0:n], [t_ps],
                                          [t_hTe[gi]])
                            self.S.end_cond()
                        its = []
                        for fb in range(nfb):
                            for gi, (g0, n) in enumerate(groups):
                                its.append(dict(fb=fb, gi=gi, g0=g0, n=n))
                        cur = {}

                        def stage_a(it):
                            fb, gi, g0, n = it["fb"], it["gi"], it["g0"], it["n"]
                            if gi == 0:
                                wg_, t_wg = wg.next()
                                wu_, t_wu = wu.next()
                                wd_, t_wd = wd.next()
                                f0 = fb * 256
                                self.dma("pool", wg_[:], wgu_d[:, f0:f0 + 256].rearrange("(c p) n -> p c n", p=128),
                                         writes=[t_wg])
                                self.dma("pool", wu_[:],
                                         wgu_d[:, F + f0:F + f0 + 256].rearrange("(c p) n -> p c n", p=128),
                                         writes=[t_wu])
                                ws_, t_ws = wdst.next()
                                self.dma("sp", ws_[:], wdn_d[f0:f0 + 256, :].rearrange("(c p) n -> p c n", p=128),
                                         writes=[t_ws])
                                self.copy("act", wd_[:], ws_[:], [t_ws], [t_wd])
                                cur["w"] = (wg_, t_wg, wu_, t_wu, wd_, t_wd)
                            wg_, t_wg, wu_, t_wu, wd_, t_wd = cur["w"]
                            it["wd"] = (wd_, t_wd)
                            self.S.begin_cond(cap, g0, NTOK)
                            a_, t_a = aT.next()
                            it["a"] = (a_, t_a)
                            for fc in range(2):
                                g_, t_g = gps.next()
                                u_, t_u = ups.next()
                                for k in range(8):
                                    self.mm(g_[:, 0:n], wg_[:, k, fc * 128:(fc + 1) * 128], hTe[:, k, g0:g0 + n],
                                            k == 0, k == 7, [t_wg, t_hTe[gi]], [t_g])
                                for k in range(8):
                                    self.mm(u_[:, 0:n], wu_[:, k, fc * 128:(fc + 1) * 128], hTe[:, k, g0:g0 + n],
                                            k == 0, k == 7, [t_wu, t_hTe[gi]], [t_u])
                                s_, t_s = sg.next()
                                self.act(s_[:, 0:n], g_[:, 0:n], AF.Silu, [t_g], [t_s])
                                self.tt("dve", a_[:, fc, 0:n], s_[:, 0:n], u_[:, 0:n], ALU.mult, [t_s, t_u], [t_a])
                            self.S.end_cond()

                        def stage_b(it):
                            fb, g0, n = it["fb"], it["g0"], it["n"]
                            a_, t_a = it["a"]
                            wd_, t_wd = it["wd"]
                            self.S.begin_cond(cap, g0, NTOK)
                            for sub in range(n // 128):
                                ti = g0 // 128 + sub
                                y_, t_y = yps.next()
                                for nb in range(2):
                                    for fc in range(2):
                                        self.mm(y_[:, nb, :], a_[:, fc, sub * 128:(sub + 1) * 128],
                                                wd_[:, fc, nb * 512:(nb + 1) * 512], fc == 0, fc == 1, [t_a, t_wd],
                                                [t_y])
                                yf = y_[:].rearrange("p a n -> p (a n)")
                                if fb == 0:
                                    self.copy("act", yacc[:, ti, :], yf, [t_y], [t_ya[ti]])
                                else:
                                    self.tt("dve", yacc[:, ti, :], yacc[:, ti, :], yf, ALU.add, [t_y, t_ya[ti]],
                                            [t_ya[ti]])
                            self.S.end_cond()
                            if fb == nfb - 1:
                                for sub in range(n // 128):
                                    ti = g0 // 128 + sub
                                    r0 = e * NSLOT + ti * 128
                                    self.dma("sp", self.ye_d[r0:r0 + 128, :], yacc[:, ti, :], reads=[t_ya[ti]])

                        for i in range(len(its) + 1):
                            if i < len(its):
                                stage_a(its[i])
                            if i >= 1:
                                stage_b(its[i - 1])
                with self.scope() as sc:
                    c = self.common(sc, njunk=1024)
                    mods = self.load_mod(sc, layer, [(5, "gate", 3)], ctx_rows=not last)
                    eoff = sc.sb("eoff", [128, NE], F32)
                    t_eo = self.S.tile()
                    self.dma("sp", eoff[:], self.eoff_d[:, :], writes=[t_eo])
                    xr = sc.rot("xr", [128, D], F32, 2)
                    tm = sc.rot("tm", [128, D], F32, 2)
                    xo = sc.rot("xo", [128, D], F32, 2)
                    yh = sc.rot("yh", [128, D], F32, 2)
                    yl = sc.rot("yl", [128, D], F32, 2)
                    mk = sc.rot("mk", [128, NE], F32, 2)
                    fl = sc.rot("fl", [128, NE], F32, 2)
                    a8 = sc.rot("a8", [128, 8], F32, 2)
                    eq = sc.rot("eq", [128, NE], F32, 4)
                    gg = sc.rot("gg", [128, 2], F32, 2)
                    ii = sc.rot("ii", [128, 2], I32, 2)
                    fi = sc.rot("fi", [128, 2], F32, 2)
                    for ti, tt_ in enumerate(half):
                        r_ = 1 if tt_ < NCTX_T else 0
                        G_ap, t_G = mods[(5, r_)]
                        rows = slice(tt_ * 128, (tt_ + 1) * 128)
                        x_, t_x = xr.next()
                        self.dma("sp", x_[:], self.xres[rows, :], writes=[t_x])
                        mk_, t_mk = mk.next()
                        self.ts("dve", mk_[:], posm[:, ti, :], 0.0, ALU.is_ge, [], [t_mk])
                        self.tt("dve", mk_[:], mk_[:], eoff[:], ALU.mult, [t_mk, t_eo], [t_mk])
                        fl_, t_fl = fl.next()
                        self.stt(fl_[:], posm[:, ti, :], 1.0, mk_[:], ALU.add, ALU.add, [t_mk], [t_fl])
                        a_, t_a = a8.next()
                        self.S.op("dve", lambda e, o=a_, i=fl_: e.max(out=o[:], in_=i[:]), [t_fl], [t_a])
                        g_, t_g = gg.next()
                        for w in range(2):
                            q_, t_q = eq.next()
                            self.ts("dve", q_[:], fl_[:], a_[:, w:w + 1], ALU.is_equal, [t_fl, t_a], [t_q])
                            self.tt("dve", q_[:], q_[:], gates[:, ti, :], ALU.mult, [t_q], [t_q])
                            self.S.op("dve", lambda e, o=g_, i=q_, w=w: e.tensor_reduce(
                                out=o[:, w:w + 1], in_=i[:], axis=AX.X, op=ALU.add), [t_q], [t_g])
                        f_, t_f = fi.next()
                        self.ts("dve", f_[:], a_[:, 0:2], -1.0, ALU.add, [t_a], [t_f])
                        i_, t_i = ii.next()
                        self.copy("dve", i_[:], f_[:], [t_f], [t_i])
                        yh_, t_yh = yh.next()
                        yl_, t_yl = yl.next()
                        self.S.dma("pool", lambda e, o=yh_, i=i_: e.indirect_dma_start(
                            out=o[:], out_offset=None, in_=self.ye_d[:, :],
                            in_offset=bass.IndirectOffsetOnAxis(ap=i[:, 0:1], axis=0)), [t_i], [t_yh])
                        self.S.dma("pool", lambda e, o=yl_, i=i_: e.indirect_dma_start(
                            out=o[:], out_offset=None, in_=self.ye_d[:, :],
                            in_offset=bass.IndirectOffsetOnAxis(ap=i[:, 1:2], axis=0)), [t_i], [t_yl])
                        self.ts("dve", yh_[:], yh_[:], g_[:, 0:1], ALU.mult, [t_yh, t_g], [t_yh])
                        self.stt(yh_[:], yl_[:], g_[:, 1:2], yh_[:], ALU.mult, ALU.add, [t_yl, t_g, t_yh], [t_yh])
                        r, t_r = self.rstd_of(c, yh_[:], D, [t_yh])
                        t_, t_t = tm.next()
                        self.stt(t_[:], yh_[:], r[:], G_ap[:], ALU.mult, ALU.mult, [t_yh, t_r, t_G], [t_t])
                        o_, t_o = xo.next()
                        self.tt("pool", o_[:], x_[:], t_[:], ALU.add, [t_x, t_t], [t_o])
                        if layer == self.nlayers - 1:
                            if tt_ >= NCTX_T:
                                self.dma("sp", self.out[(tt_ - NCTX_T) * 128:(tt_ - NCTX_T + 1) * 128, :], o_[:],
                                         reads=[t_o])
                            if self.debug:
                                self.dma("sp", self.xres[rows, :], o_[:], reads=[t_o])
                        else:
                            self.dma("sp", self.xres[rows, :], o_[:], reads=[t_o])

    def router(self, sc, c, idx, half, tok0, gatesT, t_gT, yps, ups):
        wr = sc.sb("wr", [128, 8, NE], F32)
        t_wr = self.S.tile()
        self.dma("sp", wr[:], self.e_w_router[idx].rearrange("(c p) n -> p c n", p=128), writes=[t_wr])
        hf = sc.rot("rhf", [128, D], F32, 2)
        hfT = sc.rot("rhfT", [128, 8, 128], F32, 2)
        lg = sc.rot("lg", [128, NE], F32, 2)
        m8 = sc.rot("m8", [128, 8], F32, 2)
        dd = sc.rot("dd", [128, NE], F32, 2)
        ee = sc.rot("ee", [128, NE], F32, 2)
        mk = sc.rot("mk", [128, NE], F32, 2)
        sm = sc.rot("sm", [128, 1], F32, 2)
        gt = sc.rot("gt", [128, NE], F32, 2)
        for ti, tt_ in enumerate(half):
            rows = slice(tt_ * 128, (tt_ + 1) * 128)
            h_, t_h = hf.next()
            self.dma("sp", h_[:], self.h2f_d[rows, :], writes=[t_h])
            y_, t_y = yps.next()
            yT = y_[:].rearrange("p a (b n) -> p (a b) n", n=128)
            for k in range(8):
                self.tr(yT[:, k, :], h_[:, k * 128:(k + 1) * 128], c.identf[:], [t_h, c.t_id], [t_y])
            hT_, t_hT = hfT.next()
            self.copy("act", hT_[:], yT, [t_y], [t_hT])
            misc, t_misc = ups.next()
            for k in range(8):
                self.mm(misc[:, 0:NE], hT_[:, k, :], wr[:, k, :], k == 0, k == 7, [t_hT, t_wr], [t_misc])
            l_, t_l = lg.next()
            self.copy("dve", l_[:], misc[:, 0:NE], [t_misc], [t_l])
            m_, t_m = m8.next()
            self.S.op("dve", lambda e, o=m_, i=l_: e.max(out=o[:], in_=i[:]), [t_l], [t_m])
            d_, t_d = dd.next()
            self.ts("dve", d_[:], l_[:], m_[:, 0:1], ALU.subtract, [t_l, t_m], [t_d])
            e_, t_e = ee.next()
            self.act(e_[:], d_[:], AF.Exp, [t_d], [t_e])
            k_, t_k = mk.next()
            self.ts("dve", k_[:], l_[:], m_[:, 1:2], ALU.is_ge, [t_l, t_m], [t_k])
            self.tt("dve", e_[:], e_[:], k_[:], ALU.mult, [t_e, t_k], [t_e])
            s_, t_s = sm.next()
            self.S.op("dve", lambda e, o=s_, i=e_: e.tensor_reduce(out=o[:], in_=i[:], axis=AX.X, op=ALU.add),
                      [t_e], [t_s])
            self.recip(s_[:], s_[:], [t_s], [t_s])
            g_, t_g = gt.next()
            self.ts("dve", g_[:], e_[:], s_[:], ALU.mult, [t_e, t_s], [t_g])
            self.tr(misc[0:NE, 128:256], g_[:], c.identf[:], [t_g, c.t_id], [t_misc])
            self.copy("dve", gatesT[:, ti * 128:(ti + 1) * 128], misc[0:NE, 128:256], [t_misc], [t_gT])

    def build(self):
        try:
            self._build()
        except _Stop:
            pass
        return self.nc

    def _build(self):
        with self.es:
            self.declare()
            self.phase_mod()
            for layer in range(self.nlayers):
                last = layer == DEPTH - 1
                kind = layer % 3
                j = layer // 3
                moe = layer % 2 == 1
                xsrc = self.xin if layer == 0 else self.xres
                self.cur_sparse = moe and last
                with contextlib.ExitStack() as les:
                    lay = type("L", (), {})()
                    if kind == 0:
                        lay.KT = les.enter_context(self.nc.sbuf_tensor(f"KT{layer}", [128, 4, T], BF16))
                        lay.V = les.enter_context(self.nc.sbuf_tensor(f"Vaug{layer}", [128, NT, 4, 65], BF16))
                        lay.t_KT = self.S.tile()
                        lay.t_V = self.S.tile()
                        self.phase_gqa_proj(layer, j, xsrc, lay)
                        self.phase_attn("gqa", layer, j, xsrc, lay, last, moe)
                    elif kind == 1:
                        self.phase_fourier_1(layer, xsrc)
                        self.phase_fourier_2(layer, xsrc, last, moe)
                    else:
                        lay.KrT = les.enter_context(self.nc.sbuf_tensor(f"KrT{layer}", [64, T], BF16))
                        lay.t_KrT = self.S.tile()
                        self.phase_mla_proj(layer, xsrc, lay)
                        self.phase_attn("mla", layer, j, xsrc, lay, last, moe)
                if self.cur_sparse:
                    self.phase_moe(layer, last)
                else:
                    self.phase_ffn(layer, moe, last, xsrc)
        return self.nc


def _rope_tables(rot_dim):
    nf = rot_dim // 4
    inv = (10000.0 ** (-np.arange(nf, dtype=np.float32) / nf)).astype(np.float32)
    rows = np.repeat(np.arange(64, dtype=np.float32), 64)
    cols = np.tile(np.arange(64, dtype=np.float32), 64)
    ar = rows[:, None] * inv[None, :]
    ac = cols[:, None] * inv[None, :]
    cr, sr, cc, sc_ = np.cos(ar), np.sin(ar), np.cos(ac), np.sin(ac)
    cos = np.concatenate([cr, cr, cc, cc], axis=1).astype(np.float32)
    sin = np.concatenate([-sr, sr, -sc_, sc_], axis=1).astype(np.float32)
    cos = np.concatenate([np.ones((256, rot_dim), np.float32), cos], axis=0)
    sin = np.concatenate([np.zeros((256, rot_dim), np.float32), sin], axis=0)
    return cos, sin


_CONST = {}


def _constants():
    if _CONST:
        return _CONST
    c = {}
    c["ident"] = np.eye(128, dtype=np.float32)
    c["ropeA_cos"], c["ropeA_sin"] = _rope_tables(64)
    mc, ms = _rope_tables(32)
    c["ropeM_cos"], c["ropeM_sin"] = mc, ms
    c["ropeMT_cos"] = np.ascontiguousarray(np.concatenate([mc, mc], axis=1).T)
    c["ropeMT_sin"] = np.ascontiguousarray(np.concatenate([ms, ms], axis=1).T)
    k = np.arange(256)
    ang = 2 * np.pi * ((k[:, None] * k[None, :]) % 256) / 256.0
    c["dft256"] = np.concatenate([np.cos(ang), np.sin(ang)], axis=1).astype(np.float32)
    c["cos256"] = np.cos(ang).astype(ml_dtypes.bfloat16)
    c["nsin256"] = (-np.sin(ang)).astype(ml_dtypes.bfloat16)
    k = np.arange(4096, dtype=np.int64)
    ang = 2 * np.pi * ((k[:, None] * k[None, :]) % 4096) / 4096.0
    c["cos4096"] = np.cos(ang).astype(ml_dtypes.bfloat16)
    c["nsin4096"] = (-np.sin(ang)).astype(ml_dtypes.bfloat16)
    sel = np.zeros((NE, NE, 128), np.float32)
    for e in range(NE):
        sel[e, e, :] = 1.0
    c["sel"] = sel.reshape(NE, NE * 128)
    c["tri"] = np.triu(np.ones((128, 128), np.float32))
    c["iota512"] = np.tile(np.arange(512, dtype=np.float32)[None, :], (128, 1))
    c["eoff"] = np.tile((np.arange(NE, dtype=np.float32) * NSLOT)[None, :], (128, 1))
    _CONST.update(c)
    return _CONST


def _prep_inputs(inputs):
    f = lambda a: np.ascontiguousarray(np.asarray(a, dtype=np.float32))
    shared = dict(_constants())
    for k_ in ["w_mod", "b_mod", "norm_g", "a_w_qkv", "a_g_q", "a_g_k", "a_w_o", "f_w", "m_w_down", "m_g_cq", "m_g_ckv",
               "m_w_o", "d_w_gu", "d_w_down", "e_w_router", "e_w_gu", "e_w_down"]:
        shared[k_] = f(inputs[k_])
    wuq = f(inputs["m_w_uq"])[0].reshape(384, 16, 96)
    shared["m_wuqn"] = np.ascontiguousarray(wuq[:, :, 0:64].reshape(384, 1024))
    rope = wuq[:, :, 64:96]
    shared["m_wuqr"] = np.ascontiguousarray(rope.reshape(384, 512))
    perm = np.array([(r + 8) if (r % 16) < 8 else (r - 8) for r in range(32)])
    shared["m_wuqrs"] = np.ascontiguousarray(rope[:, :, perm].reshape(384, 512))
    wukv = f(inputs["m_w_ukv"])[0].reshape(256, 16, 128)
    shared["m_wuk"] = np.ascontiguousarray(wukv[:, :, 0:64].reshape(256, 1024))
    shared["m_wuv"] = np.ascontiguousarray(wukv[:, :, 64:128].reshape(256, 1024))
    x = f(inputs["x"])
    ctx = f(inputs["ctx"])
    cc = f(inputs["c"])
    c_ctx = f(inputs["c_ctx"])
    maps = []
    for b in range(8):
        m = dict(shared)
        m["xin"] = np.ascontiguousarray(np.concatenate([ctx[b], x[b]], axis=0))
        cv = np.stack([cc[b], c_ctx], axis=1)
        m["cvT"] = np.ascontiguousarray(cv.reshape(8, 128, 2).transpose(1, 0, 2).reshape(128, 16))
        maps.append(m)
    return maps


_NC_CACHE = {}


def kernel(**inputs):
    maps = _prep_inputs(inputs)
    if "nc" not in _NC_CACHE:
        _NC_CACHE["nc"] = Builder().build()
    nc = _NC_CACHE["nc"]
    res = run_bass_kernel_spmd(nc, maps, core_ids=list(range(8)))
    out = np.stack([np.asarray(r["out"], dtype=np.float32) for r in res.results], axis=0)
    return out
```

```python
import contextlib
import numpy as np
import ml_dtypes
import concourse.bass as bass
import concourse.mybir as mybir
from concourse.bass_utils import run_bass_kernel_spmd

F32 = mybir.dt.float32
BF16 = mybir.dt.bfloat16
AF = mybir.ActivationFunctionType
ALU = mybir.AluOpType
AX = mybir.AxisListType

D = 1024
T = 4352
NT = 34
NCTX_T = 2
DEPTH = 4
EPS = 1e-6
FF_D = 2816
FF_E = 3584
NE = 8
NSLOT = 2176
I32 = mybir.dt.int32


class Tile:
    __slots__ = ("name", "writers", "readers")

    def __init__(self, name=""):
        self.name = name
        self.writers = {}
        self.readers = {}


class Op:
    __slots__ = ("stream", "tl", "deps", "fn", "marked", "epoch", "value", "seq", "cond")


class Timeline:
    def __init__(self, name, inc, limit):
        self.name = name
        self.inc = inc
        self.limit = limit
        self.ops = []
        self.epoch = None
        self.cnt = 0
        self.last = None


class Sched:
    STREAMS = ("sp", "act", "dve", "pool", "pe")
    ENG = {"sp": "sync", "act": "scalar", "dve": "vector", "pool": "gpsimd", "pe": "tensor"}

    def __init__(self, nc, semalloc, ndma=8):
        self.nc = nc
        self.semalloc = semalloc
        self.ops = {s: [] for s in self.STREAMS}
        self.tls = {s: Timeline(s, 1, 30000) for s in ("act", "dve", "pool", "pe")}
        self.dma_tls = {q: [Timeline(f"dma_{q}{i}", 16, 1800) for i in range(ndma)] for q in ("sp", "pool")}
        self.dma_rr = {q: 0 for q in self.dma_tls}
        self.all_tiles = []
        self.nseq = 0
        self.waited = {s: {} for s in self.STREAMS}
        self.nops = 0
        self.cur_cond = None
        self.valcache = {}

    def begin_cond(self, cnt_ap, thr, maxv):
        self.cur_cond = (cnt_ap, thr, maxv)

    def end_cond(self):
        self.cur_cond = None

    def all_tls(self):
        return list(self.tls.values()) + [t for q in self.dma_tls.values() for t in q]

    def tile(self, name=""):
        t = Tile(name)
        self.all_tiles.append(t)
        return t

    def _mk(self, stream, tl, fn, reads, writes, extra=()):
        op = Op()
        op.stream = stream
        op.tl = tl
        op.fn = fn
        op.marked = False
        op.epoch = None
        op.value = None
        op.seq = self.nseq
        op.cond = self.cur_cond
        assert not (op.cond is not None and tl.inc == 16), "no DMA inside conditional regions"
        self.nseq += 1
        deps = {}

        def add(d):
            k = d.tl
            if k not in deps or deps[k].seq < d.seq:
                deps[k] = d

        for t in reads:
            for d in t.writers.values():
                add(d)
        for t in writes:
            for d in t.writers.values():
                add(d)
            for d in t.readers.values():
                add(d)
        for d in extra:
            add(d)
        if stream == "pe" and self.tls["pe"] in deps:
            del deps[self.tls["pe"]]
        op.deps = list(deps.values())
        for d in op.deps:
            d.marked = True
        for t in reads:
            t.readers[tl] = op
        for t in writes:
            t.writers = {tl: op}
            t.readers = {}
        tl.ops.append(op)
        tl.last = op
        self.ops[stream].append(op)
        self.nops += 1
        return op

    def op(self, stream, fn, reads=(), writes=()):
        return self._mk(stream, self.tls[stream], fn, reads, writes)

    def dma(self, queue, fn, reads=(), writes=()):
        tls = self.dma_tls[queue]
        i = self.dma_rr[queue]
        self.dma_rr[queue] = (i + 1) % len(tls)
        tl = tls[i]
        prev = tl.ops[-1] if tl.ops else None
        op = self._mk(queue, tl, fn, reads, writes, extra=(prev,) if prev is not None else ())
        op.marked = True
        return op

    def barrier_and_emit(self):
        lasts = [tl.ops[-1] for tl in self.all_tls() if tl.ops]
        for s in self.STREAMS:
            op = Op()
            op.stream = s
            op.tl = None
            op.fn = None
            op.marked = False
            op.epoch = None
            op.value = None
            op.seq = self.nseq
            op.cond = None
            self.nseq += 1
            op.deps = [d for d in lasts if not (s == "pe" and d.tl is self.tls["pe"])]
            for d in op.deps:
                d.marked = True
            self.ops[s].append(op)
        for t in self.all_tiles:
            t.writers = {}
            t.readers = {}
        self._emit()

    def _emit(self):
        nc = self.nc
        for tl in self.all_tls():
            for op in tl.ops:
                if not op.marked:
                    continue
                if tl.epoch is None or tl.cnt >= tl.limit:
                    tl.epoch = self.semalloc(tl.name)
                    tl.cnt = 0
                tl.cnt += 1
                op.epoch = tl.epoch
                op.value = tl.cnt * tl.inc
        with nc.Block() as block:
            for s in self.STREAMS:
                ops = self.ops[s]
                if not ops:
                    continue

                def body(e, ops=ops, waited=self.waited[s], sname=s):
                    def emit_op(op):
                        for d in op.deps:
                            key = id(d.epoch)
                            if waited.get(key, 0) >= d.value:
                                continue
                            e.wait_ge(d.epoch, d.value)
                            waited[key] = d.value
                        if op.fn is None:
                            return
                        ins = op.fn(e)
                        if op.marked:
                            ins.then_inc(op.epoch, op.tl.inc)

                    i = 0
                    while i < len(ops):
                        op = ops[i]
                        if op.cond is None:
                            emit_op(op)
                            i += 1
                            continue
                        j = i
                        while j < len(ops) and ops[j].cond is op.cond:
                            j += 1
                        region = ops[i:j]
                        cnt_ap, thr, maxv = op.cond
                        ck = id(cnt_ap)
                        st_ = self.valcache.setdefault(sname, {"reg": None, "key": None, "val": None})
                        if st_["reg"] is None:
                            st_["reg"] = self.cond_regs[sname]
                        if st_["key"] != ck:
                            e.reg_load(st_["reg"], cnt_ap)
                            st_["val"] = e.snap(st_["reg"])
                            st_["key"] = ck
                        val = st_["val"]
                        saved = dict(waited)
                        with e.If(val > thr):
                            for rop in region:
                                emit_op(rop)
                        with e.Else():
                            incs = {}
                            for rop in region:
                                if rop.marked:
                                    k_ = id(rop.epoch)
                                    if k_ not in incs:
                                        incs[k_] = [rop.epoch, 0]
                                    incs[k_][1] += rop.tl.inc
                            if incs:
                                e.drain()
                            for ep_, n_ in incs.values():
                                e.sem_inc(ep_, n_)
                        waited.clear()
                        waited.update(saved)
                        i = j

                getattr(block, self.ENG[s])(body)
        for s in self.STREAMS:
            self.ops[s] = []
        for tl in self.all_tls():
            tl.ops = []
        for st_ in self.valcache.values():
            st_["key"] = None
            st_["val"] = None


class Rot:
    def __init__(self, S, alloc, name, shape, dt, n):
        self.bufs = [alloc(f"{name}{i}", shape, dt) for i in range(n)]
        self.tiles = [S.tile(f"{name}{i}") for i in range(n)]
        self.i = 0

    def next(self):
        b, t = self.bufs[self.i], self.tiles[self.i]
        self.i = (self.i + 1) % len(self.bufs)
        return b, t


class _Stop(Exception):
    pass


class Builder:
    def __init__(self, nlayers=DEPTH, debug=False, nphases=None):
        self.nphases = nphases
        self.phase_i = 0
        self.nlayers = nlayers
        self.debug = debug
        nc = self.nc = bass.Bass("TRN2", target_bir_lowering=False)
        self.es = contextlib.ExitStack()
        self.uid = 0

        def semalloc(name):
            self.uid += 1
            return self.es.enter_context(nc.semaphore(f"{name}_{self.uid}"))

        self.S = Sched(nc, semalloc)
        self.S.cond_regs = {"pe": nc.tensor.alloc_register("cr_pe"), "act": nc.scalar.alloc_register("cr_act"),
                            "dve": nc.vector.alloc_register("cr_dve"), "pool": nc.gpsimd.alloc_register("cr_pool")}

    def din(self, name, shape, dt=F32):
        return self.nc.dram_tensor(name, list(shape), dt, kind="ExternalInput").ap()

    def dscr(self, name, shape, dt):
        return self.nc.dram_tensor(name, list(shape), dt, kind="Internal").ap()

    def declare(self):
        d = self.din
        self.xin = d("xin", [T, D])
        self.cvT = d("cvT", [128, 16])
        self.ident_d = d("ident", [128, 128])
        self.w_mod = d("w_mod", [DEPTH, D, 6 * D])
        self.b_mod = d("b_mod", [DEPTH, 6 * D])
        self.norm_g = d("norm_g", [DEPTH, 4, D])
        self.a_w_qkv = d("a_w_qkv", [2, D, 1536])
        self.a_g_q = d("a_g_q", [2, 64])
        self.a_g_k = d("a_g_k", [2, 64])
        self.a_w_o = d("a_w_o", [2, D, D])
        self.f_w = d("f_w", [1, D, D])
        self.m_w_down = d("m_w_down", [1, D, 672])
        self.m_g_cq = d("m_g_cq", [1, 384])
        self.m_g_ckv = d("m_g_ckv", [1, 256])
        self.m_wuqn = d("m_wuqn", [384, 1024])
        self.m_wuqr = d("m_wuqr", [384, 512])
        self.m_wuqrs = d("m_wuqrs", [384, 512])
        self.m_wuk = d("m_wuk", [256, 1024])
        self.m_wuv = d("m_wuv", [256, 1024])
        self.m_w_o = d("m_w_o", [1, D, D])
        self.d_w_gu = d("d_w_gu", [2, D, 2 * FF_D])
        self.d_w_down = d("d_w_down", [2, FF_D, D])
        self.e_w_router = d("e_w_router", [2, D, NE])
        self.e_w_gu = d("e_w_gu", [2, NE, D, 2 * FF_E])
        self.e_w_down = d("e_w_down", [2, NE, FF_E, D])
        self.ropeA_cos = d("ropeA_cos", [T, 64])
        self.ropeA_sin = d("ropeA_sin", [T, 64])
        self.ropeM_cos = d("ropeM_cos", [T, 32])
        self.ropeM_sin = d("ropeM_sin", [T, 32])
        self.ropeMT_cos = d("ropeMT_cos", [64, T])
        self.ropeMT_sin = d("ropeMT_sin", [64, T])
        self.dft256 = d("dft256", [256, 512])
        self.cos4096 = d("cos4096", [4096, 4096], BF16)
        self.nsin4096 = d("nsin4096", [4096, 4096], BF16)
        self.cos256 = d("cos256", [256, 256], BF16)
        self.nsin256 = d("nsin256", [256, 256], BF16)
        self.sel_d = d("sel", [NE, NE * 128])
        self.tri_d = d("tri", [128, 128])
        self.iota_d = d("iota512", [128, 512])
        self.eoff_d = d("eoff", [128, NE])
        self.out = self.nc.dram_tensor("out", [4096, D], F32, kind="ExternalOutput").ap()
        s = self.dscr
        if self.debug:
            self.xres = self.nc.dram_tensor("xres", [T, D], F32, kind="ExternalOutput").ap()
        else:
            self.xres = s("xres", [T, D], F32)
        self.modrows = s("modrows", [DEPTH, 2, 6 * D], F32)
        self.ye_d = s("arena", [NE * NSLOT, D], F32)
        ab = self.ye_d.bitcast(BF16)
        r0 = [0]

        def carve(nrows):
            v = ab[r0[0]:r0[0] + nrows, :].rearrange("r c -> (r c)")
            r0[0] += nrows
            return v

        self.QT_d = carve(2176).rearrange("(c p n) -> c p n", c=8, p=128)
        self.QrT_d = carve(1088).rearrange("(c p n) -> c p n", c=8, p=64)
        self.KnT_d = carve(2176).rearrange("(c p n) -> c p n", c=8, p=128)
        self.V_d = carve(2210).rearrange("(t p n) -> t p n", t=NT, p=128)
        self.XCS_d = carve(4352).rearrange("(g t n) -> g t n", g=4, t=T)
        self.h2T_d = carve(2176).rearrange("(p c n) -> p c n", p=128, c=8)
        self.h2f_d = s("h2f_d", [T, D], F32)

    def scope(self):
        b = self

        class _Scope:
            def __enter__(s):
                if b.nphases is not None and b.phase_i >= b.nphases:
                    raise _Stop()
                b.phase_i += 1
                s.es = contextlib.ExitStack()
                s.es.__enter__()
                return s

            def sb(s, name, shape, dt):
                b.uid += 1
                return s.es.enter_context(b.nc.sbuf_tensor(f"{name}_{b.uid}", list(shape), dt))

            def ps(s, name, shape, dt):
                b.uid += 1
                return s.es.enter_context(b.nc.psum_tensor(f"{name}_{b.uid}", list(shape), dt))

            def rot(s, name, shape, dt, n, psum=False):
                return Rot(b.S, s.ps if psum else s.sb, name, shape, dt, n)

            def __exit__(s, *a):
                if a[0] is None:
                    b.S.barrier_and_emit()
                return s.es.__exit__(*a)

        return _Scope()

    def dma(self, q, out, in_, reads=(), writes=()):
        return self.S.dma(q, lambda e: e.dma_start(out=out, in_=in_), reads, writes)

    def act(self, out, in_, func, reads, writes, **kw):
        return self.S.op("act", lambda e: e.activation(out=out, in_=in_, func=func, **kw), reads, writes)

    def tt(self, eng, out, in0, in1, op, reads, writes):
        return self.S.op(eng, lambda e: e.tensor_tensor(out=out, in0=in0, in1=in1, op=op), reads, writes)

    def stt(self, out, in0, scalar, in1, op0, op1, reads, writes):
        return self.S.op("dve", lambda e: e.scalar_tensor_tensor(out=out, in0=in0, scalar=scalar, in1=in1,
                                                                  op0=op0, op1=op1), reads, writes)

    def ts(self, eng, out, in0, s1, op0, reads, writes, s2=None, op1=None):
        if op1 is None:
            return self.S.op(eng, lambda e: e.tensor_scalar(out=out, in0=in0, scalar1=s1, scalar2=None, op0=op0),
                             reads, writes)
        return self.S.op(eng, lambda e: e.tensor_scalar(out=out, in0=in0, scalar1=s1, scalar2=s2, op0=op0, op1=op1),
                         reads, writes)

    def copy(self, eng, out, in_, reads, writes):
        if eng == "act":
            return self.act(out, in_, AF.Copy, reads, writes)
        return self.S.op(eng, lambda e: e.tensor_copy(out=out, in_=in_), reads, writes)

    def mm(self, out, lhsT, rhs, start, stop, reads, writes):
        return self.S.op("pe", lambda e: e.matmul(out, lhsT=lhsT, rhs=rhs, start=start, stop=stop), reads, writes)

    def tr(self, out, in_, ident, reads, writes):
        return self.S.op("pe", lambda e: e.transpose(out=out, in_=in_, identity=ident), reads, writes)

    def recip(self, out, in_, reads, writes):
        return self.S.op("dve", lambda e: e.reciprocal(out=out, in_=in_), reads, writes)

    def memset(self, eng, ap, val, writes):
        return self.S.op(eng, lambda e: e.memset(ap, val), (), writes)

    def common(self, sc, njunk=1536):
        c = type("C", (), {})()
        c.ss = sc.rot("ss", [128, 1], F32, 4)
        c.rs = sc.rot("rs", [128, 1], F32, 4)
        c.junk = sc.rot("junk", [128, njunk], BF16, 2)
        c.identb = sc.sb("identb", [128, 128], BF16)
        c.identf = sc.sb("identf", [128, 128], F32)
        c.t_id = self.S.tile("ident")
        self.dma("sp", c.identf[:], self.ident_d[:, :], writes=[c.t_id])
        self.copy("dve", c.identb[:], c.identf[:], [c.t_id], [c.t_id])
        return c

    def rstd_of(self, c, src, n, reads, eps=EPS):
        ss, t_ss = c.ss.next()
        jk, t_jk = c.junk.next()
        self.act(jk[:, 0:n], src, AF.Square, reads, [t_jk, t_ss], accum_out=ss[:])
        rs, t_rs = c.rs.next()
        self.act(rs[:], ss[:], AF.Sqrt, [t_ss], [t_rs], scale=1.0 / n, bias=eps)
        self.recip(rs[:], rs[:], [t_rs], [t_rs])
        return rs, t_rs

    def load_mod(self, sc, layer, which, ctx_rows=True):
        res = {}
        gt = {}
        for (j, kind, nrow) in which:
            if kind != "shift" and nrow not in gt:
                g = sc.sb(f"gbc{nrow}", [128, D], F32)
                tg = self.S.tile()
                self.dma("sp", g[:], self.norm_g[layer, nrow:nrow + 1, :].partition_broadcast(128), writes=[tg])
                gt[nrow] = (g, tg)
            for r in range(2 if ctx_rows else 1):
                m = sc.sb(f"mod{j}_{r}", [128, D], F32)
                tm = self.S.tile()
                self.dma("sp", m[:], self.modrows[layer, r:r + 1, j * D:(j + 1) * D].partition_broadcast(128),
                         writes=[tm])
                if kind == "scale":
                    g, tg = gt[nrow]
                    self.stt(m[:], m[:], 1.0, g[:], ALU.add, ALU.mult, [tm, tg], [tm])
                elif kind == "gate":
                    g, tg = gt[nrow]
                    self.tt("dve", m[:], m[:], g[:], ALU.mult, [tm, tg], [tm])
                res[(j, r)] = (m, tm)
        return res

    def phase_mod(self):
        with self.scope() as sc:
            cv = sc.sb("cv", [128, 16], F32)
            scT = sc.sb("scT", [128, 16], F32)
            t_cv = self.S.tile()
            self.dma("sp", cv[:], self.cvT[:, :], writes=[t_cv])
            self.act(scT[:], cv[:], AF.Silu, [t_cv], [t_cv])
            wm = sc.rot("wm", [128, 8, 512], F32, 3)
            bm = sc.rot("bm", [2, 512], F32, 3)
            mr = sc.rot("mr", [2, 512], F32, 3)
            ps = sc.rot("mps", [2, 512], F32, 2, psum=True)
            for i in range(self.nlayers):
                for nb in range(12):
                    w, t_w = wm.next()
                    self.dma("sp", w[:], self.w_mod[i, :, nb * 512:(nb + 1) * 512].rearrange("(c p) n -> p c n", p=128),
                             writes=[t_w])
                    b_, t_b = bm.next()
                    self.dma("sp", b_[:], self.b_mod[i:i + 1, nb * 512:(nb + 1) * 512].partition_broadcast(2),
                             writes=[t_b])
                    p, t_p = ps.next()
                    for k in range(8):
                        self.mm(p[:], scT[:, 2 * k:2 * k + 2], w[:, k, :], k == 0, k == 7, [t_cv, t_w], [t_p])
                    m, t_m = mr.next()
                    self.tt("dve", m[:], p[:], b_[:], ALU.add, [t_p, t_b], [t_m])
                    self.dma("sp", self.modrows[i, :, nb * 512:(nb + 1) * 512], m[:], reads=[t_m])

    def mk_pre(self, sc, c):
        p = type("P", (), {})()
        p.x = sc.rot("px", [128, D], F32, 3)
        p.tmp = sc.rot("ptmp", [128, D], F32, 2)
        p.hb = sc.rot("phb", [128, D], BF16, 2)
        p.hT = sc.rot("phT", [128, 8, 128], BF16, 3)
        return p

    def prenorm(self, c, p, pT, xsrc, tt_, A, B):
        A_ap, t_A = A
        B_ap, t_B = B
        xt, t_x = p.x.next()
        self.dma("sp", xt[:], xsrc[tt_ * 128:(tt_ + 1) * 128, :], writes=[t_x])
        r, t_r = self.rstd_of(c, xt[:], D, [t_x])
        tmp, t_tmp = p.tmp.next()
        self.stt(tmp[:], xt[:], r[:], A_ap[:], ALU.mult, ALU.mult, [t_x, t_r, t_A], [t_tmp])
        hb, t_hb = p.hb.next()
        self.tt("pool", hb[:], tmp[:], B_ap[:], ALU.add, [t_tmp, t_B], [t_hb])
        return self.transpose8(c, p, pT, hb, t_hb)

    def transpose8(self, c, p, pT, hb, t_hb):
        ps, t_ps = pT.next()
        for k in range(8):
            self.tr(ps[:, k, :], hb[:, k * 128:(k + 1) * 128], c.identb[:], [t_hb, c.t_id], [t_ps])
        hT, t_hT = p.hT.next()
        self.copy("act", hT[:], ps[:], [t_ps], [t_hT])
        return hT, t_hT

    def mk_epi(self, sc, c, moe):
        p = type("E", (), {})()
        p.x = sc.rot("ex", [128, D], F32, 2)
        p.tmp = sc.rot("etmp", [128, D], F32, 2)
        p.xn = sc.rot("exn", [128, D], F32, 2)
        p.hb = sc.rot("ehb", [128, D], BF16, 2)
        p.hT = sc.rot("ehT", [128, 8, 128], BF16, 2)
        p.hf = sc.rot("ehf", [128, D], F32, 2) if moe else None
        return p

    def epilogue(self, c, p, pT, xsrc, tt_, y_ap, y_tiles, G, A2, B2, do_h2, moe):
        G_ap, t_G = G
        rows = slice(tt_ * 128, (tt_ + 1) * 128)
        xt, t_x = p.x.next()
        self.dma("sp", xt[:], xsrc[rows, :], writes=[t_x])
        r, t_r = self.rstd_of(c, y_ap, D, y_tiles)
        tmp, t_tmp = p.tmp.next()
        self.stt(tmp[:], y_ap, r[:], G_ap[:], ALU.mult, ALU.mult, list(y_tiles) + [t_r, t_G], [t_tmp])
        xn, t_xn = p.xn.next()
        self.tt("pool", xn[:], xt[:], tmp[:], ALU.add, [t_x, t_tmp], [t_xn])
        self.dma("sp", self.xres[rows, :], xn[:], reads=[t_xn])
        if not do_h2:
            return
        A_ap, t_A = A2
        B_ap, t_B = B2
        r2, t_r2 = self.rstd_of(c, xn[:], D, [t_xn])
        t2, t_t2 = p.tmp.next()
        self.stt(t2[:], xn[:], r2[:], A_ap[:], ALU.mult, ALU.mult, [t_xn, t_r2, t_A], [t_t2])
        hb, t_hb = p.hb.next()
        if moe:
            hf, t_hf = p.hf.next()
            self.tt("pool", hf[:], t2[:], B_ap[:], ALU.add, [t_t2, t_B], [t_hf])
            self.dma("sp", self.h2f_d[rows, :], hf[:], reads=[t_hf])
            if self.cur_sparse:
                return
            self.copy("pool", hb[:], hf[:], [t_hf], [t_hb])
        else:
            self.tt("pool", hb[:], t2[:], B_ap[:], ALU.add, [t_t2, t_B], [t_hb])
        hT, t_hT = self.transpose8(c, p, pT, hb, t_hb)
        self.dma("sp", self.h2T_d[:, :, rows], hT[:], reads=[t_hT])

    def phase_gqa_proj(self, layer, j, xsrc, lay):
        KT, V_aug = lay.KT, lay.V
        with self.scope() as sc:
            c = self.common(sc)
            p = self.mk_pre(sc, c)
            mods = self.load_mod(sc, layer, [(0, "shift", None), (1, "scale", 0)])
            wq = sc.sb("wqkv", [128, 8, 1536], BF16)
            t_w = self.S.tile()
            for nb in range(3):
                self.dma("pool", wq[:, :, nb * 512:(nb + 1) * 512],
                         self.a_w_qkv[j, :, nb * 512:(nb + 1) * 512].rearrange("(c p) n -> p c n", p=128), writes=[t_w])
            gain = sc.sb("gain", [128, 20, 64], F32)
            t_g = self.S.tile()
            for hh in range(20):
                src = self.a_g_q if hh < 16 else self.a_g_k
                self.dma("sp", gain[:, hh, :], src[j:j + 1, :].partition_broadcast(128), writes=[t_g])
            cosT = sc.sb("cosT", [128, NT, 64], F32)
            sinT = sc.sb("sinT", [128, NT, 64], F32)
            t_tab = self.S.tile()
            self.dma("sp", cosT[:], self.ropeA_cos.rearrange("(t p) d -> p t d", p=128), writes=[t_tab])
            self.dma("sp", sinT[:], self.ropeA_sin.rearrange("(t p) d -> p t d", p=128), writes=[t_tab])
            self.memset("pool", V_aug[:, :, :, 64:65], 1.0, [lay.t_V])
            pT = sc.rot("pT", [128, 8, 128], BF16, 2, psum=True)
            qps = sc.rot("qps", [128, 1536], F32, 2, psum=True)
            sq = sc.rot("sq", [128, 1280], F32, 1)
            ssq = sc.rot("ssq", [128, 20], F32, 2)
            qn = sc.rot("qn", [128, 20, 64], F32, 2)
            qa = sc.rot("qa", [128, 20, 64], F32, 1)
            qb = sc.rot("qb", [128, 20, 64], F32, 1)
            qr = sc.rot("qr", [128, 20, 64], BF16, 2)
            kd = sc.rot("kd", [128, 4, 2, 64], BF16, 2)
            qst = sc.rot("qst", [128, 8, 128], BF16, 2)
            for tt_ in range(NT):
                r_ = 1 if tt_ < NCTX_T else 0
                hT, t_hT = self.prenorm(c, p, pT, xsrc, tt_, mods[(1, r_)], mods[(0, r_)])
                ps, t_ps = qps.next()
                for nb in range(3):
                    for k in range(8):
                        self.mm(ps[:, nb * 512:(nb + 1) * 512], hT[:, k, :], wq[:, k, nb * 512:(nb + 1) * 512],
                                k == 0, k == 7, [t_hT, t_w], [t_ps])
                s_, t_s = sq.next()
                self.act(s_[:], ps[:, 0:1280], AF.Square, [t_ps], [t_s])
                ss_, t_ss = ssq.next()
                self.S.op("dve", lambda e, o=ss_, i=s_: e.tensor_reduce(
                    out=o[:], in_=i[:].rearrange("p (h d) -> p h d", d=64), axis=AX.X, op=ALU.add), [t_s], [t_ss])
                self.act(ss_[:], ss_[:], AF.Sqrt, [t_ss], [t_ss], scale=1.0 / 64, bias=EPS)
                self.recip(ss_[:], ss_[:], [t_ss], [t_ss])
                q_, t_q = qn.next()
                self.tt("dve", q_[:], ps[:, 0:1280].rearrange("p (h d) -> p h d", d=64),
                        ss_[:].unsqueeze(2).to_broadcast([128, 20, 64]), ALU.mult, [t_ps, t_ss], [t_q])
                self.tt("pool", q_[:], q_[:], gain[:], ALU.mult, [t_q, t_g], [t_q])
                a_, t_a = qa.next()
                self.tt("dve", a_[:], q_[:], cosT[:, tt_, :].unsqueeze(1).to_broadcast([128, 20, 64]), ALU.mult,
                        [t_q, t_tab], [t_a])
                b_, t_b = qb.next()
                q5 = q_[:].rearrange("p h (b s f) -> p h b s f", b=2, s=2)
                b5 = b_[:].rearrange("p h (b s f) -> p h b s f", b=2, s=2)
                s5 = sinT[:, tt_, :].rearrange("p (b s f) -> p b s f", b=2, s=2)
                for s in range(2):
                    self.tt("pool", b5[:, :, :, s, :], q5[:, :, :, 1 - s, :],
                            s5[:, :, s, :].unsqueeze(1).to_broadcast([128, 20, 2, 16]), ALU.mult, [t_q, t_tab], [t_b])
                o_, t_o = qr.next()
                self.tt("dve", o_[:], a_[:], b_[:], ALU.add, [t_a, t_b], [t_o])
                self.copy("act", V_aug[:, tt_, :, 0:64], ps[:, 1280:1536].rearrange("p (h d) -> p h d", d=64),
                          [t_ps], [lay.t_V])
                kd_, t_kd = kd.next()
                for dd in range(2):
                    self.copy("pool", kd_[:, :, dd, :], o_[:, 16:20, :], [t_o], [t_kd])
                pk, t_pk = pT.next()
                for kv in range(4):
                    self.tr(pk[:, kv, :], kd_[:, kv, :, :].rearrange("p a d -> p (a d)"), c.identb[:], [t_kd, c.t_id],
                            [t_pk])
                self.copy("act", KT[:, :, tt_ * 128:(tt_ + 1) * 128], pk[:, 0:4, :], [t_pk], [lay.t_KT])
                pq, t_pq = pT.next()
                of = o_[:].rearrange("p h d -> p (h d)")
                for k in range(8):
                    self.tr(pq[:, k, :], of[:, k * 128:(k + 1) * 128], c.identb[:], [t_o, c.t_id], [t_pq])
                st, t_st = qst.next()
                self.copy("dve", st[:], pq[:], [t_pq], [t_st])
                self.dma("sp", self.QT_d[:, :, tt_ * 128:(tt_ + 1) * 128].rearrange("c p n -> p c n"), st[:],
                         reads=[t_st])

    def phase_attn(self, kind, layer, j, xsrc, lay, last, moe):
        scale = 0.125 if kind == "gqa" else (96.0 ** -0.5)
        w_o_d = self.a_w_o[j] if kind == "gqa" else self.m_w_o[0]
        with self.scope() as sc:
            c = self.common(sc, njunk=1024)
            ep = self.mk_epi(sc, c, moe)
            wl = [(2, "gate", 1), (3, "shift", None), (4, "scale", 2)]
            mods = self.load_mod(sc, layer, wl, ctx_rows=not last)
            wo = sc.sb("wo", [128, 8, D], BF16)
            t_wo = self.S.tile()
            self.dma("pool", wo[:], w_o_d.rearrange("(c p) n -> p c n", p=128), writes=[t_wo])
            Sps = sc.ps("Sps", [128, 2, 512], F32)
            t_S = [self.S.tile(), self.S.tile()]
            Ops = [sc.ps(f"Ops{i}", [128, 512], F32) for i in range(4)]
            t_O = [self.S.tile() for _ in range(4)]
            pT = sc.rot("pT", [128, 8, 128], BF16, 2, psum=True)
            PT = sc.rot("PT", [128, 512], BF16, 4)
            qp = sc.rot("qp", [128, 512], BF16, 3)
            attn = sc.rot("attn", [128, 4, D], BF16, 2)
            aT = sc.rot("aT", [128, 8, 128], BF16, 2)
            rec = sc.rot("rec", [128, 1], F32, 8)
            if kind == "mla":
                qrp = sc.rot("qrp", [64, 512], BF16, 3)
                knp = sc.rot("knp", [128, T], BF16, 2)
                vpp = sc.rot("vpp", [128, NT, 130], BF16, 2)
            groups = []
            if not last:
                groups.append((0, 2, [0, 1]))
            for g in range(8):
                groups.append((256 + g * 512, 4, list(range(NT))))
            sidx = 0
            for (tok0, nsub, kts) in groups:
                N = nsub * 128
                nk = len(kts)
                at, t_at = attn.next()
                its = []
                for pj in range(8):
                    for r in range(2):
                        for ki, kt in enumerate(kts):
                            its.append(dict(pj=pj, r=r, ki=ki, kt=kt))
                cur = {}

                def emit_S(it):
                    nonlocal sidx
                    pj, r, ki, kt = it["pj"], it["r"], it["ki"], it["kt"]
                    if r == 0 and ki == 0:
                        q_, t_q = qp.next()
                        self.dma("sp", q_[:, 0:N], self.QT_d[pj, :, tok0:tok0 + N], writes=[t_q])
                        cur["q"] = (q_, t_q)
                        if kind == "mla":
                            qr_, t_qr = qrp.next()
                            self.dma("sp", qr_[:, 0:N], self.QrT_d[pj, :, tok0:tok0 + N], writes=[t_qr])
                            kn_, t_kn = knp.next()
                            self.dma("sp", kn_[:, 0:nk * 128], self.KnT_d[pj, :, 0:nk * 128], writes=[t_kn])
                            vp_, t_vp = vpp.next()
                            self.dma("sp", vp_[:, 0:nk, :],
                                     self.V_d[0:nk, :, pj * 130:(pj + 1) * 130].rearrange("t p c -> p t c"),
                                     writes=[t_vp])
                            cur["qr"] = (qr_, t_qr)
                            cur["kn"] = (kn_, t_kn)
                            cur["vp"] = (vp_, t_vp)
                    q_, t_q = cur["q"]
                    h = 2 * pj + r
                    kvh = h // 4
                    sb_ = sidx % 2
                    sidx += 1
                    s_ap = Sps[:, sb_, 0:N]
                    ksl = slice(kt * 128, (kt + 1) * 128)
                    if kind == "gqa":
                        self.mm(s_ap, lay.KT[64 * r:64 * r + 64, kvh, ksl], q_[64 * r:64 * r + 64, 0:N],
                                True, True, [t_q], [t_S[sb_]])
                    else:
                        qr_, t_qr = cur["qr"]
                        kn_, t_kn = cur["kn"]
                        it["vp"] = cur["vp"]
                        self.mm(s_ap, kn_[64 * r:64 * r + 64, ksl], q_[64 * r:64 * r + 64, 0:N],
                                True, False, [t_q, t_kn], [t_S[sb_]])
                        self.mm(s_ap, lay.KrT[32 * r:32 * r + 32, ksl], qr_[32 * r:32 * r + 32, 0:N],
                                False, True, [t_qr], [t_S[sb_]])
                    pt, t_pt = PT.next()
                    self.act(pt[:, 0:N], s_ap, AF.Exp, [t_S[sb_]], [t_pt], scale=scale)
                    it["pt"] = (pt, t_pt)

                def emit_PV(it):
                    pj, r, ki, kt = it["pj"], it["r"], it["ki"], it["kt"]
                    h = 2 * pj + r
                    kvh = h // 4
                    pt, t_pt = it["pt"]
                    for sub in range(nsub):
                        if kind == "gqa":
                            v_ap = lay.V[:, kt, kvh, :]
                            rd = [t_pt]
                        else:
                            vp_, t_vp = it["vp"]
                            v_ap = vp_[:, kt, r * 65:(r + 1) * 65]
                            rd = [t_pt, t_vp]
                        self.mm(Ops[sub][:, 0:65], pt[:, sub * 128:(sub + 1) * 128], v_ap,
                                ki == 0, ki == nk - 1, rd, [t_O[sub]])
                    if ki == nk - 1:
                        for sub in range(nsub):
                            rc, t_rc = rec.next()
                            self.recip(rc[:], Ops[sub][:, 64:65], [t_O[sub]], [t_rc])
                            self.ts("dve", at[:, sub, h * 64:(h + 1) * 64], Ops[sub][:, 0:64], rc[:], ALU.mult,
                                    [t_O[sub], t_rc], [t_at])

                for i in range(len(its) + 1):
                    if i < len(its):
                        emit_S(its[i])
                    if i >= 1:
                        emit_PV(its[i - 1])
                for sub in range(nsub):
                    tt_ = tok0 // 128 + sub
                    r_ = 1 if tt_ < NCTX_T else 0
                    ps, t_ps = pT.next()
                    for k in range(8):
                        self.tr(ps[:, k, :], at[:, sub, k * 128:(k + 1) * 128], c.identb[:], [t_at, c.t_id], [t_ps])
                    a_, t_a = aT.next()
                    self.copy("act", a_[:], ps[:], [t_ps], [t_a])
                    y_ap = Sps[:].rearrange("p a n -> p (a n)")
                    for nb in range(2):
                        for k in range(8):
                            self.mm(Sps[:, nb, :], a_[:, k, :], wo[:, k, nb * 512:(nb + 1) * 512], k == 0, k == 7,
                                    [t_a, t_wo], [t_S[nb]])
                    self.epilogue(c, ep, pT, xsrc, tt_, y_ap, t_S, mods[(2, r_)], mods[(4, r_)], mods[(3, r_)],
                                  True, moe)

    def phase_fourier_1(self, layer, xsrc):
        with self.scope() as sc:
            c = self.common(sc)
            p = self.mk_pre(sc, c)
            mods = self.load_mod(sc, layer, [(0, "shift", None), (1, "scale", 0)])
            cs = sc.sb("cs", [128, 2, 512], BF16)
            t_cs = self.S.tile()
            self.dma("pool", cs[:], self.dft256.rearrange("(c p) n -> p c n", p=128), writes=[t_cs])
            pT = sc.rot("pT", [128, 8, 128], BF16, 2, psum=True)
            xps = sc.rot("xps", [128, 4, 512], F32, 1, psum=True)
            xsb = sc.rot("xsb", [128, 4, 512], BF16, 2)
            for tt_ in range(NT):
                r_ = 1 if tt_ < NCTX_T else 0
                hT, t_hT = self.prenorm(c, p, pT, xsrc, tt_, mods[(1, r_)], mods[(0, r_)])
                ps, t_ps = xps.next()
                for g in range(4):
                    for k in range(2):
                        self.mm(ps[:, g, :], hT[:, 2 * g + k, :], cs[:, k, :], k == 0, k == 1, [t_hT, t_cs], [t_ps])
                xs, t_xs = xsb.next()
                self.copy("act", xs[:], ps[:], [t_ps], [t_xs])
                self.dma("sp", self.XCS_d[:, tt_ * 128:(tt_ + 1) * 128, :].rearrange("g p n -> p g n"), xs[:],
                         reads=[t_xs])

    def phase_fourier_2(self, layer, xsrc, last, moe):
        with self.scope() as sc:
            c = self.common(sc, njunk=1024)
            ep = self.mk_epi(sc, c, moe)
            mods = self.load_mod(sc, layer, [(2, "gate", 1), (3, "shift", None), (4, "scale", 2)], ctx_rows=not last)
            fw = sc.sb("fw", [128, 8, D], BF16)
            t_fw = self.S.tile()
            self.dma("pool", fw[:], self.f_w[0].rearrange("(c p) n -> p c n", p=128), writes=[t_fw])
            Tc = sc.sb("Tc", [128, 32, 512], BF16)
            Ts = sc.sb("Ts", [128, 32, 512], BF16)
            t_T = self.S.tile()
            X = sc.rot("X", [128, 32, 512], BF16, 1)
            fT = sc.rot("fT", [128, 8, 512], BF16, 1)
            pT = sc.rot("pT", [128, 8, 128], BF16, 2, psum=True)
            fps = sc.rot("fps", [128, 512], F32, 2, psum=True)
            yps = sc.ps("yps", [128, 2, 512], F32)
            t_y = [self.S.tile(), self.S.tile()]
            groups = []
            if not last:
                groups.append(("ctx", 0, 2, 2))
            for g in range(8):
                groups.append(("lat", 256 + g * 512, 4, 32))
            for (kind, tok0, nsub, na) in groups:
                N = nsub * 128
                if kind == "ctx":
                    self.dma("sp", Tc[:, 0:2, 0:256], self.cos256.rearrange("(a p) n -> p a n", p=128), writes=[t_T])
                    self.dma("sp", Ts[:, 0:2, 0:256], self.nsin256.rearrange("(a p) n -> p a n", p=128), writes=[t_T])
                    fscale = 1.0 / 256
                else:
                    k0 = tok0 - 256
                    self.dma("sp", Tc[:], self.cos4096[:, k0:k0 + 512].rearrange("(a p) n -> p a n", p=128),
                             writes=[t_T])
                    self.dma("sp", Ts[:], self.nsin4096[:, k0:k0 + 512].rearrange("(a p) n -> p a n", p=128),
                             writes=[t_T])
                    fscale = 1.0 / 1024
                f_, t_f = fT.next()
                for g in range(4):
                    x_, t_x = X.next()
                    if kind == "ctx":
                        self.dma("sp", x_[:, 0:2, :], self.XCS_d[g, 0:256, :].rearrange("(a p) n -> p a n", p=128),
                                 writes=[t_x])
                    else:
                        self.dma("sp", x_[:], self.XCS_d[g, 256:T, :].rearrange("(a p) n -> p a n", p=128),
                                 writes=[t_x])
                    for lc in range(2):
                        ps, t_ps = fps.next()
                        n_mm = 2 * na
                        i_mm = 0
                        for (tab, off) in ((Tc, 0), (Ts, 256)):
                            for a in range(na):
                                self.mm(ps[:, 0:N], x_[:, a, off + lc * 128:off + (lc + 1) * 128], tab[:, a, 0:N],
                                        i_mm == 0, i_mm == n_mm - 1, [t_x, t_T], [t_ps])
                                i_mm += 1
                        self.act(f_[:, 2 * g + lc, 0:N], ps[:, 0:N], AF.Copy, [t_ps], [t_f], scale=fscale)
                for sub in range(nsub):
                    tt_ = tok0 // 128 + sub
                    r_ = 1 if tt_ < NCTX_T else 0
                    for nb in range(2):
                        for k in range(8):
                            self.mm(yps[:, nb, :], f_[:, k, sub * 128:(sub + 1) * 128], fw[:, k, nb * 512:(nb + 1) * 512],
                                    k == 0, k == 7, [t_f, t_fw], [t_y[nb]])
                    self.epilogue(c, ep, pT, xsrc, tt_, yps[:].rearrange("p a n -> p (a n)"), t_y, mods[(2, r_)],
                                  mods[(4, r_)], mods[(3, r_)], True, moe)

    def phase_mla_proj(self, layer, xsrc, lay):
        with self.scope() as sc:
            c = self.common(sc)
            p = self.mk_pre(sc, c)
            mods = self.load_mod(sc, layer, [(0, "shift", None), (1, "scale", 0)])
            t_w = self.S.tile()
            wdn = sc.sb("wdn", [128, 8, 672], BF16)
            self.dma("pool", wdn[:], self.m_w_down[0].rearrange("(c p) n -> p c n", p=128), writes=[t_w])
            wqn = sc.sb("wqn", [128, 3, 1024], BF16)
            self.dma("pool", wqn[:], self.m_wuqn.rearrange("(c p) n -> p c n", p=128), writes=[t_w])
            wqr = sc.sb("wqr", [128, 3, 512], BF16)
            self.dma("pool", wqr[:], self.m_wuqr.rearrange("(c p) n -> p c n", p=128), writes=[t_w])
            wqs = sc.sb("wqs", [128, 3, 512], BF16)
            self.dma("pool", wqs[:], self.m_wuqrs.rearrange("(c p) n -> p c n", p=128), writes=[t_w])
            wuk = sc.sb("wuk", [128, 2, 1024], BF16)
            self.dma("pool", wuk[:], self.m_wuk.rearrange("(c p) n -> p c n", p=128), writes=[t_w])
            wuv = sc.sb("wuv", [128, 2, 1024], BF16)
            self.dma("pool", wuv[:], self.m_wuv.rearrange("(c p) n -> p c n", p=128), writes=[t_w])
            gcq = sc.sb("gcq", [128, 384], F32)
            gckv = sc.sb("gckv", [128, 256], F32)
            t_g = self.S.tile()
            self.dma("sp", gcq[:], self.m_g_cq[0:1, :].partition_broadcast(128), writes=[t_g])
            self.dma("sp", gckv[:], self.m_g_ckv[0:1, :].partition_broadcast(128), writes=[t_g])
            cosT = sc.sb("cosT", [128, NT, 32], F32)
            sinT = sc.sb("sinT", [128, NT, 32], F32)
            t_tab = self.S.tile()
            self.dma("sp", cosT[:], self.ropeM_cos.rearrange("(t p) d -> p t d", p=128), writes=[t_tab])
            self.dma("sp", sinT[:], self.ropeM_sin.rearrange("(t p) d -> p t d", p=128), writes=[t_tab])
            pT = sc.rot("pT", [128, 8, 128], BF16, 2, psum=True)
            dps = sc.rot("dps", [128, 1024], F32, 1, psum=True)
            pps = sc.rot("pps", [128, 512], F32, 2, psum=True)
            vps = sc.rot("vps", [128, 1024], F32, 1, psum=True)
            cqn = sc.rot("cqn", [128, 384], BF16, 2)
            ckvn = sc.rot("ckvn", [128, 256], BF16, 2)
            cqT = sc.rot("cqT", [128, 3, 512], BF16, 2)
            ckT = sc.rot("ckT", [128, 2, 512], BF16, 2)
            ka = sc.rot("ka", [128, 32], F32, 2)
            kb = sc.rot("kb", [128, 32], F32, 2)
            kd = sc.rot("kd", [128, 2, 32], BF16, 2)
            st = sc.rot("st", [128, 512], BF16, 3)
            ctab = sc.rot("ctab", [64, 512], F32, 2)
            stab = sc.rot("stab", [64, 512], F32, 2)
            ra = sc.rot("ra", [64, 512], F32, 2)
            rb = sc.rot("rb", [64, 512], F32, 2)
            rst = sc.rot("rst", [64, 512], BF16, 2)
            vst = sc.rot("vst", [128, 16, 65], BF16, 2)
            for i in range(2):
                self.memset("pool", vst.bufs[i][:, :, 64:65], 1.0, [vst.tiles[i]])
            groups = [(0, 2)] + [(2 + 4 * g, 4) for g in range(8)]
            for (t0, nsub) in groups:
                N = nsub * 128
                tok0 = t0 * 128
                cq_T, t_cqT = cqT.next()
                ck_T, t_ckT = ckT.next()
                for sub in range(nsub):
                    tt_ = t0 + sub
                    r_ = 1 if tt_ < NCTX_T else 0
                    hT, t_hT = self.prenorm(c, p, pT, xsrc, tt_, mods[(1, r_)], mods[(0, r_)])
                    d_, t_d = dps.next()
                    for (o0, o1) in ((0, 512), (512, 672)):
                        for k in range(8):
                            self.mm(d_[:, o0:o1], hT[:, k, :], wdn[:, k, o0:o1], k == 0, k == 7, [t_hT, t_w], [t_d])
                    r1, t_r1 = self.rstd_of(c, d_[:, 0:384], 384, [t_d])
                    cq_, t_cq = cqn.next()
                    self.stt(cq_[:], d_[:, 0:384], r1[:], gcq[:], ALU.mult, ALU.mult, [t_d, t_r1, t_g], [t_cq])
                    r2, t_r2 = self.rstd_of(c, d_[:, 384:640], 256, [t_d])
                    ck_, t_ck = ckvn.next()
                    self.stt(ck_[:], d_[:, 384:640], r2[:], gckv[:], ALU.mult, ALU.mult, [t_d, t_r2, t_g], [t_ck])
                    a_, t_a = ka.next()
                    self.tt("dve", a_[:], d_[:, 640:672], cosT[:, tt_, :], ALU.mult, [t_d, t_tab], [t_a])
                    b_, t_b = kb.next()
                    d4 = d_[:, 640:672].rearrange("p (b s f) -> p b s f", b=2, s=2)
                    b4 = b_[:].rearrange("p (b s f) -> p b s f", b=2, s=2)
                    s4 = sinT[:, tt_, :].rearrange("p (b s f) -> p b s f", b=2, s=2)
                    for s in range(2):
                        self.tt("dve", b4[:, :, s, :], d4[:, :, 1 - s, :], s4[:, :, s, :], ALU.mult, [t_d, t_tab], [t_b])
                    kd_, t_kd = kd.next()
                    for dd in range(2):
                        self.tt("pool", kd_[:, dd, :], a_[:], b_[:], ALU.add, [t_a, t_b], [t_kd])
                    ps, t_ps = pT.next()
                    for k in range(3):
                        self.tr(ps[:, k, :], cq_[:, k * 128:(k + 1) * 128], c.identb[:], [t_cq, c.t_id], [t_ps])
                    for k in range(2):
                        self.tr(ps[:, 3 + k, :], ck_[:, k * 128:(k + 1) * 128], c.identb[:], [t_ck, c.t_id], [t_ps])
                    if True:
                        self.tr(ps[0:64, 5, :], kd_[:].rearrange("p a d -> p (a d)"), c.identb[:], [t_kd, c.t_id], [t_ps])
                    self.copy("act", cq_T[:, :, sub * 128:(sub + 1) * 128], ps[:, 0:3, :], [t_ps], [t_cqT])
                    self.copy("act", ck_T[:, :, sub * 128:(sub + 1) * 128], ps[:, 3:5, :], [t_ps], [t_ckT])
                    self.copy("dve", lay.KrT[:, tt_ * 128:(tt_ + 1) * 128], ps[0:64, 5, :], [t_ps], [lay.t_KrT])
                for sub in range(nsub):
                    tt_ = t0 + sub
                    v_, t_v = vps.next()
                    for nb in range(2):
                        for k in range(2):
                            self.mm(v_[:, nb * 512:(nb + 1) * 512], ck_T[:, k, sub * 128:(sub + 1) * 128],
                                    wuv[:, k, nb * 512:(nb + 1) * 512], k == 0, k == 1, [t_ckT, t_w], [t_v])
                    vs, t_vs = vst.next()
                    self.copy("act", vs[:, :, 0:64], v_[:].rearrange("p (h d) -> p h d", d=64), [t_v], [t_vs])
                    self.dma("sp", self.V_d[tt_, :, :], vs[:].rearrange("p h d -> p (h d)"), reads=[t_vs])
                ct_, t_ct = ctab.next()
                st_, t_stb = stab.next()
                self.dma("sp", ct_[:, 0:N], self.ropeMT_cos[:, tok0:tok0 + N], writes=[t_ct])
                self.dma("sp", st_[:, 0:N], self.ropeMT_sin[:, tok0:tok0 + N], writes=[t_stb])
                for pj in range(8):
                    ps, t_ps = pps.next()
                    for k in range(3):
                        self.mm(ps[:, 0:N], wqn[:, k, pj * 128:(pj + 1) * 128], cq_T[:, k, 0:N], k == 0, k == 2,
                                [t_cqT, t_w], [t_ps])
                    s_, t_s = st.next()
                    self.copy("act", s_[:, 0:N], ps[:, 0:N], [t_ps], [t_s])
                    self.dma("sp", self.QT_d[pj, :, tok0:tok0 + N], s_[:, 0:N], reads=[t_s])
                    ps, t_ps = pps.next()
                    for k in range(2):
                        self.mm(ps[:, 0:N], wuk[:, k, pj * 128:(pj + 1) * 128], ck_T[:, k, 0:N], k == 0, k == 1,
                                [t_ckT, t_w], [t_ps])
                    s_, t_s = st.next()
                    self.copy("act", s_[:, 0:N], ps[:, 0:N], [t_ps], [t_s])
                    self.dma("sp", self.KnT_d[pj, :, tok0:tok0 + N], s_[:, 0:N], reads=[t_s])
                    ps, t_ps = pps.next()
                    for k in range(3):
                        self.mm(ps[0:64, 0:N], wqr[:, k, pj * 64:(pj + 1) * 64], cq_T[:, k, 0:N], k == 0, k == 2,
                                [t_cqT, t_w], [t_ps])
                    a_, t_a = ra.next()
                    self.tt("dve", a_[:, 0:N], ps[0:64, 0:N], ct_[:, 0:N], ALU.mult, [t_ps, t_ct], [t_a])
                    ps, t_ps = pps.next()
                    for k in range(3):
                        self.mm(ps[0:64, 0:N], wqs[:, k, pj * 64:(pj + 1) * 64], cq_T[:, k, 0:N], k == 0, k == 2,
                                [t_cqT, t_w], [t_ps])
                    b_, t_b = rb.next()
                    self.tt("dve", b_[:, 0:N], ps[0:64, 0:N], st_[:, 0:N], ALU.mult, [t_ps, t_stb], [t_b])
                    o_, t_o = rst.next()
                    self.tt("pool", o_[:, 0:N], a_[:, 0:N], b_[:, 0:N], ALU.add, [t_a, t_b], [t_o])
                    self.dma("sp", self.QrT_d[pj, :, tok0:tok0 + N], o_[:, 0:N], reads=[t_o])

    def phase_ffn(self, layer, moe, last, xsrc_unused):
        idx = layer // 2
        tiles = list(range(NCTX_T, NT)) if last else list(range(NT))
        nh = len(tiles) // 2
        halves = [tiles[:nh], tiles[nh:]]
        E = NE if moe else 1
        F = FF_E if moe else FF_D
        nfb = F // 256
        for hi, half in enumerate(halves):
            nt = len(half)
            tok0 = half[0] * 128
            NTOK = nt * 128
            with contextlib.ExitStack() as hes:
                yacc = hes.enter_context(self.nc.sbuf_tensor(f"yacc{layer}_{hi}", [128, nt, D], F32))
                with self.scope() as sc:
                    c = self.common(sc, njunk=1024)
                    h2T = sc.sb("h2T", [128, 8, NTOK], BF16)
                    t_h = self.S.tile()
                    self.dma("sp", h2T[:], self.h2T_d[:, :, tok0:tok0 + NTOK], writes=[t_h])
                    t_ya = [self.S.tile() for _ in range(nt)]
                    gps = sc.rot("gps", [128, 512], F32, 2, psum=True)
                    ups = sc.rot("ups", [128, 512], F32, 2, psum=True)
                    yps = sc.rot("yps", [128, 2, 512], F32, 2, psum=True)
                    wg = sc.rot("wg", [128, 8, 256], BF16, 3)
                    wu = sc.rot("wu", [128, 8, 256], BF16, 3)
                    wd = sc.rot("wd", [128, 2, D], BF16, 3)
                    sg = sc.rot("sg", [128, 512], F32, 2)
                    aT = sc.rot("aT", [128, 2, 512], BF16, 2)
                    if moe:
                        t1 = sc.rot("t1", [128, 512], F32, 2)
                        gbs = sc.rot("gbs", [128, NTOK], F32, 2)
                        gatesT = sc.sb("gatesT", [NE, NTOK], F32)
                        t_gT = self.S.tile()
                        sel = sc.sb("sel", [NE, NE * 128], F32)
                        t_sel = self.S.tile()
                        self.dma("sp", sel[:], self.sel_d[:, :], writes=[t_sel])
                        self.router(sc, c, idx, half, tok0, gatesT, t_gT, yps, ups)
                    groups = []
                    g0 = 0
                    while g0 < NTOK:
                        n = min(512, NTOK - g0)
                        groups.append((g0, n))
                        g0 += n
                    its = []
                    for e in range(E):
                        for fb in range(nfb):
                            for gi, (g0, n) in enumerate(groups):
                                its.append(dict(e=e, fb=fb, gi=gi, g0=g0, n=n))
                    cur = {}

                    def stage_a(it):
                        e, fb, gi, g0, n = it["e"], it["fb"], it["gi"], it["g0"], it["n"]
                        if moe:
                            wgu_d = self.e_w_gu[idx, e]
                            wdn_d = self.e_w_down[idx, e]
                        else:
                            wgu_d = self.d_w_gu[idx]
                            wdn_d = self.d_w_down[idx]
                        if gi == 0:
                            if moe and fb == 0:
                                gb_, t_gb = gbs.next()
                                for (gg0, gn) in groups:
                                    m_, t_m = gps.next()
                                    self.mm(m_[:, 0:gn], sel[:, e * 128:(e + 1) * 128], gatesT[:, gg0:gg0 + gn],
                                            True, True, [t_sel, t_gT], [t_m])
                                    self.copy("act", gb_[:, gg0:gg0 + gn], m_[:, 0:gn], [t_m], [t_gb])
                                cur["gb"] = (gb_, t_gb)
                            wg_, t_wg = wg.next()
                            wu_, t_wu = wu.next()
                            wd_, t_wd = wd.next()
                            f0 = fb * 256
                            self.dma("pool", wg_[:], wgu_d[:, f0:f0 + 256].rearrange("(c p) n -> p c n", p=128),
                                     writes=[t_wg])
                            self.dma("pool", wu_[:], wgu_d[:, F + f0:F + f0 + 256].rearrange("(c p) n -> p c n", p=128),
                                     writes=[t_wu])
                            self.dma("pool", wd_[:], wdn_d[f0:f0 + 256, :].rearrange("(c p) n -> p c n", p=128),
                                     writes=[t_wd])
                            cur["w"] = (wg_, t_wg, wu_, t_wu, wd_, t_wd)
                        wg_, t_wg, wu_, t_wu, wd_, t_wd = cur["w"]
                        it["wd"] = (wd_, t_wd)
                        a_, t_a = aT.next()
                        it["a"] = (a_, t_a)
                        for fc in range(2):
                            g_, t_g = gps.next()
                            u_, t_u = ups.next()
                            for k in range(8):
                                self.mm(g_[:, 0:n], wg_[:, k, fc * 128:(fc + 1) * 128], h2T[:, k, g0:g0 + n],
                                        k == 0, k == 7, [t_wg, t_h], [t_g])
                            for k in range(8):
                                self.mm(u_[:, 0:n], wu_[:, k, fc * 128:(fc + 1) * 128], h2T[:, k, g0:g0 + n],
                                        k == 0, k == 7, [t_wu, t_h], [t_u])
                            s_, t_s = sg.next()
                            self.act(s_[:, 0:n], g_[:, 0:n], AF.Silu, [t_g], [t_s])
                            if moe:
                                gb_, t_gb = cur["gb"]
                                t1_, t_t1 = t1.next()
                                self.tt("dve", t1_[:, 0:n], s_[:, 0:n], u_[:, 0:n], ALU.mult, [t_s, t_u], [t_t1])
                                self.tt("dve", a_[:, fc, 0:n], t1_[:, 0:n], gb_[:, g0:g0 + n], ALU.mult,
                                        [t_t1, t_gb], [t_a])
                            else:
                                self.tt("dve", a_[:, fc, 0:n], s_[:, 0:n], u_[:, 0:n], ALU.mult, [t_s, t_u], [t_a])

                    def stage_b(it):
                        e, fb, g0, n = it["e"], it["fb"], it["g0"], it["n"]
                        first = (e == 0 and fb == 0)
                        a_, t_a = it["a"]
                        wd_, t_wd = it["wd"]
                        for sub in range(n // 128):
                            ti = g0 // 128 + sub
                            y_, t_y = yps.next()
                            for nb in range(2):
                                for fc in range(2):
                                    self.mm(y_[:, nb, :], a_[:, fc, sub * 128:(sub + 1) * 128],
                                            wd_[:, fc, nb * 512:(nb + 1) * 512], fc == 0, fc == 1, [t_a, t_wd], [t_y])
                            yf = y_[:].rearrange("p a n -> p (a n)")
                            if first:
                                self.copy("act", yacc[:, ti, :], yf, [t_y], [t_ya[ti]])
                            else:
                                self.tt("dve", yacc[:, ti, :], yacc[:, ti, :], yf, ALU.add, [t_y, t_ya[ti]],
                                        [t_ya[ti]])

                    for i in range(len(its) + 1):
                        if i < len(its):
                            stage_a(its[i])
                        if i >= 1:
                            stage_b(its[i - 1])
                with self.scope() as sc:
                    c = self.common(sc, njunk=1024)
                    mods = self.load_mod(sc, layer, [(5, "gate", 3)], ctx_rows=not last)
                    xr = sc.rot("xr", [128, D], F32, 3)
                    tm = sc.rot("tm", [128, D], F32, 2)
                    xo = sc.rot("xo", [128, D], F32, 3)
                    for ti, tt_ in enumerate(half):
                        r_ = 1 if tt_ < NCTX_T else 0
                        G_ap, t_G = mods[(5, r_)]
                        rows = slice(tt_ * 128, (tt_ + 1) * 128)
                        x_, t_x = xr.next()
                        self.dma("sp", x_[:], self.xres[rows, :], writes=[t_x])
                        r, t_r = self.rstd_of(c, yacc[:, ti, :], D, [])
                        t_, t_t = tm.next()
                        self.stt(t_[:], yacc[:, ti, :], r[:], G_ap[:], ALU.mult, ALU.mult, [t_r, t_G], [t_t])
                        o_, t_o = xo.next()
                        self.tt("pool", o_[:], x_[:], t_[:], ALU.add, [t_x, t_t], [t_o])
                        if layer == self.nlayers - 1:
                            if tt_ >= NCTX_T:
                                self.dma("sp", self.out[(tt_ - NCTX_T) * 128:(tt_ - NCTX_T + 1) * 128, :], o_[:],
                                         reads=[t_o])
                            if self.debug:
                                self.dma("sp", self.xres[rows, :], o_[:], reads=[t_o])
                        else:
                            self.dma("sp", self.xres[rows, :], o_[:], reads=[t_o])

    def phase_moe(self, layer, last):
        idx = layer // 2
        tiles = list(range(NCTX_T, NT)) if last else list(range(NT))
        nh = len(tiles) // 2
        halves = [tiles[:nh], tiles[nh:]]
        F = FF_E
        nfb = F // 256
        for hi, half in enumerate(halves):
            nt = len(half)
            tok0 = half[0] * 128
            NTOK = nt * 128
            groups = []
            g0 = 0
            while g0 < NTOK:
                n = min(512, NTOK - g0)
                groups.append((g0, n))
                g0 += n
            with contextlib.ExitStack() as hes:
                hsb = lambda name, shape, dt: hes.enter_context(
                    self.nc.sbuf_tensor(f"{name}{layer}_{hi}", list(shape), dt))
                gates = hsb("gates", [128, nt, NE], F32)
                posm = hsb("posm", [128, nt, NE], F32)
                cnt_i = hsb("cnti", [1, NE], I32)
                with self.scope() as sc:
                    c = self.common(sc, njunk=1024)
                    wr = sc.sb("wr", [128, 8, NE], F32)
                    t_wr = self.S.tile()
                    self.dma("sp", wr[:], self.e_w_router[idx].rearrange("(c p) n -> p c n", p=128), writes=[t_wr])
                    trif = sc.sb("trif", [128, 128], F32)
                    trib = sc.sb("trib", [128, 128], BF16)
                    oneb = sc.sb("oneb", [128, 128], BF16)
                    t_tri = self.S.tile()
                    self.dma("sp", trif[:], self.tri_d[:, :], writes=[t_tri])
                    self.copy("dve", trib[:], trif[:], [t_tri], [t_tri])
                    self.memset("pool", oneb[:], 1.0, [t_tri])
                    maskb = sc.sb("maskb", [128, nt, NE], BF16)
                    maskf = sc.sb("maskf", [128, nt, NE], F32)
                    t_mask = self.S.tile()
                    t_gates = self.S.tile()
                    t_posm = self.S.tile()
                    hf = sc.rot("rhf", [128, D], F32, 2)
                    hfT = sc.rot("rhfT", [128, 8, 128], F32, 2)
                    lg = sc.rot("lg", [128, NE], F32, 2)
                    m8 = sc.rot("m8", [128, 8], F32, 2)
                    dd = sc.rot("dd", [128, NE], F32, 2)
                    ee = sc.rot("ee", [128, NE], F32, 2)
                    sm = sc.rot("sm", [128, 1], F32, 2)
                    tps = sc.rot("tps", [128, 2, 512], F32, 2, psum=True)
                    lps = sc.rot("lps", [128, 512], F32, 2, psum=True)
                    for ti, tt_ in enumerate(half):
                        rows = slice(tt_ * 128, (tt_ + 1) * 128)
                        h_, t_h = hf.next()
                        self.dma("sp", h_[:], self.h2f_d[rows, :], writes=[t_h])
                        y_, t_y = tps.next()
                        yT = y_[:].rearrange("p a (b n) -> p (a b) n", n=128)
                        for k in range(8):
                            self.tr(yT[:, k, :], h_[:, k * 128:(k + 1) * 128], c.identf[:], [t_h, c.t_id], [t_y])
                        hT_, t_hT = hfT.next()
                        self.copy("act", hT_[:], yT, [t_y], [t_hT])
                        lp, t_lp = lps.next()
                        for k in range(8):
                            self.mm(lp[:, 0:NE], hT_[:, k, :], wr[:, k, :], k == 0, k == 7, [t_hT, t_wr], [t_lp])
                        l_, t_l = lg.next()
                        self.copy("dve", l_[:], lp[:, 0:NE], [t_lp], [t_l])
                        m_, t_m = m8.next()
                        self.S.op("dve", lambda e, o=m_, i=l_: e.max(out=o[:], in_=i[:]), [t_l], [t_m])
                        d_, t_d = dd.next()
                        self.ts("dve", d_[:], l_[:], m_[:, 0:1], ALU.subtract, [t_l, t_m], [t_d])
                        e_, t_e = ee.next()
                        self.act(e_[:], d_[:], AF.Exp, [t_d], [t_e])
                        self.ts("dve", maskf[:, ti, :], l_[:], m_[:, 1:2], ALU.is_ge, [t_l, t_m], [t_mask])
                        self.copy("dve", maskb[:, ti, :], maskf[:, ti, :], [t_mask], [t_mask])
                        self.tt("dve", e_[:], e_[:], maskf[:, ti, :], ALU.mult, [t_e, t_mask], [t_e])
                        s_, t_s = sm.next()
                        self.S.op("dve", lambda e, o=s_, i=e_: e.tensor_reduce(out=o[:], in_=i[:], axis=AX.X, op=ALU.add),
                                  [t_e], [t_s])
                        self.recip(s_[:], s_[:], [t_s], [t_s])
                        self.ts("dve", gates[:, ti, :], e_[:], s_[:], ALU.mult, [t_e, t_s], [t_gates])
                    for ti in range(nt):
                        lp, t_lp = lps.next()
                        self.mm(lp[:, 0:NE], trib[:], maskb[:, ti, :], True, ti == 0, [t_tri, t_mask], [t_lp])
                        for tp in range(ti):
                            self.mm(lp[:, 0:NE], oneb[:], maskb[:, tp, :], False, tp == ti - 1, [t_tri, t_mask], [t_lp])
                        self.tt("dve", posm[:, ti, :], lp[:, 0:NE], maskf[:, ti, :], ALU.mult, [t_lp, t_mask], [t_posm])
                        self.ts("dve", posm[:, ti, :], posm[:, ti, :], -1.0, ALU.add, [t_posm], [t_posm])
                    lp, t_lp = lps.next()
                    for ti in range(nt):
                        self.mm(lp[0:1, 0:NE], oneb[:, 0:1], maskb[:, ti, :], ti == 0, ti == nt - 1, [t_tri, t_mask],
                                [t_lp])
                    cf = sc.sb("cntf", [1, NE], F32)
                    t_cf = self.S.tile()
                    self.copy("dve", cf[:], lp[0:1, 0:NE], [t_lp], [t_cf])
                    self.copy("dve", cnt_i[:], cf[:], [t_cf], [t_cf])
                with self.scope() as sc:
                    yacc = sc.sb("yacc", [128, nt, D], F32)
                    t_ya = [self.S.tile() for _ in range(nt)]
                    h2tok = sc.sb("h2tok", [128, nt, D], BF16)
                    t_htok = self.S.tile()
                    self.dma("pool", h2tok[:], self.h2f_d[tok0:tok0 + NTOK, :].rearrange("(t p) d -> p t d", p=128),
                             writes=[t_htok])
                    iot = sc.sb("iot", [128, 512], F32)
                    t_io = self.S.tile()
                    self.dma("sp", iot[:], self.iota_d[:, :], writes=[t_io])
                    hTe = sc.sb("hTe", [128, 8, NTOK], BF16)
                    t_hTe = [self.S.tile() for _ in groups]
                    pb = sc.rot("pb", [128, nt, 512], BF16, 1)
                    gps = sc.rot("gps", [128, 512], F32, 2, psum=True)
                    ups = sc.rot("ups", [128, 512], F32, 2, psum=True)
                    yps = sc.rot("yps", [128, 2, 512], F32, 2, psum=True)
                    wg = sc.rot("wg", [128, 8, 512], BF16, 2)
                    wu = sc.rot("wu", [128, 8, 512], BF16, 2)
                    wd = sc.rot("wd", [128, 2, D], BF16, 2)
                    sg = sc.rot("sg", [128, 512], F32, 2)
                    aT = sc.rot("aT", [128, 2, 512], BF16, 2)
                    for e in range(NE):
                        cap = cnt_i[0:1, e:e + 1]
                        wgu_d = self.e_w_gu[idx, e]
                        wdn_d = self.e_w_down[idx, e]
                        def gather(gi, g0, n):
                            pb_, t_pb = pb.next()
                            for ti in range(nt):
                                eng = "dve" if ti % 2 == 0 else "pool"
                                self.ts(eng, pb_[:, ti, 0:n], iot[:, 0:n], float(g0), ALU.add, [t_io], [t_pb],
                                        s2=posm[:, ti, e:e + 1], op1=ALU.is_equal)
                            for k in range(8):
                                ps, t_ps = (gps if k % 2 == 0 else ups).next()
                                for ti in range(nt):
                                    self.mm(ps[:, 0:n], h2tok[:, ti, k * 128:(k + 1) * 128], pb_[:, ti, 0:n],
                                            ti == 0, ti == nt - 1, [t_htok, t_pb], [t_ps])
                                self.copy("act" if k % 2 == 0 else "dve", hTe[:, k, g0:g0 + n], ps[:, 0:n], [t_ps],
                                          [t_hTe[gi]])

                        gather(0, *groups[0])
                        if len(groups) > 1:
                            self.S.begin_cond(cap, groups[1][0], NTOK)
                            gather(1, *groups[1])
                            self.S.end_cond()
                        if len(groups) > 2:
                            self.S.begin_cond(cap, groups[2][0], NTOK)
                            for gi in range(2, len(groups)):
                                gather(gi, *groups[gi])
                            self.S.end_cond()
                        cur = {}

                        def stage_a(it):
                            fb, gi, g0, n = it["fb"], it["gi"], it["g0"], it["n"]
                            if gi == 0:
                                f0 = fb * 256
                                if fb % 2 == 0:
                                    wg_, t_wg = wg.next()
                                    wu_, t_wu = wu.next()
                                    self.dma("pool", wg_[:],
                                             wgu_d[:, f0:f0 + 512].rearrange("(c p) n -> p c n", p=128), writes=[t_wg])
                                    self.dma("pool", wu_[:],
                                             wgu_d[:, F + f0:F + f0 + 512].rearrange("(c p) n -> p c n", p=128),
                                             writes=[t_wu])
                                    cur["gu"] = (wg_, t_wg, wu_, t_wu)
                                wd_, t_wd = wd.next()
                                self.dma("pool", wd_[:], wdn_d[f0:f0 + 256, :].rearrange("(c p) n -> p c n", p=128),
                                         writes=[t_wd])
                                cur["wd"] = (wd_, t_wd)
                            wg_, t_wg, wu_, t_wu = cur["gu"]
                            wd_, t_wd = cur["wd"]
                            so = (fb % 2) * 256
                            it["wd"] = (wd_, t_wd)
                            a_, t_a = aT.next()
                            it["a"] = (a_, t_a)
                            for fc in range(2):
                                g_, t_g = gps.next()
                                u_, t_u = ups.next()
                                for k in range(8):
                                    self.mm(g_[:, 0:n], wg_[:, k, so + fc * 128:so + (fc + 1) * 128], hTe[:, k, g0:g0 + n],
                                            k == 0, k == 7, [t_wg, t_hTe[gi]], [t_g])
                                for k in range(8):
                                    self.mm(u_[:, 0:n], wu_[:, k, so + fc * 128:so + (fc + 1) * 128], hTe[:, k, g0:g0 + n],
                                            k == 0, k == 7, [t_wu, t_hTe[gi]], [t_u])
                                s_, t_s = sg.next()
                                self.act(s_[:, 0:n], g_[:, 0:n], AF.Silu, [t_g], [t_s])
                                self.tt("dve", a_[:, fc, 0:n], s_[:, 0:n], u_[:, 0:n], ALU.mult, [t_s, t_u], [t_a])

                        def stage_b(it):
                            fb, g0, n = it["fb"], it["g0"], it["n"]
                            a_, t_a = it["a"]
                            wd_, t_wd = it["wd"]
                            stores = []
                            for sub in range(n // 128):
                                ti = g0 // 128 + sub
                                y_, t_y = yps.next()
                                for nb in range(2):
                                    for fc in range(2):
                                        self.mm(y_[:, nb, :], a_[:, fc, sub * 128:(sub + 1) * 128],
                                                wd_[:, fc, nb * 512:(nb + 1) * 512], fc == 0, fc == 1, [t_a, t_wd],
                                                [t_y])
                                yf = y_[:].rearrange("p a n -> p (a n)")
                                if fb == 0:
                                    self.copy("act", yacc[:, ti, :], yf, [t_y], [t_ya[ti]])
                                else:
                                    self.tt("dve", yacc[:, ti, :], yacc[:, ti, :], yf, ALU.add, [t_y, t_ya[ti]],
                                            [t_ya[ti]])
                            if fb == nfb - 1:
                                for sub in range(n // 128):
                                    ti = g0 // 128 + sub
                                    r0 = e * NSLOT + ti * 128
                                    stores.append((r0, ti))
                            return stores

                        for fb in range(nfb):
                            itsf = [dict(fb=fb, gi=gi, g0=g0, n=n) for gi, (g0, n) in enumerate(groups)]
                            stores = []
                            stage_a(itsf[0])
                            if len(groups) > 1:
                                self.S.begin_cond(cap, groups[1][0], NTOK)
                                stage_a(itsf[1])
                                self.S.end_cond()
                            stores += stage_b(itsf[0])
                            if len(groups) > 1:
                                self.S.begin_cond(cap, groups[1][0], NTOK)
                                stores += stage_b(itsf[1])
                                self.S.end_cond()
                            if len(groups) > 2:
                                self.S.begin_cond(cap, groups[2][0], NTOK)
                                for it in itsf[2:]:
                                    stage_a(it)
                                    stores += stage_b(it)
                                self.S.end_cond()
                            for (r0, ti) in stores:
                                self.dma("sp", self.ye_d[r0:r0 + 128, :], yacc[:, ti, :], reads=[t_ya[ti]])
                with self.scope() as sc:
                    c = self.common(sc, njunk=1024)
                    mods = self.load_mod(sc, layer, [(5, "gate", 3)], ctx_rows=not last)
                    eoff = sc.sb("eoff", [128, NE], F32)
                    t_eo = self.S.tile()
                    self.dma("sp", eoff[:], self.eoff_d[:, :], writes=[t_eo])
                    xr = sc.rot("xr", [128, D], F32, 2)
                    tm = sc.rot("tm", [128, D], F32, 2)
                    xo = sc.rot("xo", [128, D], F32, 2)
                    yh = sc.rot("yh", [128, D], F32, 2)
                    yl = sc.rot("yl", [128, D], F32, 2)
                    mk = sc.rot("mk", [128, NE], F32, 2)
                    fl = sc.rot("fl", [128, NE], F32, 2)
                    a8 = sc.rot("a8", [128, 8], F32, 2)
                    eq = sc.rot("eq", [128, NE], F32, 4)
                    gg = sc.rot("gg", [128, 2], F32, 2)
                    ii = sc.rot("ii", [128, 2], I32, 2)
                    fi = sc.rot("fi", [128, 2], F32, 2)
                    for ti, tt_ in enumerate(half):
                        r_ = 1 if tt_ < NCTX_T else 0
                        G_ap, t_G = mods[(5, r_)]
                        rows = slice(tt_ * 128, (tt_ + 1) * 128)
                        x_, t_x = xr.next()
                        self.dma("sp", x_[:], self.xres[rows, :], writes=[t_x])
                        mk_, t_mk = mk.next()
                        self.ts("dve", mk_[:], posm[:, ti, :], 0.0, ALU.is_ge, [], [t_mk])
                        self.tt("dve", mk_[:], mk_[:], eoff[:], ALU.mult, [t_mk, t_eo], [t_mk])
                        fl_, t_fl = fl.next()
                        self.stt(fl_[:], posm[:, ti, :], 1.0, mk_[:], ALU.add, ALU.add, [t_mk], [t_fl])
                        a_, t_a = a8.next()
                        self.S.op("dve", lambda e, o=a_, i=fl_: e.max(out=o[:], in_=i[:]), [t_fl], [t_a])
                        g_, t_g = gg.next()
                        for w in range(2):
                            q_, t_q = eq.next()
                            self.ts("dve", q_[:], fl_[:], a_[:, w:w + 1], ALU.is_equal, [t_fl, t_a], [t_q])
                            self.tt("dve", q_[:], q_[:], gates[:, ti, :], ALU.mult, [t_q], [t_q])
                            self.S.op("dve", lambda e, o=g_, i=q_, w=w: e.tensor_reduce(
                                out=o[:, w:w + 1], in_=i[:], axis=AX.X, op=ALU.add), [t_q], [t_g])
                        f_, t_f = fi.next()
                        self.ts("dve", f_[:], a_[:, 0:2], -1.0, ALU.add, [t_a], [t_f])
                        i_, t_i = ii.next()
                        self.copy("dve", i_[:], f_[:], [t_f], [t_i])
                        yh_, t_yh = yh.next()
                        yl_, t_yl = yl.next()
                        self.S.dma("pool", lambda e, o=yh_, i=i_: e.indirect_dma_start(
                            out=o[:], out_offset=None, in_=self.ye_d[:, :],
                            in_offset=bass.IndirectOffsetOnAxis(ap=i[:, 0:1], axis=0)), [t_i], [t_yh])
                        self.S.dma("pool", lambda e, o=yl_, i=i_: e.indirect_dma_start(
                            out=o[:], out_offset=None, in_=self.ye_d[:, :],
                            in_offset=bass.IndirectOffsetOnAxis(ap=i[:, 1:2], axis=0)), [t_i], [t_yl])
                        self.ts("dve", yh_[:], yh_[:], g_[:, 0:1], ALU.mult, [t_yh, t_g], [t_yh])
                        self.stt(yh_[:], yl_[:], g_[:, 1:2], yh_[:], ALU.mult, ALU.add, [t_yl, t_g, t_yh], [t_yh])
                        r, t_r = self.rstd_of(c, yh_[:], D, [t_yh])
                        t_, t_t = tm.next()
                        self.stt(t_[:], yh_[:], r[:], G_ap[:], ALU.mult, ALU.mult, [t_yh, t_r, t_G], [t_t])
                        o_, t_o = xo.next()
                        self.tt("pool", o_[:], x_[:], t_[:], ALU.add, [t_x, t_t], [t_o])
                        if layer == self.nlayers - 1:
                            if tt_ >= NCTX_T:
                                self.dma("sp", self.out[(tt_ - NCTX_T) * 128:(tt_ - NCTX_T + 1) * 128, :], o_[:],
                                         reads=[t_o])
                            if self.debug:
                                self.dma("sp", self.xres[rows, :], o_[:], reads=[t_o])
                        else:
                            self.dma("sp", self.xres[rows, :], o_[:], reads=[t_o])

    def router(self, sc, c, idx, half, tok0, gatesT, t_gT, yps, ups):
        wr = sc.sb("wr", [128, 8, NE], F32)
        t_wr = self.S.tile()
        self.dma("sp", wr[:], self.e_w_router[idx].rearrange("(c p) n -> p c n", p=128), writes=[t_wr])
        hf = sc.rot("rhf", [128, D], F32, 2)
        hfT = sc.rot("rhfT", [128, 8, 128], F32, 2)
        lg = sc.rot("lg", [128, NE], F32, 2)
        m8 = sc.rot("m8", [128, 8], F32, 2)
        dd = sc.rot("dd", [128, NE], F32, 2)
        ee = sc.rot("ee", [128, NE], F32, 2)
        mk = sc.rot("mk", [128, NE], F32, 2)
        sm = sc.rot("sm", [128, 1], F32, 2)
        gt = sc.rot("gt", [128, NE], F32, 2)
        for ti, tt_ in enumerate(half):
            rows = slice(tt_ * 128, (tt_ + 1) * 128)
            h_, t_h = hf.next()
            self.dma("sp", h_[:], self.h2f_d[rows, :], writes=[t_h])
            y_, t_y = yps.next()
            yT = y_[:].rearrange("p a (b n) -> p (a b) n", n=128)
            for k in range(8):
                self.tr(yT[:, k, :], h_[:, k * 128:(k + 1) * 128], c.identf[:], [t_h, c.t_id], [t_y])
            hT_, t_hT = hfT.next()
            self.copy("act", hT_[:], yT, [t_y], [t_hT])
            misc, t_misc = ups.next()
            for k in range(8):
                self.mm(misc[:, 0:NE], hT_[:, k, :], wr[:, k, :], k == 0, k == 7, [t_hT, t_wr], [t_misc])
            l_, t_l = lg.next()
            self.copy("dve", l_[:], misc[:, 0:NE], [t_misc], [t_l])
            m_, t_m = m8.next()
            self.S.op("dve", lambda e, o=m_, i=l_: e.max(out=o[:], in_=i[:]), [t_l], [t_m])
            d_, t_d = dd.next()
            self.ts("dve", d_[:], l_[:], m_[:, 0:1], ALU.subtract, [t_l, t_m], [t_d])
            e_, t_e = ee.next()
            self.act(e_[:], d_[:], AF.Exp, [t_d], [t_e])
            k_, t_k = mk.next()
            self.ts("dve", k_[:], l_[:], m_[:, 1:2], ALU.is_ge, [t_l, t_m], [t_k])
            self.tt("dve", e_[:], e_[:], k_[:], ALU.mult, [t_e, t_k], [t_e])
            s_, t_s = sm.next()
            self.S.op("dve", lambda e, o=s_, i=e_: e.tensor_reduce(out=o[:], in_=i[:], axis=AX.X, op=ALU.add),
                      [t_e], [t_s])
            self.recip(s_[:], s_[:], [t_s], [t_s])
            g_, t_g = gt.next()
            self.ts("dve", g_[:], e_[:], s_[:], ALU.mult, [t_e, t_s], [t_g])
            self.tr(misc[0:NE, 128:256], g_[:], c.identf[:], [t_g, c.t_id], [t_misc])
            self.copy("dve", gatesT[:, ti * 128:(ti + 1) * 128], misc[0:NE, 128:256], [t_misc], [t_gT])

    def build(self):
        try:
            self._build()
        except _Stop:
            pass
        return self.nc

    def _build(self):
        with self.es:
            self.declare()
            self.phase_mod()
            for layer in range(self.nlayers):
                last = layer == DEPTH - 1
                kind = layer % 3
                j = layer // 3
                moe = layer % 2 == 1
                xsrc = self.xin if layer == 0 else self.xres
                self.cur_sparse = moe and last
                with contextlib.ExitStack() as les:
                    lay = type("L", (), {})()
                    if kind == 0:
                        lay.KT = les.enter_context(self.nc.sbuf_tensor(f"KT{layer}", [128, 4, T], BF16))
                        lay.V = les.enter_context(self.nc.sbuf_tensor(f"Vaug{layer}", [128, NT, 4, 65], BF16))
                        lay.t_KT = self.S.tile()
                        lay.t_V = self.S.tile()
                        self.phase_gqa_proj(layer, j, xsrc, lay)
                        self.phase_attn("gqa", layer, j, xsrc, lay, last, moe)
                    elif kind == 1:
                        self.phase_fourier_1(layer, xsrc)
                        self.phase_fourier_2(layer, xsrc, last, moe)
                    else:
                        lay.KrT = les.enter_context(self.nc.sbuf_tensor(f"KrT{layer}", [64, T], BF16))
                        lay.t_KrT = self.S.tile()
                        self.phase_mla_proj(layer, xsrc, lay)
                        self.phase_attn("mla", layer, j, xsrc, lay, last, moe)
                if self.cur_sparse:
                    self.phase_moe(layer, last)
                else:
                    self.phase_ffn(layer, moe, last, xsrc)
        return self.nc


def _rope_tables(rot_dim):
    nf = rot_dim // 4
    inv = (10000.0 ** (-np.arange(nf, dtype=np.float32) / nf)).astype(np.float32)
    rows = np.repeat(np.arange(64, dtype=np.float32), 64)
    cols = np.tile(np.arange(64, dtype=np.float32), 64)
    ar = rows[:, None] * inv[None, :]
    ac = cols[:, None] * inv[None, :]
    cr, sr, cc, sc_ = np.cos(ar), np.sin(ar), np.cos(ac), np.sin(ac)
    cos = np.concatenate([cr, cr, cc, cc], axis=1).astype(np.float32)
    sin = np.concatenate([-sr, sr, -sc_, sc_], axis=1).astype(np.float32)
    cos = np.concatenate([np.ones((256, rot_dim), np.float32), cos], axis=0)
    sin = np.concatenate([np.zeros((256, rot_dim), np.float32), sin], axis=0)
    return cos, sin


_CONST = {}


def _constants():
    if _CONST:
        return _CONST
    c = {}
    c["ident"] = np.eye(128, dtype=np.float32)
    c["ropeA_cos"], c["ropeA_sin"] = _rope_tables(64)
    mc, ms = _rope_tables(32)
    c["ropeM_cos"], c["ropeM_sin"] = mc, ms
    c["ropeMT_cos"] = np.ascontiguousarray(np.concatenate([mc, mc], axis=1).T)
    c["ropeMT_sin"] = np.ascontiguousarray(np.concatenate([ms, ms], axis=1).T)
    k = np.arange(256)
    ang = 2 * np.pi * ((k[:, None] * k[None, :]) % 256) / 256.0
    c["dft256"] = np.concatenate([np.cos(ang), np.sin(ang)], axis=1).astype(np.float32)
    c["cos256"] = np.cos(ang).astype(ml_dtypes.bfloat16)
    c["nsin256"] = (-np.sin(ang)).astype(ml_dtypes.bfloat16)
    k = np.arange(4096, dtype=np.int64)
    ang = 2 * np.pi * ((k[:, None] * k[None, :]) % 4096) / 4096.0
    c["cos4096"] = np.cos(ang).astype(ml_dtypes.bfloat16)
    c["nsin4096"] = (-np.sin(ang)).astype(ml_dtypes.bfloat16)
    sel = np.zeros((NE, NE, 128), np.float32)
    for e in range(NE):
        sel[e, e, :] = 1.0
    c["sel"] = sel.reshape(NE, NE * 128)
    c["tri"] = np.triu(np.ones((128, 128), np.float32))
    c["iota512"] = np.tile(np.arange(512, dtype=np.float32)[None, :], (128, 1))
    c["eoff"] = np.tile((np.arange(NE, dtype=np.float32) * NSLOT)[None, :], (128, 1))
    _CONST.update(c)
    return _CONST


def _prep_inputs(inputs):
    f = lambda a: np.ascontiguousarray(np.asarray(a, dtype=np.float32))
    shared = dict(_constants())
    for k_ in ["w_mod", "b_mod", "norm_g", "a_w_qkv", "a_g_q", "a_g_k", "a_w_o", "f_w", "m_w_down", "m_g_cq", "m_g_ckv",
               "m_w_o", "d_w_gu", "d_w_down", "e_w_router", "e_w_gu", "e_w_down"]:
        shared[k_] = f(inputs[k_])
    wuq = f(inputs["m_w_uq"])[0].reshape(384, 16, 96)
    shared["m_wuqn"] = np.ascontiguousarray(wuq[:, :, 0:64].reshape(384, 1024))
    rope = wuq[:, :, 64:96]
    shared["m_wuqr"] = np.ascontiguousarray(rope.reshape(384, 512))
    perm = np.array([(r + 8) if (r % 16) < 8 else (r - 8) for r in range(32)])
    shared["m_wuqrs"] = np.ascontiguousarray(rope[:, :, perm].reshape(384, 512))
    wukv = f(inputs["m_w_ukv"])[0].reshape(256, 16, 128)
    shared["m_wuk"] = np.ascontiguousarray(wukv[:, :, 0:64].reshape(256, 1024))
    shared["m_wuv"] = np.ascontiguousarray(wukv[:, :, 64:128].reshape(256, 1024))
    x = f(inputs["x"])
    ctx = f(inputs["ctx"])
    cc = f(inputs["c"])
    c_ctx = f(inputs["c_ctx"])
    maps = []
    for b in range(8):
        m = dict(shared)
        m["xin"] = np.ascontiguousarray(np.concatenate([ctx[b], x[b]], axis=0))
        cv = np.stack([cc[b], c_ctx], axis=1)
        m["cvT"] = np.ascontiguousarray(cv.reshape(8, 128, 2).transpose(1, 0, 2).reshape(128, 16))
        maps.append(m)
    return maps


_NC_CACHE = {}


def kernel(**inputs):
    maps = _prep_inputs(inputs)
    if "nc" not in _NC_CACHE:
        _NC_CACHE["nc"] = Builder().build()
    nc = _NC_CACHE["nc"]
    res = run_bass_kernel_spmd(nc, maps, core_ids=list(range(8)))
    out = np.stack([np.asarray(r["out"], dtype=np.float32) for r in res.results], axis=0)
    return out
```
